# Optimizing a Trainium2 kernel written in Bass

```python
import math
import jax
import jax.numpy as jnp
from jax import lax
import numpy as np

D_MODEL = 1024
BATCH = 8
SEQ = 4096
DEPTH = 2

CTX_LEN = 256
GRID_W = 64
D_MIX = D_MODEL
RET_W = D_MIX // 4
GLA_W = D_MIX // 4
S5_W = D_MIX // 4
HY_W = D_MIX - RET_W - GLA_W - S5_W
RET_HEADS = 4
RET_DH = RET_W // RET_HEADS
ROPE_BASE = 10000.0
GLA_HEADS = 4
GLA_DK = GLA_W // (2 * GLA_HEADS)
GLA_DV = GLA_W // GLA_HEADS
GLA_LOWRANK = 16
GLA_TAU = 16.0
S5_GROUP = 16
S5_GROUPS = S5_W // S5_GROUP
S5_STATE = 64
S5_DT_MIN = 1e-3
S5_DT_MAX = 1e-1
HY_CONV = 3
HY_EMB = 33
HY_BANDS = (HY_EMB - 1) // 2
HY_FFN = 64
HY_DECAY_SLOW = -math.log(1e-2) / 1.5
HY_DECAY_FAST = -math.log(1e-2) / 0.3
CHUNK = 128
D_FF = ((8 * D_MODEL // 3 + 255) // 256) * 256
N_EXPERTS = 8
TOP_K = 2
MOE_BLOCK = 256
N_DENSE = (DEPTH + 1) // 2
N_MOE = DEPTH // 2
ALPHA = (2 * DEPTH) ** 0.25
BETA = (8 * DEPTH) ** -0.25
LN_EPS = 1e-5
PROJ_LAYOUT = (('ret_q', RET_W), ('ret_k', RET_W), ('ret_v', RET_W), ('ret_g', RET_W),
               ('gla_q', GLA_HEADS * GLA_DK), ('gla_k', GLA_HEADS * GLA_DK), ('gla_v', GLA_W),
               ('gla_r', GLA_W), ('gla_a', 2 * GLA_LOWRANK), ('s5_u', S5_W), ('hy_p', 3 * HY_W))
D_PROJ = sum(w for _, w in PROJ_LAYOUT)
CTX_STATE_FIELDS = ('ret_k', 'ret_v', 'gla_k', 'gla_v', 'gla_a', 's5_u')

kernel_name = 'hybrid_ret_gla_s5_hyena_moe_dit'


def _ln(x, g=None, b=None):
    xf = x.astype(jnp.float32)
    mu = jnp.mean(xf, axis=-1, keepdims=True)
    var = jnp.mean(jnp.square(xf - mu), axis=-1, keepdims=True)
    y = (xf - mu) * lax.rsqrt(var + LN_EPS)
    if g is not None:
        y = y * g.astype(jnp.float32) + b.astype(jnp.float32)
    return y.astype(x.dtype)


def _same(t):
    return t


def _flip_pos(t):
    return jnp.flip(t, axis=2)


def _split_heads(t, n_heads):
    b, n, w = t.shape
    return t.reshape(b, n, n_heads, w // n_heads).transpose(0, 2, 1, 3)


def _merge_heads(t):
    b, h, n, d = t.shape
    return t.transpose(0, 2, 1, 3).reshape(b, n, h * d)


def _to_chunks(t):
    b, h, n, d = t.shape
    return t.reshape(b, h, n // CHUNK, CHUNK, d).transpose(2, 0, 1, 3, 4)


def _from_chunks(t):
    nc, b, h, c, d = t.shape
    return t.transpose(1, 2, 0, 3, 4).reshape(b, h, nc * c, d)


def _project(u, w_in, fields):
    offsets, start = {}, 0
    for name, width in PROJ_LAYOUT:
        offsets[name] = (start, width)
        start += width
    cols = jnp.concatenate([w_in[:, offsets[n][0]:offsets[n][0] + offsets[n][1]] for n in fields], axis=1)
    y = u @ cols
    out, start = {}, 0
    for n in fields:
        w = offsets[n][1]
        out[n] = y[..., start:start + w]
        start += w
    return out


def _grid_rotary(n_tokens):
    rows = n_tokens // GRID_W
    row = jnp.broadcast_to(jnp.arange(rows, dtype=jnp.float32)[:, None], (rows, GRID_W)).reshape(-1)
    col = jnp.broadcast_to(jnp.arange(GRID_W, dtype=jnp.float32)[None, :], (rows, GRID_W)).reshape(-1)
    n_freq = RET_DH // 4
    inv = ROPE_BASE ** (-jnp.arange(n_freq, dtype=jnp.float32) / n_freq)
    ang = jnp.concatenate([row[:, None] * inv, col[:, None] * inv], axis=-1)
    return jnp.cos(ang), jnp.sin(ang)


def _retention_scan(q, k, v, log_g, s0):
    pos = jnp.arange(CHUNK, dtype=jnp.float32)
    k_to_end = jnp.exp(log_g[:, None] * (CHUNK - 1.0 - pos))[None, :, :, None]
    chunk_decay = jnp.exp(log_g * CHUNK)[None, :, None, None]

    def update(s, kb, vb):
        return chunk_decay * s + jnp.einsum('bhcd,bhce->bhde', kb * k_to_end, vb)

    if q is None:
        def state_step(s, kv):
            return update(s, kv[0], kv[1]), None
        s, _ = lax.scan(state_step, s0, (_to_chunks(k), _to_chunks(v)))
        return None, s
    rel = pos[:, None] - pos[None, :]
    lower = rel >= 0
    dmat = jnp.where(lower, jnp.exp(log_g[:, None, None] * jnp.where(lower, rel, 0.0)), 0.0)[None]
    q_from_state = jnp.exp(log_g[:, None] * (pos + 1.0))[None, :, :, None]

    def step(s, qkv):
        qb, kb, vb = qkv
        att = jnp.einsum('bhid,bhjd->bhij', qb, kb) * dmat
        o = jnp.einsum('bhij,bhje->bhie', att, vb) + jnp.einsum('bhid,bhde->bhie', qb * q_from_state, s)
        return update(s, kb, vb), o

    s, o = lax.scan(step, s0, (_to_chunks(q), _to_chunks(k), _to_chunks(v)))
    return _from_chunks(o), s


def _gla_scan(q, k, v, log_a, s0):
    pos = jnp.arange(CHUNK)
    lower = (pos[:, None] >= pos[None, :])[:, :, None]

    def update(s, kb, vb, cum):
        end = cum[:, :, -1]
        return jnp.exp(end)[..., None] * s + jnp.einsum('bhcd,bhce->bhde', kb * jnp.exp(end[:, :, None] - cum), vb)

    if q is None:
        def state_step(s, inp):
            kb, vb, ab = inp
            return update(s, kb, vb, jnp.cumsum(ab, axis=2)), None
        s, _ = lax.scan(state_step, s0, (_to_chunks(k), _to_chunks(v), _to_chunks(log_a)))
        return None, s

    def step(s, inp):
        qb, kb, vb, ab = inp
        cum = jnp.cumsum(ab, axis=2)
        diff = jnp.where(lower, cum[:, :, :, None, :] - cum[:, :, None, :, :], -jnp.inf)
        att = jnp.einsum('bhid,bhjd,bhijd->bhij', qb, kb, jnp.exp(diff))
        o = jnp.einsum('bhij,bhje->bhie', att, vb) + jnp.einsum('bhid,bhde->bhie', qb * jnp.exp(cum), s)
        return update(s, kb, vb, cum), o

    s, o = lax.scan(step, s0, (_to_chunks(q), _to_chunks(k), _to_chunks(v), _to_chunks(log_a)))
    return _from_chunks(o), s


def _retention_mixer(pl, pc, lp, need_ctx):
    f32 = jnp.float32
    cos, sin = _grid_rotary(pl['ret_q'].shape[1])

    def rotate(t):
        b, n, _ = t.shape
        t = t.astype(f32).reshape(b, n, RET_HEADS, RET_DH)
        t1, t2 = jnp.split(t, 2, axis=-1)
        cs, sn = cos[None, :, None, :], sin[None, :, None, :]
        return jnp.concatenate([t1 * cs - t2 * sn, t1 * sn + t2 * cs], axis=-1).transpose(0, 2, 1, 3)

    def heads(t):
        return _split_heads(t.astype(f32), RET_HEADS)

    scale = RET_DH ** -0.5
    q_l, k_l, v_l = rotate(pl['ret_q']), rotate(pl['ret_k']) * scale, heads(pl['ret_v'])
    q_c = heads(pc['ret_q']) if need_ctx else None
    k_c, v_c = heads(pc['ret_k']) * scale, heads(pc['ret_v'])
    s0 = jnp.zeros((k_c.shape[0], RET_HEADS, RET_DH, RET_DH), f32)
    o_l, o_c = [], []
    for d in range(2):
        log_g = jax.nn.log_sigmoid(lp['ret_decay'][d].astype(f32))
        fl = _same if d == 0 else _flip_pos
        oc, s_ctx = _retention_scan(None if q_c is None else fl(q_c), fl(k_c), fl(v_c), log_g, s0)
        ol, _ = _retention_scan(fl(q_l), fl(k_l), fl(v_l), log_g, s_ctx)
        o_l.append(fl(ol))
        if need_ctx:
            o_c.append(fl(oc))

    def finish(o, gate):
        o = o[0] + o[1]
        mu = jnp.mean(o, axis=-1, keepdims=True)
        var = jnp.mean(jnp.square(o - mu), axis=-1, keepdims=True)
        y = _merge_heads((o - mu) * lax.rsqrt(var + LN_EPS)) * lp['ret_gn_g'].astype(f32) + lp['ret_gn_b'].astype(f32)
        return (y * jax.nn.silu(gate.astype(f32))).astype(gate.dtype)

    y_l = finish(o_l, pl['ret_g'])
    y_c = finish(o_c, pc['ret_g']) if need_ctx else None
    return y_l, y_c


def _gla_mixer(pl, pc, lp, need_ctx):
    f32 = jnp.float32
    scale = GLA_DK ** -0.5

    def heads(t):
        return _split_heads(t.astype(f32), GLA_HEADS)

    def log_gate(a, d):
        a = a.astype(f32)[..., d * GLA_LOWRANK:(d + 1) * GLA_LOWRANK]
        g = a @ lp['gla_wa'][d].astype(f32) + lp['gla_ba'][d].astype(f32)
        return heads(jax.nn.log_sigmoid(g) / GLA_TAU)

    q_l, k_l, v_l = heads(pl['gla_q']) * scale, heads(pl['gla_k']), heads(pl['gla_v'])
    q_c = heads(pc['gla_q']) * scale if need_ctx else None
    k_c, v_c = heads(pc['gla_k']), heads(pc['gla_v'])
    s0 = jnp.zeros((k_c.shape[0], GLA_HEADS, GLA_DK, GLA_DV), f32)
    o_l, o_c = [], []
    for d in range(2):
        fl = _same if d == 0 else _flip_pos
        oc, s_ctx = _gla_scan(None if q_c is None else fl(q_c), fl(k_c), fl(v_c), fl(log_gate(pc['gla_a'], d)), s0)
        ol, _ = _gla_scan(fl(q_l), fl(k_l), fl(v_l), fl(log_gate(pl['gla_a'], d)), s_ctx)
        o_l.append(fl(ol))
        if need_ctx:
            o_c.append(fl(oc))

    def finish(o, r):
        o = o[0] + o[1]
        o = o * lax.rsqrt(jnp.mean(jnp.square(o), axis=-1, keepdims=True) + LN_EPS)
        return (_merge_heads(o) * lp['gla_norm_g'].astype(f32) * jax.nn.silu(r.astype(f32))).astype(r.dtype)

    y_l = finish(o_l, pl['gla_r'])
    y_c = finish(o_c, pc['gla_r']) if need_ctx else None
    return y_l, y_c


def _s5_scan(lam_bar, bu, h0):
    bu = bu.at[:, 0].add(lam_bar * h0)
    a = jnp.broadcast_to(lam_bar, bu.shape)

    def combine(left, right):
        a_l, b_l = left
        a_r, b_r = right
        return a_r * a_l, a_r * b_l + b_r

    return lax.associative_scan(combine, (a, bu), axis=1)[1]


def _s5_mixer(u_lat, u_ctx, lp, need_ctx):
    f32 = jnp.float32

    def groups(u):
        b, n, _ = u.shape
        return u.astype(f32).reshape(b, n, S5_GROUPS, S5_GROUP).astype(jnp.complex64)

    def flip_seq(t):
        return jnp.flip(t, axis=1)

    ul, uc = groups(u_lat), groups(u_ctx)
    h0 = jnp.zeros((ul.shape[0], S5_GROUPS, S5_STATE), jnp.complex64)
    y_l, y_c = [], []
    for d in range(2):
        lam = lp['s5_lam_re'][d].astype(f32) + 1j * lp['s5_lam_im'][d].astype(f32)
        dt = jnp.exp(lp['s5_log_dt'][d].astype(f32))[:, None]
        lam_bar = jnp.exp(lam * dt)
        b_mat = lp['s5_b_re'][d].astype(f32) + 1j * lp['s5_b_im'][d].astype(f32)
        b_bar = ((lam_bar - 1.0) / lam)[:, :, None] * b_mat
        c_mat = lp['s5_c_re'][d].astype(f32) + 1j * lp['s5_c_im'][d].astype(f32)
        fl = _same if d == 0 else flip_seq
        h_c = _s5_scan(lam_bar, jnp.einsum('blgp,gnp->blgn', fl(uc), b_bar), h0)
        h_l = _s5_scan(lam_bar, jnp.einsum('blgp,gnp->blgn', fl(ul), b_bar), h_c[:, -1])
        y_l.append(fl(jnp.real(jnp.einsum('blgn,gpn->blgp', h_l, c_mat))))
        if need_ctx:
            y_c.append(fl(jnp.real(jnp.einsum('blgn,gpn->blgp', h_c, c_mat))))

    def finish(ys, u):
        b, n = u.shape[:2]
        y = (ys[0] + ys[1]).reshape(b, n, S5_W) + lp['s5_d'].astype(f32) * u.astype(f32)
        g = jax.nn.gelu(y)
        return (g * jax.nn.sigmoid(g @ lp['s5_glu_w'].astype(f32) + lp['s5_glu_b'].astype(f32))).astype(u.dtype)

    out_l = finish(y_l, u_lat)
    out_c = finish(y_c, u_ctx) if need_ctx else None
    return out_l, out_c


def _hyena_response(n, lp):
    f32 = jnp.float32
    t = jnp.linspace(0.0, 1.0, n, dtype=f32)[:, None]
    idx = jnp.arange(n, dtype=f32)[:, None]
    bands = jnp.linspace(1e-4, HY_BANDS - 1, HY_BANDS, dtype=f32)[None, :]
    ang = (2.0 * math.pi / n) * bands * idx
    z = jnp.concatenate([t, jnp.cos(ang), -jnp.sin(ang)], axis=-1)
    fr = lp['hy_freq'].astype(f32)
    hdn = jnp.sin(fr * (z @ lp['hy_fw1'].astype(f32) + lp['hy_fb1'].astype(f32)))
    hdn = jnp.sin(fr * (hdn @ lp['hy_fw2'].astype(f32) + lp['hy_fb2'].astype(f32)))
    window = jnp.exp(-t[:, :, None] * jnp.abs(lp['hy_decay'].astype(f32)))
    filt = (hdn @ lp['hy_fw3'].astype(f32)).reshape(n, 2, HY_W) * window
    circ = jnp.concatenate([filt[:, 0], jnp.zeros((1, HY_W), f32), jnp.flip(filt[1:, 1], axis=0)], axis=0)
    circ = circ / jnp.sum(jnp.abs(circ), axis=0, keepdims=True)
    return jnp.fft.rfft(circ, axis=0)


def _hyena(p, lp):
    f32 = jnp.float32
    n = p.shape[1]
    pad = HY_CONV // 2
    pp = jnp.pad(p.astype(f32), ((0, 0), (pad, pad), (0, 0)))
    cw = lp['hy_conv_w'].astype(f32)
    s = lp['hy_conv_b'].astype(f32)
    for j in range(HY_CONV):
        s = s + pp[:, j:j + n] * cw[j]
    x0, x1, v = jnp.split(s, 3, axis=-1)
    z = x1 * v
    resp = _hyena_response(n, lp)
    y = jnp.fft.irfft(jnp.fft.rfft(z, n=2 * n, axis=1) * resp[None], n=2 * n, axis=1)[:, :n]
    return (x0 * (y + z * lp['hy_bias'].astype(f32))).astype(p.dtype)


def _token_mixer(u_lat, u_ctx, lp, need_ctx):
    all_fields = tuple(name for name, _ in PROJ_LAYOUT)
    pl = _project(u_lat, lp['w_in'], all_fields)
    pc = _project(u_ctx, lp['w_in'], all_fields if need_ctx else CTX_STATE_FIELDS)
    ret_l, ret_c = _retention_mixer(pl, pc, lp, need_ctx)
    gla_l, gla_c = _gla_mixer(pl, pc, lp, need_ctx)
    s5_l, s5_c = _s5_mixer(pl['s5_u'], pc['s5_u'], lp, need_ctx)
    y_l = jnp.concatenate([ret_l, gla_l, s5_l, _hyena(pl['hy_p'], lp)], axis=-1) @ lp['w_out']
    if not need_ctx:
        return y_l, None
    y_c = jnp.concatenate([ret_c, gla_c, s5_c, _hyena(pc['hy_p'], lp)], axis=-1) @ lp['w_out']
    return y_l, y_c


def _swiglu(h, w1, w3, w2):
    return (jax.nn.silu(h @ w1) * (h @ w3)) @ w2


def _moe_swiglu(h, router_w, router_b, w1, w3, w2):
    shape = h.shape
    t = h.reshape(-1, shape[-1])
    n_tok = t.shape[0]
    n_asg = n_tok * TOP_K
    logits = (t @ router_w).astype(jnp.float32) + router_b.astype(jnp.float32)
    top_logit, top_idx = lax.top_k(logits, TOP_K)
    gate = jax.nn.softmax(top_logit, axis=-1).reshape(-1)
    expert = top_idx.reshape(-1)
    token = jnp.repeat(jnp.arange(n_tok, dtype=jnp.int32), TOP_K)
    order = jnp.argsort(expert)
    expert_s, token_s, gate_s = expert[order], token[order], gate[order]
    count = jnp.bincount(expert, length=N_EXPERTS)
    padded = (count + MOE_BLOCK - 1) // MOE_BLOCK * MOE_BLOCK
    pad_end = jnp.cumsum(padded)
    pad_start = pad_end - padded
    sort_start = jnp.cumsum(count) - count
    slot = pad_start[expert_s] + jnp.arange(n_asg, dtype=jnp.int32) - sort_start[expert_s]
    n_blocks = -(-n_asg // MOE_BLOCK) + N_EXPERTS
    cap = n_blocks * MOE_BLOCK
    slot_token = jnp.full((cap,), n_tok, jnp.int32).at[slot].set(token_s)
    slot_gate = jnp.zeros((cap,), jnp.float32).at[slot].set(gate_s)
    block_expert = jnp.minimum(jnp.searchsorted(pad_end, jnp.arange(n_blocks, dtype=jnp.int32) * MOE_BLOCK, side='right'), N_EXPERTS - 1)
    xb = jnp.take(t, slot_token, axis=0, mode='clip').reshape(n_blocks, MOE_BLOCK, -1)

    def expert_block(args):
        xe, e = args
        return (jax.nn.silu(xe @ w1[e]) * (xe @ w3[e])) @ w2[e]

    yb = lax.map(expert_block, (xb, block_expert)).reshape(cap, -1)
    out = jnp.zeros_like(t).at[slot_token].add(yb * slot_gate[:, None].astype(yb.dtype), mode='drop')
    return out.reshape(shape)


def setup_inputs(seed: int = 0) -> dict:
    key = jax.random.key(seed)
    keys = jax.random.split(key, 64)
    counter = [0]
    f32 = jnp.float32

    def nxt():
        counter[0] += 1
        return keys[counter[0] - 1]

    def nrm(shape, scale):
        return jax.random.normal(nxt(), shape, f32) * scale

    L, D = DEPTH, D_MODEL
    G, N, P = S5_GROUPS, S5_STATE, S5_GROUP
    GK = GLA_HEADS * GLA_DK
    return {
        'x': nrm((BATCH, SEQ, D), 1.0),
        'c': nrm((BATCH, D), 1.0),
        'ctx': nrm((BATCH, CTX_LEN, D), 1.0),
        'c_ctx': nrm((D,), 1.0),
        'ada_w': nrm((L, D, 6 * D), 0.5 * D ** -0.5),
        'ada_b': nrm((L, 6 * D), 0.02),
        'w_in': nrm((L, D, D_PROJ), D ** -0.5),
        'ret_decay': jnp.log(2.0 ** (5.0 + jnp.arange(RET_HEADS, dtype=f32)) - 1.0) + nrm((L, 2, RET_HEADS), 0.01),
        'ret_gn_g': 1.0 + nrm((L, RET_W), 0.02),
        'ret_gn_b': nrm((L, RET_W), 0.02),
        'gla_wa': nrm((L, 2, GLA_LOWRANK, GK), GLA_LOWRANK ** -0.5),
        'gla_ba': nrm((L, 2, GK), 0.02),
        'gla_norm_g': 1.0 + nrm((L, GLA_W), 0.02),
        's5_lam_re': -0.5 + nrm((L, 2, G, N), 0.01),
        's5_lam_im': math.pi * jnp.arange(N, dtype=f32) + nrm((L, 2, G, N), 0.01),
        's5_log_dt': jax.random.uniform(nxt(), (L, 2, G), f32, math.log(S5_DT_MIN), math.log(S5_DT_MAX)),
        's5_b_re': nrm((L, 2, G, N, P), (2 * P) ** -0.5),
        's5_b_im': nrm((L, 2, G, N, P), (2 * P) ** -0.5),
        's5_c_re': nrm((L, 2, G, P, N), N ** -0.5),
        's5_c_im': nrm((L, 2, G, P, N), N ** -0.5),
        's5_d': nrm((L, S5_W), 1.0),
        's5_glu_w': nrm((L, S5_W, S5_W), S5_W ** -0.5),
        's5_glu_b': nrm((L, S5_W), 0.02),
        'hy_conv_w': nrm((L, HY_CONV, 3 * HY_W), HY_CONV ** -0.5),
        'hy_conv_b': nrm((L, 3 * HY_W), 0.02),
        'hy_fw1': nrm((L, HY_EMB, HY_FFN), HY_EMB ** -0.5),
        'hy_fb1': nrm((L, HY_FFN), 0.02),
        'hy_freq': 1.0 + nrm((L, HY_FFN), 0.02),
        'hy_fw2': nrm((L, HY_FFN, HY_FFN), HY_FFN ** -0.5),
        'hy_fb2': nrm((L, HY_FFN), 0.02),
        'hy_fw3': nrm((L, HY_FFN, 2 * HY_W), HY_FFN ** -0.5),
        'hy_decay': jnp.linspace(HY_DECAY_SLOW, HY_DECAY_FAST, HY_W, dtype=f32) * jnp.exp(nrm((L, 2, HY_W), 0.01)),
        'hy_bias': nrm((L, HY_W), 1.0),
        'w_out': nrm((L, D_MIX, D), BETA * D_MIX ** -0.5),
        'ln_mix_g': 1.0 + nrm((L, D), 0.02),
        'ln_mix_b': nrm((L, D), 0.02),
        'ln_ffn_g': 1.0 + nrm((L, D), 0.02),
        'ln_ffn_b': nrm((L, D), 0.02),
        'ffn_w1': nrm((N_DENSE, D, D_FF), D ** -0.5),
        'ffn_w3': nrm((N_DENSE, D, D_FF), D ** -0.5),
        'ffn_w2': nrm((N_DENSE, D_FF, D), BETA * D_FF ** -0.5),
        'router_w': nrm((N_MOE, D, N_EXPERTS), D ** -0.5),
        'router_b': nrm((N_MOE, N_EXPERTS), 0.01),
        'moe_w1': nrm((N_MOE, N_EXPERTS, D, D_FF), D ** -0.5),
        'moe_w3': nrm((N_MOE, N_EXPERTS, D, D_FF), D ** -0.5),
        'moe_w2': nrm((N_MOE, N_EXPERTS, D_FF, D), BETA * D_FF ** -0.5),
    }


def reference(x, c, ctx, c_ctx, ada_w, ada_b, w_in, ret_decay, ret_gn_g, ret_gn_b, gla_wa, gla_ba, gla_norm_g,
              s5_lam_re, s5_lam_im, s5_log_dt, s5_b_re, s5_b_im, s5_c_re, s5_c_im, s5_d, s5_glu_w, s5_glu_b,
              hy_conv_w, hy_conv_b, hy_fw1, hy_fb1, hy_freq, hy_fw2, hy_fb2, hy_fw3, hy_decay, hy_bias,
              w_out, ln_mix_g, ln_mix_b, ln_ffn_g, ln_ffn_b, ffn_w1, ffn_w3, ffn_w2,
              router_w, router_b, moe_w1, moe_w3, moe_w2):
    h_ctx = ctx
    for i in range(DEPTH):
        last = i == DEPTH - 1
        lp = {
            'w_in': w_in[i], 'ret_decay': ret_decay[i], 'ret_gn_g': ret_gn_g[i], 'ret_gn_b': ret_gn_b[i],
            'gla_wa': gla_wa[i], 'gla_ba': gla_ba[i], 'gla_norm_g': gla_norm_g[i],
            's5_lam_re': s5_lam_re[i], 's5_lam_im': s5_lam_im[i], 's5_log_dt': s5_log_dt[i],
            's5_b_re': s5_b_re[i], 's5_b_im': s5_b_im[i], 's5_c_re': s5_c_re[i], 's5_c_im': s5_c_im[i],
            's5_d': s5_d[i], 's5_glu_w': s5_glu_w[i], 's5_glu_b': s5_glu_b[i],
            'hy_conv_w': hy_conv_w[i], 'hy_conv_b': hy_conv_b[i], 'hy_fw1': hy_fw1[i], 'hy_fb1': hy_fb1[i],
            'hy_freq': hy_freq[i], 'hy_fw2': hy_fw2[i], 'hy_fb2': hy_fb2[i], 'hy_fw3': hy_fw3[i],
            'hy_decay': hy_decay[i], 'hy_bias': hy_bias[i], 'w_out': w_out[i],
        }
        j = i // 2
        if i % 2 == 0:
            ffn = lambda h, j=j: _swiglu(h, ffn_w1[j], ffn_w3[j], ffn_w2[j])
        else:
            ffn = lambda h, j=j: _moe_swiglu(h, router_w[j], router_b[j], moe_w1[j], moe_w3[j], moe_w2[j])
        mod = (jax.nn.silu(c) @ ada_w[i] + ada_b[i])[:, None, :]
        sh_m, sc_m, g_m, sh_f, sc_f, g_f = jnp.split(mod, 6, axis=-1)
        n_ctx_mod = 2 if last else 6
        mod_c = jax.nn.silu(c_ctx) @ ada_w[i][:, :n_ctx_mod * D_MODEL] + ada_b[i][:n_ctx_mod * D_MODEL]
        mods_c = jnp.split(mod_c, n_ctx_mod)
        u_lat = _ln(x) * (1.0 + sc_m) + sh_m
        u_ctx = _ln(h_ctx) * (1.0 + mods_c[1]) + mods_c[0]
        y_lat, y_ctx = _token_mixer(u_lat, u_ctx, lp, not last)
        x = _ln(ALPHA * x + g_m * y_lat, ln_mix_g[i], ln_mix_b[i])
        v_lat = _ln(x) * (1.0 + sc_f) + sh_f
        x = _ln(ALPHA * x + g_f * ffn(v_lat), ln_ffn_g[i], ln_ffn_b[i])
        if not last:
            h_ctx = _ln(ALPHA * h_ctx + mods_c[2] * y_ctx, ln_mix_g[i], ln_mix_b[i])
            v_ctx = _ln(h_ctx) * (1.0 + mods_c[4]) + mods_c[3]
            h_ctx = _ln(ALPHA * h_ctx + mods_c[5] * ffn(v_ctx), ln_ffn_g[i], ln_ffn_b[i])
    return x
```

```python
import math
import os
from contextlib import ExitStack
import numpy as np
import ml_dtypes
import concourse.bass as bass
import concourse.mybir as mybir
from concourse.bass_utils import run_bass_kernel_spmd

F32 = mybir.dt.float32
BF16 = mybir.dt.bfloat16
I32 = mybir.dt.int32
AF = mybir.ActivationFunctionType
ALU = mybir.AluOpType
AX = mybir.AxisListType

COMPUTE = ('pe', 'act', 'dve', 'pool')
NDMA = 24


class Tok:
    __slots__ = ('w', 'r')

    def __init__(self):
        self.w = None
        self.r = {}


class Sched:
    def __init__(self, nc, same_engine_sync=True):
        self.nc = nc
        self.es = ExitStack()
        self.E = {'pe': nc.tensor, 'act': nc.scalar, 'dve': nc.vector, 'pool': nc.gpsimd, 'sp': nc.sync}
        self.sem = {e: self.es.enter_context(nc.semaphore('sem_' + e)) for e in COMPUTE}
        self.cnt = {e: 0 for e in COMPUTE}
        self.seen = {f: {} for f in self.E}
        self.dsem = [self.es.enter_context(nc.semaphore('dsem%d' % i)) for i in range(NDMA)]
        self.dcnt = [0] * NDMA
        self.dnext = 0
        self.same = same_engine_sync
        self.ninst = 0

    def _semobj(self, key):
        return self.sem[key] if isinstance(key, str) else self.dsem[key[1]]

    def wait(self, f, ev):
        if ev is None:
            return
        key, val = ev
        if key == f and (f == 'pe' or not self.same):
            return
        if self.seen[f].get(key, 0) >= val:
            return
        self.E[f].wait_ge(self._semobj(key), val)
        self.seen[f][key] = val

    def _deps(self, f, R, W):
        for t in R:
            self.wait(f, t.w)
        for t in W:
            self.wait(f, t.w)
            for k, v in t.r.items():
                self.wait(f, (k, v))

    def op(self, eng, fn, R=(), W=()):
        self._deps(eng, R, W)
        ins = fn(self.E[eng])
        self.cnt[eng] += 1
        ins.then_inc(self.sem[eng], 1)
        ev = (eng, self.cnt[eng])
        self.seen[eng][eng] = self.seen[eng].get(eng, 0)
        for t in R:
            t.r[eng] = self.cnt[eng]
        for t in W:
            t.w = ev
            t.r = {}
        self.ninst += 1
        return ins

    def dma(self, out, in_, R=(), W=(), q='sp', **kw):
        i = self.dnext
        self.dnext = (i + 1) % NDMA
        key = ('d', i)
        if self.dcnt[i] > 0:
            self.wait(q, (key, self.dcnt[i]))
        self._deps(q, R, W)
        ins = self.E[q].dma_start(out=out, in_=in_, **kw)
        self.dcnt[i] += 16
        ins.then_inc(self.dsem[i], 16)
        for t in R:
            t.r[key] = self.dcnt[i]
        for t in W:
            t.w = (key, self.dcnt[i])
            t.r = {}
        self.ninst += 1
        return ins

    def barrier(self):
        for f in self.E:
            for e in COMPUTE:
                if self.cnt[e] > 0:
                    self.wait_force(f, (e, self.cnt[e]))
            for i in range(NDMA):
                if self.dcnt[i] > 0:
                    self.wait(f, (('d', i), self.dcnt[i]))

    def wait_force(self, f, ev):
        key, val = ev
        if self.seen[f].get(key, 0) >= val:
            return
        self.E[f].wait_ge(self._semobj(key), val)
        self.seen[f][key] = val


D_MODEL = 1024
SEQ = 4096
CTXL = 256
T = SEQ + CTXL
NCH = T // 128
DEPTH = 2
D_PROJ = 2848
D_FF = 2816
NF = D_FF // 128
N_EXP = 8
LN_EPS = 1e-5
ALPHA = (2 * DEPTH) ** 0.25
OFF = {}
_o = 0
for _n, _w in (('ret_q', 256), ('ret_k', 256), ('ret_v', 256), ('ret_g', 256), ('gla_q', 128), ('gla_k', 128),
               ('gla_v', 256), ('gla_r', 256), ('gla_a', 32), ('s5_u', 256), ('hy_p', 768)):
    OFF[_n] = _o
    _o += _w


class G:
    pass


_uid = [0]


def _nm(name):
    _uid[0] += 1
    return '%s_%d' % (name, _uid[0])


def build_program(stop_after=None, dbg=(), dbg_layer=0):
    nc = bass.Bass("TRN2", target_bir_lowering=False)
    K = Sched(nc, same_engine_sync=(os.environ.get('SAME', '1') == '1'))
    g = G()
    g.nc, g.K, g.dbg, g.stop_after, g.dbg_layer = nc, K, set(dbg), stop_after, dbg_layer
    g.D = {}
    g.outs = {}

    def din(name, shape, dt=F32):
        g.D[name] = nc.dram_tensor(name, list(shape), dt, kind="ExternalInput").ap()
        return g.D[name]

    def dscr(name, shape, dt=F32):
        g.D[name] = nc.dram_tensor(name, list(shape), dt, kind="Internal").ap()
        return g.D[name]

    def dout(name, shape, dt=F32):
        g.outs[name] = nc.dram_tensor(name, list(shape), dt, kind="ExternalOutput").ap()
        return g.outs[name]
    g.din, g.dscr, g.dout = din, dscr, dout

    din('xin', [T, D_MODEL])
    din('cvec', [128, 8, 2])
    din('ada_w', [DEPTH, D_MODEL, 6 * D_MODEL])
    din('ada_bT', [DEPTH, 128, 48])
    din('ada_b', [DEPTH, 6 * D_MODEL])
    din('w_out', [DEPTH, D_MODEL, D_MODEL])
    din('ln_g', [DEPTH, 2, D_MODEL])
    din('ln_b', [DEPTH, 2, D_MODEL])
    din('ffn_w1', [1, D_MODEL, D_FF])
    din('ffn_w3', [1, D_MODEL, D_FF])
    din('ffn_w2', [1, D_FF, D_MODEL])
    din('router_w', [1, D_MODEL, N_EXP])
    din('router_b', [1, N_EXP])
    din('moe_w1', [1, N_EXP, D_MODEL, D_FF])
    din('moe_w3', [1, N_EXP, D_MODEL, D_FF])
    din('moe_w2', [1, N_EXP, D_FF, D_MODEL])
    din('w_in', [DEPTH, D_MODEL, D_PROJ])
    din('ident', [128, 128])
    din('maskL', [128, 128])
    din('maskU', [128, 128])
    din('antiI', [128, 128])
    din('poscols', [128, 4])
    din('rot_cos', [128, 32, 32])
    din('rot_sin', [128, 32, 32])
    din('gla_wa', [DEPTH, 2, 16, 128])
    din('gla_ba', [DEPTH, 2, 128])
    din('gla_ng', [DEPTH, 256])
    din('ret_decay', [DEPTH, 8])
    din('ret_dec_fm', [DEPTH, 2, 128, 2])
    din('ret_gng', [DEPTH, 256])
    din('ret_gnb', [DEPTH, 256])
    din('s5_lam_fm', [DEPTH, 128, 16, 2])
    din('s5_dt_fm', [DEPTH, 128, 16])
    din('s5_BT', [DEPTH, 16, 2, 128, 128])
    din('s5_CT', [DEPTH, 16, 2, 128, 32])
    din('s5_dcol', [DEPTH, 128, 2])
    din('s5_glub', [DEPTH, 128, 2])
    din('s5_glu_w', [DEPTH, 256, 256])
    din('iota_t', [T])
    din('hy_cw', [DEPTH, 128, 6, 3])
    din('hy_cb', [DEPTH, 128, 6])
    din('hy_biasc', [DEPTH, 128, 2])
    din('hy_fw1', [DEPTH, 33, 64])
    din('hy_fw2', [DEPTH, 64, 64])
    din('hy_fw3', [DEPTH, 64, 512])
    din('hy_fcol', [DEPTH, 64, 5])
    din('hy_decay', [DEPTH, 2, 256])
    for sfx, n in (('L', SEQ), ('C', CTXL)):
        nt = n // 128
        din('hy_zemb' + sfx, [33, n])
        din('hy_tn' + sfx, [128, nt])
        din('hy_wk' + sfx, [128, nt + 1])
        din('dftc' + sfx, [nt + 1, 128, nt + 1, 128], BF16)
        din('dfts' + sfx, [nt + 1, 128, nt + 1, 128], BF16)

    pst = ExitStack()
    K.es.enter_context(pst)

    def sbp(name, shape, dt=F32):
        return pst.enter_context(nc.sbuf_tensor(_nm(name), list(shape), dt))
    g.ident = sbp('ident', [128, 128]); g.t_const = Tok()
    g.identb = sbp('identb', [128, 128], BF16)
    g.maskL = sbp('maskL', [128, 128]); g.maskU = sbp('maskU', [128, 128]); g.antiI = sbp('antiI', [128, 128])
    g.antiIb = sbp('antiIb', [128, 128], BF16)
    g.mask2 = sbp('mask2', [128, 2, 128])
    g.ones = sbp('ones', [128, 128]); g.onesb = sbp('onesb', [128, 128], BF16)
    g.epsc = sbp('epsc', [128, 1])
    g.poscols = sbp('poscols', [128, 4])
    g.lnc = sbp('lnc', [128, 2])
    g.modT = sbp('modT', [128, 48, 2]); g.t_mod = Tok()
    g.t_gbc = Tok(); g.t_gbcd = Tok()
    g.mod1 = sbp('mod1', [128, 48, 2])
    NPS = 6
    g.psum = [pst.enter_context(nc.psum_tensor('ps%d' % i, [128, 512], F32)) for i in range(NPS)]
    g.pst = [Tok() for _ in range(NPS)]
    g.pnext = 0
    g.psb = [pst.enter_context(nc.psum_tensor('psb%d' % i, [128, 1024], BF16)) for i in range(2)]
    g.t_psb = [Tok(), Tok()]
    g.psb_next = 0

    def ps():
        i = g.pnext
        g.pnext = (i + 1) % NPS
        return g.psum[i], g.pst[i]
    g.ps = ps

    def psb_half():
        i = g.psb_next
        g.psb_next = 1 - i
        return g.psb[i][:, 0:512], g.t_psb[i]
    g.psb_half = psb_half

    tc = g.t_const
    K.dma(g.ident[:], g.D['ident'][:, :], W=[tc])
    K.dma(g.maskL[:], g.D['maskL'][:, :], W=[tc])
    K.dma(g.maskU[:], g.D['maskU'][:, :], W=[tc])
    K.dma(g.mask2[:, 0, :], g.D['maskL'][:, :], W=[tc])
    K.dma(g.mask2[:, 1, :], g.D['maskU'][:, :], W=[tc])
    K.dma(g.antiI[:], g.D['antiI'][:, :], W=[tc])
    K.dma(g.poscols[:], g.D['poscols'][:, :], W=[tc])
    K.op('pool', lambda e: e.memset(g.ones[:], 1.0), W=[tc])
    K.op('pool', lambda e: e.memset(g.onesb[:], 1.0), W=[tc])
    K.op('pool', lambda e: e.memset(g.epsc[:], LN_EPS), W=[tc])
    K.op('pool', lambda e: e.memset(g.lnc[:, 0:1], math.log(0.125)), W=[tc])
    K.op('pool', lambda e: e.memset(g.lnc[:, 1:2], math.log(32 ** -0.5)), W=[tc])
    K.op('dve', lambda e: e.tensor_copy(out=g.identb[:], in_=g.ident[:]), R=[tc], W=[tc])
    K.op('dve', lambda e: e.tensor_copy(out=g.antiIb[:], in_=g.antiI[:]), R=[tc], W=[tc])
    K.barrier()

    dscr('x_cur', [T, D_MODEL])

    dscr('uT_d', [NCH, 128, 8, 128], BF16)
    dscr('gbc_d', [128, 2, 2, D_MODEL])
    dscr('vT_d', [NCH, 128, 8, 128], BF16)
    dscr('mixT_d', [D_MODEL, T], BF16)
    dscr('x1_d', [T, D_MODEL])
    dscr('ffn_d', [T, D_MODEL])
    dscr('gates_d', [NCH, 128, N_EXP])
    dout('out', [SEQ, D_MODEL])
    g.t_uT = [Tok() for _ in range(NCH)]
    g.t_mix = Tok()
    t_vT = [Tok() for _ in range(NCH)]
    t_x1 = [Tok() for _ in range(NCH)]
    t_ffn = [Tok() for _ in range(NCH)]
    t_gates = [Tok() for _ in range(NCH)]
    t_xcur = [Tok() for _ in range(NCH)]
    t_out = Tok()
    D = g.D
    x_src, t_xsrc = D['xin'], None
    for l in range(DEPTH):
        last = l == DEPTH - 1
        phase_mod(g, l)
        K.barrier()
        if stop_after == ('mod', l):
            break
        phase_lnmod(g, l, x_src, D['uT_d'], g.t_uT, sh_idx=0, sc_idx=8, t_src=t_xsrc)
        K.barrier()
        if 'uT' in g.dbg and l == g.dbg_layer:
            o = dout('dbg_uT', [NCH, 128, 8, 128], BF16)
            K.dma(o[:, :, :, :], D['uT_d'][:, :, :, :], R=g.t_uT, q='pool')
            K.barrier()
        if stop_after == ('lnmod', l):
            break
        _ps = os.environ.get('LA_PASSES', 'g0,g1,r0,r1').split(',')
        for p in range(2):
            if 'g%d' % p in _ps:
                la_pass(g, l, 'gla', p, last)
                K.barrier()
        for p in range(2):
            if 'r%d' % p in _ps:
                la_pass(g, l, 'ret', p, last)
                K.barrier()
        if 's5' in os.environ.get('MIXERS', 's5,hy'):
            phase_s5(g, l)
            K.barrier()
        if 'hy' in os.environ.get('MIXERS', 's5,hy'):
            phase_hyena(g, l, SEQ, 2, 'L')
            K.barrier()
            if not last:
                phase_hyena(g, l, CTXL, 0, 'C')
                K.barrier()
        if 'mix' in g.dbg and l == g.dbg_layer:
            o = dout('dbg_mix', [D_MODEL, T], BF16)
            K.dma(o[:, :], D['mixT_d'][:, :], R=[g.t_mix], q='pool')
            K.barrier()
        if stop_after == ('la', l):
            break
        chunks = list(range(2, NCH)) if last else list(range(NCH))
        phase_wout(g, l, x_src, t_xsrc, chunks, D['x1_d'], t_x1)
        K.barrier()
        if 'gbc' in g.dbg and l == g.dbg_layer:
            o = dout('dbg_gbc', [128, 2, 2, D_MODEL])
            K.dma(o[:, :, :, :], D['gbc_d'][:, :, :, :], R=[g.t_gbcd], q='pool')
            K.barrier()
        if 'x1' in g.dbg and l == g.dbg_layer:
            o = dout('dbg_x1', [T, D_MODEL])
            K.dma(o[:, :], D['x1_d'][:, :], R=t_x1, q='pool')
            K.barrier()
        if stop_after == ('wout', l):
            break
        j = l // 2
        if l % 2 == 0:
            phase_lnmod(g, l, D['x1_d'], D['vT_d'], t_vT, sh_idx=24, sc_idx=32, chunks=chunks, t_src=t_x1)
            K.barrier()
            phase_ffn2(g, l, chunks, [(D['ffn_w1'][j], D['ffn_w3'][j], D['ffn_w2'][j])], D['vT_d'], t_vT, D['ffn_d'], t_ffn)
            K.barrier()
        else:
            phase_lnmod(g, l, D['x1_d'], D['vT_d'], t_vT, sh_idx=24, sc_idx=32, chunks=chunks, t_src=t_x1, router=(j, D['gates_d'], t_gates))
            K.barrier()
            phase_ffn2(g, l, chunks, [(D['moe_w1'][j, e_], D['moe_w3'][j, e_], D['moe_w2'][j, e_]) for e_ in range(N_EXP)],
                       D['vT_d'], t_vT, D['ffn_d'], t_ffn, gates=(D['gates_d'], t_gates))
            K.barrier()
        if 'ffn' in g.dbg and l == g.dbg_layer:
            o = dout('dbg_ffn', [T, D_MODEL])
            K.dma(o[:, :], D['ffn_d'][:, :], R=t_ffn, q='pool')
            K.barrier()
        if stop_after == ('ffn', l):
            break
        if last:
            def dst_fn(c):
                return g.outs['out'][(c - 2) * 128:(c - 1) * 128, :], t_out
        else:
            def dst_fn(c):
                return D['x_cur'][c * 128:(c + 1) * 128, :], t_xcur[c]
        phase_ln2(g, l, chunks, D['x1_d'], t_x1, D['ffn_d'], t_ffn, dst_fn)
        K.barrier()
        if 'x2' in g.dbg and l == g.dbg_layer and not last:
            o = dout('dbg_x2', [T, D_MODEL])
            K.dma(o[:, :], D['x_cur'][:, :], R=t_xcur, q='pool')
            K.barrier()
        if stop_after == ('ln2', l):
            break
        x_src, t_xsrc = D['x_cur'], t_xcur
    K.barrier()
    return nc, g


def phase_mod(g, l):
    nc, K = g.nc, g.K
    with ExitStack() as st:
        def sb(name, shape, dt=F32):
            return st.enter_context(nc.sbuf_tensor(_nm(name), list(shape), dt))
        cv = sb('cv', [128, 8, 2]); t_cv = Tok()
        sv = sb('sv', [128, 8, 2])
        abT = sb('abT', [128, 48]); t_ab = Tok()
        wbuf = [sb('adaw%d' % i, [128, 8, 512]) for i in range(2)]
        t_w = [Tok(), Tok()]
        K.dma(cv[:], g.D['cvec'][:, :, :], W=[t_cv])
        K.dma(abT[:], g.D['ada_bT'][l, :, :], W=[t_ab])
        K.op('act', lambda e: e.activation(out=sv[:], in_=cv[:], func=AF.Silu), R=[t_cv], W=[t_cv])
        wsrc = g.D['ada_w'][l].rearrange("(kt p) n -> p kt n", p=128)
        svrep = sb('svrep', [128, 8, 2, 128])
        g.gbc = sb('gbc', [128, 2, 2, D_MODEL])
        K.op('dve', lambda e: e.tensor_copy(out=svrep[:], in_=sv[:].unsqueeze(3).to_broadcast([128, 8, 2, 128])), R=[t_cv], W=[t_cv])
        K.dma(g.gbc[:, 0, 0, :], g.D['ada_b'][l, 2048:3072].partition_broadcast(128), W=[g.t_gbc])
        K.dma(g.gbc[:, 0, 1, :], g.D['ada_b'][l, 2048:3072].partition_broadcast(128), W=[g.t_gbc])
        K.dma(g.gbc[:, 1, 0, :], g.D['ada_b'][l, 5120:6144].partition_broadcast(128), W=[g.t_gbc])
        K.dma(g.gbc[:, 1, 1, :], g.D['ada_b'][l, 5120:6144].partition_broadcast(128), W=[g.t_gbc])
        for grp in range(12):
            b = grp % 2
            K.dma(wbuf[b][:], wsrc[:, :, grp * 512:(grp + 1) * 512], W=[t_w[b]])
            if grp in (4, 5, 10, 11):
                mf = 0 if grp < 6 else 1
                hf = grp % 2
                for cls in range(2):
                    pq, pqt = g.ps()
                    for kt in range(8):
                        K.op('pe', lambda e, kt=kt, b=b, cls=cls, pq=pq: e.matmul(pq[:, 0:512], lhsT=svrep[:, kt, cls, :], rhs=wbuf[b][:, kt, :],
                                                                                  start=(kt == 0), stop=(kt == 7)), R=[t_w[b], t_cv], W=[pqt])
                    dst = g.gbc[:, mf, cls, hf * 512:(hf + 1) * 512]
                    K.op('dve', lambda e, pq=pq, dst=dst: e.tensor_tensor(out=dst, in0=pq[:, 0:512], in1=dst, op=ALU.add), R=[pqt, g.t_gbc], W=[g.t_gbc])
            pt, ptk = g.ps()
            for jj in range(4):
                for kt in range(8):
                    K.op('pe', lambda e, jj=jj, kt=kt, b=b, pt=pt: e.matmul(
                        pt[:, 2 * jj:2 * jj + 2], lhsT=wbuf[b][:, kt, jj * 128:(jj + 1) * 128], rhs=sv[:, kt, :],
                        start=(kt == 0), stop=(kt == 7)), R=[t_w[b], t_cv], W=[ptk])
            K.op('dve', lambda e, pt=pt, grp=grp: e.tensor_tensor(
                out=g.modT[:, grp * 4:(grp + 1) * 4, :], in0=pt[:, 0:8].rearrange("p (j k) -> p j k", k=2),
                in1=abT[:, grp * 4:(grp + 1) * 4].unsqueeze(2).to_broadcast([128, 4, 2]), op=ALU.add), R=[ptk, t_ab], W=[g.t_mod])
        K.op('dve', lambda e: e.tensor_scalar_add(out=g.mod1[:], in0=g.modT[:], scalar1=1.0), R=[g.t_mod], W=[g.t_mod])
        K.dma(g.D['gbc_d'][:, :, :, :], g.gbc[:], R=[g.t_gbc], W=[g.t_gbcd], q='pool')
        K.barrier()


def ln_stats(g, st_tiles, xc, t_xc, eps=LN_EPS):
    K = g.K
    stt, mv, lnv, rstd, tok = st_tiles
    for h in range(2):
        K.op('dve', lambda e, h=h: e.bn_stats(out=stt[:, h, :], in_=xc[:, h * 512:(h + 1) * 512]), R=[t_xc], W=[tok])
    K.op('dve', lambda e: e.bn_aggr(out=mv[:], in_=stt[:].rearrange("p a b -> p (a b)")), R=[tok], W=[tok])
    K.op('act', lambda e: e.activation(out=lnv[:], in_=mv[:, 1:2], func=AF.Ln, bias=g.epsc[:, 0:1]), R=[tok, g.t_const], W=[tok])
    K.op('act', lambda e: e.activation(out=rstd[:], in_=lnv[:], func=AF.Exp, scale=-0.5), R=[tok], W=[tok])
    return mv, rstd, tok


def phase_lnmod(g, l, x_src, uT_d, t_uT, sh_idx, sc_idx, chunks=None, t_src=None, router=None):
    nc, K = g.nc, g.K
    with ExitStack() as st:
        def sb(name, shape, dt=F32):
            return st.enter_context(nc.sbuf_tensor(_nm(name), list(shape), dt))
        xc = [sb('xc%d' % i, [128, 1024]) for i in range(2)]
        t_xc = [Tok(), Tok()]
        xn = [sb('xn%d' % i, [128, 1024]) for i in range(2)]
        t_xn = [Tok(), Tok()]
        uc = [sb('uc%d' % i, [128, 8, 128], BF16) for i in range(2)]
        t_uc = [Tok(), Tok()]
        sts = [(sb('stt%d' % i, [128, 2, 6]), sb('mv%d' % i, [128, 2]), sb('lnv%d' % i, [128, 1]), sb('rstd%d' % i, [128, 1]), Tok())
               for i in range(2)]
        if router is not None:
            j_moe, gates_d, t_gates = router
            uc32 = [sb('uc32_%d' % i, [128, 8, 128]) for i in range(2)]; t_uc32 = [Tok(), Tok()]
            rw = sb('rw', [128, 8, N_EXP]); rb = sb('rb', [128, N_EXP]); t_rw = Tok()
            K.dma(rw[:], g.D['router_w'][j_moe].rearrange("(kt p) n -> p kt n", p=128), W=[t_rw])
            K.dma(rb[:], g.D['router_b'][j_moe].partition_broadcast(128), W=[t_rw])
            rl = [sb('rl%d' % i, [128, 6, N_EXP]) for i in range(2)]; rs = [sb('rs%d' % i, [128, 4]) for i in range(2)]; t_rl = [Tok(), Tok()]
        for n, c in enumerate(chunks if chunks is not None else range(NCH)):
            b = n % 2
            col = 1 if c < 2 else 0
            K.dma(xc[b][:], x_src[c * 128:(c + 1) * 128, :], R=([t_src[c]] if t_src is not None else []), W=[t_xc[b]])
            mv, rstd, tk = ln_stats(g, sts[b], xc[b], t_xc[b])
            K.op('dve', lambda e, b=b, mv=mv, rstd=rstd: e.tensor_scalar(
                out=xn[b][:], in0=xc[b][:], scalar1=mv[:, 0:1], scalar2=rstd[:, 0:1], op0=ALU.subtract, op1=ALU.mult),
                R=[t_xc[b], tk], W=[t_xn[b]])
            for half in range(2):
                pt, ptk = g.ps()
                for q in range(4):
                    kt = half * 4 + q
                    K.op('pe', lambda e, b=b, kt=kt, q=q, pt=pt: e.transpose(
                        out=pt[:, q * 128:(q + 1) * 128], in_=xn[b][:, kt * 128:(kt + 1) * 128], identity=g.ident[:]),
                        R=[t_xn[b], g.t_const], W=[ptk])
                for q in range(4):
                    kt = half * 4 + q
                    dst, t_dst = (uc[b], t_uc[b]) if router is None else (uc32[b], t_uc32[b])
                    K.op('act', lambda e, kt=kt, q=q, pt=pt, dst=dst, col=col: e.activation(
                        out=dst[:, kt, :], in_=pt[:, q * 128:(q + 1) * 128], func=AF.Identity,
                        scale=g.mod1[:, sc_idx + kt, col:col + 1], bias=g.modT[:, sh_idx + kt, col:col + 1]),
                        R=[ptk, g.t_mod], W=[t_dst])
            if router is not None:
                K.op('pool', lambda e, b=b: e.tensor_copy(out=uc[b][:], in_=uc32[b][:]), R=[t_uc32[b]], W=[t_uc[b]])
                pr, prt = g.ps()
                for kt in range(8):
                    K.op('pe', lambda e, kt=kt, b=b, pr=pr: e.matmul(pr[:, 0:N_EXP], lhsT=uc32[b][:, kt, :], rhs=rw[:, kt, :], start=(kt == 0), stop=(kt == 7)),
                         R=[t_uc32[b], t_rw], W=[prt])
                L_, r4, tk2 = rl[b], rs[b], t_rl[b]
                K.op('dve', lambda e, pr=pr, L_=L_: e.tensor_tensor(out=L_[:, 0, :], in0=pr[:, 0:N_EXP], in1=rb[:], op=ALU.add), R=[prt, t_rw], W=[tk2])
                K.op('dve', lambda e, L_=L_, r4=r4: e.tensor_reduce(out=r4[:, 0:1], in_=L_[:, 0, :], axis=AX.X, op=ALU.max), R=[tk2], W=[tk2])
                K.op('dve', lambda e, L_=L_, r4=r4: e.tensor_scalar(out=L_[:, 1, :], in0=L_[:, 0, :], scalar1=r4[:, 0:1], scalar2=None, op0=ALU.is_equal), R=[tk2], W=[tk2])
                K.op('dve', lambda e, L_=L_: e.scalar_tensor_tensor(out=L_[:, 2, :], in0=L_[:, 1, :], scalar=-1e30, in1=L_[:, 0, :], op0=ALU.mult, op1=ALU.add), R=[tk2], W=[tk2])
                K.op('dve', lambda e, L_=L_, r4=r4: e.tensor_reduce(out=r4[:, 1:2], in_=L_[:, 2, :], axis=AX.X, op=ALU.max), R=[tk2], W=[tk2])
                K.op('dve', lambda e, L_=L_, r4=r4: e.tensor_scalar(out=L_[:, 3, :], in0=L_[:, 0, :], scalar1=r4[:, 1:2], scalar2=None, op0=ALU.is_ge), R=[tk2], W=[tk2])
                K.op('dve', lambda e, r4=r4: e.tensor_scalar_mul(out=r4[:, 2:3], in0=r4[:, 0:1], scalar1=-1.0), R=[tk2], W=[tk2])
                K.op('act', lambda e, L_=L_, r4=r4: e.activation(out=L_[:, 4, :], in_=L_[:, 0, :], func=AF.Exp, bias=r4[:, 2:3]), R=[tk2], W=[tk2])
                K.op('dve', lambda e, L_=L_: e.tensor_tensor(out=L_[:, 4, :], in0=L_[:, 4, :], in1=L_[:, 3, :], op=ALU.mult), R=[tk2], W=[tk2])
                K.op('dve', lambda e, L_=L_, r4=r4: e.tensor_reduce(out=r4[:, 3:4], in_=L_[:, 4, :], axis=AX.X, op=ALU.add), R=[tk2], W=[tk2])
                K.op('dve', lambda e, r4=r4: e.reciprocal(out=r4[:, 3:4], in_=r4[:, 3:4]), R=[tk2], W=[tk2])
                K.op('dve', lambda e, L_=L_, r4=r4: e.tensor_scalar_mul(out=L_[:, 5, :], in0=L_[:, 4, :], scalar1=r4[:, 3:4]), R=[tk2], W=[tk2])
                K.dma(gates_d[c, :, :], L_[:, 5, :], R=[tk2], W=[t_gates[c]], q='pool')
            K.dma(uT_d[c, :, :, :], uc[b][:], R=[t_uc[b]], W=[t_uT[c]], q='pool')


def const_inputs():
    idx = np.arange(128)
    c = {}
    c['ident'] = np.eye(128, dtype=np.float32)
    c['maskL'] = (idx[:, None] <= idx[None, :]).astype(np.float32)
    c['maskU'] = (idx[:, None] >= idx[None, :]).astype(np.float32)
    c['antiI'] = np.ascontiguousarray(np.eye(128, dtype=np.float32)[::-1])
    i = idx.astype(np.float32)
    c['poscols'] = np.stack([i + 1, 128 - i, -(i + 1), -(128 - i)], axis=1).astype(np.float32)
    tpos = np.arange(SEQ)
    row = (tpos // 64).astype(np.float32)
    colp = (tpos % 64).astype(np.float32)
    n_freq = 16
    inv = (10000.0 ** (-np.arange(n_freq, dtype=np.float32) / n_freq)).astype(np.float32)
    ang = np.concatenate([row[:, None] * inv, colp[:, None] * inv], axis=-1).astype(np.float32)
    for sfx, n in (('L', SEQ), ('C', CTXL)):
        nt = n // 128
        tlin = np.linspace(0.0, 1.0, n, dtype=np.float32)
        ii = np.arange(n, dtype=np.float32)[:, None]
        bands = np.linspace(1e-4, 15, 16, dtype=np.float32)[None, :]
        ang2 = (np.float32(2.0 * math.pi / n) * bands * ii).astype(np.float32)
        zemb = np.concatenate([tlin[:, None], np.cos(ang2), -np.sin(ang2)], axis=-1).astype(np.float32)
        c['hy_zemb' + sfx] = np.ascontiguousarray(zemb.T)
        c['hy_tn' + sfx] = np.ascontiguousarray(tlin.reshape(nt, 128).T)
        N2 = 2 * n
        kk = np.arange((nt + 1) * 128)
        wkv = np.where(kk > n, 0.0, np.where((kk == 0) | (kk == n), 1.0 / N2, 2.0 / N2)).astype(np.float32)
        c['hy_wk' + sfx] = np.ascontiguousarray(wkv.reshape(nt + 1, 128).T)
        a = kk.reshape(nt + 1, 128)
        prod = (a.T[None, :, :, None].astype(np.int64) * a[:, None, None, :].astype(np.int64)) % N2
        lut_c = np.cos(2.0 * np.pi * np.arange(N2) / N2)
        lut_s = np.sin(2.0 * np.pi * np.arange(N2) / N2)
        valid = (a.T[None, :, :, None] <= n) & (a[:, None, None, :] <= n)
        c['dftc' + sfx] = np.where(valid, lut_c[prod], 0.0).astype(ml_dtypes.bfloat16)
        c['dfts' + sfx] = np.where(valid, lut_s[prod], 0.0).astype(ml_dtypes.bfloat16)
    c['rot_cos'] = np.ascontiguousarray(np.cos(ang).astype(np.float32).reshape(32, 128, 32).transpose(1, 0, 2))
    c['rot_sin'] = np.ascontiguousarray(np.sin(ang).astype(np.float32).reshape(32, 128, 32).transpose(1, 0, 2))
    return c


def prep_core_inputs(inp, b, shared):
    m = dict(shared)
    m['xin'] = np.ascontiguousarray(np.concatenate([inp['ctx'][b], inp['x'][b]], axis=0))
    cv = np.stack([inp['c'][b], inp['c_ctx']], axis=-1)
    m['cvec'] = np.ascontiguousarray(cv.reshape(8, 128, 2).transpose(1, 0, 2))
    return m


def prep_shared(inp):
    m = const_inputs()
    m['gla_wa'] = inp['gla_wa']
    m['gla_ba'] = inp['gla_ba']
    m['gla_ng'] = inp['gla_norm_g']
    m['ret_decay'] = np.ascontiguousarray(inp['ret_decay'].reshape(DEPTH, 8))
    rd = inp['ret_decay']
    fm = np.zeros((DEPTH, 2, 128, 2), np.float32)
    for p in range(2):
        for h2 in range(2):
            fm[:, p, h2 * 64:(h2 + 1) * 64, :] = rd[:, :, 2 * p + h2][:, None, :]
    m['ret_dec_fm'] = fm
    m['ret_gng'] = inp['ret_gn_g']
    L = DEPTH
    lam = np.stack([inp['s5_lam_re'], inp['s5_lam_im']], axis=-1)
    lam = lam.reshape(L, 2, 8, 2, 64, 2)
    m['s5_lam_fm'] = np.ascontiguousarray(lam.transpose(0, 3, 4, 1, 2, 5).reshape(L, 128, 16, 2))
    ldt = np.broadcast_to(inp['s5_log_dt'].reshape(L, 2, 8, 2, 1), (L, 2, 8, 2, 64))
    m['s5_dt_fm'] = np.ascontiguousarray(ldt.transpose(0, 3, 4, 1, 2).reshape(L, 128, 16))
    BT = np.zeros((L, 2, 8, 2, 128, 128), np.float32)
    CT = np.zeros((L, 2, 8, 2, 128, 32), np.float32)
    for j in range(8):
        for g2 in range(2):
            gi = 2 * j + g2
            r0 = (gi % 8) * 16
            for ri, (bn, cn) in enumerate((('s5_b_re', 's5_c_re'), ('s5_b_im', 's5_c_im'))):
                BT[:, :, j, ri, r0:r0 + 16, g2 * 64:(g2 + 1) * 64] = inp[bn][:, :, gi].transpose(0, 1, 3, 2)
                CT[:, :, j, ri, g2 * 64:(g2 + 1) * 64, g2 * 16:(g2 + 1) * 16] = inp[cn][:, :, gi].transpose(0, 1, 3, 2)
    m['s5_BT'] = BT.reshape(L, 16, 2, 128, 128)
    m['s5_CT'] = CT.reshape(L, 16, 2, 128, 32)
    m['s5_dcol'] = np.ascontiguousarray(inp['s5_d'].reshape(L, 2, 128).transpose(0, 2, 1))
    m['s5_glub'] = np.ascontiguousarray(inp['s5_glu_b'].reshape(L, 2, 128).transpose(0, 2, 1))
    m['s5_glu_w'] = inp['s5_glu_w']
    m['iota_t'] = np.arange(T, dtype=np.float32)
    m['hy_cw'] = np.ascontiguousarray(inp['hy_conv_w'].reshape(L, 3, 6, 128).transpose(0, 3, 2, 1))
    m['hy_cb'] = np.ascontiguousarray(inp['hy_conv_b'].reshape(L, 6, 128).transpose(0, 2, 1))
    m['hy_biasc'] = np.ascontiguousarray(inp['hy_bias'].reshape(L, 2, 128).transpose(0, 2, 1))
    m['hy_fw1'] = inp['hy_fw1']; m['hy_fw2'] = inp['hy_fw2']; m['hy_fw3'] = inp['hy_fw3']
    fc = np.zeros((L, 64, 5), np.float32)
    fc[:, :, 0] = inp['hy_fb1']; fc[:, :, 1] = inp['hy_fb2']; fc[:, :, 2] = inp['hy_freq']
    m['hy_fcol'] = fc
    m['hy_decay'] = inp['hy_decay']
    m['ret_gnb'] = inp['ret_gn_b']
    m['ada_w'] = inp['ada_w']
    m['ada_b'] = inp['ada_b']
    m['w_out'] = inp['w_out']
    m['ln_g'] = np.ascontiguousarray(np.stack([inp['ln_mix_g'], inp['ln_ffn_g']], axis=1))
    m['ln_b'] = np.ascontiguousarray(np.stack([inp['ln_mix_b'], inp['ln_ffn_b']], axis=1))
    for k_ in ('ffn_w1', 'ffn_w3', 'ffn_w2', 'router_w', 'router_b', 'moe_w1', 'moe_w3', 'moe_w2'):
        m[k_] = inp[k_]
    m['ada_bT'] = np.ascontiguousarray(inp['ada_b'].reshape(DEPTH, 48, 128).transpose(0, 2, 1))
    m['w_in'] = inp['w_in']
    return m


def load_w_bf16(g, st, wsrc_cols, ncols_total, name):
    nc, K = g.nc, g.K
    wb = st.enter_context(nc.sbuf_tensor(_nm(name), [128, 8, ncols_total], BF16))
    t_w = Tok()
    with ExitStack() as st2:
        o = 0
        k = 0
        for (src, c0, n) in wsrc_cols:
            if src is None:
                K.op('pool', lambda e, o=o, n=n: e.memset(wb[:, :, o:o + n], 0.0), W=[t_w])
                o += n
                continue
            view = src.rearrange("(kt p) n -> p kt n", p=128)
            done = 0
            while done < n:
                m = min(256, n - done)
                stg = st2.enter_context(nc.sbuf_tensor(_nm('wstg'), [128, 8, m], F32))
                ts = Tok()
                K.dma(stg[:], view[:, :, c0 + done:c0 + done + m], W=[ts])
                eng = ('dve', 'pool')[k % 2]
                K.op(eng, lambda e, stg=stg, o=o, m=m: e.tensor_copy(out=wb[:, :, o:o + m], in_=stg[:]), R=[ts], W=[t_w])
                o += m
                done += m
                k += 1
        K.barrier()
    return wb, t_w


def la_pass(g, l, kind, p, last):
    nc, K, D = g.nc, g.K, g.D
    gla = kind == 'gla'
    H = 2
    dk = 64
    NV = H * 64
    row0 = (256 if gla else 0) + p * 128
    with ExitStack() as st:
        def sb(name, shape, dt=F32):
            return st.enter_context(nc.sbuf_tensor(_nm(name), list(shape), dt))
        W = D['w_in'][l]
        if gla:
            cols = []
            for nm_ in ('gla_q', 'gla_k'):
                for h2 in range(2):
                    cols.append((W, OFF[nm_] + (2 * p + h2) * 32, 32))
                    cols.append((None, 0, 32))
            cols += [(W, OFF['gla_v'] + 128 * p, 128), (W, OFF['gla_r'] + 128 * p, 128), (W, OFF['gla_a'], 32)]
            ncols = 544
        else:
            cols = [(W, OFF['ret_q'] + 128 * p, 128), (W, OFF['ret_k'] + 128 * p, 128),
                    (W, OFF['ret_v'] + 128 * p, 128), (W, OFF['ret_g'] + 128 * p, 128)]
            ncols = 512
        wb, t_w = load_w_bf16(g, st, cols, ncols, 'w_la')
        LT = sb('LT', [128, 4, T], BF16)
        t_LT = [Tok() for _ in range(NCH)]
        vtok = sb('vtok', [128, NCH, NV], BF16); t_v = [Tok() for _ in range(NCH)]
        sgtok = sb('sgtok', [128, NCH, NV], BF16); t_sg = [Tok() for _ in range(NCH)]
        KVGb = sb('KVGb', [128, NCH, 64]); t_kvb = [Tok() for _ in range(NCH)]
        Sbf = sb('Sbf', [128, NCH, 2, 64], BF16); t_S = [[Tok(), Tok()] for _ in range(NCH)]
        Sf = sb('Sf', [128, 64]); Sb = sb('Sb', [128, 64]); t_Sf = Tok(); t_Sb = Tok()
        Gall = sb('Gall', [128, NCH, 2]); t_G = [Tok() for _ in range(NCH)]
        t_par = Tok()
        if gla:
            wa = sb('wa', [16, 2, 128]); ba = sb('ba', [1, 2, 128])
            K.op('pool', lambda e: e.memset(wa[:], 0.0), W=[t_par])
            K.op('pool', lambda e: e.memset(ba[:], 0.0), W=[t_par])
            for h2 in range(2):
                hh = (2 * p + h2) * 32
                K.dma(wa[:, :, h2 * 64:h2 * 64 + 32], D['gla_wa'][l, :, :, hh:hh + 32].rearrange("d r n -> r d n"), W=[t_par])
                K.dma(ba[:, :, h2 * 64:h2 * 64 + 32], D['gla_ba'][l:l + 1, :, hh:hh + 32], W=[t_par])
            ngb = sb('ngb', [128, NV])
            K.dma(ngb[:], D['gla_ng'][l, p * 128:(p + 1) * 128].partition_broadcast(128), W=[t_par])
        else:
            gng = sb('gng', [128, NV]); gnb = sb('gnb', [128, NV])
            K.dma(gng[:], D['ret_gng'][l, p * 128:(p + 1) * 128].partition_broadcast(128), W=[t_par])
            K.dma(gnb[:], D['ret_gnb'][l, p * 128:(p + 1) * 128].partition_broadcast(128), W=[t_par])
            dtm = sb('dtm', [128, 2, 4]); dfm = sb('dfm', [128, 2])
            K.dma(dtm[:].rearrange("p a b -> p (a b)"), D['ret_decay'][l].partition_broadcast(128), W=[t_par])
            K.dma(dfm[:], D['ret_dec_fm'][l, p, :, :], W=[t_par])
            K.op('act', lambda e: e.activation(out=dtm[:], in_=dtm[:], func=AF.Exp, scale=-1.0), R=[t_par], W=[t_par])
            K.op('act', lambda e: e.activation(out=dtm[:], in_=dtm[:], func=AF.Ln, bias=1.0), R=[t_par], W=[t_par])
            K.op('act', lambda e: e.activation(out=dfm[:], in_=dfm[:], func=AF.Exp, scale=-1.0), R=[t_par], W=[t_par])
            K.op('act', lambda e: e.activation(out=dfm[:], in_=dfm[:], func=AF.Ln, bias=1.0), R=[t_par], W=[t_par])
            argt = sb('argt', [128, 2, 2])
            K.op('dve', lambda e: e.tensor_scalar_mul(out=argt[:, 0, :], in0=dtm[:, 0, 2 * p:2 * p + 2], scalar1=g.poscols[:, 2:3]),
                 R=[t_par, g.t_const], W=[t_par])
            K.op('dve', lambda e: e.tensor_scalar_mul(out=argt[:, 1, :], in0=dtm[:, 1, 2 * p:2 * p + 2], scalar1=g.poscols[:, 3:4]),
                 R=[t_par, g.t_const], W=[t_par])
            EpR = sb('EpR', [128, 2, 2]); EmR = sb('EmR', [128, 2, 2])
            K.op('act', lambda e: e.activation(out=EpR[:], in_=argt[:], func=AF.Exp), R=[t_par], W=[t_par])
            K.op('act', lambda e: e.activation(out=EmR[:], in_=argt[:], func=AF.Exp, scale=-1.0, bias=g.lnc[:, 0:1]), R=[t_par, g.t_const], W=[t_par])
            Gret = sb('Gret', [128, 2])
            K.op('act', lambda e: e.activation(out=Gret[:], in_=dfm[:], func=AF.Exp, scale=-128.0), R=[t_par], W=[t_par])
            cosT = sb('cosT', [128, 32, 32]); sinT = sb('sinT', [128, 32, 32])
            K.dma(cosT[:], D['rot_cos'][:, :, :], W=[t_par])
            K.dma(sinT[:], D['rot_sin'][:, :, :], W=[t_par])

        def Gap(c, d):
            return (Gall[:, c, d:d + 1], t_G[c]) if gla else (Gret[:, d:d + 1], t_par)

        def dbl(name, shape, dt=F32):
            return [sb(name + str(i), shape, dt) for i in range(2)], [Tok(), Tok()]
        uc, t_uc = dbl('ucl', [128, 8, 128], BF16)
        qk, t_qk = dbl('qk', [128, 256])
        rt, t_rt = dbl('rt', [128, 4, 4, 32])
        a_sb, t_a = dbl('a_sb', [128, 32])
        aT, t_aT = dbl('aT', [16, 2, 128])
        sp, t_sp = dbl('sp', [128, 256])
        Ep, t_Ep = dbl('Ep', [128, 256]); Em, t_Em = dbl('Em', [128, 256])
        qkt, t_qkt = dbl('qkt', [128, 4, 128], BF16)
        K.op('pool', lambda e: e.memset(Sf[:], 0.0), W=[t_Sf])
        K.op('pool', lambda e: e.memset(Sb[:], 0.0), W=[t_Sb])

        _dbg = os.environ.get('LA_DEBUG', '')
        _n1 = int(_dbg.split(',')[0]) if _dbg else NCH
        _p2 = int(_dbg.split(',')[1]) if _dbg else 1
        _lvl = int(_dbg.split(',')[2]) if _dbg else 99
        def stA(c):
            b = c % 2
            K.dma(uc[b][:], D['uT_d'][c, :, :, :], R=[g.t_uT[c]], W=[t_uc[b]])
            pa, pat = g.ps()
            for kt in range(8):
                K.op('pe', lambda e, kt=kt, b=b, pa=pa: e.matmul(pa[:, 0:512], lhsT=uc[b][:, kt, :], rhs=wb[:, kt, 0:512],
                                                                  start=(kt == 0), stop=(kt == 7)), R=[t_uc[b], t_w], W=[pat])
            if gla:
                pb, pbt = g.ps()
                for kt in range(8):
                    K.op('pe', lambda e, kt=kt, b=b, pb=pb: e.matmul(pb[:, 0:32], lhsT=uc[b][:, kt, :], rhs=wb[:, kt, 512:544],
                                                                      start=(kt == 0), stop=(kt == 7)), R=[t_uc[b], t_w], W=[pbt])
            K.op('act', lambda e, b=b, pa=pa: e.activation(out=qk[b][:], in_=pa[:, 0:256], func=AF.Identity), R=[pat], W=[t_qk[b]])
            K.op('act', lambda e, c=c, pa=pa: e.activation(out=vtok[:, c, :], in_=pa[:, 256:384], func=AF.Identity), R=[pat], W=[t_v[c]])
            K.op('act', lambda e, c=c, pa=pa: e.activation(out=sgtok[:, c, :], in_=pa[:, 384:512], func=AF.Silu), R=[pat], W=[t_sg[c]])
            if gla:
                K.op('dve', lambda e, b=b, pb=pb: e.tensor_copy(out=a_sb[b][:], in_=pb[:, 0:32]), R=[pbt], W=[t_a[b]])

        def stB(c):
            b = c % 2
            if gla:
                pt, ptk = g.ps()
                for d in range(2):
                    K.op('pe', lambda e, d=d, b=b, pt=pt: e.transpose(out=pt[0:16, d * 128:(d + 1) * 128], in_=a_sb[b][:, d * 16:(d + 1) * 16],
                                                                       identity=g.ident[:]), R=[t_a[b], g.t_const], W=[ptk])
                K.op('dve', lambda e, b=b, pt=pt: e.tensor_copy(out=aT[b][:].rearrange("r d n -> r (d n)"), in_=pt[0:16, 0:256]), R=[ptk], W=[t_aT[b]])
                pg, pgt = g.ps()
                for d in range(2):
                    K.op('pe', lambda e, d=d, b=b, pg=pg: e.matmul(pg[:, d * 128:(d + 1) * 128], lhsT=aT[b][:, d, :], rhs=wa[:, d, :],
                                                                    start=True, stop=False), R=[t_aT[b], t_par], W=[pgt])
                    K.op('pe', lambda e, d=d, pg=pg: e.matmul(pg[:, d * 128:(d + 1) * 128], lhsT=g.ones[0:1, :], rhs=ba[:, d, :],
                                                               start=False, stop=True), R=[t_par, g.t_const], W=[pgt])
                K.op('act', lambda e, b=b, pg=pg: e.activation(out=sp[b][:], in_=pg[:, 0:256], func=AF.Exp, scale=-1.0), R=[pgt], W=[t_sp[b]])
                K.op('act', lambda e, b=b: e.activation(out=sp[b][:], in_=sp[b][:], func=AF.Ln, bias=1.0), R=[t_sp[b]], W=[t_sp[b]])
                pc, pct = g.ps()
                K.op('pe', lambda e, b=b, pc=pc: e.matmul(pc[:, 0:128], lhsT=g.maskL[:], rhs=sp[b][:, 0:128], start=True, stop=True),
                     R=[t_sp[b], g.t_const], W=[pct])
                K.op('pe', lambda e, b=b, pc=pc: e.matmul(pc[:, 128:256], lhsT=g.maskU[:], rhs=sp[b][:, 128:256], start=True, stop=True),
                     R=[t_sp[b], g.t_const], W=[pct])
                K.op('pe', lambda e, b=b, pc=pc: e.matmul(pc[:, 256:257], lhsT=sp[b][:, 0:128], rhs=g.ones[:, 0:1], start=True, stop=True),
                     R=[t_sp[b], g.t_const], W=[pct])
                K.op('pe', lambda e, b=b, pc=pc: e.matmul(pc[:, 257:258], lhsT=sp[b][:, 128:256], rhs=g.ones[:, 0:1], start=True, stop=True),
                     R=[t_sp[b], g.t_const], W=[pct])
                K.op('act', lambda e, b=b, pc=pc: e.activation(out=Ep[b][:], in_=pc[:, 0:256], func=AF.Exp, scale=-1.0 / 16, bias=g.lnc[:, 1:2]),
                     R=[pct, g.t_const], W=[t_Ep[b]])
                K.op('act', lambda e, b=b, pc=pc: e.activation(out=Em[b][:], in_=pc[:, 0:256], func=AF.Exp, scale=1.0 / 16), R=[pct], W=[t_Em[b]])
                K.op('act', lambda e, c=c, pc=pc: e.activation(out=Gall[:, c, :], in_=pc[:, 256:258], func=AF.Exp, scale=-1.0 / 16), R=[pct], W=[t_G[c]])
                K.op('dve', lambda e, b=b: e.tensor_tensor(out=qkt[b][:, 0:2, :], in0=qk[b][:, 0:128].unsqueeze(1).to_broadcast([128, 2, 128]),
                                                            in1=Ep[b][:].rearrange("p (d n) -> p d n", d=2), op=ALU.mult),
                     R=[t_qk[b], t_Ep[b]], W=[t_qkt[b]])
                K.op('dve', lambda e, b=b: e.tensor_tensor(out=qkt[b][:, 2:4, :], in0=qk[b][:, 128:256].unsqueeze(1).to_broadcast([128, 2, 128]),
                                                            in1=Em[b][:].rearrange("p (d n) -> p d n", d=2), op=ALU.mult),
                     R=[t_qk[b], t_Em[b]], W=[t_qkt[b]])
            else:
                if c >= 2:
                    v4 = qk[b][:].rearrange("p (a s f) -> p a s f", a=4, s=2)
                    t1, t2 = v4[:, :, 0, :], v4[:, :, 1, :]
                    cs = cosT[:, c - 2, :].unsqueeze(1).to_broadcast([128, 4, 32])
                    sn = sinT[:, c - 2, :].unsqueeze(1).to_broadcast([128, 4, 32])
                    r = rt[b]
                    K.op('dve', lambda e, r=r, t1=t1, cs=cs: e.tensor_tensor(out=r[:, 0], in0=t1, in1=cs, op=ALU.mult), R=[t_qk[b], t_par], W=[t_rt[b]])
                    K.op('pool', lambda e, r=r, t2=t2, sn=sn: e.tensor_tensor(out=r[:, 1], in0=t2, in1=sn, op=ALU.mult), R=[t_qk[b], t_par], W=[t_rt[b]])
                    K.op('dve', lambda e, r=r, t1=t1, sn=sn: e.tensor_tensor(out=r[:, 2], in0=t1, in1=sn, op=ALU.mult), R=[t_qk[b], t_par], W=[t_rt[b]])
                    K.op('pool', lambda e, r=r, t2=t2, cs=cs: e.tensor_tensor(out=r[:, 3], in0=t2, in1=cs, op=ALU.mult), R=[t_qk[b], t_par], W=[t_rt[b]])
                    K.op('dve', lambda e, r=r, t1=t1: e.tensor_tensor(out=t1, in0=r[:, 0], in1=r[:, 1], op=ALU.subtract), R=[t_rt[b]], W=[t_qk[b]])
                    K.op('dve', lambda e, r=r, t2=t2: e.tensor_tensor(out=t2, in0=r[:, 2], in1=r[:, 3], op=ALU.add), R=[t_rt[b]], W=[t_qk[b]])
                K.op('dve', lambda e, b=b: e.tensor_tensor(
                    out=qkt[b][:, 0:2, :].rearrange("p d (h f) -> p d h f", h=2),
                    in0=qk[b][:, 0:128].rearrange("p (h f) -> p h f", h=2).unsqueeze(1).to_broadcast([128, 2, 2, 64]),
                    in1=EpR[:].unsqueeze(3).to_broadcast([128, 2, 2, 64]), op=ALU.mult), R=[t_qk[b], t_par], W=[t_qkt[b]])
                K.op('dve', lambda e, b=b: e.tensor_tensor(
                    out=qkt[b][:, 2:4, :].rearrange("p d (h f) -> p d h f", h=2),
                    in0=qk[b][:, 128:256].rearrange("p (h f) -> p h f", h=2).unsqueeze(1).to_broadcast([128, 2, 2, 64]),
                    in1=EmR[:].unsqueeze(3).to_broadcast([128, 2, 2, 64]), op=ALU.mult), R=[t_qk[b], t_par], W=[t_qkt[b]])

        def stC(c):
            b = c % 2
            pbh, pbht = g.psb_half()
            for s_ in range(4):
                K.op('pe', lambda e, s_=s_, b=b, pbh=pbh: e.transpose(out=pbh[:, s_ * 128:(s_ + 1) * 128], in_=qkt[b][:, s_, :], identity=g.identb[:]),
                     R=[t_qkt[b], g.t_const], W=[pbht])
            K.op('act', lambda e, c=c, pbh=pbh: e.activation(out=LT[:, :, c * 128:(c + 1) * 128], in_=pbh.rearrange("p (s n) -> p s n", s=4),
                                                              func=AF.Identity), R=[pbht], W=[t_LT[c]])
            pk, pkt = g.ps()
            for d in range(2):
                for h in range(H):
                    K.op('pe', lambda e, d=d, h=h, b=b, c=c, pk=pk: e.matmul(
                        pk[h * dk:(h + 1) * dk, d * 64:(d + 1) * 64], lhsT=qkt[b][:, 2 + d, h * dk:(h + 1) * dk],
                        rhs=vtok[:, c, h * 64:(h + 1) * 64], start=True, stop=True), R=[t_qkt[b], t_v[c]], W=[pkt])
            Gf, tGf = Gap(c, 0)
            Gb, tGb = Gap(c, 1)
            K.op('dve', lambda e, c=c: e.tensor_copy(out=Sbf[:, c, 0, :], in_=Sf[:]), R=[t_Sf], W=[t_S[c][0]])
            K.op('dve', lambda e, pk=pk: e.tensor_tensor(out=Sf[:], in0=Sf[:], in1=pk[:, 0:64], op=ALU.add), R=[pkt, t_Sf], W=[t_Sf])
            K.op('dve', lambda e, Gf=Gf: e.tensor_scalar_mul(out=Sf[:], in0=Sf[:], scalar1=Gf), R=[t_Sf, tGf], W=[t_Sf])
            K.op('dve', lambda e, c=c, pk=pk, Gb=Gb: e.tensor_scalar_mul(out=KVGb[:, c, :], in0=pk[:, 64:128], scalar1=Gb),
                 R=[pkt, tGb], W=[t_kvb[c]])

        stA(0)
        for c in range(NCH):
            if c + 1 < NCH:
                stA(c + 1)
            stB(c)
            stC(c)
        for c in [1, 0] + list(range(NCH - 1, 1, -1)):
            Gb, tGb = Gap(c, 1)
            K.op('dve', lambda e, c=c: e.tensor_copy(out=Sbf[:, c, 1, :], in_=Sb[:]), R=[t_Sb], W=[t_S[c][1]])
            K.op('dve', lambda e, c=c, Gb=Gb: e.scalar_tensor_tensor(out=Sb[:], in0=Sb[:], scalar=Gb, in1=KVGb[:, c, :], op0=ALU.mult, op1=ALU.add),
                 R=[t_Sb, tGb, t_kvb[c]], W=[t_Sb])

        att, t_att = dbl('att', [128, 2, 2, H, 128], BF16)
        osb, t_o = dbl('osb', [128, 2, H, 64])
        sq, t_sq = dbl('sq', [128, 2, H, 64])
        stat, t_st = dbl('stat', [128, 4, 2 * H])
        yb, t_yb = dbl('yb', [128, 2, NV], BF16)
        stg, t_stg = dbl('stg', [128, 2, 128], BF16)
        H2 = 2 * H

        def stP(pi, c0_):
            b = pi % 2
            for ci_ in range(2):
                c = c0_ + ci_
                cs = slice(c * 128, (c + 1) * 128)
                pos = []
                for h in range(H):
                    hr = slice(h * dk, (h + 1) * dk)
                    pz, pzt = g.ps()
                    for d in range(2):
                        K.op('pe', lambda e, d=d, hr=hr, pz=pz, cs=cs: e.matmul(
                            pz[:, d * 128:(d + 1) * 128], lhsT=LT[hr, 2 + d, cs], rhs=LT[hr, d, cs], start=True, stop=True),
                            R=[t_LT[c]], W=[pzt])
                    K.op('dve', lambda e, h=h, b=b, pz=pz, ci_=ci_: e.tensor_tensor(
                        out=att[b][:, ci_, :, h, :], in0=pz[:, 0:256].rearrange("p (d n) -> p d n", d=2),
                        in1=g.mask2[:], op=ALU.mult), R=[pzt, g.t_const], W=[t_att[b]])
                for h in range(H):
                    hr = slice(h * dk, (h + 1) * dk)
                    oc = slice(h * 64, (h + 1) * 64)
                    po, pot = g.ps()
                    K.op('pe', lambda e, h=h, b=b, oc=oc, po=po, c=c, ci_=ci_: e.matmul(po[:, 0:64], lhsT=att[b][:, ci_, 0, h, :], rhs=vtok[:, c, oc], start=True, stop=False),
                         R=[t_att[b], t_v[c]], W=[pot])
                    K.op('pe', lambda e, h=h, b=b, oc=oc, po=po, c=c, ci_=ci_: e.matmul(po[:, 0:64], lhsT=att[b][:, ci_, 1, h, :], rhs=vtok[:, c, oc], start=False, stop=False),
                         R=[t_att[b], t_v[c]], W=[pot])
                    K.op('pe', lambda e, hr=hr, po=po, c=c, cs=cs: e.matmul(po[:, 0:64], lhsT=LT[hr, 0, cs], rhs=Sbf[hr, c, 0, :], start=False, stop=False),
                         R=[t_LT[c], t_S[c][0]], W=[pot])
                    K.op('pe', lambda e, hr=hr, po=po, c=c, cs=cs: e.matmul(po[:, 0:64], lhsT=LT[hr, 1, cs], rhs=Sbf[hr, c, 1, :], start=False, stop=True),
                         R=[t_LT[c], t_S[c][1]], W=[pot])
                    K.op('act', lambda e, po=po, b=b, h=h, ci_=ci_: e.activation(out=osb[b][:, ci_, h, :], in_=po[:, 0:64], func=AF.Identity), R=[pot], W=[t_o[b]])

        def stQ(pi, c0_):
            b = pi % 2
            o3 = osb[b][:].rearrange("p c h f -> p (c h) f")
            s4 = stat[b]
            sq3 = sq[b][:].rearrange("p c h f -> p (c h) f")
            if not gla:
                K.op('dve', lambda e: e.tensor_reduce(out=s4[:, 0, :], in_=o3, axis=AX.X, op=ALU.add), R=[t_o[b]], W=[t_st[b]])
                K.op('dve', lambda e: e.tensor_scalar_mul(out=s4[:, 0, :], in0=s4[:, 0, :], scalar1=1.0 / 64), R=[t_st[b]], W=[t_st[b]])
                K.op('dve', lambda e: e.tensor_tensor(out=o3, in0=o3, in1=s4[:, 0, :].unsqueeze(2).to_broadcast([128, H2, 64]), op=ALU.subtract),
                     R=[t_o[b], t_st[b]], W=[t_o[b]])
            K.op('pool', lambda e: e.tensor_tensor(out=sq3, in0=o3, in1=o3, op=ALU.mult), R=[t_o[b]], W=[t_sq[b]])
            K.op('dve', lambda e: e.tensor_reduce(out=s4[:, 1, :], in_=sq3, axis=AX.X, op=ALU.add), R=[t_sq[b]], W=[t_st[b]])
            K.op('act', lambda e: e.activation(out=s4[:, 2, :], in_=s4[:, 1, :], func=AF.Ln, scale=1.0 / 64, bias=g.epsc[:, 0:1]), R=[t_st[b], g.t_const], W=[t_st[b]])
            K.op('act', lambda e: e.activation(out=s4[:, 3, :], in_=s4[:, 2, :], func=AF.Exp, scale=-0.5), R=[t_st[b]], W=[t_st[b]])
            K.op('dve', lambda e: e.tensor_tensor(out=o3, in0=o3, in1=s4[:, 3, :].unsqueeze(2).to_broadcast([128, H2, 64]), op=ALU.mult),
                 R=[t_o[b], t_st[b]], W=[t_o[b]])
            o2 = osb[b][:].rearrange("p c h f -> p c (h f)")
            if gla:
                K.op('pool', lambda e: e.tensor_tensor(out=o2, in0=o2, in1=ngb[:].unsqueeze(1).to_broadcast([128, 2, NV]), op=ALU.mult), R=[t_o[b], t_par], W=[t_o[b]])
            else:
                K.op('pool', lambda e: e.tensor_tensor(out=o2, in0=o2, in1=gng[:].unsqueeze(1).to_broadcast([128, 2, NV]), op=ALU.mult), R=[t_o[b], t_par], W=[t_o[b]])
                K.op('pool', lambda e: e.tensor_tensor(out=o2, in0=o2, in1=gnb[:].unsqueeze(1).to_broadcast([128, 2, NV]), op=ALU.add), R=[t_o[b], t_par], W=[t_o[b]])
            K.op('dve', lambda e: e.tensor_tensor(out=yb[b][:], in0=o2, in1=sgtok[:, c0_:c0_ + 2, :], op=ALU.mult), R=[t_o[b], t_sg[c0_], t_sg[c0_ + 1]], W=[t_yb[b]])
            pbh, pbht = g.psb_half()
            for ci_ in range(2):
                K.op('pe', lambda e, ci_=ci_, pbh=pbh: e.transpose(out=pbh[:, ci_ * 128:(ci_ + 1) * 128], in_=yb[b][:, ci_, :], identity=g.identb[:]),
                     R=[t_yb[b], g.t_const], W=[pbht])
            K.op('act', lambda e, pbh=pbh: e.activation(out=stg[b][:].rearrange("p c n -> p (c n)"), in_=pbh[:, 0:256], func=AF.Identity), R=[pbht], W=[t_stg[b]])
            K.dma(D['mixT_d'][row0:row0 + NV, c0_ * 128:(c0_ + 2) * 128], stg[b][:].rearrange("p c n -> p (c n)"), R=[t_stg[b]], W=[g.t_mix], q='pool')

        pairs = list(range(2, NCH, 2) if last else range(0, NCH, 2))
        stP(0, pairs[0])
        for pi, c0_ in enumerate(pairs):
            if pi + 1 < len(pairs):
                stP(pi + 1, pairs[pi + 1])
            stQ(pi, c0_)


TWO_PI = 2.0 * math.pi


def phase_s5(g, l):
    nc, K, D = g.nc, g.K, g.D
    NP = (T + 511) // 512
    pieces = [(i * 512, min(512, T - i * 512)) for i in range(NP)]
    with ExitStack() as st:
        def sb(name, shape, dt=F32):
            return st.enter_context(nc.sbuf_tensor(_nm(name), list(shape), dt))
        W = D['w_in'][l]
        wb, t_w = load_w_bf16(g, st, [(W, OFF['s5_u'], 256)], 256, 'w_s5')
        t_par = Tok()
        lam = sb('lam', [128, 16, 2]); dtc = sb('dtc', [128, 16])
        K.dma(lam[:], D['s5_lam_fm'][l, :, :, :], W=[t_par])
        K.dma(dtc[:], D['s5_dt_fm'][l, :, :], W=[t_par])
        CT = sb('CT', [128, 16, 2, 32])
        K.dma(CT[:], D['s5_CT'][l].rearrange("t r k m -> k t r m"), W=[t_par])
        dcol = sb('dcol', [128, 2]); glub = sb('glub', [128, 2])
        K.dma(dcol[:], D['s5_dcol'][l, :, :], W=[t_par])
        K.dma(glub[:], D['s5_glub'][l, :, :], W=[t_par])
        gw32 = sb('gw32', [128, 2, 256]); gwb = sb('gwb', [128, 2, 256], BF16)
        K.dma(gw32[:], D['s5_glu_w'][l].rearrange("(kt p) n -> p kt n", p=128), W=[t_par])
        K.op('dve', lambda e: e.tensor_copy(out=gwb[:], in_=gw32[:]), R=[t_par], W=[t_par])
        P_ = {}
        for nm_ in ('dt', 'a', 'th', 'r', 'u', 'ui', 'fr', 'sn', 'u2', 'ui2', 'fr2', 'cs', 'x', 'y', 'den', 'rden', 't1', 't2', 'cr', 'ci', 'ncr', 'thn'):
            P_[nm_] = sb('s5p_' + nm_, [128, 16], I32 if nm_ in ('ui', 'ui2') else F32)

        def dv(fn, rd=True):
            K.op('dve', fn, R=[t_par, g.t_const], W=[t_par])

        def ac(fn):
            K.op('act', fn, R=[t_par, g.t_const], W=[t_par])
        lre, lim = lam[:, :, 0], lam[:, :, 1]
        ac(lambda e: e.activation(out=P_['dt'][:], in_=dtc[:], func=AF.Exp))
        dv(lambda e: e.tensor_tensor(out=P_['a'][:], in0=lre, in1=P_['dt'][:], op=ALU.mult))
        dv(lambda e: e.tensor_tensor(out=P_['th'][:], in0=lim, in1=P_['dt'][:], op=ALU.mult))
        ac(lambda e: e.activation(out=P_['r'][:], in_=P_['a'][:], func=AF.Exp))
        dv(lambda e: e.tensor_scalar_mul(out=P_['u'][:], in0=P_['th'][:], scalar1=1.0 / TWO_PI))
        dv(lambda e: e.tensor_copy(out=P_['ui'][:], in_=P_['u'][:]))
        dv(lambda e: e.tensor_tensor(out=P_['fr'][:], in0=P_['u'][:], in1=P_['ui'][:], op=ALU.subtract))
        ac(lambda e: e.activation(out=P_['sn'][:], in_=P_['fr'][:], func=AF.Sin, scale=TWO_PI))
        dv(lambda e: e.tensor_scalar_add(out=P_['u2'][:], in0=P_['u'][:], scalar1=0.25))
        dv(lambda e: e.tensor_copy(out=P_['ui2'][:], in_=P_['u2'][:]))
        dv(lambda e: e.tensor_tensor(out=P_['fr2'][:], in0=P_['u2'][:], in1=P_['ui2'][:], op=ALU.subtract))
        ac(lambda e: e.activation(out=P_['cs'][:], in_=P_['fr2'][:], func=AF.Sin, scale=TWO_PI))
        dv(lambda e: e.tensor_tensor(out=P_['x'][:], in0=P_['r'][:], in1=P_['cs'][:], op=ALU.mult))
        dv(lambda e: e.tensor_scalar_add(out=P_['x'][:], in0=P_['x'][:], scalar1=-1.0))
        dv(lambda e: e.tensor_tensor(out=P_['y'][:], in0=P_['r'][:], in1=P_['sn'][:], op=ALU.mult))
        dv(lambda e: e.tensor_tensor(out=P_['t1'][:], in0=lre, in1=lre, op=ALU.mult))
        dv(lambda e: e.tensor_tensor(out=P_['t2'][:], in0=lim, in1=lim, op=ALU.mult))
        dv(lambda e: e.tensor_tensor(out=P_['den'][:], in0=P_['t1'][:], in1=P_['t2'][:], op=ALU.add))
        dv(lambda e: e.reciprocal(out=P_['rden'][:], in_=P_['den'][:]))
        dv(lambda e: e.tensor_tensor(out=P_['t1'][:], in0=P_['x'][:], in1=lre, op=ALU.mult))
        dv(lambda e: e.tensor_tensor(out=P_['t2'][:], in0=P_['y'][:], in1=lim, op=ALU.mult))
        dv(lambda e: e.tensor_tensor(out=P_['cr'][:], in0=P_['t1'][:], in1=P_['t2'][:], op=ALU.add))
        dv(lambda e: e.tensor_tensor(out=P_['cr'][:], in0=P_['cr'][:], in1=P_['rden'][:], op=ALU.mult))
        dv(lambda e: e.tensor_tensor(out=P_['t1'][:], in0=P_['y'][:], in1=lre, op=ALU.mult))
        dv(lambda e: e.tensor_tensor(out=P_['t2'][:], in0=P_['x'][:], in1=lim, op=ALU.mult))
        dv(lambda e: e.tensor_tensor(out=P_['ci'][:], in0=P_['t1'][:], in1=P_['t2'][:], op=ALU.subtract))
        dv(lambda e: e.tensor_tensor(out=P_['ci'][:], in0=P_['ci'][:], in1=P_['rden'][:], op=ALU.mult))
        dv(lambda e: e.tensor_scalar_mul(out=P_['thn'][:], in0=P_['th'][:], scalar1=1.0 / TWO_PI))
        Ce = sb('Ce', [128, 16, 2, 32]); Ceb = sb('Ceb', [128, 16, 2, 32], BF16); tmpC = sb('tmpC', [128, 16, 32])
        crb = P_['cr'][:].unsqueeze(2).to_broadcast([128, 16, 32])
        cib = P_['ci'][:].unsqueeze(2).to_broadcast([128, 16, 32])
        dv(lambda e: e.tensor_tensor(out=Ce[:, :, 0, :], in0=CT[:, :, 0, :], in1=crb, op=ALU.mult))
        dv(lambda e: e.tensor_tensor(out=tmpC[:], in0=CT[:, :, 1, :], in1=cib, op=ALU.mult))
        dv(lambda e: e.tensor_tensor(out=Ce[:, :, 0, :], in0=Ce[:, :, 0, :], in1=tmpC[:], op=ALU.subtract))
        dv(lambda e: e.tensor_tensor(out=Ce[:, :, 1, :], in0=CT[:, :, 0, :], in1=cib, op=ALU.mult))
        dv(lambda e: e.tensor_tensor(out=tmpC[:], in0=CT[:, :, 1, :], in1=crb, op=ALU.mult))
        dv(lambda e: e.tensor_tensor(out=Ce[:, :, 1, :], in0=Ce[:, :, 1, :], in1=tmpC[:], op=ALU.add))
        dv(lambda e: e.tensor_scalar_mul(out=Ce[:, :, 1, :], in0=Ce[:, :, 1, :], scalar1=-1.0))
        dv(lambda e: e.tensor_copy(out=Ceb[:], in_=Ce[:]))

        uTf = sb('uTf', [128, 2, T], BF16); t_uf = Tok()
        st2 = ExitStack()

        def sb2(name, shape, dt=F32):
            return st2.enter_context(nc.sbuf_tensor(_nm(name), list(shape), dt))
        ucs = [sb('ucs%d' % i, [128, 8, 128], BF16) for i in range(2)]; t_ucs = [Tok(), Tok()]
        yT = sb('yT', [128, 2, T], BF16); t_y = Tok()
        uTb = sb2('uTb', [128, 1, T], BF16); t_ub = Tok()
        for c4 in range(0, NCH, 4):
            cc = list(range(c4, min(c4 + 4, NCH)))
            pts = [g.ps(), g.ps()]
            for ci_, c in enumerate(cc):
                b = c % 2
                K.dma(ucs[b][:], D['uT_d'][c, :, :, :], R=[g.t_uT[c]], W=[t_ucs[b]])
                for ft in range(2):
                    pt, ptk = pts[ft]
                    for kt in range(8):
                        K.op('pe', lambda e, kt=kt, b=b, ft=ft, pt=pt, ci_=ci_: e.matmul(
                            pt[:, ci_ * 128:(ci_ + 1) * 128], lhsT=wb[:, kt, ft * 128:(ft + 1) * 128], rhs=ucs[b][:, kt, :],
                            start=(kt == 0), stop=(kt == 7)), R=[t_w, t_ucs[b]], W=[ptk])
            n = len(cc) * 128
            for ft in range(2):
                pt, ptk = pts[ft]
                K.op('act', lambda e, ft=ft, pt=pt, c4=c4, n=n: e.activation(out=uTf[:, ft, c4 * 128:c4 * 128 + n], in_=pt[:, 0:n], func=AF.Identity),
                     R=[ptk], W=[t_uf])

        iot = sb2('iot', [128, 1088])
        K.dma(iot[:], D['iota_t'][0:1088].partition_broadcast(128), W=[t_par])
        offc = sb2('offc', [128, 16, 4, 2])
        for q4 in range(4):
            for which, off in ((0, 0.0), (1, 0.25)):
                dv(lambda e, q4=q4, which=which, off=off: e.tensor_scalar(out=offc[:, :, q4, which], in0=P_['thn'][:], scalar1=float(q4 * 1088), scalar2=off,
                                                                         op0=ALU.mult, op1=ALU.add))
        cosT = sb2('s5cos', [128, T]); sinT = sb2('s5sin', [128, T]); t_tab = Tok()
        uu = sb2('s5uu', [128, 1088]); ui = sb2('s5ui', [128, 1088], I32); t_uu = Tok()
        cre = sb2('s5cre', [128, T]); cim = sb2('s5cim', [128, T]); t_c = Tok()
        hh = sb2('s5h', [128, 2, 2, T], BF16); t_h = [Tok(), Tok()]
        BT32 = sb2('BT32', [128, 2, 128]); BTb = sb2('BTb', [128, 2, 128], BF16); t_B = Tok()
        m1 = [sb2('s5m%d' % i, [128, 512]) for i in range(4)]; t_m = [Tok() for _ in range(4)]
        for j in range(8):
            ft = j // 4
            if j % 4 == 0:
                K.op('pool', lambda e, ft=ft: e.tensor_copy(out=uTb[:, 0, 0:CTXL], in_=uTf[:, ft, CTXL - 1::-1]), R=[t_uf], W=[t_ub])
                K.op('pool', lambda e, ft=ft: e.tensor_copy(out=uTb[:, 0, CTXL:T], in_=uTf[:, ft, T - 1:CTXL - 1:-1]), R=[t_uf], W=[t_ub])
            for d in range(2):
                col = d * 8 + j
                usrc, t_us, uft = (uTf, t_uf, ft) if d == 0 else (uTb, t_ub, 0)
                K.dma(BT32[:], D['s5_BT'][l, col].rearrange("r k m -> k r m"), W=[t_B])
                K.op('pool', lambda e: e.tensor_copy(out=BTb[:], in_=BT32[:]), R=[t_B], W=[t_B])
                for which, tab, off in ((0, sinT, 0.0), (1, cosT, 0.25)):
                    for q4 in range(4):
                        qs = slice(q4 * 1088, (q4 + 1) * 1088)
                        K.op('dve', lambda e, col=col, which=which, q4=q4: e.tensor_scalar(out=uu[:], in0=iot[:], scalar1=P_['thn'][:, col:col + 1],
                                                                                  scalar2=offc[:, col, q4, which:which + 1], op0=ALU.mult, op1=ALU.add), R=[t_par], W=[t_uu])
                        K.op('dve', lambda e: e.tensor_copy(out=ui[:], in_=uu[:]), R=[t_uu], W=[t_uu])
                        K.op('pool', lambda e: e.tensor_tensor(out=uu[:], in0=uu[:], in1=ui[:], op=ALU.subtract), R=[t_uu], W=[t_uu])
                        K.op('act', lambda e, tab=tab, qs=qs: e.activation(out=tab[:, qs], in_=uu[:], func=AF.Sin, scale=TWO_PI), R=[t_uu], W=[t_tab])
                for (p0, n) in pieces:
                    pr, prt = g.ps()
                    pi_, pit = g.ps()
                    K.op('pe', lambda e, pr=pr, p0=p0, n=n, usrc=usrc, uft=uft: e.matmul(pr[:, 0:n], lhsT=BTb[:, 0, :], rhs=usrc[:, uft, p0:p0 + n], start=True, stop=True),
                         R=[t_B, t_us], W=[prt])
                    K.op('pe', lambda e, pi_=pi_, p0=p0, n=n, usrc=usrc, uft=uft: e.matmul(pi_[:, 0:n], lhsT=BTb[:, 1, :], rhs=usrc[:, uft, p0:p0 + n], start=True, stop=True),
                         R=[t_B, t_us], W=[pit])
                    sl = slice(p0, p0 + n)
                    K.op('dve', lambda e, pr=pr, sl=sl, n=n: e.tensor_tensor(out=m1[0][:, 0:n], in0=pr[:, 0:n], in1=cosT[:, sl], op=ALU.mult), R=[prt, t_tab], W=[t_m[0]])
                    K.op('dve', lambda e, pi_=pi_, sl=sl, n=n: e.tensor_tensor(out=m1[1][:, 0:n], in0=pi_[:, 0:n], in1=sinT[:, sl], op=ALU.mult), R=[pit, t_tab], W=[t_m[1]])
                    K.op('dve', lambda e, pi_=pi_, sl=sl, n=n: e.tensor_tensor(out=m1[2][:, 0:n], in0=pi_[:, 0:n], in1=cosT[:, sl], op=ALU.mult), R=[pit, t_tab], W=[t_m[2]])
                    K.op('dve', lambda e, pr=pr, sl=sl, n=n: e.tensor_tensor(out=m1[3][:, 0:n], in0=pr[:, 0:n], in1=sinT[:, sl], op=ALU.mult), R=[prt, t_tab], W=[t_m[3]])
                    K.op('pool', lambda e, sl=sl, n=n: e.tensor_tensor(out=cre[:, sl], in0=m1[0][:, 0:n], in1=m1[1][:, 0:n], op=ALU.add), R=[t_m[0], t_m[1]], W=[t_c])
                    K.op('pool', lambda e, sl=sl, n=n: e.tensor_tensor(out=cim[:, sl], in0=m1[2][:, 0:n], in1=m1[3][:, 0:n], op=ALU.subtract), R=[t_m[2], t_m[3]], W=[t_c])
                rb = P_['r'][:, col:col + 1].to_broadcast([128, T])
                K.op('dve', lambda e, rb=rb: e.tensor_tensor_scan(out=cre[:], data0=rb, data1=cre[:], initial=0.0, op0=ALU.mult, op1=ALU.add), R=[t_c, t_par], W=[t_c])
                K.op('dve', lambda e, rb=rb: e.tensor_tensor_scan(out=cim[:], data0=rb, data1=cim[:], initial=0.0, op0=ALU.mult, op1=ALU.add), R=[t_c, t_par], W=[t_c])
                if d == 0:
                    segs = [(slice(0, T), slice(0, T))]
                else:
                    segs = [(slice(0, CTXL), slice(CTXL - 1, None, -1)), (slice(CTXL, T), slice(T - 1, CTXL - 1, -1))]
                for (so, si) in segs:
                    n = so.stop - so.start
                    for q0 in range(0, n, 1024):
                        qn = min(1024, n - q0)
                        o_sl = slice(so.start + q0, so.start + q0 + qn)
                        if d == 0:
                            i_sl = o_sl
                        else:
                            hi = si.start - q0
                            lo = hi - qn
                            i_sl = slice(hi, lo if lo >= 0 else None, -1)
                        mA, mB, mC, mD = m1[0], m1[1], m1[2], m1[3]
                        for h0 in range(0, qn, 512):
                            hn = min(512, qn - h0)
                            oo = slice(o_sl.start + h0, o_sl.start + h0 + hn)
                            if d == 0:
                                ii = oo
                            else:
                                a_ = i_sl.start - h0
                                b_ = a_ - hn
                                ii = slice(a_, b_ if b_ >= 0 else None, -1)
                            K.op('dve', lambda e, ii=ii, hn=hn: e.tensor_tensor(out=mA[:, 0:hn], in0=cre[:, ii], in1=cosT[:, ii], op=ALU.mult), R=[t_c, t_tab], W=[t_m[0]])
                            K.op('pool', lambda e, ii=ii, hn=hn: e.tensor_tensor(out=mB[:, 0:hn], in0=cim[:, ii], in1=sinT[:, ii], op=ALU.mult), R=[t_c, t_tab], W=[t_m[1]])
                            K.op('dve', lambda e, ii=ii, hn=hn: e.tensor_tensor(out=mC[:, 0:hn], in0=cre[:, ii], in1=sinT[:, ii], op=ALU.mult), R=[t_c, t_tab], W=[t_m[2]])
                            K.op('pool', lambda e, ii=ii, hn=hn: e.tensor_tensor(out=mD[:, 0:hn], in0=cim[:, ii], in1=cosT[:, ii], op=ALU.mult), R=[t_c, t_tab], W=[t_m[3]])
                            K.op('dve', lambda e, oo=oo, hn=hn, d=d: e.tensor_tensor(out=hh[:, d, 0, oo], in0=mA[:, 0:hn], in1=mB[:, 0:hn], op=ALU.subtract), R=[t_m[0], t_m[1]], W=[t_h[d]])
                            K.op('dve', lambda e, oo=oo, hn=hn, d=d: e.tensor_tensor(out=hh[:, d, 1, oo], in0=mC[:, 0:hn], in1=mD[:, 0:hn], op=ALU.add), R=[t_m[2], t_m[3]], W=[t_h[d]])
            for (p0, n) in pieces:
                py, pyt = g.ps()
                k = 0
                for d in range(2):
                    for r_ in range(2):
                        K.op('pe', lambda e, d=d, r_=r_, py=py, p0=p0, n=n, k=k, j=j: e.matmul(
                            py[0:32, 0:n], lhsT=Ceb[:, d * 8 + j, r_, :], rhs=hh[:, d, r_, p0:p0 + n], start=(k == 0), stop=(k == 3)),
                            R=[t_par, t_h[d]], W=[pyt])
                        k += 1
                rows = slice(32 * (j % 4), 32 * (j % 4) + 32)
                K.op('act', lambda e, py=py, p0=p0, n=n, rows=rows, ft=ft: e.activation(out=yT[rows, ft, p0:p0 + n], in_=py[0:32, 0:n], func=AF.Identity),
                     R=[pyt], W=[t_y])
        K.barrier()
        st2.close()
        gT = sb('gT', [128, 2, T], BF16); t_g = Tok()
        ytmp = [sb('ytmp%d' % i, [128, 512]) for i in range(2)]; t_yt = [Tok(), Tok()]
        k = 0
        for ft in range(2):
            for (p0, n) in pieces:
                b = k % 2
                k += 1
                K.op('dve', lambda e, b=b, ft=ft, p0=p0, n=n: e.scalar_tensor_tensor(
                    out=ytmp[b][:, 0:n], in0=uTf[:, ft, p0:p0 + n], scalar=dcol[:, ft:ft + 1], in1=yT[:, ft, p0:p0 + n], op0=ALU.mult, op1=ALU.add),
                    R=[t_uf, t_y, t_par], W=[t_yt[b]])
                K.op('act', lambda e, b=b, ft=ft, p0=p0, n=n: e.activation(out=gT[:, ft, p0:p0 + n], in_=ytmp[b][:, 0:n], func=AF.Gelu), R=[t_yt[b]], W=[t_g])
        ob = [sb('s5ob%d' % i, [128, 512], BF16) for i in range(2)]; t_ob = [Tok(), Tok()]
        sg = [sb('s5sg%d' % i, [128, 512]) for i in range(2)]; t_sgm = [Tok(), Tok()]
        k = 0
        for ft in range(2):
            for (p0, n) in pieces:
                b = k % 2
                k += 1
                pz, pzt = g.ps()
                for kt in range(2):
                    K.op('pe', lambda e, kt=kt, ft=ft, pz=pz, p0=p0, n=n: e.matmul(pz[:, 0:n], lhsT=gwb[:, kt, ft * 128:(ft + 1) * 128], rhs=gT[:, kt, p0:p0 + n],
                                                                                   start=(kt == 0), stop=(kt == 1)), R=[t_par, t_g], W=[pzt])
                K.op('act', lambda e, b=b, pz=pz, n=n, ft=ft: e.activation(out=sg[b][:, 0:n], in_=pz[:, 0:n], func=AF.Sigmoid, bias=glub[:, ft:ft + 1]), R=[pzt, t_par], W=[t_sgm[b]])
                K.op('dve', lambda e, b=b, ft=ft, p0=p0, n=n: e.tensor_tensor(out=ob[b][:, 0:n], in0=gT[:, ft, p0:p0 + n], in1=sg[b][:, 0:n], op=ALU.mult), R=[t_g, t_sgm[b]], W=[t_ob[b]])
                K.dma(D['mixT_d'][512 + ft * 128:512 + (ft + 1) * 128, p0:p0 + n], ob[b][:, 0:n], R=[t_ob[b]], W=[g.t_mix], q='pool')


def phase_hyena(g, l, n, c0, sfx):
    nc, K, D = g.nc, g.K, g.D
    NT = n // 128
    NKT = NT + 1
    with ExitStack() as st:
        def sb(name, shape, dt=F32):
            return st.enter_context(nc.sbuf_tensor(_nm(name), list(shape), dt))
        W = D['w_in'][l]
        t_par = Tok()
        cw = sb('hy_cw', [128, 6, 3]); cb = sb('hy_cb', [128, 6])
        K.dma(cw[:], D['hy_cw'][l, :, :, :], W=[t_par])
        K.dma(cb[:], D['hy_cb'][l, :, :], W=[t_par])
        hbias = sb('hy_bias', [128, 2])
        K.dma(hbias[:], D['hy_biasc'][l, :, :], W=[t_par])
        zT = sb('hy_zT', [128, 2, n], BF16); t_z = Tok()
        x0T = sb('hy_x0T', [128, 2, n], BF16); t_x0 = Tok()
        data = sb('hy_data', [128, NT, 768], BF16); t_data = [Tok() for _ in range(NT)]
        rnorm = sb('hy_rnorm', [128, 2]); t_rn = Tok()
        with ExitStack() as st2:
            def sb2(name, shape, dt=F32):
                return st2.enter_context(nc.sbuf_tensor(_nm(name), list(shape), dt))
            wb, t_w = load_w_bf16(g, st2, [(W, OFF['hy_p'], 768)], 768, 'w_hy')
            ucs = [sb2('hucs%d' % i, [128, 8, 128], BF16) for i in range(2)]; t_ucs = [Tok(), Tok()]
            pp = [sb2('hy_pp%d' % i, [128, n + 2]) for i in range(2)]; t_pp = [Tok(), Tok()]
            sv = [sb2('hy_sv%d' % i, [128, n]) for i in range(2)]; t_sv = [Tok(), Tok()]
            for i in range(2):
                K.op('pool', lambda e, i=i: e.memset(pp[i][:, 0:1], 0.0), W=[t_pp[i]])
                K.op('pool', lambda e, i=i: e.memset(pp[i][:, n + 1:n + 2], 0.0), W=[t_pp[i]])

            _sub = int(os.environ.get('HY_SUB', '9'))

            def proj_conv(ft, slot):
                for c4 in range(0, NT, 4):
                    cc = list(range(c4, min(c4 + 4, NT)))
                    pt, ptk = g.ps()
                    for ci_, c in enumerate(cc):
                        b = c % 2
                        K.dma(ucs[b][:], D['uT_d'][c0 + c, :, :, :], R=[g.t_uT[c0 + c]], W=[t_ucs[b]])
                        for kt in range(8):
                            K.op('pe', lambda e, kt=kt, b=b, pt=pt, ci_=ci_: e.matmul(
                                pt[:, ci_ * 128:(ci_ + 1) * 128], lhsT=wb[:, kt, ft * 128:(ft + 1) * 128], rhs=ucs[b][:, kt, :],
                                start=(kt == 0), stop=(kt == 7)), R=[t_w, t_ucs[b]], W=[ptk])
                    nn = len(cc) * 128
                    K.op('act', lambda e, pt=pt, c4=c4, nn=nn: e.activation(out=pp[slot][:, 1 + c4 * 128:1 + c4 * 128 + nn], in_=pt[:, 0:nn], func=AF.Identity),
                         R=[ptk], W=[t_pp[slot]])
                if _sub < 2:
                    return
                for q0 in range(0, n, 2048):
                    qn = min(2048, n - q0)
                    K.op('dve', lambda e, q0=q0, qn=qn: e.tensor_scalar(out=sv[slot][:, q0:q0 + qn], in0=pp[slot][:, q0:q0 + qn], scalar1=cw[:, ft, 0:1], scalar2=cb[:, ft:ft + 1],
                                                                         op0=ALU.mult, op1=ALU.add), R=[t_pp[slot], t_par], W=[t_sv[slot]])
                    K.op('dve', lambda e, q0=q0, qn=qn: e.scalar_tensor_tensor(out=sv[slot][:, q0:q0 + qn], in0=pp[slot][:, q0 + 1:q0 + 1 + qn], scalar=cw[:, ft, 1:2],
                                                                                 in1=sv[slot][:, q0:q0 + qn], op0=ALU.mult, op1=ALU.add), R=[t_pp[slot], t_par, t_sv[slot]], W=[t_sv[slot]])
                    K.op('dve', lambda e, q0=q0, qn=qn: e.scalar_tensor_tensor(out=sv[slot][:, q0:q0 + qn], in0=pp[slot][:, q0 + 2:q0 + 2 + qn], scalar=cw[:, ft, 2:3],
                                                                                 in1=sv[slot][:, q0:q0 + qn], op0=ALU.mult, op1=ALU.add), R=[t_pp[slot], t_par, t_sv[slot]], W=[t_sv[slot]])
            for ci in range(2):
                proj_conv(2 + ci, 0)
                if _sub < 3:
                    break
                proj_conv(4 + ci, 1)
                K.op('pool', lambda e, ci=ci: e.tensor_tensor(out=zT[:, ci, :], in0=sv[0][:], in1=sv[1][:], op=ALU.mult), R=[t_sv[0], t_sv[1]], W=[t_z])
                if _sub < 4:
                    break
                proj_conv(ci, 0)
                K.op('pool', lambda e, ci=ci: e.tensor_copy(out=x0T[:, ci, :], in_=sv[0][:]), R=[t_sv[0]], W=[t_x0])
            for i in (range(NT) if _sub >= 6 else [int(x) for x in os.environ.get("HY_IT", "0,1").split(",") if int(x) < NT]) if _sub >= 5 else []:
                pbh, pbht = g.psb_half()
                for ci in range(2):
                    K.op('pe', lambda e, ci=ci, i=i, pbh=pbh: e.transpose(out=pbh[:, ci * 128:(ci + 1) * 128], in_=zT[:, ci, i * 128:(i + 1) * 128], identity=g.identb[:]),
                         R=[t_z, g.t_const], W=[pbht])
                K.op('act', lambda e, i=i, pbh=pbh: e.activation(out=data[:, i, 0:256], in_=pbh[:, 0:256], func=AF.Identity), R=[pbht], W=[t_data[i]])
            K.barrier()
        _hs = int(os.environ.get('HY_STAGE', '9'))
        if _hs < 2:
            return
        with ExitStack() as st2:
            def sb2(name, shape, dt=F32):
                return st2.enter_context(nc.sbuf_tensor(_nm(name), list(shape), dt))
            zemb = sb2('hy_zemb', [33, n]); fw1 = sb2('hy_fw1', [33, 64]); fw2 = sb2('hy_fw2', [64, 64]); fw3 = sb2('hy_fw3', [64, 512])
            fcol = sb2('hy_fcol', [64, 5]); fs = sb2('hy_fs', [64, 2])
            K.dma(zemb[:], D['hy_zemb' + sfx][:, :], W=[t_par])
            K.dma(fw1[:], D['hy_fw1'][l, :, :], W=[t_par])
            K.dma(fw2[:], D['hy_fw2'][l, :, :], W=[t_par])
            K.dma(fw3[:], D['hy_fw3'][l, :, :], W=[t_par])
            K.dma(fcol[:], D['hy_fcol'][l, :, :], W=[t_par])
            K.op('dve', lambda e: e.tensor_scalar_mul(out=fs[:, 0:1], in0=fcol[:, 2:3], scalar1=1.0 / TWO_PI), R=[t_par], W=[t_par])
            absd = sb2('hy_absd', [128, 512])
            K.dma(absd[:], D['hy_decay'][l].rearrange("d c -> (d c)").partition_broadcast(128), W=[t_par])
            K.op('dve', lambda e: e.scalar_tensor_tensor(out=absd[:], in0=absd[:], scalar=-1.0, in1=absd[:], op0=ALU.mult, op1=ALU.max), R=[t_par], W=[t_par])
            tn = sb2('hy_tn', [128, NT])
            K.dma(tn[:], D['hy_tn' + sfx][:, :], W=[t_par])
            K.op('dve', lambda e: e.tensor_scalar_mul(out=tn[:], in0=tn[:], scalar1=-1.0), R=[t_par], W=[t_par])
            h1 = sb2('hy_h1', [64, n]); h2 = sb2('hy_h2', [64, n]); t_h1 = Tok(); t_h2 = Tok()
            uu = sb2('hy_uu', [64, 512]); ui = sb2('hy_ui', [64, 512], I32); t_uu = Tok()
            for (src, t_src, wgt, bcol, dst, t_dst) in ((zemb, t_par, fw1, 0, h1, t_h1), (h1, t_h1, fw2, 1, h2, t_h2)):
                for q0 in range(0, n, 512):
                    qn = min(512, n - q0)
                    pt, ptk = g.ps()
                    K.op('pe', lambda e, pt=pt, q0=q0, qn=qn, src=src, wgt=wgt: e.matmul(pt[0:64, 0:qn], lhsT=wgt[:], rhs=src[:, q0:q0 + qn], start=True, stop=True),
                         R=[t_src, t_par], W=[ptk])
                    K.op('dve', lambda e, pt=pt, qn=qn, bcol=bcol: e.tensor_scalar(out=uu[:, 0:qn], in0=pt[0:64, 0:qn], scalar1=fcol[:, bcol:bcol + 1], scalar2=fs[:, 0:1],
                                                                                   op0=ALU.add, op1=ALU.mult), R=[ptk, t_par], W=[t_uu])
                    K.op('dve', lambda e, qn=qn: e.tensor_copy(out=ui[:, 0:qn], in_=uu[:, 0:qn]), R=[t_uu], W=[t_uu])
                    K.op('dve', lambda e, qn=qn: e.tensor_tensor(out=uu[:, 0:qn], in0=uu[:, 0:qn], in1=ui[:, 0:qn], op=ALU.subtract), R=[t_uu], W=[t_uu])
                    K.op('act', lambda e, q0=q0, qn=qn, dst=dst: e.activation(out=dst[:, q0:q0 + qn], in_=uu[:, 0:qn], func=AF.Sin, scale=TWO_PI), R=[t_uu], W=[t_dst])
            acc = sb2('hy_acc', [128, 512]); t_acc = Tok()
            K.op('pool', lambda e: e.memset(acc[:], 0.0), W=[t_acc])
            win = [sb2('hy_win%d' % i, [128, 512]) for i in range(2)]; t_win = [Tok(), Tok()]
            fl = [sb2('hy_fl%d' % i, [128, 512]) for i in range(2)]; t_fl = [Tok(), Tok()]
            fa = [sb2('hy_fa%d' % i, [128, 512]) for i in range(2)]; t_fa = [Tok(), Tok()]
            for i in range(NT):
                b = i % 2
                pt, ptk = g.ps()
                K.op('pe', lambda e, pt=pt, i=i: e.matmul(pt[:, 0:512], lhsT=h2[:, i * 128:(i + 1) * 128], rhs=fw3[:], start=True, stop=True), R=[t_h2, t_par], W=[ptk])
                K.op('act', lambda e, b=b, i=i: e.activation(out=win[b][:], in_=absd[:], func=AF.Exp, scale=tn[:, i:i + 1]), R=[t_par], W=[t_win[b]])
                K.op('dve', lambda e, b=b, pt=pt: e.tensor_tensor(out=fl[b][:], in0=pt[:, 0:512], in1=win[b][:], op=ALU.mult), R=[ptk, t_win[b]], W=[t_fl[b]])
                if i == 0:
                    K.op('dve', lambda e, b=b: e.memset(fl[b][0:1, 256:512], 0.0), W=[t_fl[b]])
                K.op('dve', lambda e, b=b: e.scalar_tensor_tensor(out=fa[b][:], in0=fl[b][:], scalar=-1.0, in1=fl[b][:], op0=ALU.mult, op1=ALU.max), R=[t_fl[b]], W=[t_fa[b]])
                K.op('pool', lambda e, b=b: e.tensor_tensor(out=acc[:], in0=acc[:], in1=fa[b][:], op=ALU.add), R=[t_fa[b], t_acc], W=[t_acc])
                K.op('pool', lambda e, b=b, i=i: e.tensor_copy(out=data[:, i, 256:768], in_=fl[b][:]), R=[t_fl[b]], W=[t_data[i]])
            pt, ptk = g.ps()
            for ci in range(2):
                K.op('pe', lambda e, ci=ci, pt=pt: e.matmul(pt[:, ci:ci + 1], lhsT=acc[:, ci * 128:(ci + 1) * 128], rhs=g.ones[:, 0:1], start=True, stop=False),
                     R=[t_acc, g.t_const], W=[ptk])
                K.op('pe', lambda e, ci=ci, pt=pt: e.matmul(pt[:, ci:ci + 1], lhsT=acc[:, 256 + ci * 128:256 + (ci + 1) * 128], rhs=g.ones[:, 0:1], start=False, stop=True),
                     R=[t_acc, g.t_const], W=[ptk])
            K.op('dve', lambda e, pt=pt: e.reciprocal(out=rnorm[:], in_=pt[:, 0:2]), R=[ptk], W=[t_rn])
            K.barrier()
        if _hs < 3:
            return
        Yw = sb('hy_Yw', [128, NKT, 2, 256], BF16); t_Y = [Tok() for _ in range(NKT)]
        wk = sb('hy_wk', [128, NKT])
        K.dma(wk[:], D['hy_wk' + sfx][:, :], W=[t_par])
        tabc = [sb('hy_tc%d' % i, [128, NKT, 128], BF16) for i in range(2)]
        tabs = [sb('hy_ts%d' % i, [128, NKT, 128], BF16) for i in range(2)]
        t_tab = [Tok(), Tok()]
        f1c = sb('hy_f1c', [128, 256]); f1s = sb('hy_f1s', [128, 256]); t_f1 = Tok()
        rre = sb('hy_rre', [128, 256]); rim = sb('hy_rim', [128, 256]); t_r = Tok()
        tt = [sb('hy_tt%d' % i, [128, 256]) for i in range(4)]; t_tt = [Tok() for _ in range(4)]
        yy = [sb('hy_yy%d' % i, [128, 256]) for i in range(2)]; t_yy = [Tok(), Tok()]
        for j in range(NKT):
            b = j % 2
            K.dma(tabc[b][:], D['dftc' + sfx][j, :, :, :], W=[t_tab[b]])
            K.dma(tabs[b][:], D['dfts' + sfx][j, :, :, :], W=[t_tab[b]])
            pA, pAt = g.ps(); pB, pBt = g.ps(); pC, pCt = g.ps(); pD, pDt = g.ps()
            for i in range(NT):
                fl_ = dict(start=(i == 0), stop=(i == NT - 1))
                K.op('pe', lambda e, i=i, b=b, pA=pA, fl_=fl_: e.matmul(pA[:, 0:512], lhsT=tabc[b][:, i, :], rhs=data[:, i, 0:512], **fl_), R=[t_tab[b], t_data[i]], W=[pAt])
                K.op('pe', lambda e, i=i, b=b, pB=pB, fl_=fl_: e.matmul(pB[:, 0:256], lhsT=tabc[b][:, i, :], rhs=data[:, i, 512:768], **fl_), R=[t_tab[b], t_data[i]], W=[pBt])
                K.op('pe', lambda e, i=i, b=b, pC=pC, fl_=fl_: e.matmul(pC[:, 0:512], lhsT=tabs[b][:, i, :], rhs=data[:, i, 0:512], **fl_), R=[t_tab[b], t_data[i]], W=[pCt])
                K.op('pe', lambda e, i=i, b=b, pD=pD, fl_=fl_: e.matmul(pD[:, 0:256], lhsT=tabs[b][:, i, :], rhs=data[:, i, 512:768], **fl_), R=[t_tab[b], t_data[i]], W=[pDt])
            K.op('act', lambda e, pB=pB: e.activation(out=f1c[:], in_=pB[:, 0:256], func=AF.Identity), R=[pBt], W=[t_f1])
            K.op('act', lambda e, pD=pD: e.activation(out=f1s[:], in_=pD[:, 0:256], func=AF.Identity), R=[pDt], W=[t_f1])
            K.op('dve', lambda e, pA=pA: e.tensor_tensor(out=rre[:], in0=pA[:, 256:512], in1=f1c[:], op=ALU.add), R=[pAt, t_f1], W=[t_r])
            K.op('dve', lambda e, pC=pC: e.tensor_tensor(out=rim[:], in0=f1s[:], in1=pC[:, 256:512], op=ALU.subtract), R=[pCt, t_f1], W=[t_r])
            K.op('dve', lambda e, pA=pA: e.tensor_tensor(out=tt[0][:], in0=pA[:, 0:256], in1=rre[:], op=ALU.mult), R=[pAt, t_r], W=[t_tt[0]])
            K.op('dve', lambda e, pC=pC: e.tensor_tensor(out=tt[1][:], in0=pC[:, 0:256], in1=rim[:], op=ALU.mult), R=[pCt, t_r], W=[t_tt[1]])
            K.op('dve', lambda e, pA=pA: e.tensor_tensor(out=tt[2][:], in0=pA[:, 0:256], in1=rim[:], op=ALU.mult), R=[pAt, t_r], W=[t_tt[2]])
            K.op('dve', lambda e, pC=pC: e.tensor_tensor(out=tt[3][:], in0=pC[:, 0:256], in1=rre[:], op=ALU.mult), R=[pCt, t_r], W=[t_tt[3]])
            K.op('pool', lambda e: e.tensor_tensor(out=yy[0][:], in0=tt[0][:], in1=tt[1][:], op=ALU.add), R=[t_tt[0], t_tt[1]], W=[t_yy[0]])
            K.op('pool', lambda e: e.tensor_tensor(out=yy[1][:], in0=tt[3][:], in1=tt[2][:], op=ALU.subtract), R=[t_tt[2], t_tt[3]], W=[t_yy[1]])
            K.op('act', lambda e, j=j: e.activation(out=Yw[:, j, 0, :], in_=yy[0][:], func=AF.Identity, scale=wk[:, j:j + 1]), R=[t_yy[0], t_par], W=[t_Y[j]])
            K.op('act', lambda e, j=j: e.activation(out=Yw[:, j, 1, :], in_=yy[1][:], func=AF.Identity, scale=wk[:, j:j + 1]), R=[t_yy[1], t_par], W=[t_Y[j]])
        if _hs < 4:
            return
        ysb = [sb('hy_ysb%d' % i, [128, 256]) for i in range(2)]; t_ys = [Tok(), Tok()]
        tmp = [sb('hy_tmp%d' % i, [128, 256]) for i in range(2)]; t_tmp = [Tok(), Tok()]
        ob = [sb('hy_ob%d' % i, [128, 2, 128], BF16) for i in range(2)]; t_ob = [Tok(), Tok()]
        for i in range(NT):
            b = i % 2
            K.dma(tabc[b][:], D['dftc' + sfx][i, :, :, :], W=[t_tab[b]])
            K.dma(tabs[b][:], D['dfts' + sfx][i, :, :, :], W=[t_tab[b]])
            py, pyt = g.ps()
            for kt in range(NKT):
                K.op('pe', lambda e, kt=kt, b=b, py=py: e.matmul(py[:, 0:256], lhsT=tabc[b][:, kt, :], rhs=Yw[:, kt, 0, :], start=(kt == 0), stop=False), R=[t_tab[b], t_Y[kt]], W=[pyt])
                K.op('pe', lambda e, kt=kt, b=b, py=py: e.matmul(py[:, 0:256], lhsT=tabs[b][:, kt, :], rhs=Yw[:, kt, 1, :], start=False, stop=(kt == NKT - 1)), R=[t_tab[b], t_Y[kt]], W=[pyt])
            K.op('act', lambda e, b=b, py=py: e.activation(out=ysb[b][:], in_=py[:, 0:256], func=AF.Identity), R=[pyt], W=[t_ys[b]])
            pt, ptk = g.ps()
            for ci in range(2):
                K.op('pe', lambda e, ci=ci, b=b, pt=pt: e.transpose(out=pt[:, ci * 128:(ci + 1) * 128], in_=ysb[b][:, ci * 128:(ci + 1) * 128], identity=g.ident[:]),
                     R=[t_ys[b], g.t_const], W=[ptk])
            cs = slice(i * 128, (i + 1) * 128)
            for ci in range(2):
                K.op('act', lambda e, ci=ci, b=b, pt=pt: e.activation(out=tmp[b][:, ci * 128:(ci + 1) * 128], in_=pt[:, ci * 128:(ci + 1) * 128], func=AF.Identity, scale=rnorm[:, ci:ci + 1]),
                     R=[ptk, t_rn], W=[t_tmp[b]])
                K.op('dve', lambda e, ci=ci, b=b, cs=cs: e.scalar_tensor_tensor(out=tmp[b][:, ci * 128:(ci + 1) * 128], in0=zT[:, ci, cs], scalar=hbias[:, ci:ci + 1],
                                                                             in1=tmp[b][:, ci * 128:(ci + 1) * 128], op0=ALU.mult, op1=ALU.add), R=[t_z, t_par, t_tmp[b]], W=[t_tmp[b]])
                K.op('dve', lambda e, ci=ci, b=b, cs=cs: e.tensor_tensor(out=ob[b][:, ci, :], in0=tmp[b][:, ci * 128:(ci + 1) * 128], in1=x0T[:, ci, cs], op=ALU.mult),
                     R=[t_tmp[b], t_x0], W=[t_ob[b]])
            col0 = (c0 + i) * 128
            K.dma(D['mixT_d'][768:1024, col0:col0 + 128].rearrange("(m p) n -> p m n", p=128), ob[b][:], R=[t_ob[b]], W=[g.t_mix], q='pool')


def load_ln_tables(g, st, l, which):
    nc, K = g.nc, g.K
    t = st.enter_context(nc.sbuf_tensor(_nm('lntab'), [128, 2, D_MODEL], F32))
    tk = Tok()
    K.dma(t[:, 0, :], g.D['ln_g'][l, which, :].partition_broadcast(128), W=[tk])
    K.dma(t[:, 1, :], g.D['ln_b'][l, which, :].partition_broadcast(128), W=[tk])
    return t, tk


def deepnorm_chunk(g, l, c, b, which, y_ap, t_y, xres, t_xres, T_, lntab, t_ln, dst_ap, t_dst_tok):
    K = g.K
    cls = 1 if c < 2 else 0
    tmp, t_tmp = T_['tmp'][b], T_['t_tmp'][b]
    for h in range(2):
        K.op('dve', lambda e, h=h: e.tensor_tensor(out=tmp[:, h * 512:(h + 1) * 512], in0=y_ap[h], in1=g.gbc[:, 0, cls, h * 512:(h + 1) * 512], op=ALU.mult),
             R=[t_y[h], g.t_gbc], W=[t_tmp])
    K.op('dve', lambda e: e.scalar_tensor_tensor(out=tmp[:], in0=xres[:], scalar=float(ALPHA), in1=tmp[:], op0=ALU.mult, op1=ALU.add), R=[t_xres, t_tmp], W=[t_tmp])
    mv, rstd, tk = ln_stats(g, T_['sts'][b], tmp, t_tmp)
    K.op('dve', lambda e: e.tensor_scalar(out=tmp[:], in0=tmp[:], scalar1=mv[:, 0:1], scalar2=rstd[:, 0:1], op0=ALU.subtract, op1=ALU.mult), R=[t_tmp, tk], W=[t_tmp])
    K.op('pool', lambda e: e.tensor_tensor(out=tmp[:], in0=tmp[:], in1=lntab[:, 0, :], op=ALU.mult), R=[t_tmp, t_ln], W=[t_tmp])
    K.op('pool', lambda e: e.tensor_tensor(out=dst_ap, in0=tmp[:], in1=lntab[:, 1, :], op=ALU.add), R=[t_tmp, t_ln], W=[t_dst_tok])


def phase_wout(g, l, x_src, t_xsrc, chunks, x1_d, t_x1):
    nc, K, D = g.nc, g.K, g.D
    with ExitStack() as st:
        def sb(name, shape, dt=F32):
            return st.enter_context(nc.sbuf_tensor(_nm(name), list(shape), dt))
        wb, t_w = load_w_bf16(g, st, [(D['w_out'][l], 0, D_MODEL)], D_MODEL, 'w_out')
        lntab, t_ln = load_ln_tables(g, st, l, 0)
        g.gbc = sb('gbcw', [128, 1, 2, D_MODEL])
        K.dma(g.gbc[:], D['gbc_d'][:, 0:1, :, :], R=[g.t_gbcd], W=[g.t_gbc])
        mx = [sb('mx%d' % i, [128, 8, 128], BF16) for i in range(2)]; t_mx = [Tok(), Tok()]
        xr = [sb('xr%d' % i, [128, D_MODEL]) for i in range(2)]; t_xr = [Tok(), Tok()]
        xo = [sb('xo%d' % i, [128, D_MODEL]) for i in range(2)]; t_xo = [Tok(), Tok()]
        T_ = {'tmp': [sb('dn_tmp%d' % i, [128, D_MODEL]) for i in range(2)], 't_tmp': [Tok(), Tok()],
              'sts': [(sb('dstt%d' % i, [128, 2, 6]), sb('dmv%d' % i, [128, 2]), sb('dlnv%d' % i, [128, 1]), sb('drstd%d' % i, [128, 1]), Tok()) for i in range(2)]}
        for n_, c in enumerate(chunks):
            b = n_ % 2
            cs = slice(c * 128, (c + 1) * 128)
            K.dma(mx[b][:], D['mixT_d'][:, cs].rearrange("(kt p) n -> p kt n", p=128), R=[g.t_mix], W=[t_mx[b]])
            K.dma(xr[b][:], x_src[cs, :], R=([t_xsrc[c]] if t_xsrc is not None else []), W=[t_xr[b]])
            pys = []
            for h in range(2):
                py, pyt = g.ps()
                pys.append((py, pyt))
                for kt in range(8):
                    K.op('pe', lambda e, kt=kt, b=b, h=h, py=py: e.matmul(py[:, 0:512], lhsT=mx[b][:, kt, :], rhs=wb[:, kt, h * 512:(h + 1) * 512],
                                                                          start=(kt == 0), stop=(kt == 7)), R=[t_mx[b], t_w], W=[pyt])
            deepnorm_chunk(g, l, c, b, 0, [pys[0][0][:, 0:512], pys[1][0][:, 0:512]], [pys[0][1], pys[1][1]], xr[b], t_xr[b], T_, lntab, t_ln, xo[b][:], t_xo[b])
            K.dma(x1_d[cs, :], xo[b][:], R=[t_xo[b]], W=[t_x1[c]], q='pool')


def phase_ffn(g, l, chunks, w1_src, w3_src, w2_src, vT_d, t_vT, ffn_d, t_ffn, gate=None, first=True):
    nc, K, D = g.nc, g.K, g.D
    with ExitStack() as st:
        def sb(name, shape, dt=F32):
            return st.enter_context(nc.sbuf_tensor(_nm(name), list(shape), dt))
        w1b = sb('w1b', [128, 8, D_FF], BF16); w3b = sb('w3b', [128, 8, D_FF], BF16); w2b = sb('w2b', [128, NF, D_MODEL], BF16)
        t_w = Tok()
        stg = [sb('fstg%d' % i, [128, 8, 256]) for i in range(3)]; t_stg = [Tok() for _ in range(3)]
        k = 0
        for (src, dstw, nk, ncol) in ((w1_src, w1b, 8, D_FF), (w3_src, w3b, 8, D_FF), (w2_src, w2b, NF, D_MODEL)):
            view = src.rearrange("(kt p) n -> p kt n", p=128)
            for k0 in range(0, nk, 8):
                kn = min(8, nk - k0)
                for c0 in range(0, ncol, 256):
                    cn = min(256, ncol - c0)
                    i = k % 3
                    K.dma(stg[i][:, 0:kn, 0:cn], view[:, k0:k0 + kn, c0:c0 + cn], W=[t_stg[i]])
                    eng = ('dve', 'pool', 'act')[k % 3]
                    if eng == 'act':
                        K.op('act', lambda e, i=i, kn=kn, cn=cn, k0=k0, c0=c0, dstw=dstw: e.activation(out=dstw[:, k0:k0 + kn, c0:c0 + cn], in_=stg[i][:, 0:kn, 0:cn], func=AF.Identity),
                             R=[t_stg[i]], W=[t_w])
                    else:
                        K.op(eng, lambda e, i=i, kn=kn, cn=cn, k0=k0, c0=c0, dstw=dstw: e.tensor_copy(out=dstw[:, k0:k0 + kn, c0:c0 + cn], in_=stg[i][:, 0:kn, 0:cn]),
                             R=[t_stg[i]], W=[t_w])
                    k += 1
        vt = [sb('vt%d' % i, [128, 8, 256], BF16) for i in range(2)]; t_vt = [Tok(), Tok()]
        hT = sb('hT', [128, NF, 256], BF16); t_hT = Tok()
        sil = [sb('sil%d' % i, [128, 256]) for i in range(2)]; t_sil = [Tok(), Tok()]
        fo = [sb('fo%d' % i, [128, D_MODEL]) for i in range(2)]; t_fo = [Tok(), Tok()]
        if gate is not None:
            e_idx, gates_d, t_gates = gate
            gt = [sb('gt%d' % i, [128, N_EXP]) for i in range(2)]; t_gt = [Tok(), Tok()]
        nfo = 0
        for ti in range(0, len(chunks), 2):
            cc = chunks[ti:ti + 2]
            b = (ti // 2) % 2
            nt_ = len(cc) * 128
            for j, c in enumerate(cc):
                K.dma(vt[b][:, :, j * 128:(j + 1) * 128], vT_d[c, :, :, :], R=[t_vT[c]], W=[t_vt[b]])
            for f in range(NF):
                ph, pht = g.ps()
                fs = slice(f * 128, (f + 1) * 128)
                for kt in range(8):
                    K.op('pe', lambda e, kt=kt, b=b, ph=ph, fs=fs, nt_=nt_: e.matmul(ph[:, 0:nt_], lhsT=w1b[:, kt, fs], rhs=vt[b][:, kt, 0:nt_], start=(kt == 0), stop=(kt == 7)),
                         R=[t_w, t_vt[b]], W=[pht])
                for kt in range(8):
                    K.op('pe', lambda e, kt=kt, b=b, ph=ph, fs=fs, nt_=nt_: e.matmul(ph[:, 256:256 + nt_], lhsT=w3b[:, kt, fs], rhs=vt[b][:, kt, 0:nt_], start=(kt == 0), stop=(kt == 7)),
                         R=[t_w, t_vt[b]], W=[pht])
                sb_ = f % 2
                K.op('act', lambda e, ph=ph, sb_=sb_, nt_=nt_: e.activation(out=sil[sb_][:, 0:nt_], in_=ph[:, 0:nt_], func=AF.Silu), R=[pht], W=[t_sil[sb_]])
                K.op('dve', lambda e, ph=ph, sb_=sb_, nt_=nt_, f=f: e.tensor_tensor(out=hT[:, f, 0:nt_], in0=ph[:, 256:256 + nt_], in1=sil[sb_][:, 0:nt_], op=ALU.mult),
                     R=[pht, t_sil[sb_]], W=[t_hT])
            for j, c in enumerate(cc):
                ob_ = nfo % 2
                nfo += 1
                if gate is not None:
                    K.dma(gt[ob_][:], gates_d[c, :, :], R=[t_gates[c]], W=[t_gt[ob_]])
                for h in range(2):
                    po, pot = g.ps()
                    for f in range(NF):
                        K.op('pe', lambda e, f=f, j=j, h=h, po=po: e.matmul(po[:, 0:512], lhsT=hT[:, f, j * 128:(j + 1) * 128], rhs=w2b[:, f, h * 512:(h + 1) * 512],
                                                                            start=(f == 0), stop=(f == NF - 1)), R=[t_hT, t_w], W=[pot])
                    if gate is None:
                        K.op('act', lambda e, po=po, ob_=ob_, h=h: e.activation(out=fo[ob_][:, h * 512:(h + 1) * 512], in_=po[:, 0:512], func=AF.Identity), R=[pot], W=[t_fo[ob_]])
                    else:
                        K.op('act', lambda e, po=po, ob_=ob_, h=h: e.activation(out=fo[ob_][:, h * 512:(h + 1) * 512], in_=po[:, 0:512], func=AF.Identity,
                                                                              scale=gt[ob_][:, e_idx:e_idx + 1]), R=[pot, t_gt[ob_]], W=[t_fo[ob_]])
                cs = slice(c * 128, (c + 1) * 128)
                if first:
                    K.dma(ffn_d[cs, :], fo[ob_][:], R=[t_fo[ob_]], W=[t_ffn[c]], q='pool')
                else:
                    K.dma(ffn_d[cs, :], fo[ob_][:], R=[t_fo[ob_]], W=[t_ffn[c]], q='pool', accum_op=ALU.add)


def phase_ln2(g, l, chunks, x1_d, t_x1, ffn_d, t_ffn, dst_fn):
    nc, K, D = g.nc, g.K, g.D
    with ExitStack() as st:
        def sb(name, shape, dt=F32):
            return st.enter_context(nc.sbuf_tensor(_nm(name), list(shape), dt))
        lntab, t_ln = load_ln_tables(g, st, l, 1)
        g.gbc = sb('gbcf', [128, 1, 2, D_MODEL])
        K.dma(g.gbc[:], D['gbc_d'][:, 1:2, :, :], R=[g.t_gbcd], W=[g.t_gbc])
        xr = [sb('l2x%d' % i, [128, D_MODEL]) for i in range(2)]; t_xr = [Tok(), Tok()]
        fr = [sb('l2f%d' % i, [128, D_MODEL]) for i in range(2)]; t_fr = [Tok(), Tok()]
        xo = [sb('l2o%d' % i, [128, D_MODEL]) for i in range(2)]; t_xo = [Tok(), Tok()]
        T_ = {'tmp': [sb('l2tmp%d' % i, [128, D_MODEL]) for i in range(2)], 't_tmp': [Tok(), Tok()],
              'sts': [(sb('l2stt%d' % i, [128, 2, 6]), sb('l2mv%d' % i, [128, 2]), sb('l2lnv%d' % i, [128, 1]), sb('l2rstd%d' % i, [128, 1]), Tok()) for i in range(2)]}
        for n_, c in enumerate(chunks):
            b = n_ % 2
            cs = slice(c * 128, (c + 1) * 128)
            K.dma(xr[b][:], x1_d[cs, :], R=[t_x1[c]], W=[t_xr[b]])
            K.dma(fr[b][:], ffn_d[cs, :], R=[t_ffn[c]], W=[t_fr[b]])
            deepnorm_chunk(g, l, c, b, 1, [fr[b][:, 0:512], fr[b][:, 512:1024]], [t_fr[b], t_fr[b]], xr[b], t_xr[b], T_, lntab, t_ln, xo[b][:], t_xo[b])
            dst, t_dst = dst_fn(c)
            K.dma(dst, xo[b][:], R=[t_xo[b]], W=[t_dst], q='pool')


_CACHE = {}


def kernel(**inputs):
    inp = {k: np.asarray(v) for k, v in inputs.items()}
    if 'prog' not in _CACHE:
        _CACHE['prog'] = build_program()
    nc, g = _CACHE['prog']
    shared = prep_shared(inp)
    maps = []
    for b in range(8):
        m = prep_core_inputs(inp, b, shared)
        maps.append({k: v for k, v in m.items() if k in g.D})
    res = run_bass_kernel_spmd(nc, maps, core_ids=list(range(8)))
    out = np.stack([np.asarray(r['out']) for r in res.results], axis=0)
    return out.astype(np.float32)


def phase_ffn2(g, l, chunks, experts, vT_d, t_vT, ffn_d, t_ffn, gates=None):
    nc, K, D = g.nc, g.K, g.D
    HF = NF // 2
    with ExitStack() as st:
        def sb(name, shape, dt=F32):
            return st.enter_context(nc.sbuf_tensor(_nm(name), list(shape), dt))
        w1s = [sb('w1s%d' % i, [128, 8, HF * 128], BF16) for i in range(2)]
        w3s = [sb('w3s%d' % i, [128, 8, HF * 128], BF16) for i in range(2)]
        w2s = [sb('w2s%d' % i, [128, HF, D_MODEL], BF16) for i in range(2)]
        t_ws = [Tok(), Tok()]
        vt = [sb('vt%d' % i, [128, 8, 512], BF16) for i in range(2)]; t_vt = [Tok(), Tok()]
        hT = [sb('hT%d' % i, [128, HF, 512], BF16) for i in range(2)]; t_hT = [Tok(), Tok()]
        sil = [sb('sil%d' % i, [128, 512]) for i in range(2)]; t_sil = [Tok(), Tok()]
        fo = [sb('fo%d' % i, [128, D_MODEL]) for i in range(2)]; t_fo = [Tok(), Tok()]
        if gates is not None:
            gates_d, t_gates = gates
            gt = [sb('gt%d' % i, [128, N_EXP]) for i in range(2)]; t_gt = [Tok(), Tok()]
        units = [(e_, hf) for e_ in range(len(experts)) for hf in range(2)]

        def load_unit(u):
            e_, hf = units[u]
            w1, w3, w2 = experts[e_]
            s_ = u % 2
            f0 = hf * HF * 128
            v1 = w1.rearrange("(kt p) n -> p kt n", p=128)
            v3 = w3.rearrange("(kt p) n -> p kt n", p=128)
            v2 = w2.rearrange("(ft p) n -> p ft n", p=128)
            for c0 in range(0, HF * 128, 704):
                K.dma(w1s[s_][:, :, c0:c0 + 704], v1[:, :, f0 + c0:f0 + c0 + 704], W=[t_ws[s_]], q='pool')
                K.dma(w3s[s_][:, :, c0:c0 + 704], v3[:, :, f0 + c0:f0 + c0 + 704], W=[t_ws[s_]], q='pool')
            for f_ in range(0, HF, 4):
                fn_ = min(4, HF - f_)
                K.dma(w2s[s_][:, f_:f_ + fn_, :], v2[:, hf * HF + f_:hf * HF + f_ + fn_, :], W=[t_ws[s_]], q='pool')
        load_unit(0)
        nfo = 0
        ntile = 0
        for u in range(len(units)):
            e_, hf = units[u]
            s_ = u % 2
            if u + 1 < len(units):
                load_unit(u + 1)
            _ft = int(os.environ.get('FFN_TILE', '2'))
            for ti in range(0, len(chunks), _ft):
                cc = chunks[ti:ti + _ft]
                b = ntile % 2
                ntile += 1
                nt_ = len(cc) * 128
                for j, c in enumerate(cc):
                    K.dma(vt[b][:, :, j * 128:(j + 1) * 128], vT_d[c, :, :, :], R=[t_vT[c]], W=[t_vt[b]])
                for f in range(HF):
                    ph, pht = g.ps()
                    ph3, pht3 = g.ps()
                    fs = slice(f * 128, (f + 1) * 128)
                    for kt in range(8):
                        K.op('pe', lambda e, kt=kt, b=b, ph=ph, fs=fs, nt_=nt_, s_=s_: e.matmul(ph[:, 0:nt_], lhsT=w1s[s_][:, kt, fs], rhs=vt[b][:, kt, 0:nt_], start=(kt == 0), stop=(kt == 7)),
                             R=[t_ws[s_], t_vt[b]], W=[pht])
                    for kt in range(8):
                        K.op('pe', lambda e, kt=kt, b=b, ph3=ph3, fs=fs, nt_=nt_, s_=s_: e.matmul(ph3[:, 0:nt_], lhsT=w3s[s_][:, kt, fs], rhs=vt[b][:, kt, 0:nt_], start=(kt == 0), stop=(kt == 7)),
                             R=[t_ws[s_], t_vt[b]], W=[pht3])
                    sb_ = f % 2
                    K.op('act', lambda e, ph=ph, sb_=sb_, nt_=nt_: e.activation(out=sil[sb_][:, 0:nt_], in_=ph[:, 0:nt_], func=AF.Silu), R=[pht], W=[t_sil[sb_]])
                    K.op('dve', lambda e, ph3=ph3, sb_=sb_, nt_=nt_, f=f, b=b: e.tensor_tensor(out=hT[b][:, f, 0:nt_], in0=ph3[:, 0:nt_], in1=sil[sb_][:, 0:nt_], op=ALU.mult),
                         R=[pht3, t_sil[sb_]], W=[t_hT[b]])
                for j, c in enumerate(cc):
                    ob_ = nfo % 2
                    nfo += 1
                    if gates is not None:
                        K.dma(gt[ob_][:], gates_d[c, :, :], R=[t_gates[c]], W=[t_gt[ob_]])
                    for h in range(2):
                        po, pot = g.ps()
                        for f in range(HF):
                            K.op('pe', lambda e, f=f, j=j, h=h, po=po, b=b, s_=s_: e.matmul(po[:, 0:512], lhsT=hT[b][:, f, j * 128:(j + 1) * 128], rhs=w2s[s_][:, f, h * 512:(h + 1) * 512],
                                                                                    start=(f == 0), stop=(f == HF - 1)), R=[t_hT[b], t_ws[s_]], W=[pot])
                        if gates is None:
                            K.op('act', lambda e, po=po, ob_=ob_, h=h: e.activation(out=fo[ob_][:, h * 512:(h + 1) * 512], in_=po[:, 0:512], func=AF.Identity), R=[pot], W=[t_fo[ob_]])
                        else:
                            K.op('act', lambda e, po=po, ob_=ob_, h=h, e_=e_: e.activation(out=fo[ob_][:, h * 512:(h + 1) * 512], in_=po[:, 0:512], func=AF.Identity,
                                                                                  scale=gt[ob_][:, e_:e_ + 1]), R=[pot, t_gt[ob_]], W=[t_fo[ob_]])
                    cs = slice(c * 128, (c + 1) * 128)
                    if u == 0:
                        K.dma(ffn_d[cs, :], fo[ob_][:], R=[t_fo[ob_]], W=[t_ffn[c]], q='pool')
                    else:
                        K.dma(ffn_d[cs, :], fo[ob_][:], R=[t_fo[ob_]], W=[t_ffn[c]], q='pool', accum_op=ALU.add)
```

```python
import math
import os
from contextlib import ExitStack
import numpy as np
import ml_dtypes
import concourse.bass as bass
import concourse.mybir as mybir
from concourse.bass_utils import run_bass_kernel_spmd

F32 = mybir.dt.float32
BF16 = mybir.dt.bfloat16
I32 = mybir.dt.int32
AF = mybir.ActivationFunctionType
ALU = mybir.AluOpType
AX = mybir.AxisListType

COMPUTE = ('pe', 'act', 'dve', 'pool')
NDMA = 24


class Tok:
    __slots__ = ('w', 'r')

    def __init__(self):
        self.w = None
        self.r = {}


class Sched:
    def __init__(self, nc, same_engine_sync=True):
        self.nc = nc
        self.es = ExitStack()
        self.E = {'pe': nc.tensor, 'act': nc.scalar, 'dve': nc.vector, 'pool': nc.gpsimd, 'sp': nc.sync}
        self.sem = {e: self.es.enter_context(nc.semaphore('sem_' + e)) for e in COMPUTE}
        self.cnt = {e: 0 for e in COMPUTE}
        self.seen = {f: {} for f in self.E}
        self.dsem = [self.es.enter_context(nc.semaphore('dsem%d' % i)) for i in range(NDMA)]
        self.dcnt = [0] * NDMA
        self.dnext = 0
        self.same = same_engine_sync
        self.ninst = 0

    def _semobj(self, key):
        return self.sem[key] if isinstance(key, str) else self.dsem[key[1]]

    def wait(self, f, ev):
        if ev is None:
            return
        key, val = ev
        if key == f and (f == 'pe' or not self.same):
            return
        if self.seen[f].get(key, 0) >= val:
            return
        self.E[f].wait_ge(self._semobj(key), val)
        self.seen[f][key] = val

    def _deps(self, f, R, W):
        for t in R:
            self.wait(f, t.w)
        for t in W:
            self.wait(f, t.w)
            for k, v in t.r.items():
                self.wait(f, (k, v))

    def op(self, eng, fn, R=(), W=()):
        self._deps(eng, R, W)
        ins = fn(self.E[eng])
        self.cnt[eng] += 1
        ins.then_inc(self.sem[eng], 1)
        ev = (eng, self.cnt[eng])
        self.seen[eng][eng] = self.seen[eng].get(eng, 0)
        for t in R:
            t.r[eng] = self.cnt[eng]
        for t in W:
            t.w = ev
            t.r = {}
        self.ninst += 1
        return ins

    def dma(self, out, in_, R=(), W=(), q='sp', **kw):
        i = self.dnext
        self.dnext = (i + 1) % NDMA
        key = ('d', i)
        if self.dcnt[i] > 0:
            self.wait(q, (key, self.dcnt[i]))
        self._deps(q, R, W)
        ins = self.E[q].dma_start(out=out, in_=in_, **kw)
        self.dcnt[i] += 16
        ins.then_inc(self.dsem[i], 16)
        for t in R:
            t.r[key] = self.dcnt[i]
        for t in W:
            t.w = (key, self.dcnt[i])
            t.r = {}
        self.ninst += 1
        return ins

    def barrier(self):
        for f in self.E:
            for e in COMPUTE:
                if self.cnt[e] > 0:
                    self.wait_force(f, (e, self.cnt[e]))
            for i in range(NDMA):
                if self.dcnt[i] > 0:
                    self.wait(f, (('d', i), self.dcnt[i]))

    def wait_force(self, f, ev):
        key, val = ev
        if self.seen[f].get(key, 0) >= val:
            return
        self.E[f].wait_ge(self._semobj(key), val)
        self.seen[f][key] = val


D_MODEL = 1024
SEQ = 4096
CTXL = 256
T = SEQ + CTXL
NCH = T // 128
DEPTH = 2
D_PROJ = 2848
D_FF = 2816
NF = D_FF // 128
N_EXP = 8
LN_EPS = 1e-5
ALPHA = (2 * DEPTH) ** 0.25
OFF = {}
_o = 0
for _n, _w in (('ret_q', 256), ('ret_k', 256), ('ret_v', 256), ('ret_g', 256), ('gla_q', 128), ('gla_k', 128),
               ('gla_v', 256), ('gla_r', 256), ('gla_a', 32), ('s5_u', 256), ('hy_p', 768)):
    OFF[_n] = _o
    _o += _w


class G:
    pass


_uid = [0]


def _nm(name):
    _uid[0] += 1
    return '%s_%d' % (name, _uid[0])


def build_program(stop_after=None, dbg=(), dbg_layer=0):
    nc = bass.Bass("TRN2", target_bir_lowering=False)
    K = Sched(nc, same_engine_sync=(os.environ.get('SAME', '1') == '1'))
    g = G()
    g.nc, g.K, g.dbg, g.stop_after, g.dbg_layer = nc, K, set(dbg), stop_after, dbg_layer
    g.D = {}
    g.outs = {}

    def din(name, shape, dt=F32):
        g.D[name] = nc.dram_tensor(name, list(shape), dt, kind="ExternalInput").ap()
        return g.D[name]

    def dscr(name, shape, dt=F32):
        g.D[name] = nc.dram_tensor(name, list(shape), dt, kind="Internal").ap()
        return g.D[name]

    def dout(name, shape, dt=F32):
        g.outs[name] = nc.dram_tensor(name, list(shape), dt, kind="ExternalOutput").ap()
        return g.outs[name]
    g.din, g.dscr, g.dout = din, dscr, dout

    din('xin', [T, D_MODEL])
    din('cvec', [128, 8, 2])
    din('ada_w', [DEPTH, D_MODEL, 6 * D_MODEL])
    din('ada_bT', [DEPTH, 128, 48])
    din('ada_b', [DEPTH, 6 * D_MODEL])
    din('w_out', [DEPTH, D_MODEL, D_MODEL])
    din('ln_g', [DEPTH, 2, D_MODEL])
    din('ln_b', [DEPTH, 2, D_MODEL])
    din('ffn_w1', [1, D_MODEL, D_FF])
    din('ffn_w3', [1, D_MODEL, D_FF])
    din('ffn_w2', [1, D_FF, D_MODEL])
    din('router_w', [1, D_MODEL, N_EXP])
    din('router_b', [1, N_EXP])
    din('moe_w1', [1, N_EXP, D_MODEL, D_FF])
    din('moe_w3', [1, N_EXP, D_MODEL, D_FF])
    din('moe_w2', [1, N_EXP, D_FF, D_MODEL])
    din('w_in', [DEPTH, D_MODEL, D_PROJ])
    din('ident', [128, 128])
    din('maskL', [128, 128])
    din('maskU', [128, 128])
    din('antiI', [128, 128])
    din('poscols', [128, 4])
    din('rot_cos', [128, 32, 32])
    din('rot_sin', [128, 32, 32])
    din('gla_wa', [DEPTH, 2, 16, 128])
    din('gla_ba', [DEPTH, 2, 128])
    din('gla_ng', [DEPTH, 256])
    din('ret_decay', [DEPTH, 8])
    din('ret_dec_fm', [DEPTH, 2, 128, 2])
    din('ret_gng', [DEPTH, 256])
    din('ret_gnb', [DEPTH, 256])
    din('s5_lam_fm', [DEPTH, 128, 16, 2])
    din('s5_dt_fm', [DEPTH, 128, 16])
    din('s5_BT', [DEPTH, 16, 2, 128, 128])
    din('s5_CT', [DEPTH, 16, 2, 128, 32])
    din('s5_dcol', [DEPTH, 128, 2])
    din('s5_glub', [DEPTH, 128, 2])
    din('s5_glu_w', [DEPTH, 256, 256])
    din('iota_t', [T])
    din('hy_cw', [DEPTH, 128, 6, 3])
    din('hy_cb', [DEPTH, 128, 6])
    din('hy_biasc', [DEPTH, 128, 2])
    din('hy_fw1', [DEPTH, 33, 64])
    din('hy_fw2', [DEPTH, 64, 64])
    din('hy_fw3', [DEPTH, 64, 512])
    din('hy_fcol', [DEPTH, 64, 5])
    din('hy_decay', [DEPTH, 2, 256])
    for sfx, n in (('L', SEQ), ('C', CTXL)):
        nt = n // 128
        din('hy_zemb' + sfx, [33, n])
        din('hy_tn' + sfx, [128, nt])
        din('hy_wk' + sfx, [128, nt + 1])
        din('dftc' + sfx, [nt + 1, 128, nt + 1, 128], BF16)
        din('dfts' + sfx, [nt + 1, 128, nt + 1, 128], BF16)

    pst = ExitStack()
    K.es.enter_context(pst)

    def sbp(name, shape, dt=F32):
        return pst.enter_context(nc.sbuf_tensor(_nm(name), list(shape), dt))
    g.ident = sbp('ident', [128, 128]); g.t_const = Tok()
    g.identb = sbp('identb', [128, 128], BF16)
    g.maskL = sbp('maskL', [128, 128]); g.maskU = sbp('maskU', [128, 128]); g.antiI = sbp('antiI', [128, 128])
    g.antiIb = sbp('antiIb', [128, 128], BF16)
    g.mask2 = sbp('mask2', [128, 2, 128])
    g.ones = sbp('ones', [128, 128]); g.onesb = sbp('onesb', [128, 128], BF16)
    g.epsc = sbp('epsc', [128, 1])
    g.poscols = sbp('poscols', [128, 4])
    g.lnc = sbp('lnc', [128, 2])
    g.modT = sbp('modT', [128, 48, 2]); g.t_mod = Tok()
    g.t_gbc = Tok(); g.t_gbcd = Tok()
    g.mod1 = sbp('mod1', [128, 48, 2])
    NPS = 6
    g.psum = [pst.enter_context(nc.psum_tensor('ps%d' % i, [128, 512], F32)) for i in range(NPS)]
    g.pst = [Tok() for _ in range(NPS)]
    g.pnext = 0
    g.psb = [pst.enter_context(nc.psum_tensor('psb%d' % i, [128, 1024], BF16)) for i in range(2)]
    g.t_psb = [Tok(), Tok()]
    g.psb_next = 0

    def ps():
        i = g.pnext
        g.pnext = (i + 1) % NPS
        return g.psum[i], g.pst[i]
    g.ps = ps

    def psb_half():
        i = g.psb_next
        g.psb_next = 1 - i
        return g.psb[i][:, 0:512], g.t_psb[i]
    g.psb_half = psb_half

    tc = g.t_const
    K.dma(g.ident[:], g.D['ident'][:, :], W=[tc])
    K.dma(g.maskL[:], g.D['maskL'][:, :], W=[tc])
    K.dma(g.maskU[:], g.D['maskU'][:, :], W=[tc])
    K.dma(g.mask2[:, 0, :], g.D['maskL'][:, :], W=[tc])
    K.dma(g.mask2[:, 1, :], g.D['maskU'][:, :], W=[tc])
    K.dma(g.antiI[:], g.D['antiI'][:, :], W=[tc])
    K.dma(g.poscols[:], g.D['poscols'][:, :], W=[tc])
    K.op('pool', lambda e: e.memset(g.ones[:], 1.0), W=[tc])
    K.op('pool', lambda e: e.memset(g.onesb[:], 1.0), W=[tc])
    K.op('pool', lambda e: e.memset(g.epsc[:], LN_EPS), W=[tc])
    K.op('pool', lambda e: e.memset(g.lnc[:, 0:1], math.log(0.125)), W=[tc])
    K.op('pool', lambda e: e.memset(g.lnc[:, 1:2], math.log(32 ** -0.5)), W=[tc])
    K.op('dve', lambda e: e.tensor_copy(out=g.identb[:], in_=g.ident[:]), R=[tc], W=[tc])
    K.op('dve', lambda e: e.tensor_copy(out=g.antiIb[:], in_=g.antiI[:]), R=[tc], W=[tc])
    K.barrier()

    dscr('x_cur', [T, D_MODEL])

    dscr('uT_d', [NCH, 128, 8, 128], BF16)
    dscr('gbc_d', [128, 2, 2, D_MODEL])
    dscr('vT_d', [NCH, 128, 8, 128], BF16)
    dscr('mixT_d', [D_MODEL, T], BF16)
    dscr('x1_d', [T, D_MODEL])
    dscr('ffn_d', [T, D_MODEL])
    dscr('gates_d', [NCH, 128, N_EXP])
    dout('out', [SEQ, D_MODEL])
    g.t_uT = [Tok() for _ in range(NCH)]
    g.t_mix = Tok()
    t_vT = [Tok() for _ in range(NCH)]
    t_x1 = [Tok() for _ in range(NCH)]
    t_ffn = [Tok() for _ in range(NCH)]
    t_gates = [Tok() for _ in range(NCH)]
    t_xcur = [Tok() for _ in range(NCH)]
    t_out = Tok()
    D = g.D
    x_src, t_xsrc = D['xin'], None
    for l in range(DEPTH):
        last = l == DEPTH - 1
        phase_mod(g, l)
        K.barrier()
        if stop_after == ('mod', l):
            break
        phase_lnmod(g, l, x_src, D['uT_d'], g.t_uT, sh_idx=0, sc_idx=8, t_src=t_xsrc)
        K.barrier()
        if 'uT' in g.dbg and l == g.dbg_layer:
            o = dout('dbg_uT', [NCH, 128, 8, 128], BF16)
            K.dma(o[:, :, :, :], D['uT_d'][:, :, :, :], R=g.t_uT, q='pool')
            K.barrier()
        if stop_after == ('lnmod', l):
            break
        _ps = os.environ.get('LA_PASSES', 'g0,g1,r0,r1').split(',')
        for p in range(2):
            if 'g%d' % p in _ps:
                la_pass(g, l, 'gla', p, last)
                K.barrier()
        for p in range(2):
            if 'r%d' % p in _ps:
                la_pass(g, l, 'ret', p, last)
                K.barrier()
        if 's5' in os.environ.get('MIXERS', 's5,hy'):
            phase_s5(g, l)
            K.barrier()
        if 'hy' in os.environ.get('MIXERS', 's5,hy'):
            phase_hyena(g, l, SEQ, 2, 'L')
            K.barrier()
            if not last:
                phase_hyena(g, l, CTXL, 0, 'C')
                K.barrier()
        if 'mix' in g.dbg and l == g.dbg_layer:
            o = dout('dbg_mix', [D_MODEL, T], BF16)
            K.dma(o[:, :], D['mixT_d'][:, :], R=[g.t_mix], q='pool')
            K.barrier()
        if stop_after == ('la', l):
            break
        chunks = list(range(2, NCH)) if last else list(range(NCH))
        phase_wout(g, l, x_src, t_xsrc, chunks, D['x1_d'], t_x1)
        K.barrier()
        if 'gbc' in g.dbg and l == g.dbg_layer:
            o = dout('dbg_gbc', [128, 2, 2, D_MODEL])
            K.dma(o[:, :, :, :], D['gbc_d'][:, :, :, :], R=[g.t_gbcd], q='pool')
            K.barrier()
        if 'x1' in g.dbg and l == g.dbg_layer:
            o = dout('dbg_x1', [T, D_MODEL])
            K.dma(o[:, :], D['x1_d'][:, :], R=t_x1, q='pool')
            K.barrier()
        if stop_after == ('wout', l):
            break
        j = l // 2
        if l % 2 == 0:
            phase_lnmod(g, l, D['x1_d'], D['vT_d'], t_vT, sh_idx=24, sc_idx=32, chunks=chunks, t_src=t_x1)
            K.barrier()
            phase_ffn2(g, l, chunks, [(D['ffn_w1'][j], D['ffn_w3'][j], D['ffn_w2'][j])], D['vT_d'], t_vT, D['ffn_d'], t_ffn)
            K.barrier()
        else:
            phase_lnmod(g, l, D['x1_d'], D['vT_d'], t_vT, sh_idx=24, sc_idx=32, chunks=chunks, t_src=t_x1, router=(j, D['gates_d'], t_gates))
            K.barrier()
            phase_ffn2(g, l, chunks, [(D['moe_w1'][j, e_], D['moe_w3'][j, e_], D['moe_w2'][j, e_]) for e_ in range(N_EXP)],
                       D['vT_d'], t_vT, D['ffn_d'], t_ffn, gates=(D['gates_d'], t_gates))
            K.barrier()
        if 'ffn' in g.dbg and l == g.dbg_layer:
            o = dout('dbg_ffn', [T, D_MODEL])
            K.dma(o[:, :], D['ffn_d'][:, :], R=t_ffn, q='pool')
            K.barrier()
        if stop_after == ('ffn', l):
            break
        if last:
            def dst_fn(c):
                return g.outs['out'][(c - 2) * 128:(c - 1) * 128, :], t_out
        else:
            def dst_fn(c):
                return D['x_cur'][c * 128:(c + 1) * 128, :], t_xcur[c]
        phase_ln2(g, l, chunks, D['x1_d'], t_x1, D['ffn_d'], t_ffn, dst_fn)
        K.barrier()
        if 'x2' in g.dbg and l == g.dbg_layer and not last:
            o = dout('dbg_x2', [T, D_MODEL])
            K.dma(o[:, :], D['x_cur'][:, :], R=t_xcur, q='pool')
            K.barrier()
        if stop_after == ('ln2', l):
            break
        x_src, t_xsrc = D['x_cur'], t_xcur
    K.barrier()
    return nc, g


def phase_mod(g, l):
    nc, K = g.nc, g.K
    with ExitStack() as st:
        def sb(name, shape, dt=F32):
            return st.enter_context(nc.sbuf_tensor(_nm(name), list(shape), dt))
        cv = sb('cv', [128, 8, 2]); t_cv = Tok()
        sv = sb('sv', [128, 8, 2])
        abT = sb('abT', [128, 48]); t_ab = Tok()
        wbuf = [sb('adaw%d' % i, [128, 8, 512]) for i in range(2)]
        t_w = [Tok(), Tok()]
        K.dma(cv[:], g.D['cvec'][:, :, :], W=[t_cv])
        K.dma(abT[:], g.D['ada_bT'][l, :, :], W=[t_ab])
        K.op('act', lambda e: e.activation(out=sv[:], in_=cv[:], func=AF.Silu), R=[t_cv], W=[t_cv])
        wsrc = g.D['ada_w'][l].rearrange("(kt p) n -> p kt n", p=128)
        svrep = sb('svrep', [128, 8, 2, 128])
        g.gbc = sb('gbc', [128, 2, 2, D_MODEL])
        K.op('dve', lambda e: e.tensor_copy(out=svrep[:], in_=sv[:].unsqueeze(3).to_broadcast([128, 8, 2, 128])), R=[t_cv], W=[t_cv])
        K.dma(g.gbc[:, 0, 0, :], g.D['ada_b'][l, 2048:3072].partition_broadcast(128), W=[g.t_gbc])
        K.dma(g.gbc[:, 0, 1, :], g.D['ada_b'][l, 2048:3072].partition_broadcast(128), W=[g.t_gbc])
        K.dma(g.gbc[:, 1, 0, :], g.D['ada_b'][l, 5120:6144].partition_broadcast(128), W=[g.t_gbc])
        K.dma(g.gbc[:, 1, 1, :], g.D['ada_b'][l, 5120:6144].partition_broadcast(128), W=[g.t_gbc])
        for grp in range(12):
            b = grp % 2
            K.dma(wbuf[b][:], wsrc[:, :, grp * 512:(grp + 1) * 512], W=[t_w[b]])
            if grp in (4, 5, 10, 11):
                mf = 0 if grp < 6 else 1
                hf = grp % 2
                for cls in range(2):
                    pq, pqt = g.ps()
                    for kt in range(8):
                        K.op('pe', lambda e, kt=kt, b=b, cls=cls, pq=pq: e.matmul(pq[:, 0:512], lhsT=svrep[:, kt, cls, :], rhs=wbuf[b][:, kt, :],
                                                                                  start=(kt == 0), stop=(kt == 7)), R=[t_w[b], t_cv], W=[pqt])
                    dst = g.gbc[:, mf, cls, hf * 512:(hf + 1) * 512]
                    K.op('dve', lambda e, pq=pq, dst=dst: e.tensor_tensor(out=dst, in0=pq[:, 0:512], in1=dst, op=ALU.add), R=[pqt, g.t_gbc], W=[g.t_gbc])
            pt, ptk = g.ps()
            for jj in range(4):
                for kt in range(8):
                    K.op('pe', lambda e, jj=jj, kt=kt, b=b, pt=pt: e.matmul(
                        pt[:, 2 * jj:2 * jj + 2], lhsT=wbuf[b][:, kt, jj * 128:(jj + 1) * 128], rhs=sv[:, kt, :],
                        start=(kt == 0), stop=(kt == 7)), R=[t_w[b], t_cv], W=[ptk])
            K.op('dve', lambda e, pt=pt, grp=grp: e.tensor_tensor(
                out=g.modT[:, grp * 4:(grp + 1) * 4, :], in0=pt[:, 0:8].rearrange("p (j k) -> p j k", k=2),
                in1=abT[:, grp * 4:(grp + 1) * 4].unsqueeze(2).to_broadcast([128, 4, 2]), op=ALU.add), R=[ptk, t_ab], W=[g.t_mod])
        K.op('dve', lambda e: e.tensor_scalar_add(out=g.mod1[:], in0=g.modT[:], scalar1=1.0), R=[g.t_mod], W=[g.t_mod])
        K.dma(g.D['gbc_d'][:, :, :, :], g.gbc[:], R=[g.t_gbc], W=[g.t_gbcd], q='pool')
        K.barrier()


def ln_stats(g, st_tiles, xc, t_xc, eps=LN_EPS):
    K = g.K
    stt, mv, lnv, rstd, tok = st_tiles
    for h in range(2):
        K.op('dve', lambda e, h=h: e.bn_stats(out=stt[:, h, :], in_=xc[:, h * 512:(h + 1) * 512]), R=[t_xc], W=[tok])
    K.op('dve', lambda e: e.bn_aggr(out=mv[:], in_=stt[:].rearrange("p a b -> p (a b)")), R=[tok], W=[tok])
    K.op('act', lambda e: e.activation(out=lnv[:], in_=mv[:, 1:2], func=AF.Ln, bias=g.epsc[:, 0:1]), R=[tok, g.t_const], W=[tok])
    K.op('act', lambda e: e.activation(out=rstd[:], in_=lnv[:], func=AF.Exp, scale=-0.5), R=[tok], W=[tok])
    return mv, rstd, tok


def phase_lnmod(g, l, x_src, uT_d, t_uT, sh_idx, sc_idx, chunks=None, t_src=None, router=None):
    nc, K = g.nc, g.K
    with ExitStack() as st:
        def sb(name, shape, dt=F32):
            return st.enter_context(nc.sbuf_tensor(_nm(name), list(shape), dt))
        xc = [sb('xc%d' % i, [128, 1024]) for i in range(2)]
        t_xc = [Tok(), Tok()]
        xn = [sb('xn%d' % i, [128, 1024]) for i in range(2)]
        t_xn = [Tok(), Tok()]
        uc = [sb('uc%d' % i, [128, 8, 128], BF16) for i in range(2)]
        t_uc = [Tok(), Tok()]
        sts = [(sb('stt%d' % i, [128, 2, 6]), sb('mv%d' % i, [128, 2]), sb('lnv%d' % i, [128, 1]), sb('rstd%d' % i, [128, 1]), Tok())
               for i in range(2)]
        if router is not None:
            j_moe, gates_d, t_gates = router
            uc32 = [sb('uc32_%d' % i, [128, 8, 128]) for i in range(2)]; t_uc32 = [Tok(), Tok()]
            rw = sb('rw', [128, 8, N_EXP]); rb = sb('rb', [128, N_EXP]); t_rw = Tok()
            K.dma(rw[:], g.D['router_w'][j_moe].rearrange("(kt p) n -> p kt n", p=128), W=[t_rw])
            K.dma(rb[:], g.D['router_b'][j_moe].partition_broadcast(128), W=[t_rw])
            rl = [sb('rl%d' % i, [128, 6, N_EXP]) for i in range(2)]; rs = [sb('rs%d' % i, [128, 4]) for i in range(2)]; t_rl = [Tok(), Tok()]
        cl_ = list(chunks if chunks is not None else range(NCH))

        def stX(n, c):
            b = n % 2
            col = 1 if c < 2 else 0
            K.dma(xc[b][:], x_src[c * 128:(c + 1) * 128, :], R=([t_src[c]] if t_src is not None else []), W=[t_xc[b]])
            mv, rstd, tk = ln_stats(g, sts[b], xc[b], t_xc[b])
            K.op('dve', lambda e, b=b, mv=mv, rstd=rstd: e.tensor_scalar(
                out=xn[b][:], in0=xc[b][:], scalar1=mv[:, 0:1], scalar2=rstd[:, 0:1], op0=ALU.subtract, op1=ALU.mult),
                R=[t_xc[b], tk], W=[t_xn[b]])

        def stY(n, c):
            b = n % 2
            col = 1 if c < 2 else 0
            mv, rstd, tk = None, None, None
            for half in range(2):
                pt, ptk = g.ps()
                for q in range(4):
                    kt = half * 4 + q
                    K.op('pe', lambda e, b=b, kt=kt, q=q, pt=pt: e.transpose(
                        out=pt[:, q * 128:(q + 1) * 128], in_=xn[b][:, kt * 128:(kt + 1) * 128], identity=g.ident[:]),
                        R=[t_xn[b], g.t_const], W=[ptk])
                for q in range(4):
                    kt = half * 4 + q
                    dst, t_dst = (uc[b], t_uc[b]) if router is None else (uc32[b], t_uc32[b])
                    K.op('act', lambda e, kt=kt, q=q, pt=pt, dst=dst, col=col: e.activation(
                        out=dst[:, kt, :], in_=pt[:, q * 128:(q + 1) * 128], func=AF.Identity,
                        scale=g.mod1[:, sc_idx + kt, col:col + 1], bias=g.modT[:, sh_idx + kt, col:col + 1]),
                        R=[ptk, g.t_mod], W=[t_dst])
            if router is not None:
                K.op('pool', lambda e, b=b: e.tensor_copy(out=uc[b][:], in_=uc32[b][:]), R=[t_uc32[b]], W=[t_uc[b]])
                pr, prt = g.ps()
                for kt in range(8):
                    K.op('pe', lambda e, kt=kt, b=b, pr=pr: e.matmul(pr[:, 0:N_EXP], lhsT=uc32[b][:, kt, :], rhs=rw[:, kt, :], start=(kt == 0), stop=(kt == 7)),
                         R=[t_uc32[b], t_rw], W=[prt])
                L_, r4, tk2 = rl[b], rs[b], t_rl[b]
                K.op('dve', lambda e, pr=pr, L_=L_: e.tensor_tensor(out=L_[:, 0, :], in0=pr[:, 0:N_EXP], in1=rb[:], op=ALU.add), R=[prt, t_rw], W=[tk2])
                K.op('dve', lambda e, L_=L_, r4=r4: e.tensor_reduce(out=r4[:, 0:1], in_=L_[:, 0, :], axis=AX.X, op=ALU.max), R=[tk2], W=[tk2])
                K.op('dve', lambda e, L_=L_, r4=r4: e.tensor_scalar(out=L_[:, 1, :], in0=L_[:, 0, :], scalar1=r4[:, 0:1], scalar2=None, op0=ALU.is_equal), R=[tk2], W=[tk2])
                K.op('dve', lambda e, L_=L_: e.scalar_tensor_tensor(out=L_[:, 2, :], in0=L_[:, 1, :], scalar=-1e30, in1=L_[:, 0, :], op0=ALU.mult, op1=ALU.add), R=[tk2], W=[tk2])
                K.op('dve', lambda e, L_=L_, r4=r4: e.tensor_reduce(out=r4[:, 1:2], in_=L_[:, 2, :], axis=AX.X, op=ALU.max), R=[tk2], W=[tk2])
                K.op('dve', lambda e, L_=L_, r4=r4: e.tensor_scalar(out=L_[:, 3, :], in0=L_[:, 0, :], scalar1=r4[:, 1:2], scalar2=None, op0=ALU.is_ge), R=[tk2], W=[tk2])
                K.op('dve', lambda e, r4=r4: e.tensor_scalar_mul(out=r4[:, 2:3], in0=r4[:, 0:1], scalar1=-1.0), R=[tk2], W=[tk2])
                K.op('act', lambda e, L_=L_, r4=r4: e.activation(out=L_[:, 4, :], in_=L_[:, 0, :], func=AF.Exp, bias=r4[:, 2:3]), R=[tk2], W=[tk2])
                K.op('dve', lambda e, L_=L_: e.tensor_tensor(out=L_[:, 4, :], in0=L_[:, 4, :], in1=L_[:, 3, :], op=ALU.mult), R=[tk2], W=[tk2])
                K.op('dve', lambda e, L_=L_, r4=r4: e.tensor_reduce(out=r4[:, 3:4], in_=L_[:, 4, :], axis=AX.X, op=ALU.add), R=[tk2], W=[tk2])
                K.op('dve', lambda e, r4=r4: e.reciprocal(out=r4[:, 3:4], in_=r4[:, 3:4]), R=[tk2], W=[tk2])
                K.op('dve', lambda e, L_=L_, r4=r4: e.tensor_scalar_mul(out=L_[:, 5, :], in0=L_[:, 4, :], scalar1=r4[:, 3:4]), R=[tk2], W=[tk2])
                K.dma(gates_d[c, :, :], L_[:, 5, :], R=[tk2], W=[t_gates[c]], q='pool')
            K.dma(uT_d[c, :, :, :], uc[b][:], R=[t_uc[b]], W=[t_uT[c]], q='pool')

        stX(0, cl_[0])
        for n, c in enumerate(cl_):
            if n + 1 < len(cl_):
                stX(n + 1, cl_[n + 1])
            stY(n, c)


def const_inputs():
    idx = np.arange(128)
    c = {}
    c['ident'] = np.eye(128, dtype=np.float32)
    c['maskL'] = (idx[:, None] <= idx[None, :]).astype(np.float32)
    c['maskU'] = (idx[:, None] >= idx[None, :]).astype(np.float32)
    c['antiI'] = np.ascontiguousarray(np.eye(128, dtype=np.float32)[::-1])
    i = idx.astype(np.float32)
    c['poscols'] = np.stack([i + 1, 128 - i, -(i + 1), -(128 - i)], axis=1).astype(np.float32)
    tpos = np.arange(SEQ)
    row = (tpos // 64).astype(np.float32)
    colp = (tpos % 64).astype(np.float32)
    n_freq = 16
    inv = (10000.0 ** (-np.arange(n_freq, dtype=np.float32) / n_freq)).astype(np.float32)
    ang = np.concatenate([row[:, None] * inv, colp[:, None] * inv], axis=-1).astype(np.float32)
    for sfx, n in (('L', SEQ), ('C', CTXL)):
        nt = n // 128
        tlin = np.linspace(0.0, 1.0, n, dtype=np.float32)
        ii = np.arange(n, dtype=np.float32)[:, None]
        bands = np.linspace(1e-4, 15, 16, dtype=np.float32)[None, :]
        ang2 = (np.float32(2.0 * math.pi / n) * bands * ii).astype(np.float32)
        zemb = np.concatenate([tlin[:, None], np.cos(ang2), -np.sin(ang2)], axis=-1).astype(np.float32)
        c['hy_zemb' + sfx] = np.ascontiguousarray(zemb.T)
        c['hy_tn' + sfx] = np.ascontiguousarray(tlin.reshape(nt, 128).T)
        N2 = 2 * n
        kk = np.arange((nt + 1) * 128)
        wkv = np.where(kk > n, 0.0, np.where((kk == 0) | (kk == n), 1.0 / N2, 2.0 / N2)).astype(np.float32)
        c['hy_wk' + sfx] = np.ascontiguousarray(wkv.reshape(nt + 1, 128).T)
        a = kk.reshape(nt + 1, 128)
        prod = (a.T[None, :, :, None].astype(np.int64) * a[:, None, None, :].astype(np.int64)) % N2
        lut_c = np.cos(2.0 * np.pi * np.arange(N2) / N2)
        lut_s = np.sin(2.0 * np.pi * np.arange(N2) / N2)
        valid = (a.T[None, :, :, None] <= n) & (a[:, None, None, :] <= n)
        c['dftc' + sfx] = np.where(valid, lut_c[prod], 0.0).astype(ml_dtypes.bfloat16)
        c['dfts' + sfx] = np.where(valid, lut_s[prod], 0.0).astype(ml_dtypes.bfloat16)
    c['rot_cos'] = np.ascontiguousarray(np.cos(ang).astype(np.float32).reshape(32, 128, 32).transpose(1, 0, 2))
    c['rot_sin'] = np.ascontiguousarray(np.sin(ang).astype(np.float32).reshape(32, 128, 32).transpose(1, 0, 2))
    return c


def prep_core_inputs(inp, b, shared):
    m = dict(shared)
    m['xin'] = np.ascontiguousarray(np.concatenate([inp['ctx'][b], inp['x'][b]], axis=0))
    cv = np.stack([inp['c'][b], inp['c_ctx']], axis=-1)
    m['cvec'] = np.ascontiguousarray(cv.reshape(8, 128, 2).transpose(1, 0, 2))
    return m


def prep_shared(inp):
    m = const_inputs()
    m['gla_wa'] = inp['gla_wa']
    m['gla_ba'] = inp['gla_ba']
    m['gla_ng'] = inp['gla_norm_g']
    m['ret_decay'] = np.ascontiguousarray(inp['ret_decay'].reshape(DEPTH, 8))
    rd = inp['ret_decay']
    fm = np.zeros((DEPTH, 2, 128, 2), np.float32)
    for p in range(2):
        for h2 in range(2):
            fm[:, p, h2 * 64:(h2 + 1) * 64, :] = rd[:, :, 2 * p + h2][:, None, :]
    m['ret_dec_fm'] = fm
    m['ret_gng'] = inp['ret_gn_g']
    L = DEPTH
    lam = np.stack([inp['s5_lam_re'], inp['s5_lam_im']], axis=-1)
    lam = lam.reshape(L, 2, 8, 2, 64, 2)
    m['s5_lam_fm'] = np.ascontiguousarray(lam.transpose(0, 3, 4, 1, 2, 5).reshape(L, 128, 16, 2))
    ldt = np.broadcast_to(inp['s5_log_dt'].reshape(L, 2, 8, 2, 1), (L, 2, 8, 2, 64))
    m['s5_dt_fm'] = np.ascontiguousarray(ldt.transpose(0, 3, 4, 1, 2).reshape(L, 128, 16))
    BT = np.zeros((L, 2, 8, 2, 128, 128), np.float32)
    CT = np.zeros((L, 2, 8, 2, 128, 32), np.float32)
    for j in range(8):
        for g2 in range(2):
            gi = 2 * j + g2
            r0 = (gi % 8) * 16
            for ri, (bn, cn) in enumerate((('s5_b_re', 's5_c_re'), ('s5_b_im', 's5_c_im'))):
                BT[:, :, j, ri, r0:r0 + 16, g2 * 64:(g2 + 1) * 64] = inp[bn][:, :, gi].transpose(0, 1, 3, 2)
                CT[:, :, j, ri, g2 * 64:(g2 + 1) * 64, g2 * 16:(g2 + 1) * 16] = inp[cn][:, :, gi].transpose(0, 1, 3, 2)
    m['s5_BT'] = BT.reshape(L, 16, 2, 128, 128)
    m['s5_CT'] = CT.reshape(L, 16, 2, 128, 32)
    m['s5_dcol'] = np.ascontiguousarray(inp['s5_d'].reshape(L, 2, 128).transpose(0, 2, 1))
    m['s5_glub'] = np.ascontiguousarray(inp['s5_glu_b'].reshape(L, 2, 128).transpose(0, 2, 1))
    m['s5_glu_w'] = inp['s5_glu_w']
    m['iota_t'] = np.arange(T, dtype=np.float32)
    m['hy_cw'] = np.ascontiguousarray(inp['hy_conv_w'].reshape(L, 3, 6, 128).transpose(0, 3, 2, 1))
    m['hy_cb'] = np.ascontiguousarray(inp['hy_conv_b'].reshape(L, 6, 128).transpose(0, 2, 1))
    m['hy_biasc'] = np.ascontiguousarray(inp['hy_bias'].reshape(L, 2, 128).transpose(0, 2, 1))
    m['hy_fw1'] = inp['hy_fw1']; m['hy_fw2'] = inp['hy_fw2']; m['hy_fw3'] = inp['hy_fw3']
    fc = np.zeros((L, 64, 5), np.float32)
    fc[:, :, 0] = inp['hy_fb1']; fc[:, :, 1] = inp['hy_fb2']; fc[:, :, 2] = inp['hy_freq']
    m['hy_fcol'] = fc
    m['hy_decay'] = inp['hy_decay']
    m['ret_gnb'] = inp['ret_gn_b']
    m['ada_w'] = inp['ada_w']
    m['ada_b'] = inp['ada_b']
    m['w_out'] = inp['w_out']
    m['ln_g'] = np.ascontiguousarray(np.stack([inp['ln_mix_g'], inp['ln_ffn_g']], axis=1))
    m['ln_b'] = np.ascontiguousarray(np.stack([inp['ln_mix_b'], inp['ln_ffn_b']], axis=1))
    for k_ in ('ffn_w1', 'ffn_w3', 'ffn_w2', 'router_w', 'router_b', 'moe_w1', 'moe_w3', 'moe_w2'):
        m[k_] = inp[k_]
    m['ada_bT'] = np.ascontiguousarray(inp['ada_b'].reshape(DEPTH, 48, 128).transpose(0, 2, 1))
    m['w_in'] = inp['w_in']
    return m


def load_w_bf16(g, st, wsrc_cols, ncols_total, name):
    nc, K = g.nc, g.K
    wb = st.enter_context(nc.sbuf_tensor(_nm(name), [128, 8, ncols_total], BF16))
    t_w = Tok()
    o = 0
    for (src, c0, n) in wsrc_cols:
        if src is None:
            K.op('pool', lambda e, o=o, n=n: e.memset(wb[:, :, o:o + n], 0.0), W=[t_w])
            o += n
            continue
        view = src.rearrange("(kt p) n -> p kt n", p=128)
        done = 0
        while done < n:
            m = min(512, n - done)
            K.dma(wb[:, :, o:o + m], view[:, :, c0 + done:c0 + done + m], W=[t_w], q='pool')
            o += m
            done += m
    return wb, t_w


def la_pass(g, l, kind, p, last):
    nc, K, D = g.nc, g.K, g.D
    gla = kind == 'gla'
    H = 2
    dk = 64
    NV = H * 64
    row0 = (256 if gla else 0) + p * 128
    with ExitStack() as st:
        def sb(name, shape, dt=F32):
            return st.enter_context(nc.sbuf_tensor(_nm(name), list(shape), dt))
        W = D['w_in'][l]
        if gla:
            cols = []
            for nm_ in ('gla_q', 'gla_k'):
                for h2 in range(2):
                    cols.append((W, OFF[nm_] + (2 * p + h2) * 32, 32))
                    cols.append((None, 0, 32))
            cols += [(W, OFF['gla_v'] + 128 * p, 128), (W, OFF['gla_r'] + 128 * p, 128), (W, OFF['gla_a'], 32)]
            ncols = 544
        else:
            cols = [(W, OFF['ret_q'] + 128 * p, 128), (W, OFF['ret_k'] + 128 * p, 128),
                    (W, OFF['ret_v'] + 128 * p, 128), (W, OFF['ret_g'] + 128 * p, 128)]
            ncols = 512
        wb, t_w = load_w_bf16(g, st, cols, ncols, 'w_la')
        LT = sb('LT', [128, 4, T], BF16)
        t_LT = [Tok() for _ in range(NCH)]
        vtok = sb('vtok', [128, NCH, NV], BF16); t_v = [Tok() for _ in range(NCH)]
        sgtok = sb('sgtok', [128, NCH, NV], BF16); t_sg = [Tok() for _ in range(NCH)]
        KVGb = sb('KVGb', [128, NCH, 64]); t_kvb = [Tok() for _ in range(NCH)]
        Sbf = sb('Sbf', [128, NCH, 2, 64], BF16); t_S = [[Tok(), Tok()] for _ in range(NCH)]
        Sf = sb('Sf', [128, 64]); Sb = sb('Sb', [128, 64]); t_Sf = Tok(); t_Sb = Tok()
        Gall = sb('Gall', [128, NCH, 2]); t_G = [Tok() for _ in range(NCH)]
        t_par = Tok()
        if gla:
            wa = sb('wa', [16, 2, 128]); ba = sb('ba', [1, 2, 128])
            K.op('pool', lambda e: e.memset(wa[:], 0.0), W=[t_par])
            K.op('pool', lambda e: e.memset(ba[:], 0.0), W=[t_par])
            for h2 in range(2):
                hh = (2 * p + h2) * 32
                K.dma(wa[:, :, h2 * 64:h2 * 64 + 32], D['gla_wa'][l, :, :, hh:hh + 32].rearrange("d r n -> r d n"), W=[t_par])
                K.dma(ba[:, :, h2 * 64:h2 * 64 + 32], D['gla_ba'][l:l + 1, :, hh:hh + 32], W=[t_par])
            ngb = sb('ngb', [128, NV])
            K.dma(ngb[:], D['gla_ng'][l, p * 128:(p + 1) * 128].partition_broadcast(128), W=[t_par])
        else:
            gng = sb('gng', [128, NV]); gnb = sb('gnb', [128, NV])
            K.dma(gng[:], D['ret_gng'][l, p * 128:(p + 1) * 128].partition_broadcast(128), W=[t_par])
            K.dma(gnb[:], D['ret_gnb'][l, p * 128:(p + 1) * 128].partition_broadcast(128), W=[t_par])
            dtm = sb('dtm', [128, 2, 4]); dfm = sb('dfm', [128, 2])
            K.dma(dtm[:].rearrange("p a b -> p (a b)"), D['ret_decay'][l].partition_broadcast(128), W=[t_par])
            K.dma(dfm[:], D['ret_dec_fm'][l, p, :, :], W=[t_par])
            K.op('act', lambda e: e.activation(out=dtm[:], in_=dtm[:], func=AF.Exp, scale=-1.0), R=[t_par], W=[t_par])
            K.op('act', lambda e: e.activation(out=dtm[:], in_=dtm[:], func=AF.Ln, bias=1.0), R=[t_par], W=[t_par])
            K.op('act', lambda e: e.activation(out=dfm[:], in_=dfm[:], func=AF.Exp, scale=-1.0), R=[t_par], W=[t_par])
            K.op('act', lambda e: e.activation(out=dfm[:], in_=dfm[:], func=AF.Ln, bias=1.0), R=[t_par], W=[t_par])
            argt = sb('argt', [128, 2, 2])
            K.op('dve', lambda e: e.tensor_scalar_mul(out=argt[:, 0, :], in0=dtm[:, 0, 2 * p:2 * p + 2], scalar1=g.poscols[:, 2:3]),
                 R=[t_par, g.t_const], W=[t_par])
            K.op('dve', lambda e: e.tensor_scalar_mul(out=argt[:, 1, :], in0=dtm[:, 1, 2 * p:2 * p + 2], scalar1=g.poscols[:, 3:4]),
                 R=[t_par, g.t_const], W=[t_par])
            EpR = sb('EpR', [128, 2, 2]); EmR = sb('EmR', [128, 2, 2])
            K.op('act', lambda e: e.activation(out=EpR[:], in_=argt[:], func=AF.Exp), R=[t_par], W=[t_par])
            K.op('act', lambda e: e.activation(out=EmR[:], in_=argt[:], func=AF.Exp, scale=-1.0, bias=g.lnc[:, 0:1]), R=[t_par, g.t_const], W=[t_par])
            Gret = sb('Gret', [128, 2])
            K.op('act', lambda e: e.activation(out=Gret[:], in_=dfm[:], func=AF.Exp, scale=-128.0), R=[t_par], W=[t_par])
            cosT = sb('cosT', [128, 32, 32]); sinT = sb('sinT', [128, 32, 32])
            K.dma(cosT[:], D['rot_cos'][:, :, :], W=[t_par])
            K.dma(sinT[:], D['rot_sin'][:, :, :], W=[t_par])

        def Gap(c, d):
            return (Gall[:, c, d:d + 1], t_G[c]) if gla else (Gret[:, d:d + 1], t_par)

        def dbl(name, shape, dt=F32):
            return [sb(name + str(i), shape, dt) for i in range(2)], [Tok(), Tok()]
        uc, t_uc = dbl('ucl', [128, 8, 128], BF16)
        qk, t_qk = dbl('qk', [128, 256])
        rt, t_rt = dbl('rt', [128, 4, 4, 32])
        a_sb, t_a = dbl('a_sb', [128, 32])
        aT, t_aT = dbl('aT', [16, 2, 128])
        sp, t_sp = dbl('sp', [128, 256])
        Ep, t_Ep = dbl('Ep', [128, 256]); Em, t_Em = dbl('Em', [128, 256])
        qkt, t_qkt = dbl('qkt', [128, 4, 128], BF16)
        K.op('pool', lambda e: e.memset(Sf[:], 0.0), W=[t_Sf])
        K.op('pool', lambda e: e.memset(Sb[:], 0.0), W=[t_Sb])

        _dbg = os.environ.get('LA_DEBUG', '')
        _n1 = int(_dbg.split(',')[0]) if _dbg else NCH
        _p2 = int(_dbg.split(',')[1]) if _dbg else 1
        _lvl = int(_dbg.split(',')[2]) if _dbg else 99
        def stA(c):
            b = c % 2
            K.dma(uc[b][:], D['uT_d'][c, :, :, :], R=[g.t_uT[c]], W=[t_uc[b]])
            pa, pat = g.ps()
            for kt in range(8):
                K.op('pe', lambda e, kt=kt, b=b, pa=pa: e.matmul(pa[:, 0:512], lhsT=uc[b][:, kt, :], rhs=wb[:, kt, 0:512],
                                                                  start=(kt == 0), stop=(kt == 7)), R=[t_uc[b], t_w], W=[pat])
            if gla:
                pb, pbt = g.ps()
                for kt in range(8):
                    K.op('pe', lambda e, kt=kt, b=b, pb=pb: e.matmul(pb[:, 0:32], lhsT=uc[b][:, kt, :], rhs=wb[:, kt, 512:544],
                                                                      start=(kt == 0), stop=(kt == 7)), R=[t_uc[b], t_w], W=[pbt])
            K.op('act', lambda e, b=b, pa=pa: e.activation(out=qk[b][:], in_=pa[:, 0:256], func=AF.Identity), R=[pat], W=[t_qk[b]])
            K.op('act', lambda e, c=c, pa=pa: e.activation(out=vtok[:, c, :], in_=pa[:, 256:384], func=AF.Identity), R=[pat], W=[t_v[c]])
            K.op('act', lambda e, c=c, pa=pa: e.activation(out=sgtok[:, c, :], in_=pa[:, 384:512], func=AF.Silu), R=[pat], W=[t_sg[c]])
            if gla:
                K.op('dve', lambda e, b=b, pb=pb: e.tensor_copy(out=a_sb[b][:], in_=pb[:, 0:32]), R=[pbt], W=[t_a[b]])

        def stB(c):
            b = c % 2
            if gla:
                pt, ptk = g.ps()
                for d in range(2):
                    K.op('pe', lambda e, d=d, b=b, pt=pt: e.transpose(out=pt[0:16, d * 128:(d + 1) * 128], in_=a_sb[b][:, d * 16:(d + 1) * 16],
                                                                       identity=g.ident[:]), R=[t_a[b], g.t_const], W=[ptk])
                K.op('dve', lambda e, b=b, pt=pt: e.tensor_copy(out=aT[b][:].rearrange("r d n -> r (d n)"), in_=pt[0:16, 0:256]), R=[ptk], W=[t_aT[b]])
                pg, pgt = g.ps()
                for d in range(2):
                    K.op('pe', lambda e, d=d, b=b, pg=pg: e.matmul(pg[:, d * 128:(d + 1) * 128], lhsT=aT[b][:, d, :], rhs=wa[:, d, :],
                                                                    start=True, stop=False), R=[t_aT[b], t_par], W=[pgt])
                    K.op('pe', lambda e, d=d, pg=pg: e.matmul(pg[:, d * 128:(d + 1) * 128], lhsT=g.ones[0:1, :], rhs=ba[:, d, :],
                                                               start=False, stop=True), R=[t_par, g.t_const], W=[pgt])
                K.op('act', lambda e, b=b, pg=pg: e.activation(out=sp[b][:], in_=pg[:, 0:256], func=AF.Exp, scale=-1.0), R=[pgt], W=[t_sp[b]])
                K.op('act', lambda e, b=b: e.activation(out=sp[b][:], in_=sp[b][:], func=AF.Ln, bias=1.0), R=[t_sp[b]], W=[t_sp[b]])
                pc, pct = g.ps()
                K.op('pe', lambda e, b=b, pc=pc: e.matmul(pc[:, 0:128], lhsT=g.maskL[:], rhs=sp[b][:, 0:128], start=True, stop=True),
                     R=[t_sp[b], g.t_const], W=[pct])
                K.op('pe', lambda e, b=b, pc=pc: e.matmul(pc[:, 128:256], lhsT=g.maskU[:], rhs=sp[b][:, 128:256], start=True, stop=True),
                     R=[t_sp[b], g.t_const], W=[pct])
                K.op('pe', lambda e, b=b, pc=pc: e.matmul(pc[:, 256:257], lhsT=sp[b][:, 0:128], rhs=g.ones[:, 0:1], start=True, stop=True),
                     R=[t_sp[b], g.t_const], W=[pct])
                K.op('pe', lambda e, b=b, pc=pc: e.matmul(pc[:, 257:258], lhsT=sp[b][:, 128:256], rhs=g.ones[:, 0:1], start=True, stop=True),
                     R=[t_sp[b], g.t_const], W=[pct])
                K.op('act', lambda e, b=b, pc=pc: e.activation(out=Ep[b][:], in_=pc[:, 0:256], func=AF.Exp, scale=-1.0 / 16, bias=g.lnc[:, 1:2]),
                     R=[pct, g.t_const], W=[t_Ep[b]])
                K.op('act', lambda e, b=b, pc=pc: e.activation(out=Em[b][:], in_=pc[:, 0:256], func=AF.Exp, scale=1.0 / 16), R=[pct], W=[t_Em[b]])
                K.op('act', lambda e, c=c, pc=pc: e.activation(out=Gall[:, c, :], in_=pc[:, 256:258], func=AF.Exp, scale=-1.0 / 16), R=[pct], W=[t_G[c]])
                K.op('dve', lambda e, b=b: e.tensor_tensor(out=qkt[b][:, 0:2, :], in0=qk[b][:, 0:128].unsqueeze(1).to_broadcast([128, 2, 128]),
                                                            in1=Ep[b][:].rearrange("p (d n) -> p d n", d=2), op=ALU.mult),
                     R=[t_qk[b], t_Ep[b]], W=[t_qkt[b]])
                K.op('dve', lambda e, b=b: e.tensor_tensor(out=qkt[b][:, 2:4, :], in0=qk[b][:, 128:256].unsqueeze(1).to_broadcast([128, 2, 128]),
                                                            in1=Em[b][:].rearrange("p (d n) -> p d n", d=2), op=ALU.mult),
                     R=[t_qk[b], t_Em[b]], W=[t_qkt[b]])
            else:
                if c >= 2:
                    v4 = qk[b][:].rearrange("p (a s f) -> p a s f", a=4, s=2)
                    t1, t2 = v4[:, :, 0, :], v4[:, :, 1, :]
                    cs = cosT[:, c - 2, :].unsqueeze(1).to_broadcast([128, 4, 32])
                    sn = sinT[:, c - 2, :].unsqueeze(1).to_broadcast([128, 4, 32])
                    r = rt[b]
                    K.op('dve', lambda e, r=r, t1=t1, cs=cs: e.tensor_tensor(out=r[:, 0], in0=t1, in1=cs, op=ALU.mult), R=[t_qk[b], t_par], W=[t_rt[b]])
                    K.op('pool', lambda e, r=r, t2=t2, sn=sn: e.tensor_tensor(out=r[:, 1], in0=t2, in1=sn, op=ALU.mult), R=[t_qk[b], t_par], W=[t_rt[b]])
                    K.op('dve', lambda e, r=r, t1=t1, sn=sn: e.tensor_tensor(out=r[:, 2], in0=t1, in1=sn, op=ALU.mult), R=[t_qk[b], t_par], W=[t_rt[b]])
                    K.op('pool', lambda e, r=r, t2=t2, cs=cs: e.tensor_tensor(out=r[:, 3], in0=t2, in1=cs, op=ALU.mult), R=[t_qk[b], t_par], W=[t_rt[b]])
                    K.op('dve', lambda e, r=r, t1=t1: e.tensor_tensor(out=t1, in0=r[:, 0], in1=r[:, 1], op=ALU.subtract), R=[t_rt[b]], W=[t_qk[b]])
                    K.op('dve', lambda e, r=r, t2=t2: e.tensor_tensor(out=t2, in0=r[:, 2], in1=r[:, 3], op=ALU.add), R=[t_rt[b]], W=[t_qk[b]])
                K.op('dve', lambda e, b=b: e.tensor_tensor(
                    out=qkt[b][:, 0:2, :].rearrange("p d (h f) -> p d h f", h=2),
                    in0=qk[b][:, 0:128].rearrange("p (h f) -> p h f", h=2).unsqueeze(1).to_broadcast([128, 2, 2, 64]),
                    in1=EpR[:].unsqueeze(3).to_broadcast([128, 2, 2, 64]), op=ALU.mult), R=[t_qk[b], t_par], W=[t_qkt[b]])
                K.op('dve', lambda e, b=b: e.tensor_tensor(
                    out=qkt[b][:, 2:4, :].rearrange("p d (h f) -> p d h f", h=2),
                    in0=qk[b][:, 128:256].rearrange("p (h f) -> p h f", h=2).unsqueeze(1).to_broadcast([128, 2, 2, 64]),
                    in1=EmR[:].unsqueeze(3).to_broadcast([128, 2, 2, 64]), op=ALU.mult), R=[t_qk[b], t_par], W=[t_qkt[b]])

        def stC(c):
            b = c % 2
            pbh, pbht = g.psb_half()
            for s_ in range(4):
                K.op('pe', lambda e, s_=s_, b=b, pbh=pbh: e.transpose(out=pbh[:, s_ * 128:(s_ + 1) * 128], in_=qkt[b][:, s_, :], identity=g.identb[:]),
                     R=[t_qkt[b], g.t_const], W=[pbht])
            K.op('act', lambda e, c=c, pbh=pbh: e.activation(out=LT[:, :, c * 128:(c + 1) * 128], in_=pbh.rearrange("p (s n) -> p s n", s=4),
                                                              func=AF.Identity), R=[pbht], W=[t_LT[c]])
            pk, pkt = g.ps()
            for d in range(2):
                for h in range(H):
                    K.op('pe', lambda e, d=d, h=h, b=b, c=c, pk=pk: e.matmul(
                        pk[h * dk:(h + 1) * dk, d * 64:(d + 1) * 64], lhsT=qkt[b][:, 2 + d, h * dk:(h + 1) * dk],
                        rhs=vtok[:, c, h * 64:(h + 1) * 64], start=True, stop=True), R=[t_qkt[b], t_v[c]], W=[pkt])
            Gf, tGf = Gap(c, 0)
            Gb, tGb = Gap(c, 1)
            K.op('dve', lambda e, c=c: e.tensor_copy(out=Sbf[:, c, 0, :], in_=Sf[:]), R=[t_Sf], W=[t_S[c][0]])
            K.op('dve', lambda e, pk=pk: e.tensor_tensor(out=Sf[:], in0=Sf[:], in1=pk[:, 0:64], op=ALU.add), R=[pkt, t_Sf], W=[t_Sf])
            K.op('dve', lambda e, Gf=Gf: e.tensor_scalar_mul(out=Sf[:], in0=Sf[:], scalar1=Gf), R=[t_Sf, tGf], W=[t_Sf])
            K.op('dve', lambda e, c=c, pk=pk, Gb=Gb: e.tensor_scalar_mul(out=KVGb[:, c, :], in0=pk[:, 64:128], scalar1=Gb),
                 R=[pkt, tGb], W=[t_kvb[c]])

        stA(0)
        for c in range(NCH):
            if c + 1 < NCH:
                stA(c + 1)
            stB(c)
            stC(c)
        for c in [1, 0] + list(range(NCH - 1, 1, -1)):
            Gb, tGb = Gap(c, 1)
            K.op('dve', lambda e, c=c: e.tensor_copy(out=Sbf[:, c, 1, :], in_=Sb[:]), R=[t_Sb], W=[t_S[c][1]])
            K.op('dve', lambda e, c=c, Gb=Gb: e.scalar_tensor_tensor(out=Sb[:], in0=Sb[:], scalar=Gb, in1=KVGb[:, c, :], op0=ALU.mult, op1=ALU.add),
                 R=[t_Sb, tGb, t_kvb[c]], W=[t_Sb])

        att, t_att = dbl('att', [128, 2, 2, H, 128], BF16)
        osb, t_o = dbl('osb', [128, 2, H, 64])
        sq, t_sq = dbl('sq', [128, 2, H, 64])
        stat, t_st = dbl('stat', [128, 4, 2 * H])
        yb, t_yb = dbl('yb', [128, 2, NV], BF16)
        stg, t_stg = dbl('stg', [128, 2, 128], BF16)
        H2 = 2 * H

        def stP(pi, c0_):
            b = pi % 2
            for ci_ in range(2):
                c = c0_ + ci_
                cs = slice(c * 128, (c + 1) * 128)
                pos = []
                for h in range(H):
                    hr = slice(h * dk, (h + 1) * dk)
                    pz, pzt = g.ps()
                    for d in range(2):
                        K.op('pe', lambda e, d=d, hr=hr, pz=pz, cs=cs: e.matmul(
                            pz[:, d * 128:(d + 1) * 128], lhsT=LT[hr, 2 + d, cs], rhs=LT[hr, d, cs], start=True, stop=True),
                            R=[t_LT[c]], W=[pzt])
                    K.op('dve', lambda e, h=h, b=b, pz=pz, ci_=ci_: e.tensor_tensor(
                        out=att[b][:, ci_, :, h, :], in0=pz[:, 0:256].rearrange("p (d n) -> p d n", d=2),
                        in1=g.mask2[:], op=ALU.mult), R=[pzt, g.t_const], W=[t_att[b]])
                for h in range(H):
                    hr = slice(h * dk, (h + 1) * dk)
                    oc = slice(h * 64, (h + 1) * 64)
                    po, pot = g.ps()
                    K.op('pe', lambda e, h=h, b=b, oc=oc, po=po, c=c, ci_=ci_: e.matmul(po[:, 0:64], lhsT=att[b][:, ci_, 0, h, :], rhs=vtok[:, c, oc], start=True, stop=False),
                         R=[t_att[b], t_v[c]], W=[pot])
                    K.op('pe', lambda e, h=h, b=b, oc=oc, po=po, c=c, ci_=ci_: e.matmul(po[:, 0:64], lhsT=att[b][:, ci_, 1, h, :], rhs=vtok[:, c, oc], start=False, stop=False),
                         R=[t_att[b], t_v[c]], W=[pot])
                    K.op('pe', lambda e, hr=hr, po=po, c=c, cs=cs: e.matmul(po[:, 0:64], lhsT=LT[hr, 0, cs], rhs=Sbf[hr, c, 0, :], start=False, stop=False),
                         R=[t_LT[c], t_S[c][0]], W=[pot])
                    K.op('pe', lambda e, hr=hr, po=po, c=c, cs=cs: e.matmul(po[:, 0:64], lhsT=LT[hr, 1, cs], rhs=Sbf[hr, c, 1, :], start=False, stop=True),
                         R=[t_LT[c], t_S[c][1]], W=[pot])
                    K.op('act', lambda e, po=po, b=b, h=h, ci_=ci_: e.activation(out=osb[b][:, ci_, h, :], in_=po[:, 0:64], func=AF.Identity), R=[pot], W=[t_o[b]])

        def stQ(pi, c0_):
            b = pi % 2
            o3 = osb[b][:].rearrange("p c h f -> p (c h) f")
            s4 = stat[b]
            sq3 = sq[b][:].rearrange("p c h f -> p (c h) f")
            if not gla:
                K.op('dve', lambda e: e.tensor_reduce(out=s4[:, 0, :], in_=o3, axis=AX.X, op=ALU.add), R=[t_o[b]], W=[t_st[b]])
                K.op('dve', lambda e: e.tensor_scalar_mul(out=s4[:, 0, :], in0=s4[:, 0, :], scalar1=1.0 / 64), R=[t_st[b]], W=[t_st[b]])
                K.op('dve', lambda e: e.tensor_tensor(out=o3, in0=o3, in1=s4[:, 0, :].unsqueeze(2).to_broadcast([128, H2, 64]), op=ALU.subtract),
                     R=[t_o[b], t_st[b]], W=[t_o[b]])
            K.op('pool', lambda e: e.tensor_tensor(out=sq3, in0=o3, in1=o3, op=ALU.mult), R=[t_o[b]], W=[t_sq[b]])
            K.op('dve', lambda e: e.tensor_reduce(out=s4[:, 1, :], in_=sq3, axis=AX.X, op=ALU.add), R=[t_sq[b]], W=[t_st[b]])
            K.op('act', lambda e: e.activation(out=s4[:, 2, :], in_=s4[:, 1, :], func=AF.Ln, scale=1.0 / 64, bias=g.epsc[:, 0:1]), R=[t_st[b], g.t_const], W=[t_st[b]])
            K.op('act', lambda e: e.activation(out=s4[:, 3, :], in_=s4[:, 2, :], func=AF.Exp, scale=-0.5), R=[t_st[b]], W=[t_st[b]])
            K.op('dve', lambda e: e.tensor_tensor(out=o3, in0=o3, in1=s4[:, 3, :].unsqueeze(2).to_broadcast([128, H2, 64]), op=ALU.mult),
                 R=[t_o[b], t_st[b]], W=[t_o[b]])
            o2 = osb[b][:].rearrange("p c h f -> p c (h f)")
            if gla:
                K.op('pool', lambda e: e.tensor_tensor(out=o2, in0=o2, in1=ngb[:].unsqueeze(1).to_broadcast([128, 2, NV]), op=ALU.mult), R=[t_o[b], t_par], W=[t_o[b]])
            else:
                K.op('pool', lambda e: e.tensor_tensor(out=o2, in0=o2, in1=gng[:].unsqueeze(1).to_broadcast([128, 2, NV]), op=ALU.mult), R=[t_o[b], t_par], W=[t_o[b]])
                K.op('pool', lambda e: e.tensor_tensor(out=o2, in0=o2, in1=gnb[:].unsqueeze(1).to_broadcast([128, 2, NV]), op=ALU.add), R=[t_o[b], t_par], W=[t_o[b]])
            K.op('dve', lambda e: e.tensor_tensor(out=yb[b][:], in0=o2, in1=sgtok[:, c0_:c0_ + 2, :], op=ALU.mult), R=[t_o[b], t_sg[c0_], t_sg[c0_ + 1]], W=[t_yb[b]])
            pbh, pbht = g.psb_half()
            for ci_ in range(2):
                K.op('pe', lambda e, ci_=ci_, pbh=pbh: e.transpose(out=pbh[:, ci_ * 128:(ci_ + 1) * 128], in_=yb[b][:, ci_, :], identity=g.identb[:]),
                     R=[t_yb[b], g.t_const], W=[pbht])
            K.op('act', lambda e, pbh=pbh: e.activation(out=stg[b][:].rearrange("p c n -> p (c n)"), in_=pbh[:, 0:256], func=AF.Identity), R=[pbht], W=[t_stg[b]])
            K.dma(D['mixT_d'][row0:row0 + NV, c0_ * 128:(c0_ + 2) * 128], stg[b][:].rearrange("p c n -> p (c n)"), R=[t_stg[b]], W=[g.t_mix], q='pool')

        pairs = list(range(2, NCH, 2) if last else range(0, NCH, 2))
        stP(0, pairs[0])
        for pi, c0_ in enumerate(pairs):
            if pi + 1 < len(pairs):
                stP(pi + 1, pairs[pi + 1])
            stQ(pi, c0_)


TWO_PI = 2.0 * math.pi


def phase_s5(g, l):
    nc, K, D = g.nc, g.K, g.D
    NP = (T + 511) // 512
    pieces = [(i * 512, min(512, T - i * 512)) for i in range(NP)]
    with ExitStack() as st:
        def sb(name, shape, dt=F32):
            return st.enter_context(nc.sbuf_tensor(_nm(name), list(shape), dt))
        W = D['w_in'][l]
        wb, t_w = load_w_bf16(g, st, [(W, OFF['s5_u'], 256)], 256, 'w_s5')
        t_par = Tok()
        lam = sb('lam', [128, 16, 2]); dtc = sb('dtc', [128, 16])
        K.dma(lam[:], D['s5_lam_fm'][l, :, :, :], W=[t_par])
        K.dma(dtc[:], D['s5_dt_fm'][l, :, :], W=[t_par])
        CT = sb('CT', [128, 16, 2, 32])
        K.dma(CT[:], D['s5_CT'][l].rearrange("t r k m -> k t r m"), W=[t_par])
        dcol = sb('dcol', [128, 2]); glub = sb('glub', [128, 2])
        K.dma(dcol[:], D['s5_dcol'][l, :, :], W=[t_par])
        K.dma(glub[:], D['s5_glub'][l, :, :], W=[t_par])
        gw32 = sb('gw32', [128, 2, 256]); gwb = sb('gwb', [128, 2, 256], BF16)
        K.dma(gw32[:], D['s5_glu_w'][l].rearrange("(kt p) n -> p kt n", p=128), W=[t_par])
        K.op('dve', lambda e: e.tensor_copy(out=gwb[:], in_=gw32[:]), R=[t_par], W=[t_par])
        P_ = {}
        for nm_ in ('dt', 'a', 'th', 'r', 'u', 'ui', 'fr', 'sn', 'u2', 'ui2', 'fr2', 'cs', 'x', 'y', 'den', 'rden', 't1', 't2', 'cr', 'ci', 'ncr', 'thn'):
            P_[nm_] = sb('s5p_' + nm_, [128, 16], I32 if nm_ in ('ui', 'ui2') else F32)

        def dv(fn, rd=True):
            K.op('dve', fn, R=[t_par, g.t_const], W=[t_par])

        def ac(fn):
            K.op('act', fn, R=[t_par, g.t_const], W=[t_par])
        lre, lim = lam[:, :, 0], lam[:, :, 1]
        ac(lambda e: e.activation(out=P_['dt'][:], in_=dtc[:], func=AF.Exp))
        dv(lambda e: e.tensor_tensor(out=P_['a'][:], in0=lre, in1=P_['dt'][:], op=ALU.mult))
        dv(lambda e: e.tensor_tensor(out=P_['th'][:], in0=lim, in1=P_['dt'][:], op=ALU.mult))
        ac(lambda e: e.activation(out=P_['r'][:], in_=P_['a'][:], func=AF.Exp))
        dv(lambda e: e.tensor_scalar_mul(out=P_['u'][:], in0=P_['th'][:], scalar1=1.0 / TWO_PI))
        dv(lambda e: e.tensor_copy(out=P_['ui'][:], in_=P_['u'][:]))
        dv(lambda e: e.tensor_tensor(out=P_['fr'][:], in0=P_['u'][:], in1=P_['ui'][:], op=ALU.subtract))
        ac(lambda e: e.activation(out=P_['sn'][:], in_=P_['fr'][:], func=AF.Sin, scale=TWO_PI))
        dv(lambda e: e.tensor_scalar_add(out=P_['u2'][:], in0=P_['u'][:], scalar1=0.25))
        dv(lambda e: e.tensor_copy(out=P_['ui2'][:], in_=P_['u2'][:]))
        dv(lambda e: e.tensor_tensor(out=P_['fr2'][:], in0=P_['u2'][:], in1=P_['ui2'][:], op=ALU.subtract))
        ac(lambda e: e.activation(out=P_['cs'][:], in_=P_['fr2'][:], func=AF.Sin, scale=TWO_PI))
        dv(lambda e: e.tensor_tensor(out=P_['x'][:], in0=P_['r'][:], in1=P_['cs'][:], op=ALU.mult))
        dv(lambda e: e.tensor_scalar_add(out=P_['x'][:], in0=P_['x'][:], scalar1=-1.0))
        dv(lambda e: e.tensor_tensor(out=P_['y'][:], in0=P_['r'][:], in1=P_['sn'][:], op=ALU.mult))
        dv(lambda e: e.tensor_tensor(out=P_['t1'][:], in0=lre, in1=lre, op=ALU.mult))
        dv(lambda e: e.tensor_tensor(out=P_['t2'][:], in0=lim, in1=lim, op=ALU.mult))
        dv(lambda e: e.tensor_tensor(out=P_['den'][:], in0=P_['t1'][:], in1=P_['t2'][:], op=ALU.add))
        dv(lambda e: e.reciprocal(out=P_['rden'][:], in_=P_['den'][:]))
        dv(lambda e: e.tensor_tensor(out=P_['t1'][:], in0=P_['x'][:], in1=lre, op=ALU.mult))
        dv(lambda e: e.tensor_tensor(out=P_['t2'][:], in0=P_['y'][:], in1=lim, op=ALU.mult))
        dv(lambda e: e.tensor_tensor(out=P_['cr'][:], in0=P_['t1'][:], in1=P_['t2'][:], op=ALU.add))
        dv(lambda e: e.tensor_tensor(out=P_['cr'][:], in0=P_['cr'][:], in1=P_['rden'][:], op=ALU.mult))
        dv(lambda e: e.tensor_tensor(out=P_['t1'][:], in0=P_['y'][:], in1=lre, op=ALU.mult))
        dv(lambda e: e.tensor_tensor(out=P_['t2'][:], in0=P_['x'][:], in1=lim, op=ALU.mult))
        dv(lambda e: e.tensor_tensor(out=P_['ci'][:], in0=P_['t1'][:], in1=P_['t2'][:], op=ALU.subtract))
        dv(lambda e: e.tensor_tensor(out=P_['ci'][:], in0=P_['ci'][:], in1=P_['rden'][:], op=ALU.mult))
        dv(lambda e: e.tensor_scalar_mul(out=P_['thn'][:], in0=P_['th'][:], scalar1=1.0 / TWO_PI))
        Ce = sb('Ce', [128, 16, 2, 32]); Ceb = sb('Ceb', [128, 16, 2, 32], BF16); tmpC = sb('tmpC', [128, 16, 32])
        crb = P_['cr'][:].unsqueeze(2).to_broadcast([128, 16, 32])
        cib = P_['ci'][:].unsqueeze(2).to_broadcast([128, 16, 32])
        dv(lambda e: e.tensor_tensor(out=Ce[:, :, 0, :], in0=CT[:, :, 0, :], in1=crb, op=ALU.mult))
        dv(lambda e: e.tensor_tensor(out=tmpC[:], in0=CT[:, :, 1, :], in1=cib, op=ALU.mult))
        dv(lambda e: e.tensor_tensor(out=Ce[:, :, 0, :], in0=Ce[:, :, 0, :], in1=tmpC[:], op=ALU.subtract))
        dv(lambda e: e.tensor_tensor(out=Ce[:, :, 1, :], in0=CT[:, :, 0, :], in1=cib, op=ALU.mult))
        dv(lambda e: e.tensor_tensor(out=tmpC[:], in0=CT[:, :, 1, :], in1=crb, op=ALU.mult))
        dv(lambda e: e.tensor_tensor(out=Ce[:, :, 1, :], in0=Ce[:, :, 1, :], in1=tmpC[:], op=ALU.add))
        dv(lambda e: e.tensor_scalar_mul(out=Ce[:, :, 1, :], in0=Ce[:, :, 1, :], scalar1=-1.0))
        dv(lambda e: e.tensor_copy(out=Ceb[:], in_=Ce[:]))

        uTf = sb('uTf', [128, 2, T], BF16); t_uf = Tok()
        st2 = ExitStack()

        def sb2(name, shape, dt=F32):
            return st2.enter_context(nc.sbuf_tensor(_nm(name), list(shape), dt))
        ucs = [sb('ucs%d' % i, [128, 8, 128], BF16) for i in range(2)]; t_ucs = [Tok(), Tok()]
        yT = sb('yT', [128, 2, T], BF16); t_y = Tok()
        uTb = sb2('uTb', [128, 1, T], BF16); t_ub = Tok()
        for c4 in range(0, NCH, 4):
            cc = list(range(c4, min(c4 + 4, NCH)))
            pts = [g.ps(), g.ps()]
            for ci_, c in enumerate(cc):
                b = c % 2
                K.dma(ucs[b][:], D['uT_d'][c, :, :, :], R=[g.t_uT[c]], W=[t_ucs[b]])
                for ft in range(2):
                    pt, ptk = pts[ft]
                    for kt in range(8):
                        K.op('pe', lambda e, kt=kt, b=b, ft=ft, pt=pt, ci_=ci_: e.matmul(
                            pt[:, ci_ * 128:(ci_ + 1) * 128], lhsT=wb[:, kt, ft * 128:(ft + 1) * 128], rhs=ucs[b][:, kt, :],
                            start=(kt == 0), stop=(kt == 7)), R=[t_w, t_ucs[b]], W=[ptk])
            n = len(cc) * 128
            for ft in range(2):
                pt, ptk = pts[ft]
                K.op('act', lambda e, ft=ft, pt=pt, c4=c4, n=n: e.activation(out=uTf[:, ft, c4 * 128:c4 * 128 + n], in_=pt[:, 0:n], func=AF.Identity),
                     R=[ptk], W=[t_uf])

        iot = sb2('iot', [128, 1088])
        K.dma(iot[:], D['iota_t'][0:1088].partition_broadcast(128), W=[t_par])
        offc = sb2('offc', [128, 16, 4, 2])
        for q4 in range(4):
            for which, off in ((0, 0.0), (1, 0.25)):
                dv(lambda e, q4=q4, which=which, off=off: e.tensor_scalar(out=offc[:, :, q4, which], in0=P_['thn'][:], scalar1=float(q4 * 1088), scalar2=off,
                                                                         op0=ALU.mult, op1=ALU.add))
        cosT = sb2('s5cos', [128, T]); sinT = sb2('s5sin', [128, T]); t_tab = Tok()
        uu = sb2('s5uu', [128, 1088]); ui = sb2('s5ui', [128, 1088], I32); t_uu = Tok()
        cre = sb2('s5cre', [128, T]); cim = sb2('s5cim', [128, T]); t_c = Tok()
        hh = sb2('s5h', [128, 2, 2, T], BF16); t_h = [Tok(), Tok()]
        BT32 = sb2('BT32', [128, 2, 128]); BTb = sb2('BTb', [128, 2, 128], BF16); t_B = Tok()
        m1 = [sb2('s5m%d' % i, [128, 512]) for i in range(4)]; t_m = [Tok() for _ in range(4)]
        for j in range(8):
            ft = j // 4
            if j % 4 == 0:
                K.op('pool', lambda e, ft=ft: e.tensor_copy(out=uTb[:, 0, 0:CTXL], in_=uTf[:, ft, CTXL - 1::-1]), R=[t_uf], W=[t_ub])
                K.op('pool', lambda e, ft=ft: e.tensor_copy(out=uTb[:, 0, CTXL:T], in_=uTf[:, ft, T - 1:CTXL - 1:-1]), R=[t_uf], W=[t_ub])
            for d in range(2):
                col = d * 8 + j
                usrc, t_us, uft = (uTf, t_uf, ft) if d == 0 else (uTb, t_ub, 0)
                K.dma(BT32[:], D['s5_BT'][l, col].rearrange("r k m -> k r m"), W=[t_B])
                K.op('pool', lambda e: e.tensor_copy(out=BTb[:], in_=BT32[:]), R=[t_B], W=[t_B])
                for which, tab, off in ((0, sinT, 0.0), (1, cosT, 0.25)):
                    for q4 in range(4):
                        qs = slice(q4 * 1088, (q4 + 1) * 1088)
                        K.op('dve', lambda e, col=col, which=which, q4=q4: e.tensor_scalar(out=uu[:], in0=iot[:], scalar1=P_['thn'][:, col:col + 1],
                                                                                  scalar2=offc[:, col, q4, which:which + 1], op0=ALU.mult, op1=ALU.add), R=[t_par], W=[t_uu])
                        K.op('dve', lambda e: e.tensor_copy(out=ui[:], in_=uu[:]), R=[t_uu], W=[t_uu])
                        K.op('pool', lambda e: e.tensor_tensor(out=uu[:], in0=uu[:], in1=ui[:], op=ALU.subtract), R=[t_uu], W=[t_uu])
                        K.op('act', lambda e, tab=tab, qs=qs: e.activation(out=tab[:, qs], in_=uu[:], func=AF.Sin, scale=TWO_PI), R=[t_uu], W=[t_tab])
                for (p0, n) in pieces:
                    pr, prt = g.ps()
                    pi_, pit = g.ps()
                    K.op('pe', lambda e, pr=pr, p0=p0, n=n, usrc=usrc, uft=uft: e.matmul(pr[:, 0:n], lhsT=BTb[:, 0, :], rhs=usrc[:, uft, p0:p0 + n], start=True, stop=True),
                         R=[t_B, t_us], W=[prt])
                    K.op('pe', lambda e, pi_=pi_, p0=p0, n=n, usrc=usrc, uft=uft: e.matmul(pi_[:, 0:n], lhsT=BTb[:, 1, :], rhs=usrc[:, uft, p0:p0 + n], start=True, stop=True),
                         R=[t_B, t_us], W=[pit])
                    sl = slice(p0, p0 + n)
                    K.op('dve', lambda e, pr=pr, sl=sl, n=n: e.tensor_tensor(out=m1[0][:, 0:n], in0=pr[:, 0:n], in1=cosT[:, sl], op=ALU.mult), R=[prt, t_tab], W=[t_m[0]])
                    K.op('dve', lambda e, pi_=pi_, sl=sl, n=n: e.tensor_tensor(out=m1[1][:, 0:n], in0=pi_[:, 0:n], in1=sinT[:, sl], op=ALU.mult), R=[pit, t_tab], W=[t_m[1]])
                    K.op('dve', lambda e, pi_=pi_, sl=sl, n=n: e.tensor_tensor(out=m1[2][:, 0:n], in0=pi_[:, 0:n], in1=cosT[:, sl], op=ALU.mult), R=[pit, t_tab], W=[t_m[2]])
                    K.op('dve', lambda e, pr=pr, sl=sl, n=n: e.tensor_tensor(out=m1[3][:, 0:n], in0=pr[:, 0:n], in1=sinT[:, sl], op=ALU.mult), R=[prt, t_tab], W=[t_m[3]])
                    K.op('pool', lambda e, sl=sl, n=n: e.tensor_tensor(out=cre[:, sl], in0=m1[0][:, 0:n], in1=m1[1][:, 0:n], op=ALU.add), R=[t_m[0], t_m[1]], W=[t_c])
                    K.op('pool', lambda e, sl=sl, n=n: e.tensor_tensor(out=cim[:, sl], in0=m1[2][:, 0:n], in1=m1[3][:, 0:n], op=ALU.subtract), R=[t_m[2], t_m[3]], W=[t_c])
                rb = P_['r'][:, col:col + 1].to_broadcast([128, T])
                K.op('dve', lambda e, rb=rb: e.tensor_tensor_scan(out=cre[:], data0=rb, data1=cre[:], initial=0.0, op0=ALU.mult, op1=ALU.add), R=[t_c, t_par], W=[t_c])
                K.op('dve', lambda e, rb=rb: e.tensor_tensor_scan(out=cim[:], data0=rb, data1=cim[:], initial=0.0, op0=ALU.mult, op1=ALU.add), R=[t_c, t_par], W=[t_c])
                if d == 0:
                    segs = [(slice(0, T), slice(0, T))]
                else:
                    segs = [(slice(0, CTXL), slice(CTXL - 1, None, -1)), (slice(CTXL, T), slice(T - 1, CTXL - 1, -1))]
                for (so, si) in segs:
                    n = so.stop - so.start
                    for q0 in range(0, n, 1024):
                        qn = min(1024, n - q0)
                        o_sl = slice(so.start + q0, so.start + q0 + qn)
                        if d == 0:
                            i_sl = o_sl
                        else:
                            hi = si.start - q0
                            lo = hi - qn
                            i_sl = slice(hi, lo if lo >= 0 else None, -1)
                        mA, mB, mC, mD = m1[0], m1[1], m1[2], m1[3]
                        for h0 in range(0, qn, 512):
                            hn = min(512, qn - h0)
                            oo = slice(o_sl.start + h0, o_sl.start + h0 + hn)
                            if d == 0:
                                ii = oo
                            else:
                                a_ = i_sl.start - h0
                                b_ = a_ - hn
                                ii = slice(a_, b_ if b_ >= 0 else None, -1)
                            K.op('dve', lambda e, ii=ii, hn=hn: e.tensor_tensor(out=mA[:, 0:hn], in0=cre[:, ii], in1=cosT[:, ii], op=ALU.mult), R=[t_c, t_tab], W=[t_m[0]])
                            K.op('pool', lambda e, ii=ii, hn=hn: e.tensor_tensor(out=mB[:, 0:hn], in0=cim[:, ii], in1=sinT[:, ii], op=ALU.mult), R=[t_c, t_tab], W=[t_m[1]])
                            K.op('dve', lambda e, ii=ii, hn=hn: e.tensor_tensor(out=mC[:, 0:hn], in0=cre[:, ii], in1=sinT[:, ii], op=ALU.mult), R=[t_c, t_tab], W=[t_m[2]])
                            K.op('pool', lambda e, ii=ii, hn=hn: e.tensor_tensor(out=mD[:, 0:hn], in0=cim[:, ii], in1=cosT[:, ii], op=ALU.mult), R=[t_c, t_tab], W=[t_m[3]])
                            K.op('dve', lambda e, oo=oo, hn=hn, d=d: e.tensor_tensor(out=hh[:, d, 0, oo], in0=mA[:, 0:hn], in1=mB[:, 0:hn], op=ALU.subtract), R=[t_m[0], t_m[1]], W=[t_h[d]])
                            K.op('dve', lambda e, oo=oo, hn=hn, d=d: e.tensor_tensor(out=hh[:, d, 1, oo], in0=mC[:, 0:hn], in1=mD[:, 0:hn], op=ALU.add), R=[t_m[2], t_m[3]], W=[t_h[d]])
            for (p0, n) in pieces:
                py, pyt = g.ps()
                k = 0
                for d in range(2):
                    for r_ in range(2):
                        K.op('pe', lambda e, d=d, r_=r_, py=py, p0=p0, n=n, k=k, j=j: e.matmul(
                            py[0:32, 0:n], lhsT=Ceb[:, d * 8 + j, r_, :], rhs=hh[:, d, r_, p0:p0 + n], start=(k == 0), stop=(k == 3)),
                            R=[t_par, t_h[d]], W=[pyt])
                        k += 1
                rows = slice(32 * (j % 4), 32 * (j % 4) + 32)
                K.op('act', lambda e, py=py, p0=p0, n=n, rows=rows, ft=ft: e.activation(out=yT[rows, ft, p0:p0 + n], in_=py[0:32, 0:n], func=AF.Identity),
                     R=[pyt], W=[t_y])
        K.barrier()
        st2.close()
        gT = sb('gT', [128, 2, T], BF16); t_g = Tok()
        ytmp = [sb('ytmp%d' % i, [128, 512]) for i in range(2)]; t_yt = [Tok(), Tok()]
        k = 0
        for ft in range(2):
            for (p0, n) in pieces:
                b = k % 2
                k += 1
                K.op('dve', lambda e, b=b, ft=ft, p0=p0, n=n: e.scalar_tensor_tensor(
                    out=ytmp[b][:, 0:n], in0=uTf[:, ft, p0:p0 + n], scalar=dcol[:, ft:ft + 1], in1=yT[:, ft, p0:p0 + n], op0=ALU.mult, op1=ALU.add),
                    R=[t_uf, t_y, t_par], W=[t_yt[b]])
                K.op('act', lambda e, b=b, ft=ft, p0=p0, n=n: e.activation(out=gT[:, ft, p0:p0 + n], in_=ytmp[b][:, 0:n], func=AF.Gelu), R=[t_yt[b]], W=[t_g])
        ob = [sb('s5ob%d' % i, [128, 512], BF16) for i in range(2)]; t_ob = [Tok(), Tok()]
        sg = [sb('s5sg%d' % i, [128, 512]) for i in range(2)]; t_sgm = [Tok(), Tok()]
        k = 0
        for ft in range(2):
            for (p0, n) in pieces:
                b = k % 2
                k += 1
                pz, pzt = g.ps()
                for kt in range(2):
                    K.op('pe', lambda e, kt=kt, ft=ft, pz=pz, p0=p0, n=n: e.matmul(pz[:, 0:n], lhsT=gwb[:, kt, ft * 128:(ft + 1) * 128], rhs=gT[:, kt, p0:p0 + n],
                                                                                   start=(kt == 0), stop=(kt == 1)), R=[t_par, t_g], W=[pzt])
                K.op('act', lambda e, b=b, pz=pz, n=n, ft=ft: e.activation(out=sg[b][:, 0:n], in_=pz[:, 0:n], func=AF.Sigmoid, bias=glub[:, ft:ft + 1]), R=[pzt, t_par], W=[t_sgm[b]])
                K.op('dve', lambda e, b=b, ft=ft, p0=p0, n=n: e.tensor_tensor(out=ob[b][:, 0:n], in0=gT[:, ft, p0:p0 + n], in1=sg[b][:, 0:n], op=ALU.mult), R=[t_g, t_sgm[b]], W=[t_ob[b]])
                K.dma(D['mixT_d'][512 + ft * 128:512 + (ft + 1) * 128, p0:p0 + n], ob[b][:, 0:n], R=[t_ob[b]], W=[g.t_mix], q='pool')


def phase_hyena(g, l, n, c0, sfx):
    nc, K, D = g.nc, g.K, g.D
    NT = n // 128
    NKT = NT + 1
    with ExitStack() as st:
        def sb(name, shape, dt=F32):
            return st.enter_context(nc.sbuf_tensor(_nm(name), list(shape), dt))
        W = D['w_in'][l]
        t_par = Tok()
        cw = sb('hy_cw', [128, 6, 3]); cb = sb('hy_cb', [128, 6])
        K.dma(cw[:], D['hy_cw'][l, :, :, :], W=[t_par])
        K.dma(cb[:], D['hy_cb'][l, :, :], W=[t_par])
        hbias = sb('hy_bias', [128, 2])
        K.dma(hbias[:], D['hy_biasc'][l, :, :], W=[t_par])
        zT = sb('hy_zT', [128, 2, n], BF16); t_z = Tok()
        x0T = sb('hy_x0T', [128, 2, n], BF16); t_x0 = Tok()
        data = sb('hy_data', [128, NT, 768], BF16); t_data = [Tok() for _ in range(NT)]
        rnorm = sb('hy_rnorm', [128, 2]); t_rn = Tok()
        with ExitStack() as st2:
            def sb2(name, shape, dt=F32):
                return st2.enter_context(nc.sbuf_tensor(_nm(name), list(shape), dt))
            wb, t_w = load_w_bf16(g, st2, [(W, OFF['hy_p'], 768)], 768, 'w_hy')
            ucs = [sb2('hucs%d' % i, [128, 8, 128], BF16) for i in range(2)]; t_ucs = [Tok(), Tok()]
            pp = [sb2('hy_pp%d' % i, [128, n + 2]) for i in range(2)]; t_pp = [Tok(), Tok()]
            sv = [sb2('hy_sv%d' % i, [128, n]) for i in range(2)]; t_sv = [Tok(), Tok()]
            for i in range(2):
                K.op('pool', lambda e, i=i: e.memset(pp[i][:, 0:1], 0.0), W=[t_pp[i]])
                K.op('pool', lambda e, i=i: e.memset(pp[i][:, n + 1:n + 2], 0.0), W=[t_pp[i]])

            _sub = int(os.environ.get('HY_SUB', '9'))

            def proj_conv(ft, slot):
                for c4 in range(0, NT, 4):
                    cc = list(range(c4, min(c4 + 4, NT)))
                    pt, ptk = g.ps()
                    for ci_, c in enumerate(cc):
                        b = c % 2
                        K.dma(ucs[b][:], D['uT_d'][c0 + c, :, :, :], R=[g.t_uT[c0 + c]], W=[t_ucs[b]])
                        for kt in range(8):
                            K.op('pe', lambda e, kt=kt, b=b, pt=pt, ci_=ci_: e.matmul(
                                pt[:, ci_ * 128:(ci_ + 1) * 128], lhsT=wb[:, kt, ft * 128:(ft + 1) * 128], rhs=ucs[b][:, kt, :],
                                start=(kt == 0), stop=(kt == 7)), R=[t_w, t_ucs[b]], W=[ptk])
                    nn = len(cc) * 128
                    K.op('act', lambda e, pt=pt, c4=c4, nn=nn: e.activation(out=pp[slot][:, 1 + c4 * 128:1 + c4 * 128 + nn], in_=pt[:, 0:nn], func=AF.Identity),
                         R=[ptk], W=[t_pp[slot]])
                if _sub < 2:
                    return
                for q0 in range(0, n, 2048):
                    qn = min(2048, n - q0)
                    K.op('dve', lambda e, q0=q0, qn=qn: e.tensor_scalar(out=sv[slot][:, q0:q0 + qn], in0=pp[slot][:, q0:q0 + qn], scalar1=cw[:, ft, 0:1], scalar2=cb[:, ft:ft + 1],
                                                                         op0=ALU.mult, op1=ALU.add), R=[t_pp[slot], t_par], W=[t_sv[slot]])
                    K.op('dve', lambda e, q0=q0, qn=qn: e.scalar_tensor_tensor(out=sv[slot][:, q0:q0 + qn], in0=pp[slot][:, q0 + 1:q0 + 1 + qn], scalar=cw[:, ft, 1:2],
                                                                                 in1=sv[slot][:, q0:q0 + qn], op0=ALU.mult, op1=ALU.add), R=[t_pp[slot], t_par, t_sv[slot]], W=[t_sv[slot]])
                    K.op('dve', lambda e, q0=q0, qn=qn: e.scalar_tensor_tensor(out=sv[slot][:, q0:q0 + qn], in0=pp[slot][:, q0 + 2:q0 + 2 + qn], scalar=cw[:, ft, 2:3],
                                                                                 in1=sv[slot][:, q0:q0 + qn], op0=ALU.mult, op1=ALU.add), R=[t_pp[slot], t_par, t_sv[slot]], W=[t_sv[slot]])
            for ci in range(2):
                proj_conv(2 + ci, 0)
                if _sub < 3:
                    break
                proj_conv(4 + ci, 1)
                K.op('pool', lambda e, ci=ci: e.tensor_tensor(out=zT[:, ci, :], in0=sv[0][:], in1=sv[1][:], op=ALU.mult), R=[t_sv[0], t_sv[1]], W=[t_z])
                if _sub < 4:
                    break
                proj_conv(ci, 0)
                K.op('pool', lambda e, ci=ci: e.tensor_copy(out=x0T[:, ci, :], in_=sv[0][:]), R=[t_sv[0]], W=[t_x0])
            for i in (range(NT) if _sub >= 6 else [int(x) for x in os.environ.get("HY_IT", "0,1").split(",") if int(x) < NT]) if _sub >= 5 else []:
                pbh, pbht = g.psb_half()
                for ci in range(2):
                    K.op('pe', lambda e, ci=ci, i=i, pbh=pbh: e.transpose(out=pbh[:, ci * 128:(ci + 1) * 128], in_=zT[:, ci, i * 128:(i + 1) * 128], identity=g.identb[:]),
                         R=[t_z, g.t_const], W=[pbht])
                K.op('act', lambda e, i=i, pbh=pbh: e.activation(out=data[:, i, 0:256], in_=pbh[:, 0:256], func=AF.Identity), R=[pbht], W=[t_data[i]])
            K.barrier()
        _hs = int(os.environ.get('HY_STAGE', '9'))
        if _hs < 2:
            return
        with ExitStack() as st2:
            def sb2(name, shape, dt=F32):
                return st2.enter_context(nc.sbuf_tensor(_nm(name), list(shape), dt))
            zemb = sb2('hy_zemb', [33, n]); fw1 = sb2('hy_fw1', [33, 64]); fw2 = sb2('hy_fw2', [64, 64]); fw3 = sb2('hy_fw3', [64, 512])
            fcol = sb2('hy_fcol', [64, 5]); fs = sb2('hy_fs', [64, 2])
            K.dma(zemb[:], D['hy_zemb' + sfx][:, :], W=[t_par])
            K.dma(fw1[:], D['hy_fw1'][l, :, :], W=[t_par])
            K.dma(fw2[:], D['hy_fw2'][l, :, :], W=[t_par])
            K.dma(fw3[:], D['hy_fw3'][l, :, :], W=[t_par])
            K.dma(fcol[:], D['hy_fcol'][l, :, :], W=[t_par])
            K.op('dve', lambda e: e.tensor_scalar_mul(out=fs[:, 0:1], in0=fcol[:, 2:3], scalar1=1.0 / TWO_PI), R=[t_par], W=[t_par])
            absd = sb2('hy_absd', [128, 512])
            K.dma(absd[:], D['hy_decay'][l].rearrange("d c -> (d c)").partition_broadcast(128), W=[t_par])
            K.op('dve', lambda e: e.scalar_tensor_tensor(out=absd[:], in0=absd[:], scalar=-1.0, in1=absd[:], op0=ALU.mult, op1=ALU.max), R=[t_par], W=[t_par])
            tn = sb2('hy_tn', [128, NT])
            K.dma(tn[:], D['hy_tn' + sfx][:, :], W=[t_par])
            K.op('dve', lambda e: e.tensor_scalar_mul(out=tn[:], in0=tn[:], scalar1=-1.0), R=[t_par], W=[t_par])
            h1 = sb2('hy_h1', [64, n]); h2 = sb2('hy_h2', [64, n]); t_h1 = Tok(); t_h2 = Tok()
            uu = sb2('hy_uu', [64, 512]); ui = sb2('hy_ui', [64, 512], I32); t_uu = Tok()
            for (src, t_src, wgt, bcol, dst, t_dst) in ((zemb, t_par, fw1, 0, h1, t_h1), (h1, t_h1, fw2, 1, h2, t_h2)):
                for q0 in range(0, n, 512):
                    qn = min(512, n - q0)
                    pt, ptk = g.ps()
                    K.op('pe', lambda e, pt=pt, q0=q0, qn=qn, src=src, wgt=wgt: e.matmul(pt[0:64, 0:qn], lhsT=wgt[:], rhs=src[:, q0:q0 + qn], start=True, stop=True),
                         R=[t_src, t_par], W=[ptk])
                    K.op('dve', lambda e, pt=pt, qn=qn, bcol=bcol: e.tensor_scalar(out=uu[:, 0:qn], in0=pt[0:64, 0:qn], scalar1=fcol[:, bcol:bcol + 1], scalar2=fs[:, 0:1],
                                                                                   op0=ALU.add, op1=ALU.mult), R=[ptk, t_par], W=[t_uu])
                    K.op('dve', lambda e, qn=qn: e.tensor_copy(out=ui[:, 0:qn], in_=uu[:, 0:qn]), R=[t_uu], W=[t_uu])
                    K.op('dve', lambda e, qn=qn: e.tensor_tensor(out=uu[:, 0:qn], in0=uu[:, 0:qn], in1=ui[:, 0:qn], op=ALU.subtract), R=[t_uu], W=[t_uu])
                    K.op('act', lambda e, q0=q0, qn=qn, dst=dst: e.activation(out=dst[:, q0:q0 + qn], in_=uu[:, 0:qn], func=AF.Sin, scale=TWO_PI), R=[t_uu], W=[t_dst])
            acc = sb2('hy_acc', [128, 512]); t_acc = Tok()
            K.op('pool', lambda e: e.memset(acc[:], 0.0), W=[t_acc])
            win = [sb2('hy_win%d' % i, [128, 512]) for i in range(2)]; t_win = [Tok(), Tok()]
            fl = [sb2('hy_fl%d' % i, [128, 512]) for i in range(2)]; t_fl = [Tok(), Tok()]
            fa = [sb2('hy_fa%d' % i, [128, 512]) for i in range(2)]; t_fa = [Tok(), Tok()]
            for i in range(NT):
                b = i % 2
                pt, ptk = g.ps()
                K.op('pe', lambda e, pt=pt, i=i: e.matmul(pt[:, 0:512], lhsT=h2[:, i * 128:(i + 1) * 128], rhs=fw3[:], start=True, stop=True), R=[t_h2, t_par], W=[ptk])
                K.op('act', lambda e, b=b, i=i: e.activation(out=win[b][:], in_=absd[:], func=AF.Exp, scale=tn[:, i:i + 1]), R=[t_par], W=[t_win[b]])
                K.op('dve', lambda e, b=b, pt=pt: e.tensor_tensor(out=fl[b][:], in0=pt[:, 0:512], in1=win[b][:], op=ALU.mult), R=[ptk, t_win[b]], W=[t_fl[b]])
                if i == 0:
                    K.op('dve', lambda e, b=b: e.memset(fl[b][0:1, 256:512], 0.0), W=[t_fl[b]])
                K.op('dve', lambda e, b=b: e.scalar_tensor_tensor(out=fa[b][:], in0=fl[b][:], scalar=-1.0, in1=fl[b][:], op0=ALU.mult, op1=ALU.max), R=[t_fl[b]], W=[t_fa[b]])
                K.op('pool', lambda e, b=b: e.tensor_tensor(out=acc[:], in0=acc[:], in1=fa[b][:], op=ALU.add), R=[t_fa[b], t_acc], W=[t_acc])
                K.op('pool', lambda e, b=b, i=i: e.tensor_copy(out=data[:, i, 256:768], in_=fl[b][:]), R=[t_fl[b]], W=[t_data[i]])
            pt, ptk = g.ps()
            for ci in range(2):
                K.op('pe', lambda e, ci=ci, pt=pt: e.matmul(pt[:, ci:ci + 1], lhsT=acc[:, ci * 128:(ci + 1) * 128], rhs=g.ones[:, 0:1], start=True, stop=False),
                     R=[t_acc, g.t_const], W=[ptk])
                K.op('pe', lambda e, ci=ci, pt=pt: e.matmul(pt[:, ci:ci + 1], lhsT=acc[:, 256 + ci * 128:256 + (ci + 1) * 128], rhs=g.ones[:, 0:1], start=False, stop=True),
                     R=[t_acc, g.t_const], W=[ptk])
            K.op('dve', lambda e, pt=pt: e.reciprocal(out=rnorm[:], in_=pt[:, 0:2]), R=[ptk], W=[t_rn])
            K.barrier()
        if _hs < 3:
            return
        Yw = sb('hy_Yw', [128, NKT, 2, 256], BF16); t_Y = [Tok() for _ in range(NKT)]
        wk = sb('hy_wk', [128, NKT])
        K.dma(wk[:], D['hy_wk' + sfx][:, :], W=[t_par])
        tabc = [sb('hy_tc%d' % i, [128, NKT, 128], BF16) for i in range(2)]
        tabs = [sb('hy_ts%d' % i, [128, NKT, 128], BF16) for i in range(2)]
        t_tab = [Tok(), Tok()]
        f1c = sb('hy_f1c', [128, 256]); f1s = sb('hy_f1s', [128, 256]); t_f1 = Tok()
        rre = sb('hy_rre', [128, 256]); rim = sb('hy_rim', [128, 256]); t_r = Tok()
        tt = [sb('hy_tt%d' % i, [128, 256]) for i in range(4)]; t_tt = [Tok() for _ in range(4)]
        yy = [sb('hy_yy%d' % i, [128, 256]) for i in range(2)]; t_yy = [Tok(), Tok()]
        for j in range(NKT):
            b = j % 2
            K.dma(tabc[b][:], D['dftc' + sfx][j, :, :, :], W=[t_tab[b]])
            K.dma(tabs[b][:], D['dfts' + sfx][j, :, :, :], W=[t_tab[b]])
            pA, pAt = g.ps(); pB, pBt = g.ps(); pC, pCt = g.ps(); pD, pDt = g.ps()
            for i in range(NT):
                fl_ = dict(start=(i == 0), stop=(i == NT - 1))
                K.op('pe', lambda e, i=i, b=b, pA=pA, fl_=fl_: e.matmul(pA[:, 0:512], lhsT=tabc[b][:, i, :], rhs=data[:, i, 0:512], **fl_), R=[t_tab[b], t_data[i]], W=[pAt])
                K.op('pe', lambda e, i=i, b=b, pB=pB, fl_=fl_: e.matmul(pB[:, 0:256], lhsT=tabc[b][:, i, :], rhs=data[:, i, 512:768], **fl_), R=[t_tab[b], t_data[i]], W=[pBt])
                K.op('pe', lambda e, i=i, b=b, pC=pC, fl_=fl_: e.matmul(pC[:, 0:512], lhsT=tabs[b][:, i, :], rhs=data[:, i, 0:512], **fl_), R=[t_tab[b], t_data[i]], W=[pCt])
                K.op('pe', lambda e, i=i, b=b, pD=pD, fl_=fl_: e.matmul(pD[:, 0:256], lhsT=tabs[b][:, i, :], rhs=data[:, i, 512:768], **fl_), R=[t_tab[b], t_data[i]], W=[pDt])
            K.op('act', lambda e, pB=pB: e.activation(out=f1c[:], in_=pB[:, 0:256], func=AF.Identity), R=[pBt], W=[t_f1])
            K.op('act', lambda e, pD=pD: e.activation(out=f1s[:], in_=pD[:, 0:256], func=AF.Identity), R=[pDt], W=[t_f1])
            K.op('dve', lambda e, pA=pA: e.tensor_tensor(out=rre[:], in0=pA[:, 256:512], in1=f1c[:], op=ALU.add), R=[pAt, t_f1], W=[t_r])
            K.op('dve', lambda e, pC=pC: e.tensor_tensor(out=rim[:], in0=f1s[:], in1=pC[:, 256:512], op=ALU.subtract), R=[pCt, t_f1], W=[t_r])
            K.op('dve', lambda e, pA=pA: e.tensor_tensor(out=tt[0][:], in0=pA[:, 0:256], in1=rre[:], op=ALU.mult), R=[pAt, t_r], W=[t_tt[0]])
            K.op('dve', lambda e, pC=pC: e.tensor_tensor(out=tt[1][:], in0=pC[:, 0:256], in1=rim[:], op=ALU.mult), R=[pCt, t_r], W=[t_tt[1]])
            K.op('dve', lambda e, pA=pA: e.tensor_tensor(out=tt[2][:], in0=pA[:, 0:256], in1=rim[:], op=ALU.mult), R=[pAt, t_r], W=[t_tt[2]])
            K.op('dve', lambda e, pC=pC: e.tensor_tensor(out=tt[3][:], in0=pC[:, 0:256], in1=rre[:], op=ALU.mult), R=[pCt, t_r], W=[t_tt[3]])
            K.op('pool', lambda e: e.tensor_tensor(out=yy[0][:], in0=tt[0][:], in1=tt[1][:], op=ALU.add), R=[t_tt[0], t_tt[1]], W=[t_yy[0]])
            K.op('pool', lambda e: e.tensor_tensor(out=yy[1][:], in0=tt[3][:], in1=tt[2][:], op=ALU.subtract), R=[t_tt[2], t_tt[3]], W=[t_yy[1]])
            K.op('act', lambda e, j=j: e.activation(out=Yw[:, j, 0, :], in_=yy[0][:], func=AF.Identity, scale=wk[:, j:j + 1]), R=[t_yy[0], t_par], W=[t_Y[j]])
            K.op('act', lambda e, j=j: e.activation(out=Yw[:, j, 1, :], in_=yy[1][:], func=AF.Identity, scale=wk[:, j:j + 1]), R=[t_yy[1], t_par], W=[t_Y[j]])
        if _hs < 4:
            return
        ysb = [sb('hy_ysb%d' % i, [128, 256]) for i in range(2)]; t_ys = [Tok(), Tok()]
        tmp = [sb('hy_tmp%d' % i, [128, 256]) for i in range(2)]; t_tmp = [Tok(), Tok()]
        ob = [sb('hy_ob%d' % i, [128, 2, 128], BF16) for i in range(2)]; t_ob = [Tok(), Tok()]
        for i in range(NT):
            b = i % 2
            K.dma(tabc[b][:], D['dftc' + sfx][i, :, :, :], W=[t_tab[b]])
            K.dma(tabs[b][:], D['dfts' + sfx][i, :, :, :], W=[t_tab[b]])
            py, pyt = g.ps()
            for kt in range(NKT):
                K.op('pe', lambda e, kt=kt, b=b, py=py: e.matmul(py[:, 0:256], lhsT=tabc[b][:, kt, :], rhs=Yw[:, kt, 0, :], start=(kt == 0), stop=False), R=[t_tab[b], t_Y[kt]], W=[pyt])
                K.op('pe', lambda e, kt=kt, b=b, py=py: e.matmul(py[:, 0:256], lhsT=tabs[b][:, kt, :], rhs=Yw[:, kt, 1, :], start=False, stop=(kt == NKT - 1)), R=[t_tab[b], t_Y[kt]], W=[pyt])
            K.op('act', lambda e, b=b, py=py: e.activation(out=ysb[b][:], in_=py[:, 0:256], func=AF.Identity), R=[pyt], W=[t_ys[b]])
            pt, ptk = g.ps()
            for ci in range(2):
                K.op('pe', lambda e, ci=ci, b=b, pt=pt: e.transpose(out=pt[:, ci * 128:(ci + 1) * 128], in_=ysb[b][:, ci * 128:(ci + 1) * 128], identity=g.ident[:]),
                     R=[t_ys[b], g.t_const], W=[ptk])
            cs = slice(i * 128, (i + 1) * 128)
            for ci in range(2):
                K.op('act', lambda e, ci=ci, b=b, pt=pt: e.activation(out=tmp[b][:, ci * 128:(ci + 1) * 128], in_=pt[:, ci * 128:(ci + 1) * 128], func=AF.Identity, scale=rnorm[:, ci:ci + 1]),
                     R=[ptk, t_rn], W=[t_tmp[b]])
                K.op('dve', lambda e, ci=ci, b=b, cs=cs: e.scalar_tensor_tensor(out=tmp[b][:, ci * 128:(ci + 1) * 128], in0=zT[:, ci, cs], scalar=hbias[:, ci:ci + 1],
                                                                             in1=tmp[b][:, ci * 128:(ci + 1) * 128], op0=ALU.mult, op1=ALU.add), R=[t_z, t_par, t_tmp[b]], W=[t_tmp[b]])
                K.op('dve', lambda e, ci=ci, b=b, cs=cs: e.tensor_tensor(out=ob[b][:, ci, :], in0=tmp[b][:, ci * 128:(ci + 1) * 128], in1=x0T[:, ci, cs], op=ALU.mult),
                     R=[t_tmp[b], t_x0], W=[t_ob[b]])
            col0 = (c0 + i) * 128
            K.dma(D['mixT_d'][768:1024, col0:col0 + 128].rearrange("(m p) n -> p m n", p=128), ob[b][:], R=[t_ob[b]], W=[g.t_mix], q='pool')


def load_ln_tables(g, st, l, which):
    nc, K = g.nc, g.K
    t = st.enter_context(nc.sbuf_tensor(_nm('lntab'), [128, 2, D_MODEL], F32))
    tk = Tok()
    K.dma(t[:, 0, :], g.D['ln_g'][l, which, :].partition_broadcast(128), W=[tk])
    K.dma(t[:, 1, :], g.D['ln_b'][l, which, :].partition_broadcast(128), W=[tk])
    return t, tk


def deepnorm_chunk(g, l, c, b, which, y_ap, t_y, xres, t_xres, T_, lntab, t_ln, dst_ap, t_dst_tok):
    K = g.K
    cls = 1 if c < 2 else 0
    tmp, t_tmp = T_['tmp'][b], T_['t_tmp'][b]
    for h in range(2):
        K.op('dve', lambda e, h=h: e.tensor_tensor(out=tmp[:, h * 512:(h + 1) * 512], in0=y_ap[h], in1=g.gbc[:, 0, cls, h * 512:(h + 1) * 512], op=ALU.mult),
             R=[t_y[h], g.t_gbc], W=[t_tmp])
    K.op('dve', lambda e: e.scalar_tensor_tensor(out=tmp[:], in0=xres[:], scalar=float(ALPHA), in1=tmp[:], op0=ALU.mult, op1=ALU.add), R=[t_xres, t_tmp], W=[t_tmp])
    mv, rstd, tk = ln_stats(g, T_['sts'][b], tmp, t_tmp)
    K.op('dve', lambda e: e.tensor_scalar(out=tmp[:], in0=tmp[:], scalar1=mv[:, 0:1], scalar2=rstd[:, 0:1], op0=ALU.subtract, op1=ALU.mult), R=[t_tmp, tk], W=[t_tmp])
    K.op('pool', lambda e: e.tensor_tensor(out=tmp[:], in0=tmp[:], in1=lntab[:, 0, :], op=ALU.mult), R=[t_tmp, t_ln], W=[t_tmp])
    K.op('pool', lambda e: e.tensor_tensor(out=dst_ap, in0=tmp[:], in1=lntab[:, 1, :], op=ALU.add), R=[t_tmp, t_ln], W=[t_dst_tok])


def phase_wout(g, l, x_src, t_xsrc, chunks, x1_d, t_x1):
    nc, K, D = g.nc, g.K, g.D
    with ExitStack() as st:
        def sb(name, shape, dt=F32):
            return st.enter_context(nc.sbuf_tensor(_nm(name), list(shape), dt))
        wb, t_w = load_w_bf16(g, st, [(D['w_out'][l], 0, D_MODEL)], D_MODEL, 'w_out')
        lntab, t_ln = load_ln_tables(g, st, l, 0)
        g.gbc = sb('gbcw', [128, 1, 2, D_MODEL])
        K.dma(g.gbc[:], D['gbc_d'][:, 0:1, :, :], R=[g.t_gbcd], W=[g.t_gbc])
        mx = [sb('mx%d' % i, [128, 8, 128], BF16) for i in range(2)]; t_mx = [Tok(), Tok()]
        xr = [sb('xr%d' % i, [128, D_MODEL]) for i in range(2)]; t_xr = [Tok(), Tok()]
        xo = [sb('xo%d' % i, [128, D_MODEL]) for i in range(2)]; t_xo = [Tok(), Tok()]
        T_ = {'tmp': [sb('dn_tmp%d' % i, [128, D_MODEL]) for i in range(2)], 't_tmp': [Tok(), Tok()],
              'sts': [(sb('dstt%d' % i, [128, 2, 6]), sb('dmv%d' % i, [128, 2]), sb('dlnv%d' % i, [128, 1]), sb('drstd%d' % i, [128, 1]), Tok()) for i in range(2)]}
        for n_, c in enumerate(chunks):
            b = n_ % 2
            cs = slice(c * 128, (c + 1) * 128)
            K.dma(mx[b][:], D['mixT_d'][:, cs].rearrange("(kt p) n -> p kt n", p=128), R=[g.t_mix], W=[t_mx[b]])
            K.dma(xr[b][:], x_src[cs, :], R=([t_xsrc[c]] if t_xsrc is not None else []), W=[t_xr[b]])
            pys = []
            for h in range(2):
                py, pyt = g.ps()
                pys.append((py, pyt))
                for kt in range(8):
                    K.op('pe', lambda e, kt=kt, b=b, h=h, py=py: e.matmul(py[:, 0:512], lhsT=mx[b][:, kt, :], rhs=wb[:, kt, h * 512:(h + 1) * 512],
                                                                          start=(kt == 0), stop=(kt == 7)), R=[t_mx[b], t_w], W=[pyt])
            deepnorm_chunk(g, l, c, b, 0, [pys[0][0][:, 0:512], pys[1][0][:, 0:512]], [pys[0][1], pys[1][1]], xr[b], t_xr[b], T_, lntab, t_ln, xo[b][:], t_xo[b])
            K.dma(x1_d[cs, :], xo[b][:], R=[t_xo[b]], W=[t_x1[c]], q='pool')


def phase_ffn(g, l, chunks, w1_src, w3_src, w2_src, vT_d, t_vT, ffn_d, t_ffn, gate=None, first=True):
    nc, K, D = g.nc, g.K, g.D
    with ExitStack() as st:
        def sb(name, shape, dt=F32):
            return st.enter_context(nc.sbuf_tensor(_nm(name), list(shape), dt))
        w1b = sb('w1b', [128, 8, D_FF], BF16); w3b = sb('w3b', [128, 8, D_FF], BF16); w2b = sb('w2b', [128, NF, D_MODEL], BF16)
        t_w = Tok()
        stg = [sb('fstg%d' % i, [128, 8, 256]) for i in range(3)]; t_stg = [Tok() for _ in range(3)]
        k = 0
        for (src, dstw, nk, ncol) in ((w1_src, w1b, 8, D_FF), (w3_src, w3b, 8, D_FF), (w2_src, w2b, NF, D_MODEL)):
            view = src.rearrange("(kt p) n -> p kt n", p=128)
            for k0 in range(0, nk, 8):
                kn = min(8, nk - k0)
                for c0 in range(0, ncol, 256):
                    cn = min(256, ncol - c0)
                    i = k % 3
                    K.dma(stg[i][:, 0:kn, 0:cn], view[:, k0:k0 + kn, c0:c0 + cn], W=[t_stg[i]])
                    eng = ('dve', 'pool', 'act')[k % 3]
                    if eng == 'act':
                        K.op('act', lambda e, i=i, kn=kn, cn=cn, k0=k0, c0=c0, dstw=dstw: e.activation(out=dstw[:, k0:k0 + kn, c0:c0 + cn], in_=stg[i][:, 0:kn, 0:cn], func=AF.Identity),
                             R=[t_stg[i]], W=[t_w])
                    else:
                        K.op(eng, lambda e, i=i, kn=kn, cn=cn, k0=k0, c0=c0, dstw=dstw: e.tensor_copy(out=dstw[:, k0:k0 + kn, c0:c0 + cn], in_=stg[i][:, 0:kn, 0:cn]),
                             R=[t_stg[i]], W=[t_w])
                    k += 1
        vt = [sb('vt%d' % i, [128, 8, 256], BF16) for i in range(2)]; t_vt = [Tok(), Tok()]
        hT = sb('hT', [128, NF, 256], BF16); t_hT = Tok()
        sil = [sb('sil%d' % i, [128, 256]) for i in range(2)]; t_sil = [Tok(), Tok()]
        fo = [sb('fo%d' % i, [128, D_MODEL]) for i in range(2)]; t_fo = [Tok(), Tok()]
        if gate is not None:
            e_idx, gates_d, t_gates = gate
            gt = [sb('gt%d' % i, [128, N_EXP]) for i in range(2)]; t_gt = [Tok(), Tok()]
        nfo = 0
        for ti in range(0, len(chunks), 2):
            cc = chunks[ti:ti + 2]
            b = (ti // 2) % 2
            nt_ = len(cc) * 128
            for j, c in enumerate(cc):
                K.dma(vt[b][:, :, j * 128:(j + 1) * 128], vT_d[c, :, :, :], R=[t_vT[c]], W=[t_vt[b]])
            for f in range(NF):
                ph, pht = g.ps()
                fs = slice(f * 128, (f + 1) * 128)
                for kt in range(8):
                    K.op('pe', lambda e, kt=kt, b=b, ph=ph, fs=fs, nt_=nt_: e.matmul(ph[:, 0:nt_], lhsT=w1b[:, kt, fs], rhs=vt[b][:, kt, 0:nt_], start=(kt == 0), stop=(kt == 7)),
                         R=[t_w, t_vt[b]], W=[pht])
                for kt in range(8):
                    K.op('pe', lambda e, kt=kt, b=b, ph=ph, fs=fs, nt_=nt_: e.matmul(ph[:, 256:256 + nt_], lhsT=w3b[:, kt, fs], rhs=vt[b][:, kt, 0:nt_], start=(kt == 0), stop=(kt == 7)),
                         R=[t_w, t_vt[b]], W=[pht])
                sb_ = f % 2
                K.op('act', lambda e, ph=ph, sb_=sb_, nt_=nt_: e.activation(out=sil[sb_][:, 0:nt_], in_=ph[:, 0:nt_], func=AF.Silu), R=[pht], W=[t_sil[sb_]])
                K.op('dve', lambda e, ph=ph, sb_=sb_, nt_=nt_, f=f: e.tensor_tensor(out=hT[:, f, 0:nt_], in0=ph[:, 256:256 + nt_], in1=sil[sb_][:, 0:nt_], op=ALU.mult),
                     R=[pht, t_sil[sb_]], W=[t_hT])
            for j, c in enumerate(cc):
                ob_ = nfo % 2
                nfo += 1
                if gate is not None:
                    K.dma(gt[ob_][:], gates_d[c, :, :], R=[t_gates[c]], W=[t_gt[ob_]])
                for h in range(2):
                    po, pot = g.ps()
                    for f in range(NF):
                        K.op('pe', lambda e, f=f, j=j, h=h, po=po: e.matmul(po[:, 0:512], lhsT=hT[:, f, j * 128:(j + 1) * 128], rhs=w2b[:, f, h * 512:(h + 1) * 512],
                                                                            start=(f == 0), stop=(f == NF - 1)), R=[t_hT, t_w], W=[pot])
                    if gate is None:
                        K.op('act', lambda e, po=po, ob_=ob_, h=h: e.activation(out=fo[ob_][:, h * 512:(h + 1) * 512], in_=po[:, 0:512], func=AF.Identity), R=[pot], W=[t_fo[ob_]])
                    else:
                        K.op('act', lambda e, po=po, ob_=ob_, h=h: e.activation(out=fo[ob_][:, h * 512:(h + 1) * 512], in_=po[:, 0:512], func=AF.Identity,
                                                                              scale=gt[ob_][:, e_idx:e_idx + 1]), R=[pot, t_gt[ob_]], W=[t_fo[ob_]])
                cs = slice(c * 128, (c + 1) * 128)
                if first:
                    K.dma(ffn_d[cs, :], fo[ob_][:], R=[t_fo[ob_]], W=[t_ffn[c]], q='pool')
                else:
                    K.dma(ffn_d[cs, :], fo[ob_][:], R=[t_fo[ob_]], W=[t_ffn[c]], q='pool', accum_op=ALU.add)


def phase_ln2(g, l, chunks, x1_d, t_x1, ffn_d, t_ffn, dst_fn):
    nc, K, D = g.nc, g.K, g.D
    with ExitStack() as st:
        def sb(name, shape, dt=F32):
            return st.enter_context(nc.sbuf_tensor(_nm(name), list(shape), dt))
        lntab, t_ln = load_ln_tables(g, st, l, 1)
        g.gbc = sb('gbcf', [128, 1, 2, D_MODEL])
        K.dma(g.gbc[:], D['gbc_d'][:, 1:2, :, :], R=[g.t_gbcd], W=[g.t_gbc])
        xr = [sb('l2x%d' % i, [128, D_MODEL]) for i in range(2)]; t_xr = [Tok(), Tok()]
        fr = [sb('l2f%d' % i, [128, D_MODEL]) for i in range(2)]; t_fr = [Tok(), Tok()]
        xo = [sb('l2o%d' % i, [128, D_MODEL]) for i in range(2)]; t_xo = [Tok(), Tok()]
        T_ = {'tmp': [sb('l2tmp%d' % i, [128, D_MODEL]) for i in range(2)], 't_tmp': [Tok(), Tok()],
              'sts': [(sb('l2stt%d' % i, [128, 2, 6]), sb('l2mv%d' % i, [128, 2]), sb('l2lnv%d' % i, [128, 1]), sb('l2rstd%d' % i, [128, 1]), Tok()) for i in range(2)]}
        for n_, c in enumerate(chunks):
            b = n_ % 2
            cs = slice(c * 128, (c + 1) * 128)
            K.dma(xr[b][:], x1_d[cs, :], R=[t_x1[c]], W=[t_xr[b]])
            K.dma(fr[b][:], ffn_d[cs, :], R=[t_ffn[c]], W=[t_fr[b]])
            deepnorm_chunk(g, l, c, b, 1, [fr[b][:, 0:512], fr[b][:, 512:1024]], [t_fr[b], t_fr[b]], xr[b], t_xr[b], T_, lntab, t_ln, xo[b][:], t_xo[b])
            dst, t_dst = dst_fn(c)
            K.dma(dst, xo[b][:], R=[t_xo[b]], W=[t_dst], q='pool')


_CACHE = {}


def kernel(**inputs):
    inp = {k: np.asarray(v) for k, v in inputs.items()}
    if 'prog' not in _CACHE:
        _CACHE['prog'] = build_program()
    nc, g = _CACHE['prog']
    shared = prep_shared(inp)
    maps = []
    for b in range(8):
        m = prep_core_inputs(inp, b, shared)
        maps.append({k: v for k, v in m.items() if k in g.D})
    res = run_bass_kernel_spmd(nc, maps, core_ids=list(range(8)))
    out = np.stack([np.asarray(r['out']) for r in res.results], axis=0)
    return out.astype(np.float32)


def phase_ffn2(g, l, chunks, experts, vT_d, t_vT, ffn_d, t_ffn, gates=None):
    nc, K, D = g.nc, g.K, g.D
    HF = NF // 2
    with ExitStack() as st:
        def sb(name, shape, dt=F32):
            return st.enter_context(nc.sbuf_tensor(_nm(name), list(shape), dt))
        w1s = [sb('w1s%d' % i, [128, 8, HF * 128], BF16) for i in range(2)]
        w3s = [sb('w3s%d' % i, [128, 8, HF * 128], BF16) for i in range(2)]
        w2s = [sb('w2s%d' % i, [128, HF, D_MODEL], BF16) for i in range(2)]
        t_ws = [Tok(), Tok()]
        vt = [sb('vt%d' % i, [128, 8, 512], BF16) for i in range(2)]; t_vt = [Tok(), Tok()]
        hT = [sb('hT%d' % i, [128, HF, 512], BF16) for i in range(2)]; t_hT = [Tok(), Tok()]
        sil = [sb('sil%d' % i, [128, 512]) for i in range(2)]; t_sil = [Tok(), Tok()]
        fo = [sb('fo%d' % i, [128, D_MODEL]) for i in range(2)]; t_fo = [Tok(), Tok()]
        if gates is not None:
            gates_d, t_gates = gates
            gt = [sb('gt%d' % i, [128, N_EXP]) for i in range(2)]; t_gt = [Tok(), Tok()]
        units = [(e_, hf) for e_ in range(len(experts)) for hf in range(2)]

        def load_unit(u):
            e_, hf = units[u]
            w1, w3, w2 = experts[e_]
            s_ = u % 2
            f0 = hf * HF * 128
            v1 = w1.rearrange("(kt p) n -> p kt n", p=128)
            v3 = w3.rearrange("(kt p) n -> p kt n", p=128)
            v2 = w2.rearrange("(ft p) n -> p ft n", p=128)
            for c0 in range(0, HF * 128, 704):
                K.dma(w1s[s_][:, :, c0:c0 + 704], v1[:, :, f0 + c0:f0 + c0 + 704], W=[t_ws[s_]], q='pool')
                K.dma(w3s[s_][:, :, c0:c0 + 704], v3[:, :, f0 + c0:f0 + c0 + 704], W=[t_ws[s_]], q='pool')
            for f_ in range(0, HF, 4):
                fn_ = min(4, HF - f_)
                K.dma(w2s[s_][:, f_:f_ + fn_, :], v2[:, hf * HF + f_:hf * HF + f_ + fn_, :], W=[t_ws[s_]], q='pool')
        load_unit(0)
        nfo = 0
        ntile = 0
        for u in range(len(units)):
            e_, hf = units[u]
            s_ = u % 2
            if u + 1 < len(units):
                load_unit(u + 1)
            _ft = int(os.environ.get('FFN_TILE', '2'))
            for ti in range(0, len(chunks), _ft):
                cc = chunks[ti:ti + _ft]
                b = ntile % 2
                ntile += 1
                nt_ = len(cc) * 128
                for j, c in enumerate(cc):
                    K.dma(vt[b][:, :, j * 128:(j + 1) * 128], vT_d[c, :, :, :], R=[t_vT[c]], W=[t_vt[b]])
                for f in range(HF):
                    ph, pht = g.ps()
                    ph3, pht3 = g.ps()
                    fs = slice(f * 128, (f + 1) * 128)
                    for kt in range(8):
                        K.op('pe', lambda e, kt=kt, b=b, ph=ph, fs=fs, nt_=nt_, s_=s_: e.matmul(ph[:, 0:nt_], lhsT=w1s[s_][:, kt, fs], rhs=vt[b][:, kt, 0:nt_], start=(kt == 0), stop=(kt == 7)),
                             R=[t_ws[s_], t_vt[b]], W=[pht])
                    for kt in range(8):
                        K.op('pe', lambda e, kt=kt, b=b, ph3=ph3, fs=fs, nt_=nt_, s_=s_: e.matmul(ph3[:, 0:nt_], lhsT=w3s[s_][:, kt, fs], rhs=vt[b][:, kt, 0:nt_], start=(kt == 0), stop=(kt == 7)),
                             R=[t_ws[s_], t_vt[b]], W=[pht3])
                    sb_ = f % 2
                    K.op('act', lambda e, ph=ph, sb_=sb_, nt_=nt_: e.activation(out=sil[sb_][:, 0:nt_], in_=ph[:, 0:nt_], func=AF.Silu), R=[pht], W=[t_sil[sb_]])
                    K.op('dve', lambda e, ph3=ph3, sb_=sb_, nt_=nt_, f=f, b=b: e.tensor_tensor(out=hT[b][:, f, 0:nt_], in0=ph3[:, 0:nt_], in1=sil[sb_][:, 0:nt_], op=ALU.mult),
                         R=[pht3, t_sil[sb_]], W=[t_hT[b]])
                for j, c in enumerate(cc):
                    ob_ = nfo % 2
                    nfo += 1
                    if gates is not None:
                        K.dma(gt[ob_][:], gates_d[c, :, :], R=[t_gates[c]], W=[t_gt[ob_]])
                    for h in range(2):
                        po, pot = g.ps()
                        for f in range(HF):
                            K.op('pe', lambda e, f=f, j=j, h=h, po=po, b=b, s_=s_: e.matmul(po[:, 0:512], lhsT=hT[b][:, f, j * 128:(j + 1) * 128], rhs=w2s[s_][:, f, h * 512:(h + 1) * 512],
                                                                                    start=(f == 0), stop=(f == HF - 1)), R=[t_hT[b], t_ws[s_]], W=[pot])
                        if gates is None:
                            K.op('act', lambda e, po=po, ob_=ob_, h=h: e.activation(out=fo[ob_][:, h * 512:(h + 1) * 512], in_=po[:, 0:512], func=AF.Identity), R=[pot], W=[t_fo[ob_]])
                        else:
                            K.op('act', lambda e, po=po, ob_=ob_, h=h, e_=e_: e.activation(out=fo[ob_][:, h * 512:(h + 1) * 512], in_=po[:, 0:512], func=AF.Identity,
                                                                                  scale=gt[ob_][:, e_:e_ + 1]), R=[pot, t_gt[ob_]], W=[t_fo[ob_]])
                    cs = slice(c * 128, (c + 1) * 128)
                    if u == 0:
                        K.dma(ffn_d[cs, :], fo[ob_][:], R=[t_fo[ob_]], W=[t_ffn[c]], q='pool')
                    else:
                        K.dma(ffn_d[cs, :], fo[ob_][:], R=[t_fo[ob_]], W=[t_ffn[c]], q='pool', accum_op=ALU.add)
```

```python
import math
import os
from contextlib import ExitStack
import numpy as np
import ml_dtypes
import concourse.bass as bass
import concourse.mybir as mybir
from concourse.bass_utils import run_bass_kernel_spmd

F32 = mybir.dt.float32
BF16 = mybir.dt.bfloat16
I32 = mybir.dt.int32
AF = mybir.ActivationFunctionType
ALU = mybir.AluOpType
AX = mybir.AxisListType

COMPUTE = ('pe', 'act', 'dve', 'pool')
NDMA = 24


class Tok:
    __slots__ = ('w', 'r')

    def __init__(self):
        self.w = None
        self.r = {}


class Sched:
    def __init__(self, nc, same_engine_sync=True):
        self.nc = nc
        self.es = ExitStack()
        self.E = {'pe': nc.tensor, 'act': nc.scalar, 'dve': nc.vector, 'pool': nc.gpsimd, 'sp': nc.sync}
        self.sem = {e: self.es.enter_context(nc.semaphore('sem_' + e)) for e in COMPUTE}
        self.cnt = {e: 0 for e in COMPUTE}
        self.seen = {f: {} for f in self.E}
        self.dsem = [self.es.enter_context(nc.semaphore('dsem%d' % i)) for i in range(NDMA)]
        self.dcnt = [0] * NDMA
        self.dnext = 0
        self.same = same_engine_sync
        self.ninst = 0

    def _semobj(self, key):
        return self.sem[key] if isinstance(key, str) else self.dsem[key[1]]

    def wait(self, f, ev):
        if ev is None:
            return
        key, val = ev
        if key == f and (f == 'pe' or not self.same):
            return
        if self.seen[f].get(key, 0) >= val:
            return
        self.E[f].wait_ge(self._semobj(key), val)
        self.seen[f][key] = val

    def _deps(self, f, R, W):
        for t in R:
            self.wait(f, t.w)
        for t in W:
            self.wait(f, t.w)
            for k, v in t.r.items():
                self.wait(f, (k, v))

    def op(self, eng, fn, R=(), W=()):
        self._deps(eng, R, W)
        ins = fn(self.E[eng])
        self.cnt[eng] += 1
        ins.then_inc(self.sem[eng], 1)
        ev = (eng, self.cnt[eng])
        self.seen[eng][eng] = self.seen[eng].get(eng, 0)
        for t in R:
            t.r[eng] = self.cnt[eng]
        for t in W:
            t.w = ev
            t.r = {}
        self.ninst += 1
        return ins

    def dma(self, out, in_, R=(), W=(), q='sp', **kw):
        i = self.dnext
        self.dnext = (i + 1) % NDMA
        key = ('d', i)
        if self.dcnt[i] > 0:
            self.wait(q, (key, self.dcnt[i]))
        self._deps(q, R, W)
        ins = self.E[q].dma_start(out=out, in_=in_, **kw)
        self.dcnt[i] += 16
        ins.then_inc(self.dsem[i], 16)
        for t in R:
            t.r[key] = self.dcnt[i]
        for t in W:
            t.w = (key, self.dcnt[i])
            t.r = {}
        self.ninst += 1
        return ins

    def barrier(self):
        for f in self.E:
            for e in COMPUTE:
                if self.cnt[e] > 0:
                    self.wait_force(f, (e, self.cnt[e]))
            for i in range(NDMA):
                if self.dcnt[i] > 0:
                    self.wait(f, (('d', i), self.dcnt[i]))

    def wait_force(self, f, ev):
        key, val = ev
        if self.seen[f].get(key, 0) >= val:
            return
        self.E[f].wait_ge(self._semobj(key), val)
        self.seen[f][key] = val


D_MODEL = 1024
SEQ = 4096
CTXL = 256
T = SEQ + CTXL
NCH = T // 128
DEPTH = 2
D_PROJ = 2848
D_FF = 2816
NF = D_FF // 128
N_EXP = 8
LN_EPS = 1e-5
ALPHA = (2 * DEPTH) ** 0.25
OFF = {}
_o = 0
for _n, _w in (('ret_q', 256), ('ret_k', 256), ('ret_v', 256), ('ret_g', 256), ('gla_q', 128), ('gla_k', 128),
               ('gla_v', 256), ('gla_r', 256), ('gla_a', 32), ('s5_u', 256), ('hy_p', 768)):
    OFF[_n] = _o
    _o += _w


class G:
    pass


_uid = [0]


def _nm(name):
    _uid[0] += 1
    return '%s_%d' % (name, _uid[0])


def build_program(stop_after=None, dbg=(), dbg_layer=0):
    nc = bass.Bass("TRN2", target_bir_lowering=False)
    K = Sched(nc, same_engine_sync=(os.environ.get('SAME', '1') == '1'))
    g = G()
    g.nc, g.K, g.dbg, g.stop_after, g.dbg_layer = nc, K, set(dbg), stop_after, dbg_layer
    g.D = {}
    g.outs = {}

    def din(name, shape, dt=F32):
        g.D[name] = nc.dram_tensor(name, list(shape), dt, kind="ExternalInput").ap()
        return g.D[name]

    def dscr(name, shape, dt=F32):
        g.D[name] = nc.dram_tensor(name, list(shape), dt, kind="Internal").ap()
        return g.D[name]

    def dout(name, shape, dt=F32):
        g.outs[name] = nc.dram_tensor(name, list(shape), dt, kind="ExternalOutput").ap()
        return g.outs[name]
    g.din, g.dscr, g.dout = din, dscr, dout

    din('xin', [T, D_MODEL])
    din('cvec', [128, 8, 2])
    din('ada_w', [DEPTH, D_MODEL, 6 * D_MODEL])
    din('ada_bT', [DEPTH, 128, 48])
    din('ada_b', [DEPTH, 6 * D_MODEL])
    din('w_out', [DEPTH, D_MODEL, D_MODEL])
    din('ln_g', [DEPTH, 2, D_MODEL])
    din('ln_b', [DEPTH, 2, D_MODEL])
    din('ffn_w1', [1, D_MODEL, D_FF])
    din('ffn_w3', [1, D_MODEL, D_FF])
    din('ffn_w2', [1, D_FF, D_MODEL])
    din('router_w', [1, D_MODEL, N_EXP])
    din('router_b', [1, N_EXP])
    din('moe_w1', [1, N_EXP, D_MODEL, D_FF])
    din('moe_w3', [1, N_EXP, D_MODEL, D_FF])
    din('moe_w2', [1, N_EXP, D_FF, D_MODEL])
    din('w_in', [DEPTH, D_MODEL, D_PROJ])
    din('ident', [128, 128])
    din('maskL', [128, 128])
    din('maskU', [128, 128])
    din('antiI', [128, 128])
    din('poscols', [128, 4])
    din('rot_cos', [128, 32, 32])
    din('rot_sin', [128, 32, 32])
    din('gla_wa', [DEPTH, 2, 16, 128])
    din('gla_ba', [DEPTH, 2, 128])
    din('gla_ng', [DEPTH, 256])
    din('ret_decay', [DEPTH, 8])
    din('ret_dec_fm', [DEPTH, 2, 128, 2])
    din('ret_gng', [DEPTH, 256])
    din('ret_gnb', [DEPTH, 256])
    din('s5_lam_fm', [DEPTH, 128, 16, 2])
    din('s5_dt_fm', [DEPTH, 128, 16])
    din('s5_BT', [DEPTH, 16, 2, 128, 128])
    din('s5_CT', [DEPTH, 16, 2, 128, 32])
    din('s5_dcol', [DEPTH, 128, 2])
    din('s5_glub', [DEPTH, 128, 2])
    din('s5_glu_w', [DEPTH, 256, 256])
    din('iota_t', [T])
    din('hy_cw', [DEPTH, 128, 6, 3])
    din('hy_cb', [DEPTH, 128, 6])
    din('hy_biasc', [DEPTH, 128, 2])
    din('hy_fw1', [DEPTH, 33, 64])
    din('hy_fw2', [DEPTH, 64, 64])
    din('hy_fw3', [DEPTH, 64, 512])
    din('hy_fcol', [DEPTH, 64, 5])
    din('hy_decay', [DEPTH, 2, 256])
    for sfx, n in (('L', SEQ), ('C', CTXL)):
        nt = n // 128
        din('hy_zemb' + sfx, [33, n])
        din('hy_tn' + sfx, [128, nt])
        din('hy_wk' + sfx, [128, nt + 1])
        din('dftc' + sfx, [nt + 1, 128, nt + 1, 128], BF16)
        din('dfts' + sfx, [nt + 1, 128, nt + 1, 128], BF16)

    pst = ExitStack()
    K.es.enter_context(pst)

    def sbp(name, shape, dt=F32):
        return pst.enter_context(nc.sbuf_tensor(_nm(name), list(shape), dt))
    g.ident = sbp('ident', [128, 128]); g.t_const = Tok()
    g.identb = sbp('identb', [128, 128], BF16)
    g.maskL = sbp('maskL', [128, 128]); g.maskU = sbp('maskU', [128, 128]); g.antiI = sbp('antiI', [128, 128])
    g.antiIb = sbp('antiIb', [128, 128], BF16)
    g.mask2 = sbp('mask2', [128, 2, 128])
    g.ones = sbp('ones', [128, 128]); g.onesb = sbp('onesb', [128, 128], BF16)
    g.epsc = sbp('epsc', [128, 1])
    g.poscols = sbp('poscols', [128, 4])
    g.lnc = sbp('lnc', [128, 2])
    g.modT = sbp('modT', [128, 48, 2]); g.t_mod = Tok()
    g.t_gbc = Tok(); g.t_gbcd = Tok()
    g.mod1 = sbp('mod1', [128, 48, 2])
    NPS = 6
    g.psum = [pst.enter_context(nc.psum_tensor('ps%d' % i, [128, 512], F32)) for i in range(NPS)]
    g.pst = [Tok() for _ in range(NPS)]
    g.pnext = 0
    g.psb = [pst.enter_context(nc.psum_tensor('psb%d' % i, [128, 1024], BF16)) for i in range(2)]
    g.t_psb = [Tok(), Tok()]
    g.psb_next = 0

    def ps():
        i = g.pnext
        g.pnext = (i + 1) % NPS
        return g.psum[i], g.pst[i]
    g.ps = ps

    def psb_half():
        i = g.psb_next
        g.psb_next = 1 - i
        return g.psb[i][:, 0:512], g.t_psb[i]
    g.psb_half = psb_half

    tc = g.t_const
    K.dma(g.ident[:], g.D['ident'][:, :], W=[tc])
    K.dma(g.maskL[:], g.D['maskL'][:, :], W=[tc])
    K.dma(g.maskU[:], g.D['maskU'][:, :], W=[tc])
    K.dma(g.mask2[:, 0, :], g.D['maskL'][:, :], W=[tc])
    K.dma(g.mask2[:, 1, :], g.D['maskU'][:, :], W=[tc])
    K.dma(g.antiI[:], g.D['antiI'][:, :], W=[tc])
    K.dma(g.poscols[:], g.D['poscols'][:, :], W=[tc])
    K.op('pool', lambda e: e.memset(g.ones[:], 1.0), W=[tc])
    K.op('pool', lambda e: e.memset(g.onesb[:], 1.0), W=[tc])
    K.op('pool', lambda e: e.memset(g.epsc[:], LN_EPS), W=[tc])
    K.op('pool', lambda e: e.memset(g.lnc[:, 0:1], math.log(0.125)), W=[tc])
    K.op('pool', lambda e: e.memset(g.lnc[:, 1:2], math.log(32 ** -0.5)), W=[tc])
    K.op('dve', lambda e: e.tensor_copy(out=g.identb[:], in_=g.ident[:]), R=[tc], W=[tc])
    K.op('dve', lambda e: e.tensor_copy(out=g.antiIb[:], in_=g.antiI[:]), R=[tc], W=[tc])
    K.barrier()

    dscr('x_cur', [T, D_MODEL])

    dscr('uT_d', [NCH, 128, 8, 128], BF16)
    dscr('gbc_d', [128, 2, 2, D_MODEL])
    dscr('vT_d', [NCH, 128, 8, 128], BF16)
    dscr('mixT_d', [D_MODEL, T], BF16)
    dscr('x1_d', [T, D_MODEL])
    dscr('ffn_d', [T, D_MODEL])
    dscr('gates_d', [NCH, 128, N_EXP])
    dout('out', [SEQ, D_MODEL])
    g.t_uT = [Tok() for _ in range(NCH)]
    g.t_mix = Tok()
    t_vT = [Tok() for _ in range(NCH)]
    t_x1 = [Tok() for _ in range(NCH)]
    t_ffn = [Tok() for _ in range(NCH)]
    t_gates = [Tok() for _ in range(NCH)]
    t_xcur = [Tok() for _ in range(NCH)]
    t_out = Tok()
    D = g.D
    x_src, t_xsrc = D['xin'], None
    for l in range(DEPTH):
        last = l == DEPTH - 1
        phase_mod(g, l)
        K.barrier()
        if stop_after == ('mod', l):
            break
        phase_lnmod(g, l, x_src, D['uT_d'], g.t_uT, sh_idx=0, sc_idx=8, t_src=t_xsrc)
        K.barrier()
        if 'uT' in g.dbg and l == g.dbg_layer:
            o = dout('dbg_uT', [NCH, 128, 8, 128], BF16)
            K.dma(o[:, :, :, :], D['uT_d'][:, :, :, :], R=g.t_uT, q='pool')
            K.barrier()
        if stop_after == ('lnmod', l):
            break
        _ps = os.environ.get('LA_PASSES', 'g0,g1,r0,r1').split(',')
        for p in range(2):
            if 'g%d' % p in _ps:
                la_pass(g, l, 'gla', p, last)
                K.barrier()
        for p in range(2):
            if 'r%d' % p in _ps:
                la_pass(g, l, 'ret', p, last)
                K.barrier()
        if 's5' in os.environ.get('MIXERS', 's5,hy'):
            phase_s5(g, l)
            K.barrier()
        if 'hy' in os.environ.get('MIXERS', 's5,hy'):
            phase_hyena(g, l, SEQ, 2, 'L')
            K.barrier()
            if not last:
                phase_hyena(g, l, CTXL, 0, 'C')
                K.barrier()
        if 'mix' in g.dbg and l == g.dbg_layer:
            o = dout('dbg_mix', [D_MODEL, T], BF16)
            K.dma(o[:, :], D['mixT_d'][:, :], R=[g.t_mix], q='pool')
            K.barrier()
        if stop_after == ('la', l):
            break
        chunks = list(range(2, NCH)) if last else list(range(NCH))
        phase_wout(g, l, x_src, t_xsrc, chunks, D['x1_d'], t_x1)
        K.barrier()
        if 'gbc' in g.dbg and l == g.dbg_layer:
            o = dout('dbg_gbc', [128, 2, 2, D_MODEL])
            K.dma(o[:, :, :, :], D['gbc_d'][:, :, :, :], R=[g.t_gbcd], q='pool')
            K.barrier()
        if 'x1' in g.dbg and l == g.dbg_layer:
            o = dout('dbg_x1', [T, D_MODEL])
            K.dma(o[:, :], D['x1_d'][:, :], R=t_x1, q='pool')
            K.barrier()
        if stop_after == ('wout', l):
            break
        j = l // 2
        if l % 2 == 0:
            phase_lnmod(g, l, D['x1_d'], D['vT_d'], t_vT, sh_idx=24, sc_idx=32, chunks=chunks, t_src=t_x1)
            K.barrier()
            phase_ffn2(g, l, chunks, [(D['ffn_w1'][j], D['ffn_w3'][j], D['ffn_w2'][j])], D['vT_d'], t_vT, D['ffn_d'], t_ffn)
            K.barrier()
        else:
            phase_lnmod(g, l, D['x1_d'], D['vT_d'], t_vT, sh_idx=24, sc_idx=32, chunks=chunks, t_src=t_x1, router=(j, D['gates_d'], t_gates))
            K.barrier()
            phase_ffn2(g, l, chunks, [(D['moe_w1'][j, e_], D['moe_w3'][j, e_], D['moe_w2'][j, e_]) for e_ in range(N_EXP)],
                       D['vT_d'], t_vT, D['ffn_d'], t_ffn, gates=(D['gates_d'], t_gates))
            K.barrier()
        if 'ffn' in g.dbg and l == g.dbg_layer:
            o = dout('dbg_ffn', [T, D_MODEL])
            K.dma(o[:, :], D['ffn_d'][:, :], R=t_ffn, q='pool')
            K.barrier()
        if stop_after == ('ffn', l):
            break
        if last:
            def dst_fn(c):
                return g.outs['out'][(c - 2) * 128:(c - 1) * 128, :], t_out
        else:
            def dst_fn(c):
                return D['x_cur'][c * 128:(c + 1) * 128, :], t_xcur[c]
        phase_ln2(g, l, chunks, D['x1_d'], t_x1, D['ffn_d'], t_ffn, dst_fn)
        K.barrier()
        if 'x2' in g.dbg and l == g.dbg_layer and not last:
            o = dout('dbg_x2', [T, D_MODEL])
            K.dma(o[:, :], D['x_cur'][:, :], R=t_xcur, q='pool')
            K.barrier()
        if stop_after == ('ln2', l):
            break
        x_src, t_xsrc = D['x_cur'], t_xcur
    K.barrier()
    return nc, g


def phase_mod(g, l):
    nc, K = g.nc, g.K
    with ExitStack() as st:
        def sb(name, shape, dt=F32):
            return st.enter_context(nc.sbuf_tensor(_nm(name), list(shape), dt))
        cv = sb('cv', [128, 8, 2]); t_cv = Tok()
        sv = sb('sv', [128, 8, 2])
        abT = sb('abT', [128, 48]); t_ab = Tok()
        wbuf = [sb('adaw%d' % i, [128, 8, 512]) for i in range(2)]
        t_w = [Tok(), Tok()]
        K.dma(cv[:], g.D['cvec'][:, :, :], W=[t_cv])
        K.dma(abT[:], g.D['ada_bT'][l, :, :], W=[t_ab])
        K.op('act', lambda e: e.activation(out=sv[:], in_=cv[:], func=AF.Silu), R=[t_cv], W=[t_cv])
        wsrc = g.D['ada_w'][l].rearrange("(kt p) n -> p kt n", p=128)
        svrep = sb('svrep', [128, 8, 2, 128])
        g.gbc = sb('gbc', [128, 2, 2, D_MODEL])
        K.op('dve', lambda e: e.tensor_copy(out=svrep[:], in_=sv[:].unsqueeze(3).to_broadcast([128, 8, 2, 128])), R=[t_cv], W=[t_cv])
        K.dma(g.gbc[:, 0, 0, :], g.D['ada_b'][l, 2048:3072].partition_broadcast(128), W=[g.t_gbc])
        K.dma(g.gbc[:, 0, 1, :], g.D['ada_b'][l, 2048:3072].partition_broadcast(128), W=[g.t_gbc])
        K.dma(g.gbc[:, 1, 0, :], g.D['ada_b'][l, 5120:6144].partition_broadcast(128), W=[g.t_gbc])
        K.dma(g.gbc[:, 1, 1, :], g.D['ada_b'][l, 5120:6144].partition_broadcast(128), W=[g.t_gbc])
        for grp in range(12):
            b = grp % 2
            K.dma(wbuf[b][:], wsrc[:, :, grp * 512:(grp + 1) * 512], W=[t_w[b]])
            if grp in (4, 5, 10, 11):
                mf = 0 if grp < 6 else 1
                hf = grp % 2
                for cls in range(2):
                    pq, pqt = g.ps()
                    for kt in range(8):
                        K.op('pe', lambda e, kt=kt, b=b, cls=cls, pq=pq: e.matmul(pq[:, 0:512], lhsT=svrep[:, kt, cls, :], rhs=wbuf[b][:, kt, :],
                                                                                  start=(kt == 0), stop=(kt == 7)), R=[t_w[b], t_cv], W=[pqt])
                    dst = g.gbc[:, mf, cls, hf * 512:(hf + 1) * 512]
                    K.op('dve', lambda e, pq=pq, dst=dst: e.tensor_tensor(out=dst, in0=pq[:, 0:512], in1=dst, op=ALU.add), R=[pqt, g.t_gbc], W=[g.t_gbc])
            pt, ptk = g.ps()
            for jj in range(4):
                for kt in range(8):
                    K.op('pe', lambda e, jj=jj, kt=kt, b=b, pt=pt: e.matmul(
                        pt[:, 2 * jj:2 * jj + 2], lhsT=wbuf[b][:, kt, jj * 128:(jj + 1) * 128], rhs=sv[:, kt, :],
                        start=(kt == 0), stop=(kt == 7)), R=[t_w[b], t_cv], W=[ptk])
            K.op('dve', lambda e, pt=pt, grp=grp: e.tensor_tensor(
                out=g.modT[:, grp * 4:(grp + 1) * 4, :], in0=pt[:, 0:8].rearrange("p (j k) -> p j k", k=2),
                in1=abT[:, grp * 4:(grp + 1) * 4].unsqueeze(2).to_broadcast([128, 4, 2]), op=ALU.add), R=[ptk, t_ab], W=[g.t_mod])
        K.op('dve', lambda e: e.tensor_scalar_add(out=g.mod1[:], in0=g.modT[:], scalar1=1.0), R=[g.t_mod], W=[g.t_mod])
        K.dma(g.D['gbc_d'][:, :, :, :], g.gbc[:], R=[g.t_gbc], W=[g.t_gbcd], q='pool')
        K.barrier()


def ln_stats(g, st_tiles, xc, t_xc, eps=LN_EPS):
    K = g.K
    stt, mv, lnv, rstd, tok = st_tiles
    for h in range(2):
        K.op('dve', lambda e, h=h: e.bn_stats(out=stt[:, h, :], in_=xc[:, h * 512:(h + 1) * 512]), R=[t_xc], W=[tok])
    K.op('dve', lambda e: e.bn_aggr(out=mv[:], in_=stt[:].rearrange("p a b -> p (a b)")), R=[tok], W=[tok])
    K.op('act', lambda e: e.activation(out=lnv[:], in_=mv[:, 1:2], func=AF.Ln, bias=g.epsc[:, 0:1]), R=[tok, g.t_const], W=[tok])
    K.op('act', lambda e: e.activation(out=rstd[:], in_=lnv[:], func=AF.Exp, scale=-0.5), R=[tok], W=[tok])
    return mv, rstd, tok


def phase_lnmod(g, l, x_src, uT_d, t_uT, sh_idx, sc_idx, chunks=None, t_src=None, router=None):
    nc, K = g.nc, g.K
    with ExitStack() as st:
        def sb(name, shape, dt=F32):
            return st.enter_context(nc.sbuf_tensor(_nm(name), list(shape), dt))
        xc = [sb('xc%d' % i, [128, 1024]) for i in range(2)]
        t_xc = [Tok(), Tok()]
        xn = [sb('xn%d' % i, [128, 1024]) for i in range(2)]
        t_xn = [Tok(), Tok()]
        uc = [sb('uc%d' % i, [128, 8, 128], BF16) for i in range(2)]
        t_uc = [Tok(), Tok()]
        sts = [(sb('stt%d' % i, [128, 2, 6]), sb('mv%d' % i, [128, 2]), sb('lnv%d' % i, [128, 1]), sb('rstd%d' % i, [128, 1]), Tok())
               for i in range(2)]
        if router is not None:
            j_moe, gates_d, t_gates = router
            uc32 = [sb('uc32_%d' % i, [128, 8, 128]) for i in range(2)]; t_uc32 = [Tok(), Tok()]
            rw = sb('rw', [128, 8, N_EXP]); rb = sb('rb', [128, N_EXP]); t_rw = Tok()
            K.dma(rw[:], g.D['router_w'][j_moe].rearrange("(kt p) n -> p kt n", p=128), W=[t_rw])
            K.dma(rb[:], g.D['router_b'][j_moe].partition_broadcast(128), W=[t_rw])
            rl = [sb('rl%d' % i, [128, 6, N_EXP]) for i in range(2)]; rs = [sb('rs%d' % i, [128, 4]) for i in range(2)]; t_rl = [Tok(), Tok()]
        cl_ = list(chunks if chunks is not None else range(NCH))

        def stX(n, c):
            b = n % 2
            col = 1 if c < 2 else 0
            K.dma(xc[b][:], x_src[c * 128:(c + 1) * 128, :], R=([t_src[c]] if t_src is not None else []), W=[t_xc[b]])
            mv, rstd, tk = ln_stats(g, sts[b], xc[b], t_xc[b])
            K.op('dve', lambda e, b=b, mv=mv, rstd=rstd: e.tensor_scalar(
                out=xn[b][:], in0=xc[b][:], scalar1=mv[:, 0:1], scalar2=rstd[:, 0:1], op0=ALU.subtract, op1=ALU.mult),
                R=[t_xc[b], tk], W=[t_xn[b]])

        def stY(n, c):
            b = n % 2
            col = 1 if c < 2 else 0
            mv, rstd, tk = None, None, None
            for half in range(2):
                pt, ptk = g.ps()
                for q in range(4):
                    kt = half * 4 + q
                    K.op('pe', lambda e, b=b, kt=kt, q=q, pt=pt: e.transpose(
                        out=pt[:, q * 128:(q + 1) * 128], in_=xn[b][:, kt * 128:(kt + 1) * 128], identity=g.ident[:]),
                        R=[t_xn[b], g.t_const], W=[ptk])
                for q in range(4):
                    kt = half * 4 + q
                    dst, t_dst = (uc[b], t_uc[b]) if router is None else (uc32[b], t_uc32[b])
                    K.op('act', lambda e, kt=kt, q=q, pt=pt, dst=dst, col=col: e.activation(
                        out=dst[:, kt, :], in_=pt[:, q * 128:(q + 1) * 128], func=AF.Identity,
                        scale=g.mod1[:, sc_idx + kt, col:col + 1], bias=g.modT[:, sh_idx + kt, col:col + 1]),
                        R=[ptk, g.t_mod], W=[t_dst])
            if router is not None:
                K.op('pool', lambda e, b=b: e.tensor_copy(out=uc[b][:], in_=uc32[b][:]), R=[t_uc32[b]], W=[t_uc[b]])
                pr, prt = g.ps()
                for kt in range(8):
                    K.op('pe', lambda e, kt=kt, b=b, pr=pr: e.matmul(pr[:, 0:N_EXP], lhsT=uc32[b][:, kt, :], rhs=rw[:, kt, :], start=(kt == 0), stop=(kt == 7)),
                         R=[t_uc32[b], t_rw], W=[prt])
                L_, r4, tk2 = rl[b], rs[b], t_rl[b]
                K.op('dve', lambda e, pr=pr, L_=L_: e.tensor_tensor(out=L_[:, 0, :], in0=pr[:, 0:N_EXP], in1=rb[:], op=ALU.add), R=[prt, t_rw], W=[tk2])
                K.op('dve', lambda e, L_=L_, r4=r4: e.tensor_reduce(out=r4[:, 0:1], in_=L_[:, 0, :], axis=AX.X, op=ALU.max), R=[tk2], W=[tk2])
                K.op('dve', lambda e, L_=L_, r4=r4: e.tensor_scalar(out=L_[:, 1, :], in0=L_[:, 0, :], scalar1=r4[:, 0:1], scalar2=None, op0=ALU.is_equal), R=[tk2], W=[tk2])
                K.op('dve', lambda e, L_=L_: e.scalar_tensor_tensor(out=L_[:, 2, :], in0=L_[:, 1, :], scalar=-1e30, in1=L_[:, 0, :], op0=ALU.mult, op1=ALU.add), R=[tk2], W=[tk2])
                K.op('dve', lambda e, L_=L_, r4=r4: e.tensor_reduce(out=r4[:, 1:2], in_=L_[:, 2, :], axis=AX.X, op=ALU.max), R=[tk2], W=[tk2])
                K.op('dve', lambda e, L_=L_, r4=r4: e.tensor_scalar(out=L_[:, 3, :], in0=L_[:, 0, :], scalar1=r4[:, 1:2], scalar2=None, op0=ALU.is_ge), R=[tk2], W=[tk2])
                K.op('dve', lambda e, r4=r4: e.tensor_scalar_mul(out=r4[:, 2:3], in0=r4[:, 0:1], scalar1=-1.0), R=[tk2], W=[tk2])
                K.op('act', lambda e, L_=L_, r4=r4: e.activation(out=L_[:, 4, :], in_=L_[:, 0, :], func=AF.Exp, bias=r4[:, 2:3]), R=[tk2], W=[tk2])
                K.op('dve', lambda e, L_=L_: e.tensor_tensor(out=L_[:, 4, :], in0=L_[:, 4, :], in1=L_[:, 3, :], op=ALU.mult), R=[tk2], W=[tk2])
                K.op('dve', lambda e, L_=L_, r4=r4: e.tensor_reduce(out=r4[:, 3:4], in_=L_[:, 4, :], axis=AX.X, op=ALU.add), R=[tk2], W=[tk2])
                K.op('dve', lambda e, r4=r4: e.reciprocal(out=r4[:, 3:4], in_=r4[:, 3:4]), R=[tk2], W=[tk2])
                K.op('dve', lambda e, L_=L_, r4=r4: e.tensor_scalar_mul(out=L_[:, 5, :], in0=L_[:, 4, :], scalar1=r4[:, 3:4]), R=[tk2], W=[tk2])
                K.dma(gates_d[c, :, :], L_[:, 5, :], R=[tk2], W=[t_gates[c]], q='pool')
            K.dma(uT_d[c, :, :, :], uc[b][:], R=[t_uc[b]], W=[t_uT[c]], q='pool')

        stX(0, cl_[0])
        for n, c in enumerate(cl_):
            if n + 1 < len(cl_):
                stX(n + 1, cl_[n + 1])
            stY(n, c)


def const_inputs():
    idx = np.arange(128)
    c = {}
    c['ident'] = np.eye(128, dtype=np.float32)
    c['maskL'] = (idx[:, None] <= idx[None, :]).astype(np.float32)
    c['maskU'] = (idx[:, None] >= idx[None, :]).astype(np.float32)
    c['antiI'] = np.ascontiguousarray(np.eye(128, dtype=np.float32)[::-1])
    i = idx.astype(np.float32)
    c['poscols'] = np.stack([i + 1, 128 - i, -(i + 1), -(128 - i)], axis=1).astype(np.float32)
    tpos = np.arange(SEQ)
    row = (tpos // 64).astype(np.float32)
    colp = (tpos % 64).astype(np.float32)
    n_freq = 16
    inv = (10000.0 ** (-np.arange(n_freq, dtype=np.float32) / n_freq)).astype(np.float32)
    ang = np.concatenate([row[:, None] * inv, colp[:, None] * inv], axis=-1).astype(np.float32)
    for sfx, n in (('L', SEQ), ('C', CTXL)):
        nt = n // 128
        tlin = np.linspace(0.0, 1.0, n, dtype=np.float32)
        ii = np.arange(n, dtype=np.float32)[:, None]
        bands = np.linspace(1e-4, 15, 16, dtype=np.float32)[None, :]
        ang2 = (np.float32(2.0 * math.pi / n) * bands * ii).astype(np.float32)
        zemb = np.concatenate([tlin[:, None], np.cos(ang2), -np.sin(ang2)], axis=-1).astype(np.float32)
        c['hy_zemb' + sfx] = np.ascontiguousarray(zemb.T)
        c['hy_tn' + sfx] = np.ascontiguousarray(tlin.reshape(nt, 128).T)
        N2 = 2 * n
        kk = np.arange((nt + 1) * 128)
        wkv = np.where(kk > n, 0.0, np.where((kk == 0) | (kk == n), 1.0 / N2, 2.0 / N2)).astype(np.float32)
        c['hy_wk' + sfx] = np.ascontiguousarray(wkv.reshape(nt + 1, 128).T)
        a = kk.reshape(nt + 1, 128)
        prod = (a.T[None, :, :, None].astype(np.int64) * a[:, None, None, :].astype(np.int64)) % N2
        lut_c = np.cos(2.0 * np.pi * np.arange(N2) / N2)
        lut_s = np.sin(2.0 * np.pi * np.arange(N2) / N2)
        valid = (a.T[None, :, :, None] <= n) & (a[:, None, None, :] <= n)
        c['dftc' + sfx] = np.where(valid, lut_c[prod], 0.0).astype(ml_dtypes.bfloat16)
        c['dfts' + sfx] = np.where(valid, lut_s[prod], 0.0).astype(ml_dtypes.bfloat16)
    c['rot_cos'] = np.ascontiguousarray(np.cos(ang).astype(np.float32).reshape(32, 128, 32).transpose(1, 0, 2))
    c['rot_sin'] = np.ascontiguousarray(np.sin(ang).astype(np.float32).reshape(32, 128, 32).transpose(1, 0, 2))
    return c


def prep_core_inputs(inp, b, shared):
    m = dict(shared)
    m['xin'] = np.ascontiguousarray(np.concatenate([inp['ctx'][b], inp['x'][b]], axis=0))
    cv = np.stack([inp['c'][b], inp['c_ctx']], axis=-1)
    m['cvec'] = np.ascontiguousarray(cv.reshape(8, 128, 2).transpose(1, 0, 2))
    return m


def prep_shared(inp):
    m = const_inputs()
    m['gla_wa'] = inp['gla_wa']
    m['gla_ba'] = inp['gla_ba']
    m['gla_ng'] = inp['gla_norm_g']
    m['ret_decay'] = np.ascontiguousarray(inp['ret_decay'].reshape(DEPTH, 8))
    rd = inp['ret_decay']
    fm = np.zeros((DEPTH, 2, 128, 2), np.float32)
    for p in range(2):
        for h2 in range(2):
            fm[:, p, h2 * 64:(h2 + 1) * 64, :] = rd[:, :, 2 * p + h2][:, None, :]
    m['ret_dec_fm'] = fm
    m['ret_gng'] = inp['ret_gn_g']
    L = DEPTH
    lam = np.stack([inp['s5_lam_re'], inp['s5_lam_im']], axis=-1)
    lam = lam.reshape(L, 2, 8, 2, 64, 2)
    m['s5_lam_fm'] = np.ascontiguousarray(lam.transpose(0, 3, 4, 1, 2, 5).reshape(L, 128, 16, 2))
    ldt = np.broadcast_to(inp['s5_log_dt'].reshape(L, 2, 8, 2, 1), (L, 2, 8, 2, 64))
    m['s5_dt_fm'] = np.ascontiguousarray(ldt.transpose(0, 3, 4, 1, 2).reshape(L, 128, 16))
    BT = np.zeros((L, 2, 8, 2, 128, 128), np.float32)
    CT = np.zeros((L, 2, 8, 2, 128, 32), np.float32)
    for j in range(8):
        for g2 in range(2):
            gi = 2 * j + g2
            r0 = (gi % 8) * 16
            for ri, (bn, cn) in enumerate((('s5_b_re', 's5_c_re'), ('s5_b_im', 's5_c_im'))):
                BT[:, :, j, ri, r0:r0 + 16, g2 * 64:(g2 + 1) * 64] = inp[bn][:, :, gi].transpose(0, 1, 3, 2)
                CT[:, :, j, ri, g2 * 64:(g2 + 1) * 64, g2 * 16:(g2 + 1) * 16] = inp[cn][:, :, gi].transpose(0, 1, 3, 2)
    m['s5_BT'] = BT.reshape(L, 16, 2, 128, 128)
    m['s5_CT'] = CT.reshape(L, 16, 2, 128, 32)
    m['s5_dcol'] = np.ascontiguousarray(inp['s5_d'].reshape(L, 2, 128).transpose(0, 2, 1))
    m['s5_glub'] = np.ascontiguousarray(inp['s5_glu_b'].reshape(L, 2, 128).transpose(0, 2, 1))
    m['s5_glu_w'] = inp['s5_glu_w']
    m['iota_t'] = np.arange(T, dtype=np.float32)
    m['hy_cw'] = np.ascontiguousarray(inp['hy_conv_w'].reshape(L, 3, 6, 128).transpose(0, 3, 2, 1))
    m['hy_cb'] = np.ascontiguousarray(inp['hy_conv_b'].reshape(L, 6, 128).transpose(0, 2, 1))
    m['hy_biasc'] = np.ascontiguousarray(inp['hy_bias'].reshape(L, 2, 128).transpose(0, 2, 1))
    m['hy_fw1'] = inp['hy_fw1']; m['hy_fw2'] = inp['hy_fw2']; m['hy_fw3'] = inp['hy_fw3']
    fc = np.zeros((L, 64, 5), np.float32)
    fc[:, :, 0] = inp['hy_fb1']; fc[:, :, 1] = inp['hy_fb2']; fc[:, :, 2] = inp['hy_freq']
    m['hy_fcol'] = fc
    m['hy_decay'] = inp['hy_decay']
    m['ret_gnb'] = inp['ret_gn_b']
    m['ada_w'] = inp['ada_w']
    m['ada_b'] = inp['ada_b']
    m['w_out'] = inp['w_out']
    m['ln_g'] = np.ascontiguousarray(np.stack([inp['ln_mix_g'], inp['ln_ffn_g']], axis=1))
    m['ln_b'] = np.ascontiguousarray(np.stack([inp['ln_mix_b'], inp['ln_ffn_b']], axis=1))
    for k_ in ('ffn_w1', 'ffn_w3', 'ffn_w2', 'router_w', 'router_b', 'moe_w1', 'moe_w3', 'moe_w2'):
        m[k_] = inp[k_]
    m['ada_bT'] = np.ascontiguousarray(inp['ada_b'].reshape(DEPTH, 48, 128).transpose(0, 2, 1))
    m['w_in'] = inp['w_in']
    return m


def load_w_bf16(g, st, wsrc_cols, ncols_total, name):
    nc, K = g.nc, g.K
    wb = st.enter_context(nc.sbuf_tensor(_nm(name), [128, 8, ncols_total], BF16))
    t_w = Tok()
    o = 0
    for (src, c0, n) in wsrc_cols:
        if src is None:
            K.op('pool', lambda e, o=o, n=n: e.memset(wb[:, :, o:o + n], 0.0), W=[t_w])
            o += n
            continue
        view = src.rearrange("(kt p) n -> p kt n", p=128)
        done = 0
        while done < n:
            m = min(512, n - done)
            K.dma(wb[:, :, o:o + m], view[:, :, c0 + done:c0 + done + m], W=[t_w], q='pool')
            o += m
            done += m
    return wb, t_w


def la_pass(g, l, kind, p, last):
    nc, K, D = g.nc, g.K, g.D
    gla = kind == 'gla'
    H = 2
    dk = 64
    NV = H * 64
    row0 = (256 if gla else 0) + p * 128
    with ExitStack() as st:
        def sb(name, shape, dt=F32):
            return st.enter_context(nc.sbuf_tensor(_nm(name), list(shape), dt))
        W = D['w_in'][l]
        if gla:
            cols = []
            for nm_ in ('gla_q', 'gla_k'):
                for h2 in range(2):
                    cols.append((W, OFF[nm_] + (2 * p + h2) * 32, 32))
                    cols.append((None, 0, 32))
            cols += [(W, OFF['gla_v'] + 128 * p, 128), (W, OFF['gla_r'] + 128 * p, 128), (W, OFF['gla_a'], 32)]
            ncols = 544
        else:
            cols = [(W, OFF['ret_q'] + 128 * p, 128), (W, OFF['ret_k'] + 128 * p, 128),
                    (W, OFF['ret_v'] + 128 * p, 128), (W, OFF['ret_g'] + 128 * p, 128)]
            ncols = 512
        wb, t_w = load_w_bf16(g, st, cols, ncols, 'w_la')
        LT = sb('LT', [128, 4, T], BF16)
        t_LT = [Tok() for _ in range(NCH)]
        vtok = sb('vtok', [128, NCH, NV], BF16); t_v = [Tok() for _ in range(NCH)]
        sgtok = sb('sgtok', [128, NCH, NV], BF16); t_sg = [Tok() for _ in range(NCH)]
        KVGb = sb('KVGb', [128, NCH, 64]); t_kvb = [Tok() for _ in range(NCH)]
        Sbf = sb('Sbf', [128, NCH, 2, 64], BF16); t_S = [[Tok(), Tok()] for _ in range(NCH)]
        Sf = sb('Sf', [128, 64]); Sb = sb('Sb', [128, 64]); t_Sf = Tok(); t_Sb = Tok()
        Gall = sb('Gall', [128, NCH, 2]); t_G = [Tok() for _ in range(NCH)]
        t_par = Tok()
        if gla:
            wa = sb('wa', [16, 2, 128]); ba = sb('ba', [1, 2, 128])
            K.op('pool', lambda e: e.memset(wa[:], 0.0), W=[t_par])
            K.op('pool', lambda e: e.memset(ba[:], 0.0), W=[t_par])
            for h2 in range(2):
                hh = (2 * p + h2) * 32
                K.dma(wa[:, :, h2 * 64:h2 * 64 + 32], D['gla_wa'][l, :, :, hh:hh + 32].rearrange("d r n -> r d n"), W=[t_par])
                K.dma(ba[:, :, h2 * 64:h2 * 64 + 32], D['gla_ba'][l:l + 1, :, hh:hh + 32], W=[t_par])
            ngb = sb('ngb', [128, NV])
            K.dma(ngb[:], D['gla_ng'][l, p * 128:(p + 1) * 128].partition_broadcast(128), W=[t_par])
        else:
            gng = sb('gng', [128, NV]); gnb = sb('gnb', [128, NV])
            K.dma(gng[:], D['ret_gng'][l, p * 128:(p + 1) * 128].partition_broadcast(128), W=[t_par])
            K.dma(gnb[:], D['ret_gnb'][l, p * 128:(p + 1) * 128].partition_broadcast(128), W=[t_par])
            dtm = sb('dtm', [128, 2, 4]); dfm = sb('dfm', [128, 2])
            K.dma(dtm[:].rearrange("p a b -> p (a b)"), D['ret_decay'][l].partition_broadcast(128), W=[t_par])
            K.dma(dfm[:], D['ret_dec_fm'][l, p, :, :], W=[t_par])
            K.op('act', lambda e: e.activation(out=dtm[:], in_=dtm[:], func=AF.Exp, scale=-1.0), R=[t_par], W=[t_par])
            K.op('act', lambda e: e.activation(out=dtm[:], in_=dtm[:], func=AF.Ln, bias=1.0), R=[t_par], W=[t_par])
            K.op('act', lambda e: e.activation(out=dfm[:], in_=dfm[:], func=AF.Exp, scale=-1.0), R=[t_par], W=[t_par])
            K.op('act', lambda e: e.activation(out=dfm[:], in_=dfm[:], func=AF.Ln, bias=1.0), R=[t_par], W=[t_par])
            argt = sb('argt', [128, 2, 2])
            K.op('dve', lambda e: e.tensor_scalar_mul(out=argt[:, 0, :], in0=dtm[:, 0, 2 * p:2 * p + 2], scalar1=g.poscols[:, 2:3]),
                 R=[t_par, g.t_const], W=[t_par])
            K.op('dve', lambda e: e.tensor_scalar_mul(out=argt[:, 1, :], in0=dtm[:, 1, 2 * p:2 * p + 2], scalar1=g.poscols[:, 3:4]),
                 R=[t_par, g.t_const], W=[t_par])
            EpR = sb('EpR', [128, 2, 2]); EmR = sb('EmR', [128, 2, 2])
            K.op('act', lambda e: e.activation(out=EpR[:], in_=argt[:], func=AF.Exp), R=[t_par], W=[t_par])
            K.op('act', lambda e: e.activation(out=EmR[:], in_=argt[:], func=AF.Exp, scale=-1.0, bias=g.lnc[:, 0:1]), R=[t_par, g.t_const], W=[t_par])
            Gret = sb('Gret', [128, 2])
            K.op('act', lambda e: e.activation(out=Gret[:], in_=dfm[:], func=AF.Exp, scale=-128.0), R=[t_par], W=[t_par])
            cosT = sb('cosT', [128, 32, 32]); sinT = sb('sinT', [128, 32, 32])
            K.dma(cosT[:], D['rot_cos'][:, :, :], W=[t_par])
            K.dma(sinT[:], D['rot_sin'][:, :, :], W=[t_par])

        def Gap(c, d):
            return (Gall[:, c, d:d + 1], t_G[c]) if gla else (Gret[:, d:d + 1], t_par)

        def dbl(name, shape, dt=F32):
            return [sb(name + str(i), shape, dt) for i in range(2)], [Tok(), Tok()]
        uc, t_uc = dbl('ucl', [128, 8, 128], BF16)
        qk, t_qk = dbl('qk', [128, 256])
        rt, t_rt = dbl('rt', [128, 4, 4, 32])
        a_sb, t_a = dbl('a_sb', [128, 32])
        aT, t_aT = dbl('aT', [16, 2, 128])
        sp, t_sp = dbl('sp', [128, 256])
        Ep, t_Ep = dbl('Ep', [128, 256]); Em, t_Em = dbl('Em', [128, 256])
        qkt, t_qkt = dbl('qkt', [128, 4, 128], BF16)
        K.op('pool', lambda e: e.memset(Sf[:], 0.0), W=[t_Sf])
        K.op('pool', lambda e: e.memset(Sb[:], 0.0), W=[t_Sb])

        _dbg = os.environ.get('LA_DEBUG', '')
        _n1 = int(_dbg.split(',')[0]) if _dbg else NCH
        _p2 = int(_dbg.split(',')[1]) if _dbg else 1
        _lvl = int(_dbg.split(',')[2]) if _dbg else 99
        def stA(c):
            b = c % 2
            K.dma(uc[b][:], D['uT_d'][c, :, :, :], R=[g.t_uT[c]], W=[t_uc[b]])
            pa, pat = g.ps()
            for kt in range(8):
                K.op('pe', lambda e, kt=kt, b=b, pa=pa: e.matmul(pa[:, 0:512], lhsT=uc[b][:, kt, :], rhs=wb[:, kt, 0:512],
                                                                  start=(kt == 0), stop=(kt == 7)), R=[t_uc[b], t_w], W=[pat])
            if gla:
                pb, pbt = g.ps()
                for kt in range(8):
                    K.op('pe', lambda e, kt=kt, b=b, pb=pb: e.matmul(pb[:, 0:32], lhsT=uc[b][:, kt, :], rhs=wb[:, kt, 512:544],
                                                                      start=(kt == 0), stop=(kt == 7)), R=[t_uc[b], t_w], W=[pbt])
            K.op('act', lambda e, b=b, pa=pa: e.activation(out=qk[b][:], in_=pa[:, 0:256], func=AF.Identity), R=[pat], W=[t_qk[b]])
            K.op('act', lambda e, c=c, pa=pa: e.activation(out=vtok[:, c, :], in_=pa[:, 256:384], func=AF.Identity), R=[pat], W=[t_v[c]])
            K.op('act', lambda e, c=c, pa=pa: e.activation(out=sgtok[:, c, :], in_=pa[:, 384:512], func=AF.Silu), R=[pat], W=[t_sg[c]])
            if gla:
                K.op('dve', lambda e, b=b, pb=pb: e.tensor_copy(out=a_sb[b][:], in_=pb[:, 0:32]), R=[pbt], W=[t_a[b]])

        def stB(c):
            b = c % 2
            if gla:
                pt, ptk = g.ps()
                for d in range(2):
                    K.op('pe', lambda e, d=d, b=b, pt=pt: e.transpose(out=pt[0:16, d * 128:(d + 1) * 128], in_=a_sb[b][:, d * 16:(d + 1) * 16],
                                                                       identity=g.ident[:]), R=[t_a[b], g.t_const], W=[ptk])
                K.op('dve', lambda e, b=b, pt=pt: e.tensor_copy(out=aT[b][:].rearrange("r d n -> r (d n)"), in_=pt[0:16, 0:256]), R=[ptk], W=[t_aT[b]])
                pg, pgt = g.ps()
                for d in range(2):
                    K.op('pe', lambda e, d=d, b=b, pg=pg: e.matmul(pg[:, d * 128:(d + 1) * 128], lhsT=aT[b][:, d, :], rhs=wa[:, d, :],
                                                                    start=True, stop=False), R=[t_aT[b], t_par], W=[pgt])
                    K.op('pe', lambda e, d=d, pg=pg: e.matmul(pg[:, d * 128:(d + 1) * 128], lhsT=g.ones[0:1, :], rhs=ba[:, d, :],
                                                               start=False, stop=True), R=[t_par, g.t_const], W=[pgt])
                K.op('act', lambda e, b=b, pg=pg: e.activation(out=sp[b][:], in_=pg[:, 0:256], func=AF.Exp, scale=-1.0), R=[pgt], W=[t_sp[b]])
                K.op('act', lambda e, b=b: e.activation(out=sp[b][:], in_=sp[b][:], func=AF.Ln, bias=1.0), R=[t_sp[b]], W=[t_sp[b]])
                pc, pct = g.ps()
                K.op('pe', lambda e, b=b, pc=pc: e.matmul(pc[:, 0:128], lhsT=g.maskL[:], rhs=sp[b][:, 0:128], start=True, stop=True),
                     R=[t_sp[b], g.t_const], W=[pct])
                K.op('pe', lambda e, b=b, pc=pc: e.matmul(pc[:, 128:256], lhsT=g.maskU[:], rhs=sp[b][:, 128:256], start=True, stop=True),
                     R=[t_sp[b], g.t_const], W=[pct])
                K.op('pe', lambda e, b=b, pc=pc: e.matmul(pc[:, 256:257], lhsT=sp[b][:, 0:128], rhs=g.ones[:, 0:1], start=True, stop=True),
                     R=[t_sp[b], g.t_const], W=[pct])
                K.op('pe', lambda e, b=b, pc=pc: e.matmul(pc[:, 257:258], lhsT=sp[b][:, 128:256], rhs=g.ones[:, 0:1], start=True, stop=True),
                     R=[t_sp[b], g.t_const], W=[pct])
                K.op('act', lambda e, b=b, pc=pc: e.activation(out=Ep[b][:], in_=pc[:, 0:256], func=AF.Exp, scale=-1.0 / 16, bias=g.lnc[:, 1:2]),
                     R=[pct, g.t_const], W=[t_Ep[b]])
                K.op('act', lambda e, b=b, pc=pc: e.activation(out=Em[b][:], in_=pc[:, 0:256], func=AF.Exp, scale=1.0 / 16), R=[pct], W=[t_Em[b]])
                K.op('act', lambda e, c=c, pc=pc: e.activation(out=Gall[:, c, :], in_=pc[:, 256:258], func=AF.Exp, scale=-1.0 / 16), R=[pct], W=[t_G[c]])
                K.op('dve', lambda e, b=b: e.tensor_tensor(out=qkt[b][:, 0:2, :], in0=qk[b][:, 0:128].unsqueeze(1).to_broadcast([128, 2, 128]),
                                                            in1=Ep[b][:].rearrange("p (d n) -> p d n", d=2), op=ALU.mult),
                     R=[t_qk[b], t_Ep[b]], W=[t_qkt[b]])
                K.op('dve', lambda e, b=b: e.tensor_tensor(out=qkt[b][:, 2:4, :], in0=qk[b][:, 128:256].unsqueeze(1).to_broadcast([128, 2, 128]),
                                                            in1=Em[b][:].rearrange("p (d n) -> p d n", d=2), op=ALU.mult),
                     R=[t_qk[b], t_Em[b]], W=[t_qkt[b]])
            else:
                if c >= 2:
                    v4 = qk[b][:].rearrange("p (a s f) -> p a s f", a=4, s=2)
                    t1, t2 = v4[:, :, 0, :], v4[:, :, 1, :]
                    cs = cosT[:, c - 2, :].unsqueeze(1).to_broadcast([128, 4, 32])
                    sn = sinT[:, c - 2, :].unsqueeze(1).to_broadcast([128, 4, 32])
                    r = rt[b]
                    K.op('dve', lambda e, r=r, t1=t1, cs=cs: e.tensor_tensor(out=r[:, 0], in0=t1, in1=cs, op=ALU.mult), R=[t_qk[b], t_par], W=[t_rt[b]])
                    K.op('pool', lambda e, r=r, t2=t2, sn=sn: e.tensor_tensor(out=r[:, 1], in0=t2, in1=sn, op=ALU.mult), R=[t_qk[b], t_par], W=[t_rt[b]])
                    K.op('dve', lambda e, r=r, t1=t1, sn=sn: e.tensor_tensor(out=r[:, 2], in0=t1, in1=sn, op=ALU.mult), R=[t_qk[b], t_par], W=[t_rt[b]])
                    K.op('pool', lambda e, r=r, t2=t2, cs=cs: e.tensor_tensor(out=r[:, 3], in0=t2, in1=cs, op=ALU.mult), R=[t_qk[b], t_par], W=[t_rt[b]])
                    K.op('dve', lambda e, r=r, t1=t1: e.tensor_tensor(out=t1, in0=r[:, 0], in1=r[:, 1], op=ALU.subtract), R=[t_rt[b]], W=[t_qk[b]])
                    K.op('dve', lambda e, r=r, t2=t2: e.tensor_tensor(out=t2, in0=r[:, 2], in1=r[:, 3], op=ALU.add), R=[t_rt[b]], W=[t_qk[b]])
                K.op('dve', lambda e, b=b: e.tensor_tensor(
                    out=qkt[b][:, 0:2, :].rearrange("p d (h f) -> p d h f", h=2),
                    in0=qk[b][:, 0:128].rearrange("p (h f) -> p h f", h=2).unsqueeze(1).to_broadcast([128, 2, 2, 64]),
                    in1=EpR[:].unsqueeze(3).to_broadcast([128, 2, 2, 64]), op=ALU.mult), R=[t_qk[b], t_par], W=[t_qkt[b]])
                K.op('dve', lambda e, b=b: e.tensor_tensor(
                    out=qkt[b][:, 2:4, :].rearrange("p d (h f) -> p d h f", h=2),
                    in0=qk[b][:, 128:256].rearrange("p (h f) -> p h f", h=2).unsqueeze(1).to_broadcast([128, 2, 2, 64]),
                    in1=EmR[:].unsqueeze(3).to_broadcast([128, 2, 2, 64]), op=ALU.mult), R=[t_qk[b], t_par], W=[t_qkt[b]])

        def stC(c):
            b = c % 2
            pbh, pbht = g.psb_half()
            for s_ in range(4):
                K.op('pe', lambda e, s_=s_, b=b, pbh=pbh: e.transpose(out=pbh[:, s_ * 128:(s_ + 1) * 128], in_=qkt[b][:, s_, :], identity=g.identb[:]),
                     R=[t_qkt[b], g.t_const], W=[pbht])
            K.op('act', lambda e, c=c, pbh=pbh: e.activation(out=LT[:, :, c * 128:(c + 1) * 128], in_=pbh.rearrange("p (s n) -> p s n", s=4),
                                                              func=AF.Identity), R=[pbht], W=[t_LT[c]])
            pk, pkt = g.ps()
            for d in range(2):
                for h in range(H):
                    K.op('pe', lambda e, d=d, h=h, b=b, c=c, pk=pk: e.matmul(
                        pk[h * dk:(h + 1) * dk, d * 64:(d + 1) * 64], lhsT=qkt[b][:, 2 + d, h * dk:(h + 1) * dk],
                        rhs=vtok[:, c, h * 64:(h + 1) * 64], start=True, stop=True), R=[t_qkt[b], t_v[c]], W=[pkt])
            Gf, tGf = Gap(c, 0)
            Gb, tGb = Gap(c, 1)
            K.op('dve', lambda e, c=c: e.tensor_copy(out=Sbf[:, c, 0, :], in_=Sf[:]), R=[t_Sf], W=[t_S[c][0]])
            K.op('dve', lambda e, pk=pk: e.tensor_tensor(out=Sf[:], in0=Sf[:], in1=pk[:, 0:64], op=ALU.add), R=[pkt, t_Sf], W=[t_Sf])
            K.op('dve', lambda e, Gf=Gf: e.tensor_scalar_mul(out=Sf[:], in0=Sf[:], scalar1=Gf), R=[t_Sf, tGf], W=[t_Sf])
            K.op('dve', lambda e, c=c, pk=pk, Gb=Gb: e.tensor_scalar_mul(out=KVGb[:, c, :], in0=pk[:, 64:128], scalar1=Gb),
                 R=[pkt, tGb], W=[t_kvb[c]])

        stA(0)
        for c in range(NCH):
            if c + 1 < NCH:
                stA(c + 1)
            stB(c)
            stC(c)
        for c in [1, 0] + list(range(NCH - 1, 1, -1)):
            Gb, tGb = Gap(c, 1)
            K.op('dve', lambda e, c=c: e.tensor_copy(out=Sbf[:, c, 1, :], in_=Sb[:]), R=[t_Sb], W=[t_S[c][1]])
            K.op('dve', lambda e, c=c, Gb=Gb: e.scalar_tensor_tensor(out=Sb[:], in0=Sb[:], scalar=Gb, in1=KVGb[:, c, :], op0=ALU.mult, op1=ALU.add),
                 R=[t_Sb, tGb, t_kvb[c]], W=[t_Sb])

        att, t_att = dbl('att', [128, 2, 2, H, 128], BF16)
        osb, t_o = dbl('osb', [128, 2, H, 64])
        sq, t_sq = dbl('sq', [128, 2, H, 64])
        stat, t_st = dbl('stat', [128, 4, 2 * H])
        yb, t_yb = dbl('yb', [128, 2, NV], BF16)
        stg, t_stg = dbl('stg', [128, 2, 128], BF16)
        H2 = 2 * H

        def stP(pi, c0_):
            b = pi % 2
            for ci_ in range(2):
                c = c0_ + ci_
                cs = slice(c * 128, (c + 1) * 128)
                pos = []
                for h in range(H):
                    hr = slice(h * dk, (h + 1) * dk)
                    pz, pzt = g.ps()
                    for d in range(2):
                        K.op('pe', lambda e, d=d, hr=hr, pz=pz, cs=cs: e.matmul(
                            pz[:, d * 128:(d + 1) * 128], lhsT=LT[hr, 2 + d, cs], rhs=LT[hr, d, cs], start=True, stop=True),
                            R=[t_LT[c]], W=[pzt])
                    K.op('dve', lambda e, h=h, b=b, pz=pz, ci_=ci_: e.tensor_tensor(
                        out=att[b][:, ci_, :, h, :], in0=pz[:, 0:256].rearrange("p (d n) -> p d n", d=2),
                        in1=g.mask2[:], op=ALU.mult), R=[pzt, g.t_const], W=[t_att[b]])
                for h in range(H):
                    hr = slice(h * dk, (h + 1) * dk)
                    oc = slice(h * 64, (h + 1) * 64)
                    po, pot = g.ps()
                    K.op('pe', lambda e, h=h, b=b, oc=oc, po=po, c=c, ci_=ci_: e.matmul(po[:, 0:64], lhsT=att[b][:, ci_, 0, h, :], rhs=vtok[:, c, oc], start=True, stop=False),
                         R=[t_att[b], t_v[c]], W=[pot])
                    K.op('pe', lambda e, h=h, b=b, oc=oc, po=po, c=c, ci_=ci_: e.matmul(po[:, 0:64], lhsT=att[b][:, ci_, 1, h, :], rhs=vtok[:, c, oc], start=False, stop=False),
                         R=[t_att[b], t_v[c]], W=[pot])
                    K.op('pe', lambda e, hr=hr, po=po, c=c, cs=cs: e.matmul(po[:, 0:64], lhsT=LT[hr, 0, cs], rhs=Sbf[hr, c, 0, :], start=False, stop=False),
                         R=[t_LT[c], t_S[c][0]], W=[pot])
                    K.op('pe', lambda e, hr=hr, po=po, c=c, cs=cs: e.matmul(po[:, 0:64], lhsT=LT[hr, 1, cs], rhs=Sbf[hr, c, 1, :], start=False, stop=True),
                         R=[t_LT[c], t_S[c][1]], W=[pot])
                    K.op('act', lambda e, po=po, b=b, h=h, ci_=ci_: e.activation(out=osb[b][:, ci_, h, :], in_=po[:, 0:64], func=AF.Identity), R=[pot], W=[t_o[b]])

        def stQ(pi, c0_):
            b = pi % 2
            o3 = osb[b][:].rearrange("p c h f -> p (c h) f")
            s4 = stat[b]
            sq3 = sq[b][:].rearrange("p c h f -> p (c h) f")
            if not gla:
                K.op('dve', lambda e: e.tensor_reduce(out=s4[:, 0, :], in_=o3, axis=AX.X, op=ALU.add), R=[t_o[b]], W=[t_st[b]])
                K.op('dve', lambda e: e.tensor_scalar_mul(out=s4[:, 0, :], in0=s4[:, 0, :], scalar1=1.0 / 64), R=[t_st[b]], W=[t_st[b]])
                K.op('dve', lambda e: e.tensor_tensor(out=o3, in0=o3, in1=s4[:, 0, :].unsqueeze(2).to_broadcast([128, H2, 64]), op=ALU.subtract),
                     R=[t_o[b], t_st[b]], W=[t_o[b]])
            K.op('pool', lambda e: e.tensor_tensor(out=sq3, in0=o3, in1=o3, op=ALU.mult), R=[t_o[b]], W=[t_sq[b]])
            K.op('dve', lambda e: e.tensor_reduce(out=s4[:, 1, :], in_=sq3, axis=AX.X, op=ALU.add), R=[t_sq[b]], W=[t_st[b]])
            K.op('act', lambda e: e.activation(out=s4[:, 2, :], in_=s4[:, 1, :], func=AF.Ln, scale=1.0 / 64, bias=g.epsc[:, 0:1]), R=[t_st[b], g.t_const], W=[t_st[b]])
            K.op('act', lambda e: e.activation(out=s4[:, 3, :], in_=s4[:, 2, :], func=AF.Exp, scale=-0.5), R=[t_st[b]], W=[t_st[b]])
            K.op('dve', lambda e: e.tensor_tensor(out=o3, in0=o3, in1=s4[:, 3, :].unsqueeze(2).to_broadcast([128, H2, 64]), op=ALU.mult),
                 R=[t_o[b], t_st[b]], W=[t_o[b]])
            o2 = osb[b][:].rearrange("p c h f -> p c (h f)")
            if gla:
                K.op('pool', lambda e: e.tensor_tensor(out=o2, in0=o2, in1=ngb[:].unsqueeze(1).to_broadcast([128, 2, NV]), op=ALU.mult), R=[t_o[b], t_par], W=[t_o[b]])
            else:
                K.op('pool', lambda e: e.tensor_tensor(out=o2, in0=o2, in1=gng[:].unsqueeze(1).to_broadcast([128, 2, NV]), op=ALU.mult), R=[t_o[b], t_par], W=[t_o[b]])
                K.op('pool', lambda e: e.tensor_tensor(out=o2, in0=o2, in1=gnb[:].unsqueeze(1).to_broadcast([128, 2, NV]), op=ALU.add), R=[t_o[b], t_par], W=[t_o[b]])
            K.op('dve', lambda e: e.tensor_tensor(out=yb[b][:], in0=o2, in1=sgtok[:, c0_:c0_ + 2, :], op=ALU.mult), R=[t_o[b], t_sg[c0_], t_sg[c0_ + 1]], W=[t_yb[b]])
            pbh, pbht = g.psb_half()
            for ci_ in range(2):
                K.op('pe', lambda e, ci_=ci_, pbh=pbh: e.transpose(out=pbh[:, ci_ * 128:(ci_ + 1) * 128], in_=yb[b][:, ci_, :], identity=g.identb[:]),
                     R=[t_yb[b], g.t_const], W=[pbht])
            K.op('act', lambda e, pbh=pbh: e.activation(out=stg[b][:].rearrange("p c n -> p (c n)"), in_=pbh[:, 0:256], func=AF.Identity), R=[pbht], W=[t_stg[b]])
            K.dma(D['mixT_d'][row0:row0 + NV, c0_ * 128:(c0_ + 2) * 128], stg[b][:].rearrange("p c n -> p (c n)"), R=[t_stg[b]], W=[g.t_mix], q='pool')

        pairs = list(range(2, NCH, 2) if last else range(0, NCH, 2))
        stP(0, pairs[0])
        for pi, c0_ in enumerate(pairs):
            if pi + 1 < len(pairs):
                stP(pi + 1, pairs[pi + 1])
            stQ(pi, c0_)


TWO_PI = 2.0 * math.pi


def phase_s5(g, l):
    nc, K, D = g.nc, g.K, g.D
    NP = (T + 511) // 512
    pieces = [(i * 512, min(512, T - i * 512)) for i in range(NP)]
    with ExitStack() as st:
        def sb(name, shape, dt=F32):
            return st.enter_context(nc.sbuf_tensor(_nm(name), list(shape), dt))
        W = D['w_in'][l]
        wb, t_w = load_w_bf16(g, st, [(W, OFF['s5_u'], 256)], 256, 'w_s5')
        t_par = Tok()
        lam = sb('lam', [128, 16, 2]); dtc = sb('dtc', [128, 16])
        K.dma(lam[:], D['s5_lam_fm'][l, :, :, :], W=[t_par])
        K.dma(dtc[:], D['s5_dt_fm'][l, :, :], W=[t_par])
        CT = sb('CT', [128, 16, 2, 32])
        K.dma(CT[:], D['s5_CT'][l].rearrange("t r k m -> k t r m"), W=[t_par])
        dcol = sb('dcol', [128, 2]); glub = sb('glub', [128, 2])
        K.dma(dcol[:], D['s5_dcol'][l, :, :], W=[t_par])
        K.dma(glub[:], D['s5_glub'][l, :, :], W=[t_par])
        gw32 = sb('gw32', [128, 2, 256]); gwb = sb('gwb', [128, 2, 256], BF16)
        K.dma(gw32[:], D['s5_glu_w'][l].rearrange("(kt p) n -> p kt n", p=128), W=[t_par])
        K.op('dve', lambda e: e.tensor_copy(out=gwb[:], in_=gw32[:]), R=[t_par], W=[t_par])
        P_ = {}
        for nm_ in ('dt', 'a', 'th', 'r', 'u', 'ui', 'fr', 'sn', 'u2', 'ui2', 'fr2', 'cs', 'x', 'y', 'den', 'rden', 't1', 't2', 'cr', 'ci', 'ncr', 'thn'):
            P_[nm_] = sb('s5p_' + nm_, [128, 16], I32 if nm_ in ('ui', 'ui2') else F32)

        def dv(fn, rd=True):
            K.op('dve', fn, R=[t_par, g.t_const], W=[t_par])

        def ac(fn):
            K.op('act', fn, R=[t_par, g.t_const], W=[t_par])
        lre, lim = lam[:, :, 0], lam[:, :, 1]
        ac(lambda e: e.activation(out=P_['dt'][:], in_=dtc[:], func=AF.Exp))
        dv(lambda e: e.tensor_tensor(out=P_['a'][:], in0=lre, in1=P_['dt'][:], op=ALU.mult))
        dv(lambda e: e.tensor_tensor(out=P_['th'][:], in0=lim, in1=P_['dt'][:], op=ALU.mult))
        ac(lambda e: e.activation(out=P_['r'][:], in_=P_['a'][:], func=AF.Exp))
        dv(lambda e: e.tensor_scalar_mul(out=P_['u'][:], in0=P_['th'][:], scalar1=1.0 / TWO_PI))
        dv(lambda e: e.tensor_copy(out=P_['ui'][:], in_=P_['u'][:]))
        dv(lambda e: e.tensor_tensor(out=P_['fr'][:], in0=P_['u'][:], in1=P_['ui'][:], op=ALU.subtract))
        ac(lambda e: e.activation(out=P_['sn'][:], in_=P_['fr'][:], func=AF.Sin, scale=TWO_PI))
        dv(lambda e: e.tensor_scalar_add(out=P_['u2'][:], in0=P_['u'][:], scalar1=0.25))
        dv(lambda e: e.tensor_copy(out=P_['ui2'][:], in_=P_['u2'][:]))
        dv(lambda e: e.tensor_tensor(out=P_['fr2'][:], in0=P_['u2'][:], in1=P_['ui2'][:], op=ALU.subtract))
        ac(lambda e: e.activation(out=P_['cs'][:], in_=P_['fr2'][:], func=AF.Sin, scale=TWO_PI))
        dv(lambda e: e.tensor_tensor(out=P_['x'][:], in0=P_['r'][:], in1=P_['cs'][:], op=ALU.mult))
        dv(lambda e: e.tensor_scalar_add(out=P_['x'][:], in0=P_['x'][:], scalar1=-1.0))
        dv(lambda e: e.tensor_tensor(out=P_['y'][:], in0=P_['r'][:], in1=P_['sn'][:], op=ALU.mult))
        dv(lambda e: e.tensor_tensor(out=P_['t1'][:], in0=lre, in1=lre, op=ALU.mult))
        dv(lambda e: e.tensor_tensor(out=P_['t2'][:], in0=lim, in1=lim, op=ALU.mult))
        dv(lambda e: e.tensor_tensor(out=P_['den'][:], in0=P_['t1'][:], in1=P_['t2'][:], op=ALU.add))
        dv(lambda e: e.reciprocal(out=P_['rden'][:], in_=P_['den'][:]))
        dv(lambda e: e.tensor_tensor(out=P_['t1'][:], in0=P_['x'][:], in1=lre, op=ALU.mult))
        dv(lambda e: e.tensor_tensor(out=P_['t2'][:], in0=P_['y'][:], in1=lim, op=ALU.mult))
        dv(lambda e: e.tensor_tensor(out=P_['cr'][:], in0=P_['t1'][:], in1=P_['t2'][:], op=ALU.add))
        dv(lambda e: e.tensor_tensor(out=P_['cr'][:], in0=P_['cr'][:], in1=P_['rden'][:], op=ALU.mult))
        dv(lambda e: e.tensor_tensor(out=P_['t1'][:], in0=P_['y'][:], in1=lre, op=ALU.mult))
        dv(lambda e: e.tensor_tensor(out=P_['t2'][:], in0=P_['x'][:], in1=lim, op=ALU.mult))
        dv(lambda e: e.tensor_tensor(out=P_['ci'][:], in0=P_['t1'][:], in1=P_['t2'][:], op=ALU.subtract))
        dv(lambda e: e.tensor_tensor(out=P_['ci'][:], in0=P_['ci'][:], in1=P_['rden'][:], op=ALU.mult))
        dv(lambda e: e.tensor_scalar_mul(out=P_['thn'][:], in0=P_['th'][:], scalar1=1.0 / TWO_PI))
        Ce = sb('Ce', [128, 16, 2, 32]); Ceb = sb('Ceb', [128, 16, 2, 32], BF16); tmpC = sb('tmpC', [128, 16, 32])
        crb = P_['cr'][:].unsqueeze(2).to_broadcast([128, 16, 32])
        cib = P_['ci'][:].unsqueeze(2).to_broadcast([128, 16, 32])
        dv(lambda e: e.tensor_tensor(out=Ce[:, :, 0, :], in0=CT[:, :, 0, :], in1=crb, op=ALU.mult))
        dv(lambda e: e.tensor_tensor(out=tmpC[:], in0=CT[:, :, 1, :], in1=cib, op=ALU.mult))
        dv(lambda e: e.tensor_tensor(out=Ce[:, :, 0, :], in0=Ce[:, :, 0, :], in1=tmpC[:], op=ALU.subtract))
        dv(lambda e: e.tensor_tensor(out=Ce[:, :, 1, :], in0=CT[:, :, 0, :], in1=cib, op=ALU.mult))
        dv(lambda e: e.tensor_tensor(out=tmpC[:], in0=CT[:, :, 1, :], in1=crb, op=ALU.mult))
        dv(lambda e: e.tensor_tensor(out=Ce[:, :, 1, :], in0=Ce[:, :, 1, :], in1=tmpC[:], op=ALU.add))
        dv(lambda e: e.tensor_scalar_mul(out=Ce[:, :, 1, :], in0=Ce[:, :, 1, :], scalar1=-1.0))
        dv(lambda e: e.tensor_copy(out=Ceb[:], in_=Ce[:]))

        uTf = sb('uTf', [128, 2, T], BF16); t_uf = Tok()
        st2 = ExitStack()

        def sb2(name, shape, dt=F32):
            return st2.enter_context(nc.sbuf_tensor(_nm(name), list(shape), dt))
        ucs = [sb('ucs%d' % i, [128, 8, 128], BF16) for i in range(2)]; t_ucs = [Tok(), Tok()]
        yT = sb('yT', [128, 2, T], BF16); t_y = Tok()
        uTb = sb2('uTb', [128, 1, T], BF16); t_ub = Tok()
        for c4 in range(0, NCH, 4):
            cc = list(range(c4, min(c4 + 4, NCH)))
            pts = [g.ps(), g.ps()]
            for ci_, c in enumerate(cc):
                b = c % 2
                K.dma(ucs[b][:], D['uT_d'][c, :, :, :], R=[g.t_uT[c]], W=[t_ucs[b]])
                for ft in range(2):
                    pt, ptk = pts[ft]
                    for kt in range(8):
                        K.op('pe', lambda e, kt=kt, b=b, ft=ft, pt=pt, ci_=ci_: e.matmul(
                            pt[:, ci_ * 128:(ci_ + 1) * 128], lhsT=wb[:, kt, ft * 128:(ft + 1) * 128], rhs=ucs[b][:, kt, :],
                            start=(kt == 0), stop=(kt == 7)), R=[t_w, t_ucs[b]], W=[ptk])
            n = len(cc) * 128
            for ft in range(2):
                pt, ptk = pts[ft]
                K.op('act', lambda e, ft=ft, pt=pt, c4=c4, n=n: e.activation(out=uTf[:, ft, c4 * 128:c4 * 128 + n], in_=pt[:, 0:n], func=AF.Identity),
                     R=[ptk], W=[t_uf])

        iot = sb2('iot', [128, 544])
        K.dma(iot[:], D['iota_t'][0:544].partition_broadcast(128), W=[t_par])
        offc = sb2('offc', [128, 16, 8, 2])
        for q4 in range(8):
            for which, off in ((0, 0.0), (1, 0.25)):
                dv(lambda e, q4=q4, which=which, off=off: e.tensor_scalar(out=offc[:, :, q4, which], in0=P_['thn'][:], scalar1=float(q4 * 544), scalar2=off,
                                                                         op0=ALU.mult, op1=ALU.add))
        cosT = sb2('s5cos', [128, T]); sinT = sb2('s5sin', [128, T]); t_tab = Tok()
        uus = [sb2('s5uu%d' % i, [128, 544]) for i in range(2)]; uis = [sb2('s5ui%d' % i, [128, 544], I32) for i in range(2)]; t_uus = [Tok(), Tok()]
        _tc = [0]
        cre = sb2('s5cre', [128, T]); cim = sb2('s5cim', [128, T]); t_c = Tok()
        hh = sb2('s5h', [128, 2, 2, T], BF16); t_h = [Tok(), Tok()]
        BT32 = sb2('BT32', [128, 2, 128]); BTb = sb2('BTb', [128, 2, 128], BF16); t_B = Tok()
        m1a = [sb2('s5m%d' % i, [128, 512]) for i in range(8)]; t_ma = [Tok() for _ in range(8)]
        _pc = [0]
        for j in range(8):
            ft = j // 4
            if j % 4 == 0:
                K.op('pool', lambda e, ft=ft: e.tensor_copy(out=uTb[:, 0, 0:CTXL], in_=uTf[:, ft, CTXL - 1::-1]), R=[t_uf], W=[t_ub])
                K.op('pool', lambda e, ft=ft: e.tensor_copy(out=uTb[:, 0, CTXL:T], in_=uTf[:, ft, T - 1:CTXL - 1:-1]), R=[t_uf], W=[t_ub])
            for d in range(2):
                col = d * 8 + j
                usrc, t_us, uft = (uTf, t_uf, ft) if d == 0 else (uTb, t_ub, 0)
                K.dma(BT32[:], D['s5_BT'][l, col].rearrange("r k m -> k r m"), W=[t_B])
                K.op('pool', lambda e: e.tensor_copy(out=BTb[:], in_=BT32[:]), R=[t_B], W=[t_B])
                for which, tab, off in ((0, sinT, 0.0), (1, cosT, 0.25)):
                    for q4 in range(8):
                        qs = slice(q4 * 544, (q4 + 1) * 544)
                        _b = _tc[0] % 2
                        _tc[0] += 1
                        uu, ui, t_uu = uus[_b], uis[_b], t_uus[_b]
                        K.op('dve', lambda e, col=col, which=which, q4=q4, uu=uu: e.tensor_scalar(out=uu[:], in0=iot[:], scalar1=P_['thn'][:, col:col + 1],
                                                                                  scalar2=offc[:, col, q4, which:which + 1], op0=ALU.mult, op1=ALU.add), R=[t_par], W=[t_uu])
                        K.op('dve', lambda e, uu=uu, ui=ui: e.tensor_copy(out=ui[:], in_=uu[:]), R=[t_uu], W=[t_uu])
                        K.op('pool', lambda e, uu=uu, ui=ui: e.tensor_tensor(out=uu[:], in0=uu[:], in1=ui[:], op=ALU.subtract), R=[t_uu], W=[t_uu])
                        K.op('act', lambda e, tab=tab, qs=qs, uu=uu: e.activation(out=tab[:, qs], in_=uu[:], func=AF.Sin, scale=TWO_PI), R=[t_uu], W=[t_tab])
                for (p0, n) in pieces:
                    pr, prt = g.ps()
                    pi_, pit = g.ps()
                    K.op('pe', lambda e, pr=pr, p0=p0, n=n, usrc=usrc, uft=uft: e.matmul(pr[:, 0:n], lhsT=BTb[:, 0, :], rhs=usrc[:, uft, p0:p0 + n], start=True, stop=True),
                         R=[t_B, t_us], W=[prt])
                    K.op('pe', lambda e, pi_=pi_, p0=p0, n=n, usrc=usrc, uft=uft: e.matmul(pi_[:, 0:n], lhsT=BTb[:, 1, :], rhs=usrc[:, uft, p0:p0 + n], start=True, stop=True),
                         R=[t_B, t_us], W=[pit])
                    sl = slice(p0, p0 + n)
                    _o = 4 * (_pc[0] % 2)
                    _pc[0] += 1
                    m1 = m1a[_o:_o + 4]
                    t_m = t_ma[_o:_o + 4]
                    K.op('dve', lambda e, pr=pr, sl=sl, n=n, m1=m1: e.tensor_tensor(out=m1[0][:, 0:n], in0=pr[:, 0:n], in1=cosT[:, sl], op=ALU.mult), R=[prt, t_tab], W=[t_m[0]])
                    K.op('dve', lambda e, pi_=pi_, sl=sl, n=n, m1=m1: e.tensor_tensor(out=m1[1][:, 0:n], in0=pi_[:, 0:n], in1=sinT[:, sl], op=ALU.mult), R=[pit, t_tab], W=[t_m[1]])
                    K.op('dve', lambda e, pi_=pi_, sl=sl, n=n, m1=m1: e.tensor_tensor(out=m1[2][:, 0:n], in0=pi_[:, 0:n], in1=cosT[:, sl], op=ALU.mult), R=[pit, t_tab], W=[t_m[2]])
                    K.op('dve', lambda e, pr=pr, sl=sl, n=n, m1=m1: e.tensor_tensor(out=m1[3][:, 0:n], in0=pr[:, 0:n], in1=sinT[:, sl], op=ALU.mult), R=[prt, t_tab], W=[t_m[3]])
                    K.op('pool', lambda e, sl=sl, n=n, m1=m1: e.tensor_tensor(out=cre[:, sl], in0=m1[0][:, 0:n], in1=m1[1][:, 0:n], op=ALU.add), R=[t_m[0], t_m[1]], W=[t_c])
                    K.op('pool', lambda e, sl=sl, n=n, m1=m1: e.tensor_tensor(out=cim[:, sl], in0=m1[2][:, 0:n], in1=m1[3][:, 0:n], op=ALU.subtract), R=[t_m[2], t_m[3]], W=[t_c])
                rb = P_['r'][:, col:col + 1].to_broadcast([128, T])
                K.op('dve', lambda e, rb=rb: e.tensor_tensor_scan(out=cre[:], data0=rb, data1=cre[:], initial=0.0, op0=ALU.mult, op1=ALU.add), R=[t_c, t_par], W=[t_c])
                K.op('dve', lambda e, rb=rb: e.tensor_tensor_scan(out=cim[:], data0=rb, data1=cim[:], initial=0.0, op0=ALU.mult, op1=ALU.add), R=[t_c, t_par], W=[t_c])
                if d == 0:
                    segs = [(slice(0, T), slice(0, T))]
                else:
                    segs = [(slice(0, CTXL), slice(CTXL - 1, None, -1)), (slice(CTXL, T), slice(T - 1, CTXL - 1, -1))]
                for (so, si) in segs:
                    n = so.stop - so.start
                    for q0 in range(0, n, 1024):
                        qn = min(1024, n - q0)
                        o_sl = slice(so.start + q0, so.start + q0 + qn)
                        if d == 0:
                            i_sl = o_sl
                        else:
                            hi = si.start - q0
                            lo = hi - qn
                            i_sl = slice(hi, lo if lo >= 0 else None, -1)
                        for h0 in range(0, qn, 512):
                            hn = min(512, qn - h0)
                            oo = slice(o_sl.start + h0, o_sl.start + h0 + hn)
                            _o = 4 * (_pc[0] % 2)
                            _pc[0] += 1
                            mA, mB, mC, mD = m1a[_o:_o + 4]
                            t_m = t_ma[_o:_o + 4]
                            if d == 0:
                                ii = oo
                            else:
                                a_ = i_sl.start - h0
                                b_ = a_ - hn
                                ii = slice(a_, b_ if b_ >= 0 else None, -1)
                            K.op('dve', lambda e, ii=ii, hn=hn: e.tensor_tensor(out=mA[:, 0:hn], in0=cre[:, ii], in1=cosT[:, ii], op=ALU.mult), R=[t_c, t_tab], W=[t_m[0]])
                            K.op('pool', lambda e, ii=ii, hn=hn: e.tensor_tensor(out=mB[:, 0:hn], in0=cim[:, ii], in1=sinT[:, ii], op=ALU.mult), R=[t_c, t_tab], W=[t_m[1]])
                            K.op('dve', lambda e, ii=ii, hn=hn: e.tensor_tensor(out=mC[:, 0:hn], in0=cre[:, ii], in1=sinT[:, ii], op=ALU.mult), R=[t_c, t_tab], W=[t_m[2]])
                            K.op('pool', lambda e, ii=ii, hn=hn: e.tensor_tensor(out=mD[:, 0:hn], in0=cim[:, ii], in1=cosT[:, ii], op=ALU.mult), R=[t_c, t_tab], W=[t_m[3]])
                            K.op('dve', lambda e, oo=oo, hn=hn, d=d: e.tensor_tensor(out=hh[:, d, 0, oo], in0=mA[:, 0:hn], in1=mB[:, 0:hn], op=ALU.subtract), R=[t_m[0], t_m[1]], W=[t_h[d]])
                            K.op('dve', lambda e, oo=oo, hn=hn, d=d: e.tensor_tensor(out=hh[:, d, 1, oo], in0=mC[:, 0:hn], in1=mD[:, 0:hn], op=ALU.add), R=[t_m[2], t_m[3]], W=[t_h[d]])
            for (p0, n) in pieces:
                py, pyt = g.ps()
                k = 0
                for d in range(2):
                    for r_ in range(2):
                        K.op('pe', lambda e, d=d, r_=r_, py=py, p0=p0, n=n, k=k, j=j: e.matmul(
                            py[0:32, 0:n], lhsT=Ceb[:, d * 8 + j, r_, :], rhs=hh[:, d, r_, p0:p0 + n], start=(k == 0), stop=(k == 3)),
                            R=[t_par, t_h[d]], W=[pyt])
                        k += 1
                rows = slice(32 * (j % 4), 32 * (j % 4) + 32)
                K.op('act', lambda e, py=py, p0=p0, n=n, rows=rows, ft=ft: e.activation(out=yT[rows, ft, p0:p0 + n], in_=py[0:32, 0:n], func=AF.Identity),
                     R=[pyt], W=[t_y])
        K.barrier()
        st2.close()
        gT = sb('gT', [128, 2, T], BF16); t_g = Tok()
        ytmp = [sb('ytmp%d' % i, [128, 512]) for i in range(2)]; t_yt = [Tok(), Tok()]
        k = 0
        for ft in range(2):
            for (p0, n) in pieces:
                b = k % 2
                k += 1
                K.op('dve', lambda e, b=b, ft=ft, p0=p0, n=n: e.scalar_tensor_tensor(
                    out=ytmp[b][:, 0:n], in0=uTf[:, ft, p0:p0 + n], scalar=dcol[:, ft:ft + 1], in1=yT[:, ft, p0:p0 + n], op0=ALU.mult, op1=ALU.add),
                    R=[t_uf, t_y, t_par], W=[t_yt[b]])
                K.op('act', lambda e, b=b, ft=ft, p0=p0, n=n: e.activation(out=gT[:, ft, p0:p0 + n], in_=ytmp[b][:, 0:n], func=AF.Gelu), R=[t_yt[b]], W=[t_g])
        ob = [sb('s5ob%d' % i, [128, 512], BF16) for i in range(2)]; t_ob = [Tok(), Tok()]
        sg = [sb('s5sg%d' % i, [128, 512]) for i in range(2)]; t_sgm = [Tok(), Tok()]
        k = 0
        for ft in range(2):
            for (p0, n) in pieces:
                b = k % 2
                k += 1
                pz, pzt = g.ps()
                for kt in range(2):
                    K.op('pe', lambda e, kt=kt, ft=ft, pz=pz, p0=p0, n=n: e.matmul(pz[:, 0:n], lhsT=gwb[:, kt, ft * 128:(ft + 1) * 128], rhs=gT[:, kt, p0:p0 + n],
                                                                                   start=(kt == 0), stop=(kt == 1)), R=[t_par, t_g], W=[pzt])
                K.op('act', lambda e, b=b, pz=pz, n=n, ft=ft: e.activation(out=sg[b][:, 0:n], in_=pz[:, 0:n], func=AF.Sigmoid, bias=glub[:, ft:ft + 1]), R=[pzt, t_par], W=[t_sgm[b]])
                K.op('dve', lambda e, b=b, ft=ft, p0=p0, n=n: e.tensor_tensor(out=ob[b][:, 0:n], in0=gT[:, ft, p0:p0 + n], in1=sg[b][:, 0:n], op=ALU.mult), R=[t_g, t_sgm[b]], W=[t_ob[b]])
                K.dma(D['mixT_d'][512 + ft * 128:512 + (ft + 1) * 128, p0:p0 + n], ob[b][:, 0:n], R=[t_ob[b]], W=[g.t_mix], q='pool')


def phase_hyena(g, l, n, c0, sfx):
    nc, K, D = g.nc, g.K, g.D
    NT = n // 128
    NKT = NT + 1
    with ExitStack() as st:
        def sb(name, shape, dt=F32):
            return st.enter_context(nc.sbuf_tensor(_nm(name), list(shape), dt))
        W = D['w_in'][l]
        t_par = Tok()
        cw = sb('hy_cw', [128, 6, 3]); cb = sb('hy_cb', [128, 6])
        K.dma(cw[:], D['hy_cw'][l, :, :, :], W=[t_par])
        K.dma(cb[:], D['hy_cb'][l, :, :], W=[t_par])
        hbias = sb('hy_bias', [128, 2])
        K.dma(hbias[:], D['hy_biasc'][l, :, :], W=[t_par])
        zT = sb('hy_zT', [128, 2, n], BF16); t_z = Tok()
        x0T = sb('hy_x0T', [128, 2, n], BF16); t_x0 = Tok()
        data = sb('hy_data', [128, NT, 768], BF16); t_data = [Tok() for _ in range(NT)]
        rnorm = sb('hy_rnorm', [128, 2]); t_rn = Tok()
        with ExitStack() as st2:
            def sb2(name, shape, dt=F32):
                return st2.enter_context(nc.sbuf_tensor(_nm(name), list(shape), dt))
            wb, t_w = load_w_bf16(g, st2, [(W, OFF['hy_p'], 768)], 768, 'w_hy')
            ucs = [sb2('hucs%d' % i, [128, 8, 128], BF16) for i in range(2)]; t_ucs = [Tok(), Tok()]
            pp = [sb2('hy_pp%d' % i, [128, n + 2]) for i in range(2)]; t_pp = [Tok(), Tok()]
            sv = [sb2('hy_sv%d' % i, [128, n]) for i in range(2)]; t_sv = [Tok(), Tok()]
            for i in range(2):
                K.op('pool', lambda e, i=i: e.memset(pp[i][:, 0:1], 0.0), W=[t_pp[i]])
                K.op('pool', lambda e, i=i: e.memset(pp[i][:, n + 1:n + 2], 0.0), W=[t_pp[i]])

            _sub = int(os.environ.get('HY_SUB', '9'))

            def proj_conv(ft, slot):
                for c4 in range(0, NT, 4):
                    cc = list(range(c4, min(c4 + 4, NT)))
                    pt, ptk = g.ps()
                    for ci_, c in enumerate(cc):
                        b = c % 2
                        K.dma(ucs[b][:], D['uT_d'][c0 + c, :, :, :], R=[g.t_uT[c0 + c]], W=[t_ucs[b]])
                        for kt in range(8):
                            K.op('pe', lambda e, kt=kt, b=b, pt=pt, ci_=ci_: e.matmul(
                                pt[:, ci_ * 128:(ci_ + 1) * 128], lhsT=wb[:, kt, ft * 128:(ft + 1) * 128], rhs=ucs[b][:, kt, :],
                                start=(kt == 0), stop=(kt == 7)), R=[t_w, t_ucs[b]], W=[ptk])
                    nn = len(cc) * 128
                    K.op('act', lambda e, pt=pt, c4=c4, nn=nn: e.activation(out=pp[slot][:, 1 + c4 * 128:1 + c4 * 128 + nn], in_=pt[:, 0:nn], func=AF.Identity),
                         R=[ptk], W=[t_pp[slot]])
                if _sub < 2:
                    return
                for q0 in range(0, n, 2048):
                    qn = min(2048, n - q0)
                    K.op('dve', lambda e, q0=q0, qn=qn: e.tensor_scalar(out=sv[slot][:, q0:q0 + qn], in0=pp[slot][:, q0:q0 + qn], scalar1=cw[:, ft, 0:1], scalar2=cb[:, ft:ft + 1],
                                                                         op0=ALU.mult, op1=ALU.add), R=[t_pp[slot], t_par], W=[t_sv[slot]])
                    K.op('dve', lambda e, q0=q0, qn=qn: e.scalar_tensor_tensor(out=sv[slot][:, q0:q0 + qn], in0=pp[slot][:, q0 + 1:q0 + 1 + qn], scalar=cw[:, ft, 1:2],
                                                                                 in1=sv[slot][:, q0:q0 + qn], op0=ALU.mult, op1=ALU.add), R=[t_pp[slot], t_par, t_sv[slot]], W=[t_sv[slot]])
                    K.op('dve', lambda e, q0=q0, qn=qn: e.scalar_tensor_tensor(out=sv[slot][:, q0:q0 + qn], in0=pp[slot][:, q0 + 2:q0 + 2 + qn], scalar=cw[:, ft, 2:3],
                                                                                 in1=sv[slot][:, q0:q0 + qn], op0=ALU.mult, op1=ALU.add), R=[t_pp[slot], t_par, t_sv[slot]], W=[t_sv[slot]])
            for ci in range(2):
                proj_conv(2 + ci, 0)
                if _sub < 3:
                    break
                proj_conv(4 + ci, 1)
                K.op('pool', lambda e, ci=ci: e.tensor_tensor(out=zT[:, ci, :], in0=sv[0][:], in1=sv[1][:], op=ALU.mult), R=[t_sv[0], t_sv[1]], W=[t_z])
                if _sub < 4:
                    break
                proj_conv(ci, 0)
                K.op('pool', lambda e, ci=ci: e.tensor_copy(out=x0T[:, ci, :], in_=sv[0][:]), R=[t_sv[0]], W=[t_x0])
            for i in (range(NT) if _sub >= 6 else [int(x) for x in os.environ.get("HY_IT", "0,1").split(",") if int(x) < NT]) if _sub >= 5 else []:
                pbh, pbht = g.psb_half()
                for ci in range(2):
                    K.op('pe', lambda e, ci=ci, i=i, pbh=pbh: e.transpose(out=pbh[:, ci * 128:(ci + 1) * 128], in_=zT[:, ci, i * 128:(i + 1) * 128], identity=g.identb[:]),
                         R=[t_z, g.t_const], W=[pbht])
                K.op('act', lambda e, i=i, pbh=pbh: e.activation(out=data[:, i, 0:256], in_=pbh[:, 0:256], func=AF.Identity), R=[pbht], W=[t_data[i]])
            K.barrier()
        _hs = int(os.environ.get('HY_STAGE', '9'))
        if _hs < 2:
            return
        with ExitStack() as st2:
            def sb2(name, shape, dt=F32):
                return st2.enter_context(nc.sbuf_tensor(_nm(name), list(shape), dt))
            zemb = sb2('hy_zemb', [33, n]); fw1 = sb2('hy_fw1', [33, 64]); fw2 = sb2('hy_fw2', [64, 64]); fw3 = sb2('hy_fw3', [64, 512])
            fcol = sb2('hy_fcol', [64, 5]); fs = sb2('hy_fs', [64, 2])
            K.dma(zemb[:], D['hy_zemb' + sfx][:, :], W=[t_par])
            K.dma(fw1[:], D['hy_fw1'][l, :, :], W=[t_par])
            K.dma(fw2[:], D['hy_fw2'][l, :, :], W=[t_par])
            K.dma(fw3[:], D['hy_fw3'][l, :, :], W=[t_par])
            K.dma(fcol[:], D['hy_fcol'][l, :, :], W=[t_par])
            K.op('dve', lambda e: e.tensor_scalar_mul(out=fs[:, 0:1], in0=fcol[:, 2:3], scalar1=1.0 / TWO_PI), R=[t_par], W=[t_par])
            absd = sb2('hy_absd', [128, 512])
            K.dma(absd[:], D['hy_decay'][l].rearrange("d c -> (d c)").partition_broadcast(128), W=[t_par])
            K.op('dve', lambda e: e.scalar_tensor_tensor(out=absd[:], in0=absd[:], scalar=-1.0, in1=absd[:], op0=ALU.mult, op1=ALU.max), R=[t_par], W=[t_par])
            tn = sb2('hy_tn', [128, NT])
            K.dma(tn[:], D['hy_tn' + sfx][:, :], W=[t_par])
            K.op('dve', lambda e: e.tensor_scalar_mul(out=tn[:], in0=tn[:], scalar1=-1.0), R=[t_par], W=[t_par])
            h1 = sb2('hy_h1', [64, n]); h2 = sb2('hy_h2', [64, n]); t_h1 = Tok(); t_h2 = Tok()
            uu = sb2('hy_uu', [64, 512]); ui = sb2('hy_ui', [64, 512], I32); t_uu = Tok()
            for (src, t_src, wgt, bcol, dst, t_dst) in ((zemb, t_par, fw1, 0, h1, t_h1), (h1, t_h1, fw2, 1, h2, t_h2)):
                for q0 in range(0, n, 512):
                    qn = min(512, n - q0)
                    pt, ptk = g.ps()
                    K.op('pe', lambda e, pt=pt, q0=q0, qn=qn, src=src, wgt=wgt: e.matmul(pt[0:64, 0:qn], lhsT=wgt[:], rhs=src[:, q0:q0 + qn], start=True, stop=True),
                         R=[t_src, t_par], W=[ptk])
                    K.op('dve', lambda e, pt=pt, qn=qn, bcol=bcol: e.tensor_scalar(out=uu[:, 0:qn], in0=pt[0:64, 0:qn], scalar1=fcol[:, bcol:bcol + 1], scalar2=fs[:, 0:1],
                                                                                   op0=ALU.add, op1=ALU.mult), R=[ptk, t_par], W=[t_uu])
                    K.op('dve', lambda e, qn=qn: e.tensor_copy(out=ui[:, 0:qn], in_=uu[:, 0:qn]), R=[t_uu], W=[t_uu])
                    K.op('dve', lambda e, qn=qn: e.tensor_tensor(out=uu[:, 0:qn], in0=uu[:, 0:qn], in1=ui[:, 0:qn], op=ALU.subtract), R=[t_uu], W=[t_uu])
                    K.op('act', lambda e, q0=q0, qn=qn, dst=dst: e.activation(out=dst[:, q0:q0 + qn], in_=uu[:, 0:qn], func=AF.Sin, scale=TWO_PI), R=[t_uu], W=[t_dst])
            acc = sb2('hy_acc', [128, 512]); t_acc = Tok()
            K.op('pool', lambda e: e.memset(acc[:], 0.0), W=[t_acc])
            win = [sb2('hy_win%d' % i, [128, 512]) for i in range(2)]; t_win = [Tok(), Tok()]
            fl = [sb2('hy_fl%d' % i, [128, 512]) for i in range(2)]; t_fl = [Tok(), Tok()]
            fa = [sb2('hy_fa%d' % i, [128, 512]) for i in range(2)]; t_fa = [Tok(), Tok()]
            for i in range(NT):
                b = i % 2
                pt, ptk = g.ps()
                K.op('pe', lambda e, pt=pt, i=i: e.matmul(pt[:, 0:512], lhsT=h2[:, i * 128:(i + 1) * 128], rhs=fw3[:], start=True, stop=True), R=[t_h2, t_par], W=[ptk])
                K.op('act', lambda e, b=b, i=i: e.activation(out=win[b][:], in_=absd[:], func=AF.Exp, scale=tn[:, i:i + 1]), R=[t_par], W=[t_win[b]])
                K.op('dve', lambda e, b=b, pt=pt: e.tensor_tensor(out=fl[b][:], in0=pt[:, 0:512], in1=win[b][:], op=ALU.mult), R=[ptk, t_win[b]], W=[t_fl[b]])
                if i == 0:
                    K.op('dve', lambda e, b=b: e.memset(fl[b][0:1, 256:512], 0.0), W=[t_fl[b]])
                K.op('dve', lambda e, b=b: e.scalar_tensor_tensor(out=fa[b][:], in0=fl[b][:], scalar=-1.0, in1=fl[b][:], op0=ALU.mult, op1=ALU.max), R=[t_fl[b]], W=[t_fa[b]])
                K.op('pool', lambda e, b=b: e.tensor_tensor(out=acc[:], in0=acc[:], in1=fa[b][:], op=ALU.add), R=[t_fa[b], t_acc], W=[t_acc])
                K.op('pool', lambda e, b=b, i=i: e.tensor_copy(out=data[:, i, 256:768], in_=fl[b][:]), R=[t_fl[b]], W=[t_data[i]])
            pt, ptk = g.ps()
            for ci in range(2):
                K.op('pe', lambda e, ci=ci, pt=pt: e.matmul(pt[:, ci:ci + 1], lhsT=acc[:, ci * 128:(ci + 1) * 128], rhs=g.ones[:, 0:1], start=True, stop=False),
                     R=[t_acc, g.t_const], W=[ptk])
                K.op('pe', lambda e, ci=ci, pt=pt: e.matmul(pt[:, ci:ci + 1], lhsT=acc[:, 256 + ci * 128:256 + (ci + 1) * 128], rhs=g.ones[:, 0:1], start=False, stop=True),
                     R=[t_acc, g.t_const], W=[ptk])
            K.op('dve', lambda e, pt=pt: e.reciprocal(out=rnorm[:], in_=pt[:, 0:2]), R=[ptk], W=[t_rn])
            K.barrier()
        if _hs < 3:
            return
        Yw = sb('hy_Yw', [128, NKT, 2, 256], BF16); t_Y = [Tok() for _ in range(NKT)]
        wk = sb('hy_wk', [128, NKT])
        K.dma(wk[:], D['hy_wk' + sfx][:, :], W=[t_par])
        tabc = [sb('hy_tc%d' % i, [128, NKT, 128], BF16) for i in range(2)]
        tabs = [sb('hy_ts%d' % i, [128, NKT, 128], BF16) for i in range(2)]
        t_tab = [Tok(), Tok()]
        f1c = sb('hy_f1c', [128, 256]); f1s = sb('hy_f1s', [128, 256]); t_f1 = Tok()
        rre = sb('hy_rre', [128, 256]); rim = sb('hy_rim', [128, 256]); t_r = Tok()
        tt = [sb('hy_tt%d' % i, [128, 256]) for i in range(4)]; t_tt = [Tok() for _ in range(4)]
        yy = [sb('hy_yy%d' % i, [128, 256]) for i in range(2)]; t_yy = [Tok(), Tok()]
        for j in range(NKT):
            b = j % 2
            K.dma(tabc[b][:], D['dftc' + sfx][j, :, :, :], W=[t_tab[b]])
            K.dma(tabs[b][:], D['dfts' + sfx][j, :, :, :], W=[t_tab[b]])
            pA, pAt = g.ps(); pB, pBt = g.ps(); pC, pCt = g.ps(); pD, pDt = g.ps()
            for i in range(NT):
                fl_ = dict(start=(i == 0), stop=(i == NT - 1))
                K.op('pe', lambda e, i=i, b=b, pA=pA, fl_=fl_: e.matmul(pA[:, 0:512], lhsT=tabc[b][:, i, :], rhs=data[:, i, 0:512], **fl_), R=[t_tab[b], t_data[i]], W=[pAt])
                K.op('pe', lambda e, i=i, b=b, pB=pB, fl_=fl_: e.matmul(pB[:, 0:256], lhsT=tabc[b][:, i, :], rhs=data[:, i, 512:768], **fl_), R=[t_tab[b], t_data[i]], W=[pBt])
                K.op('pe', lambda e, i=i, b=b, pC=pC, fl_=fl_: e.matmul(pC[:, 0:512], lhsT=tabs[b][:, i, :], rhs=data[:, i, 0:512], **fl_), R=[t_tab[b], t_data[i]], W=[pCt])
                K.op('pe', lambda e, i=i, b=b, pD=pD, fl_=fl_: e.matmul(pD[:, 0:256], lhsT=tabs[b][:, i, :], rhs=data[:, i, 512:768], **fl_), R=[t_tab[b], t_data[i]], W=[pDt])
            K.op('act', lambda e, pB=pB: e.activation(out=f1c[:], in_=pB[:, 0:256], func=AF.Identity), R=[pBt], W=[t_f1])
            K.op('act', lambda e, pD=pD: e.activation(out=f1s[:], in_=pD[:, 0:256], func=AF.Identity), R=[pDt], W=[t_f1])
            K.op('dve', lambda e, pA=pA: e.tensor_tensor(out=rre[:], in0=pA[:, 256:512], in1=f1c[:], op=ALU.add), R=[pAt, t_f1], W=[t_r])
            K.op('dve', lambda e, pC=pC: e.tensor_tensor(out=rim[:], in0=f1s[:], in1=pC[:, 256:512], op=ALU.subtract), R=[pCt, t_f1], W=[t_r])
            K.op('dve', lambda e, pA=pA: e.tensor_tensor(out=tt[0][:], in0=pA[:, 0:256], in1=rre[:], op=ALU.mult), R=[pAt, t_r], W=[t_tt[0]])
            K.op('dve', lambda e, pC=pC: e.tensor_tensor(out=tt[1][:], in0=pC[:, 0:256], in1=rim[:], op=ALU.mult), R=[pCt, t_r], W=[t_tt[1]])
            K.op('dve', lambda e, pA=pA: e.tensor_tensor(out=tt[2][:], in0=pA[:, 0:256], in1=rim[:], op=ALU.mult), R=[pAt, t_r], W=[t_tt[2]])
            K.op('dve', lambda e, pC=pC: e.tensor_tensor(out=tt[3][:], in0=pC[:, 0:256], in1=rre[:], op=ALU.mult), R=[pCt, t_r], W=[t_tt[3]])
            K.op('pool', lambda e: e.tensor_tensor(out=yy[0][:], in0=tt[0][:], in1=tt[1][:], op=ALU.add), R=[t_tt[0], t_tt[1]], W=[t_yy[0]])
            K.op('pool', lambda e: e.tensor_tensor(out=yy[1][:], in0=tt[3][:], in1=tt[2][:], op=ALU.subtract), R=[t_tt[2], t_tt[3]], W=[t_yy[1]])
            K.op('act', lambda e, j=j: e.activation(out=Yw[:, j, 0, :], in_=yy[0][:], func=AF.Identity, scale=wk[:, j:j + 1]), R=[t_yy[0], t_par], W=[t_Y[j]])
            K.op('act', lambda e, j=j: e.activation(out=Yw[:, j, 1, :], in_=yy[1][:], func=AF.Identity, scale=wk[:, j:j + 1]), R=[t_yy[1], t_par], W=[t_Y[j]])
        if _hs < 4:
            return
        ysb = [sb('hy_ysb%d' % i, [128, 256]) for i in range(2)]; t_ys = [Tok(), Tok()]
        tmp = [sb('hy_tmp%d' % i, [128, 256]) for i in range(2)]; t_tmp = [Tok(), Tok()]
        ob = [sb('hy_ob%d' % i, [128, 2, 128], BF16) for i in range(2)]; t_ob = [Tok(), Tok()]
        for i in range(NT):
            b = i % 2
            K.dma(tabc[b][:], D['dftc' + sfx][i, :, :, :], W=[t_tab[b]])
            K.dma(tabs[b][:], D['dfts' + sfx][i, :, :, :], W=[t_tab[b]])
            py, pyt = g.ps()
            for kt in range(NKT):
                K.op('pe', lambda e, kt=kt, b=b, py=py: e.matmul(py[:, 0:256], lhsT=tabc[b][:, kt, :], rhs=Yw[:, kt, 0, :], start=(kt == 0), stop=False), R=[t_tab[b], t_Y[kt]], W=[pyt])
                K.op('pe', lambda e, kt=kt, b=b, py=py: e.matmul(py[:, 0:256], lhsT=tabs[b][:, kt, :], rhs=Yw[:, kt, 1, :], start=False, stop=(kt == NKT - 1)), R=[t_tab[b], t_Y[kt]], W=[pyt])
            K.op('act', lambda e, b=b, py=py: e.activation(out=ysb[b][:], in_=py[:, 0:256], func=AF.Identity), R=[pyt], W=[t_ys[b]])
            pt, ptk = g.ps()
            for ci in range(2):
                K.op('pe', lambda e, ci=ci, b=b, pt=pt: e.transpose(out=pt[:, ci * 128:(ci + 1) * 128], in_=ysb[b][:, ci * 128:(ci + 1) * 128], identity=g.ident[:]),
                     R=[t_ys[b], g.t_const], W=[ptk])
            cs = slice(i * 128, (i + 1) * 128)
            for ci in range(2):
                K.op('act', lambda e, ci=ci, b=b, pt=pt: e.activation(out=tmp[b][:, ci * 128:(ci + 1) * 128], in_=pt[:, ci * 128:(ci + 1) * 128], func=AF.Identity, scale=rnorm[:, ci:ci + 1]),
                     R=[ptk, t_rn], W=[t_tmp[b]])
                K.op('dve', lambda e, ci=ci, b=b, cs=cs: e.scalar_tensor_tensor(out=tmp[b][:, ci * 128:(ci + 1) * 128], in0=zT[:, ci, cs], scalar=hbias[:, ci:ci + 1],
                                                                             in1=tmp[b][:, ci * 128:(ci + 1) * 128], op0=ALU.mult, op1=ALU.add), R=[t_z, t_par, t_tmp[b]], W=[t_tmp[b]])
                K.op('dve', lambda e, ci=ci, b=b, cs=cs: e.tensor_tensor(out=ob[b][:, ci, :], in0=tmp[b][:, ci * 128:(ci + 1) * 128], in1=x0T[:, ci, cs], op=ALU.mult),
                     R=[t_tmp[b], t_x0], W=[t_ob[b]])
            col0 = (c0 + i) * 128
            K.dma(D['mixT_d'][768:1024, col0:col0 + 128].rearrange("(m p) n -> p m n", p=128), ob[b][:], R=[t_ob[b]], W=[g.t_mix], q='pool')


def load_ln_tables(g, st, l, which):
    nc, K = g.nc, g.K
    t = st.enter_context(nc.sbuf_tensor(_nm('lntab'), [128, 2, D_MODEL], F32))
    tk = Tok()
    K.dma(t[:, 0, :], g.D['ln_g'][l, which, :].partition_broadcast(128), W=[tk])
    K.dma(t[:, 1, :], g.D['ln_b'][l, which, :].partition_broadcast(128), W=[tk])
    return t, tk


def deepnorm_chunk(g, l, c, b, which, y_ap, t_y, xres, t_xres, T_, lntab, t_ln, dst_ap, t_dst_tok):
    K = g.K
    cls = 1 if c < 2 else 0
    tmp, t_tmp = T_['tmp'][b], T_['t_tmp'][b]
    for h in range(2):
        K.op('dve', lambda e, h=h: e.tensor_tensor(out=tmp[:, h * 512:(h + 1) * 512], in0=y_ap[h], in1=g.gbc[:, 0, cls, h * 512:(h + 1) * 512], op=ALU.mult),
             R=[t_y[h], g.t_gbc], W=[t_tmp])
    K.op('dve', lambda e: e.scalar_tensor_tensor(out=tmp[:], in0=xres[:], scalar=float(ALPHA), in1=tmp[:], op0=ALU.mult, op1=ALU.add), R=[t_xres, t_tmp], W=[t_tmp])
    mv, rstd, tk = ln_stats(g, T_['sts'][b], tmp, t_tmp)
    K.op('dve', lambda e: e.tensor_scalar(out=tmp[:], in0=tmp[:], scalar1=mv[:, 0:1], scalar2=rstd[:, 0:1], op0=ALU.subtract, op1=ALU.mult), R=[t_tmp, tk], W=[t_tmp])
    K.op('pool', lambda e: e.tensor_tensor(out=tmp[:], in0=tmp[:], in1=lntab[:, 0, :], op=ALU.mult), R=[t_tmp, t_ln], W=[t_tmp])
    K.op('pool', lambda e: e.tensor_tensor(out=dst_ap, in0=tmp[:], in1=lntab[:, 1, :], op=ALU.add), R=[t_tmp, t_ln], W=[t_dst_tok])


def phase_wout(g, l, x_src, t_xsrc, chunks, x1_d, t_x1):
    nc, K, D = g.nc, g.K, g.D
    with ExitStack() as st:
        def sb(name, shape, dt=F32):
            return st.enter_context(nc.sbuf_tensor(_nm(name), list(shape), dt))
        wb, t_w = load_w_bf16(g, st, [(D['w_out'][l], 0, D_MODEL)], D_MODEL, 'w_out')
        lntab, t_ln = load_ln_tables(g, st, l, 0)
        g.gbc = sb('gbcw', [128, 1, 2, D_MODEL])
        K.dma(g.gbc[:], D['gbc_d'][:, 0:1, :, :], R=[g.t_gbcd], W=[g.t_gbc])
        mx = [sb('mx%d' % i, [128, 8, 128], BF16) for i in range(2)]; t_mx = [Tok(), Tok()]
        xr = [sb('xr%d' % i, [128, D_MODEL]) for i in range(2)]; t_xr = [Tok(), Tok()]
        xo = [sb('xo%d' % i, [128, D_MODEL]) for i in range(2)]; t_xo = [Tok(), Tok()]
        T_ = {'tmp': [sb('dn_tmp%d' % i, [128, D_MODEL]) for i in range(2)], 't_tmp': [Tok(), Tok()],
              'sts': [(sb('dstt%d' % i, [128, 2, 6]), sb('dmv%d' % i, [128, 2]), sb('dlnv%d' % i, [128, 1]), sb('drstd%d' % i, [128, 1]), Tok()) for i in range(2)]}
        for n_, c in enumerate(chunks):
            b = n_ % 2
            cs = slice(c * 128, (c + 1) * 128)
            K.dma(mx[b][:], D['mixT_d'][:, cs].rearrange("(kt p) n -> p kt n", p=128), R=[g.t_mix], W=[t_mx[b]])
            K.dma(xr[b][:], x_src[cs, :], R=([t_xsrc[c]] if t_xsrc is not None else []), W=[t_xr[b]])
            pys = []
            for h in range(2):
                py, pyt = g.ps()
                pys.append((py, pyt))
                for kt in range(8):
                    K.op('pe', lambda e, kt=kt, b=b, h=h, py=py: e.matmul(py[:, 0:512], lhsT=mx[b][:, kt, :], rhs=wb[:, kt, h * 512:(h + 1) * 512],
                                                                          start=(kt == 0), stop=(kt == 7)), R=[t_mx[b], t_w], W=[pyt])
            deepnorm_chunk(g, l, c, b, 0, [pys[0][0][:, 0:512], pys[1][0][:, 0:512]], [pys[0][1], pys[1][1]], xr[b], t_xr[b], T_, lntab, t_ln, xo[b][:], t_xo[b])
            K.dma(x1_d[cs, :], xo[b][:], R=[t_xo[b]], W=[t_x1[c]], q='pool')


def phase_ffn(g, l, chunks, w1_src, w3_src, w2_src, vT_d, t_vT, ffn_d, t_ffn, gate=None, first=True):
    nc, K, D = g.nc, g.K, g.D
    with ExitStack() as st:
        def sb(name, shape, dt=F32):
            return st.enter_context(nc.sbuf_tensor(_nm(name), list(shape), dt))
        w1b = sb('w1b', [128, 8, D_FF], BF16); w3b = sb('w3b', [128, 8, D_FF], BF16); w2b = sb('w2b', [128, NF, D_MODEL], BF16)
        t_w = Tok()
        stg = [sb('fstg%d' % i, [128, 8, 256]) for i in range(3)]; t_stg = [Tok() for _ in range(3)]
        k = 0
        for (src, dstw, nk, ncol) in ((w1_src, w1b, 8, D_FF), (w3_src, w3b, 8, D_FF), (w2_src, w2b, NF, D_MODEL)):
            view = src.rearrange("(kt p) n -> p kt n", p=128)
            for k0 in range(0, nk, 8):
                kn = min(8, nk - k0)
                for c0 in range(0, ncol, 256):
                    cn = min(256, ncol - c0)
                    i = k % 3
                    K.dma(stg[i][:, 0:kn, 0:cn], view[:, k0:k0 + kn, c0:c0 + cn], W=[t_stg[i]])
                    eng = ('dve', 'pool', 'act')[k % 3]
                    if eng == 'act':
                        K.op('act', lambda e, i=i, kn=kn, cn=cn, k0=k0, c0=c0, dstw=dstw: e.activation(out=dstw[:, k0:k0 + kn, c0:c0 + cn], in_=stg[i][:, 0:kn, 0:cn], func=AF.Identity),
                             R=[t_stg[i]], W=[t_w])
                    else:
                        K.op(eng, lambda e, i=i, kn=kn, cn=cn, k0=k0, c0=c0, dstw=dstw: e.tensor_copy(out=dstw[:, k0:k0 + kn, c0:c0 + cn], in_=stg[i][:, 0:kn, 0:cn]),
                             R=[t_stg[i]], W=[t_w])
                    k += 1
        vt = [sb('vt%d' % i, [128, 8, 256], BF16) for i in range(2)]; t_vt = [Tok(), Tok()]
        hT = sb('hT', [128, NF, 256], BF16); t_hT = Tok()
        sil = [sb('sil%d' % i, [128, 256]) for i in range(2)]; t_sil = [Tok(), Tok()]
        fo = [sb('fo%d' % i, [128, D_MODEL]) for i in range(2)]; t_fo = [Tok(), Tok()]
        if gate is not None:
            e_idx, gates_d, t_gates = gate
            gt = [sb('gt%d' % i, [128, N_EXP]) for i in range(2)]; t_gt = [Tok(), Tok()]
        nfo = 0
        for ti in range(0, len(chunks), 2):
            cc = chunks[ti:ti + 2]
            b = (ti // 2) % 2
            nt_ = len(cc) * 128
            for j, c in enumerate(cc):
                K.dma(vt[b][:, :, j * 128:(j + 1) * 128], vT_d[c, :, :, :], R=[t_vT[c]], W=[t_vt[b]])
            for f in range(NF):
                ph, pht = g.ps()
                fs = slice(f * 128, (f + 1) * 128)
                for kt in range(8):
                    K.op('pe', lambda e, kt=kt, b=b, ph=ph, fs=fs, nt_=nt_: e.matmul(ph[:, 0:nt_], lhsT=w1b[:, kt, fs], rhs=vt[b][:, kt, 0:nt_], start=(kt == 0), stop=(kt == 7)),
                         R=[t_w, t_vt[b]], W=[pht])
                for kt in range(8):
                    K.op('pe', lambda e, kt=kt, b=b, ph=ph, fs=fs, nt_=nt_: e.matmul(ph[:, 256:256 + nt_], lhsT=w3b[:, kt, fs], rhs=vt[b][:, kt, 0:nt_], start=(kt == 0), stop=(kt == 7)),
                         R=[t_w, t_vt[b]], W=[pht])
                sb_ = f % 2
                K.op('act', lambda e, ph=ph, sb_=sb_, nt_=nt_: e.activation(out=sil[sb_][:, 0:nt_], in_=ph[:, 0:nt_], func=AF.Silu), R=[pht], W=[t_sil[sb_]])
                K.op('dve', lambda e, ph=ph, sb_=sb_, nt_=nt_, f=f: e.tensor_tensor(out=hT[:, f, 0:nt_], in0=ph[:, 256:256 + nt_], in1=sil[sb_][:, 0:nt_], op=ALU.mult),
                     R=[pht, t_sil[sb_]], W=[t_hT])
            for j, c in enumerate(cc):
                ob_ = nfo % 2
                nfo += 1
                if gate is not None:
                    K.dma(gt[ob_][:], gates_d[c, :, :], R=[t_gates[c]], W=[t_gt[ob_]])
                for h in range(2):
                    po, pot = g.ps()
                    for f in range(NF):
                        K.op('pe', lambda e, f=f, j=j, h=h, po=po: e.matmul(po[:, 0:512], lhsT=hT[:, f, j * 128:(j + 1) * 128], rhs=w2b[:, f, h * 512:(h + 1) * 512],
                                                                            start=(f == 0), stop=(f == NF - 1)), R=[t_hT, t_w], W=[pot])
                    if gate is None:
                        K.op('act', lambda e, po=po, ob_=ob_, h=h: e.activation(out=fo[ob_][:, h * 512:(h + 1) * 512], in_=po[:, 0:512], func=AF.Identity), R=[pot], W=[t_fo[ob_]])
                    else:
                        K.op('act', lambda e, po=po, ob_=ob_, h=h: e.activation(out=fo[ob_][:, h * 512:(h + 1) * 512], in_=po[:, 0:512], func=AF.Identity,
                                                                              scale=gt[ob_][:, e_idx:e_idx + 1]), R=[pot, t_gt[ob_]], W=[t_fo[ob_]])
                cs = slice(c * 128, (c + 1) * 128)
                if first:
                    K.dma(ffn_d[cs, :], fo[ob_][:], R=[t_fo[ob_]], W=[t_ffn[c]], q='pool')
                else:
                    K.dma(ffn_d[cs, :], fo[ob_][:], R=[t_fo[ob_]], W=[t_ffn[c]], q='pool', accum_op=ALU.add)


def phase_ln2(g, l, chunks, x1_d, t_x1, ffn_d, t_ffn, dst_fn):
    nc, K, D = g.nc, g.K, g.D
    with ExitStack() as st:
        def sb(name, shape, dt=F32):
            return st.enter_context(nc.sbuf_tensor(_nm(name), list(shape), dt))
        lntab, t_ln = load_ln_tables(g, st, l, 1)
        g.gbc = sb('gbcf', [128, 1, 2, D_MODEL])
        K.dma(g.gbc[:], D['gbc_d'][:, 1:2, :, :], R=[g.t_gbcd], W=[g.t_gbc])
        xr = [sb('l2x%d' % i, [128, D_MODEL]) for i in range(2)]; t_xr = [Tok(), Tok()]
        fr = [sb('l2f%d' % i, [128, D_MODEL]) for i in range(2)]; t_fr = [Tok(), Tok()]
        xo = [sb('l2o%d' % i, [128, D_MODEL]) for i in range(2)]; t_xo = [Tok(), Tok()]
        T_ = {'tmp': [sb('l2tmp%d' % i, [128, D_MODEL]) for i in range(2)], 't_tmp': [Tok(), Tok()],
              'sts': [(sb('l2stt%d' % i, [128, 2, 6]), sb('l2mv%d' % i, [128, 2]), sb('l2lnv%d' % i, [128, 1]), sb('l2rstd%d' % i, [128, 1]), Tok()) for i in range(2)]}
        for n_, c in enumerate(chunks):
            b = n_ % 2
            cs = slice(c * 128, (c + 1) * 128)
            K.dma(xr[b][:], x1_d[cs, :], R=[t_x1[c]], W=[t_xr[b]])
            K.dma(fr[b][:], ffn_d[cs, :], R=[t_ffn[c]], W=[t_fr[b]])
            deepnorm_chunk(g, l, c, b, 1, [fr[b][:, 0:512], fr[b][:, 512:1024]], [t_fr[b], t_fr[b]], xr[b], t_xr[b], T_, lntab, t_ln, xo[b][:], t_xo[b])
            dst, t_dst = dst_fn(c)
            K.dma(dst, xo[b][:], R=[t_xo[b]], W=[t_dst], q='pool')


_CACHE = {}


def kernel(**inputs):
    inp = {k: np.asarray(v) for k, v in inputs.items()}
    if 'prog' not in _CACHE:
        _CACHE['prog'] = build_program()
    nc, g = _CACHE['prog']
    shared = prep_shared(inp)
    maps = []
    for b in range(8):
        m = prep_core_inputs(inp, b, shared)
        maps.append({k: v for k, v in m.items() if k in g.D})
    res = run_bass_kernel_spmd(nc, maps, core_ids=list(range(8)))
    out = np.stack([np.asarray(r['out']) for r in res.results], axis=0)
    return out.astype(np.float32)


def phase_ffn2(g, l, chunks, experts, vT_d, t_vT, ffn_d, t_ffn, gates=None):
    nc, K, D = g.nc, g.K, g.D
    HF = NF // 2
    with ExitStack() as st:
        def sb(name, shape, dt=F32):
            return st.enter_context(nc.sbuf_tensor(_nm(name), list(shape), dt))
        w1s = [sb('w1s%d' % i, [128, 8, HF * 128], BF16) for i in range(2)]
        w3s = [sb('w3s%d' % i, [128, 8, HF * 128], BF16) for i in range(2)]
        w2s = [sb('w2s%d' % i, [128, HF, D_MODEL], BF16) for i in range(2)]
        t_ws = [Tok(), Tok()]
        vt = [sb('vt%d' % i, [128, 8, 512], BF16) for i in range(2)]; t_vt = [Tok(), Tok()]
        hT = [sb('hT%d' % i, [128, HF, 512], BF16) for i in range(2)]; t_hT = [Tok(), Tok()]
        sil = [sb('sil%d' % i, [128, 512]) for i in range(2)]; t_sil = [Tok(), Tok()]
        fo = [sb('fo%d' % i, [128, D_MODEL]) for i in range(2)]; t_fo = [Tok(), Tok()]
        if gates is not None:
            gates_d, t_gates = gates
            gt = [sb('gt%d' % i, [128, N_EXP]) for i in range(2)]; t_gt = [Tok(), Tok()]
        units = [(e_, hf) for e_ in range(len(experts)) for hf in range(2)]

        def load_unit(u):
            e_, hf = units[u]
            w1, w3, w2 = experts[e_]
            s_ = u % 2
            f0 = hf * HF * 128
            v1 = w1.rearrange("(kt p) n -> p kt n", p=128)
            v3 = w3.rearrange("(kt p) n -> p kt n", p=128)
            v2 = w2.rearrange("(ft p) n -> p ft n", p=128)
            for c0 in range(0, HF * 128, 704):
                K.dma(w1s[s_][:, :, c0:c0 + 704], v1[:, :, f0 + c0:f0 + c0 + 704], W=[t_ws[s_]], q='pool')
                K.dma(w3s[s_][:, :, c0:c0 + 704], v3[:, :, f0 + c0:f0 + c0 + 704], W=[t_ws[s_]], q='pool')
            for f_ in range(0, HF, 4):
                fn_ = min(4, HF - f_)
                K.dma(w2s[s_][:, f_:f_ + fn_, :], v2[:, hf * HF + f_:hf * HF + f_ + fn_, :], W=[t_ws[s_]], q='pool')
        load_unit(0)
        nfo = 0
        ntile = 0
        for u in range(len(units)):
            e_, hf = units[u]
            s_ = u % 2
            if u + 1 < len(units):
                load_unit(u + 1)
            _ft = int(os.environ.get('FFN_TILE', '2'))
            for ti in range(0, len(chunks), _ft):
                cc = chunks[ti:ti + _ft]
                b = ntile % 2
                ntile += 1
                nt_ = len(cc) * 128
                for j, c in enumerate(cc):
                    K.dma(vt[b][:, :, j * 128:(j + 1) * 128], vT_d[c, :, :, :], R=[t_vT[c]], W=[t_vt[b]])
                for f in range(HF):
                    ph, pht = g.ps()
                    ph3, pht3 = g.ps()
                    fs = slice(f * 128, (f + 1) * 128)
                    for kt in range(8):
                        K.op('pe', lambda e, kt=kt, b=b, ph=ph, fs=fs, nt_=nt_, s_=s_: e.matmul(ph[:, 0:nt_], lhsT=w1s[s_][:, kt, fs], rhs=vt[b][:, kt, 0:nt_], start=(kt == 0), stop=(kt == 7)),
                             R=[t_ws[s_], t_vt[b]], W=[pht])
                    for kt in range(8):
                        K.op('pe', lambda e, kt=kt, b=b, ph3=ph3, fs=fs, nt_=nt_, s_=s_: e.matmul(ph3[:, 0:nt_], lhsT=w3s[s_][:, kt, fs], rhs=vt[b][:, kt, 0:nt_], start=(kt == 0), stop=(kt == 7)),
                             R=[t_ws[s_], t_vt[b]], W=[pht3])
                    sb_ = f % 2
                    K.op('act', lambda e, ph=ph, sb_=sb_, nt_=nt_: e.activation(out=sil[sb_][:, 0:nt_], in_=ph[:, 0:nt_], func=AF.Silu), R=[pht], W=[t_sil[sb_]])
                    K.op('dve', lambda e, ph3=ph3, sb_=sb_, nt_=nt_, f=f, b=b: e.tensor_tensor(out=hT[b][:, f, 0:nt_], in0=ph3[:, 0:nt_], in1=sil[sb_][:, 0:nt_], op=ALU.mult),
                         R=[pht3, t_sil[sb_]], W=[t_hT[b]])
                for j, c in enumerate(cc):
                    ob_ = nfo % 2
                    nfo += 1
                    if gates is not None:
                        K.dma(gt[ob_][:], gates_d[c, :, :], R=[t_gates[c]], W=[t_gt[ob_]])
                    for h in range(2):
                        po, pot = g.ps()
                        for f in range(HF):
                            K.op('pe', lambda e, f=f, j=j, h=h, po=po, b=b, s_=s_: e.matmul(po[:, 0:512], lhsT=hT[b][:, f, j * 128:(j + 1) * 128], rhs=w2s[s_][:, f, h * 512:(h + 1) * 512],
                                                                                    start=(f == 0), stop=(f == HF - 1)), R=[t_hT[b], t_ws[s_]], W=[pot])
                        if gates is None:
                            K.op('act', lambda e, po=po, ob_=ob_, h=h: e.activation(out=fo[ob_][:, h * 512:(h + 1) * 512], in_=po[:, 0:512], func=AF.Identity), R=[pot], W=[t_fo[ob_]])
                        else:
                            K.op('act', lambda e, po=po, ob_=ob_, h=h, e_=e_: e.activation(out=fo[ob_][:, h * 512:(h + 1) * 512], in_=po[:, 0:512], func=AF.Identity,
                                                                                  scale=gt[ob_][:, e_:e_ + 1]), R=[pot, t_gt[ob_]], W=[t_fo[ob_]])
                    cs = slice(c * 128, (c + 1) * 128)
                    if u == 0:
                        K.dma(ffn_d[cs, :], fo[ob_][:], R=[t_fo[ob_]], W=[t_ffn[c]], q='pool')
                    else:
                        K.dma(ffn_d[cs, :], fo[ob_][:], R=[t_fo[ob_]], W=[t_ffn[c]], q='pool', accum_op=ALU.add)
```

```python
import math
import os
from contextlib import ExitStack
import numpy as np
import ml_dtypes
import concourse.bass as bass
import concourse.mybir as mybir
from concourse.bass_utils import run_bass_kernel_spmd

F32 = mybir.dt.float32
BF16 = mybir.dt.bfloat16
I32 = mybir.dt.int32
AF = mybir.ActivationFunctionType
ALU = mybir.AluOpType
AX = mybir.AxisListType

COMPUTE = ('pe', 'act', 'dve', 'pool')
NDMA = 24


class Tok:
    __slots__ = ('w', 'r')

    def __init__(self):
        self.w = None
        self.r = {}


class Sched:
    def __init__(self, nc, same_engine_sync=True):
        self.nc = nc
        self.es = ExitStack()
        self.E = {'pe': nc.tensor, 'act': nc.scalar, 'dve': nc.vector, 'pool': nc.gpsimd, 'sp': nc.sync}
        self.sem = {e: self.es.enter_context(nc.semaphore('sem_' + e)) for e in COMPUTE}
        self.cnt = {e: 0 for e in COMPUTE}
        self.seen = {f: {} for f in self.E}
        self.dsem = [self.es.enter_context(nc.semaphore('dsem%d' % i)) for i in range(NDMA)]
        self.dcnt = [0] * NDMA
        self.dnext = 0
        self.same = same_engine_sync
        self.ninst = 0

    def _semobj(self, key):
        return self.sem[key] if isinstance(key, str) else self.dsem[key[1]]

    def wait(self, f, ev):
        if ev is None:
            return
        key, val = ev
        if key == f and (f == 'pe' or not self.same):
            return
        if self.seen[f].get(key, 0) >= val:
            return
        self.E[f].wait_ge(self._semobj(key), val)
        self.seen[f][key] = val

    def _deps(self, f, R, W):
        for t in R:
            self.wait(f, t.w)
        for t in W:
            self.wait(f, t.w)
            for k, v in t.r.items():
                self.wait(f, (k, v))

    def op(self, eng, fn, R=(), W=()):
        self._deps(eng, R, W)
        ins = fn(self.E[eng])
        self.cnt[eng] += 1
        ins.then_inc(self.sem[eng], 1)
        ev = (eng, self.cnt[eng])
        self.seen[eng][eng] = self.seen[eng].get(eng, 0)
        for t in R:
            t.r[eng] = self.cnt[eng]
        for t in W:
            t.w = ev
            t.r = {}
        self.ninst += 1
        return ins

    def dma(self, out, in_, R=(), W=(), q='sp', **kw):
        i = self.dnext
        self.dnext = (i + 1) % NDMA
        key = ('d', i)
        if self.dcnt[i] > 0:
            self.wait(q, (key, self.dcnt[i]))
        self._deps(q, R, W)
        ins = self.E[q].dma_start(out=out, in_=in_, **kw)
        self.dcnt[i] += 16
        ins.then_inc(self.dsem[i], 16)
        for t in R:
            t.r[key] = self.dcnt[i]
        for t in W:
            t.w = (key, self.dcnt[i])
            t.r = {}
        self.ninst += 1
        return ins

    def barrier(self):
        for f in self.E:
            for e in COMPUTE:
                if self.cnt[e] > 0:
                    self.wait_force(f, (e, self.cnt[e]))
            for i in range(NDMA):
                if self.dcnt[i] > 0:
                    self.wait(f, (('d', i), self.dcnt[i]))

    def wait_force(self, f, ev):
        key, val = ev
        if self.seen[f].get(key, 0) >= val:
            return
        self.E[f].wait_ge(self._semobj(key), val)
        self.seen[f][key] = val


D_MODEL = 1024
SEQ = 4096
CTXL = 256
T = SEQ + CTXL
NCH = T // 128
DEPTH = 2
D_PROJ = 2848
D_FF = 2816
NF = D_FF // 128
N_EXP = 8
LN_EPS = 1e-5
ALPHA = (2 * DEPTH) ** 0.25
OFF = {}
_o = 0
for _n, _w in (('ret_q', 256), ('ret_k', 256), ('ret_v', 256), ('ret_g', 256), ('gla_q', 128), ('gla_k', 128),
               ('gla_v', 256), ('gla_r', 256), ('gla_a', 32), ('s5_u', 256), ('hy_p', 768)):
    OFF[_n] = _o
    _o += _w


class G:
    pass


_uid = [0]


def _nm(name):
    _uid[0] += 1
    return '%s_%d' % (name, _uid[0])


def build_program(stop_after=None, dbg=(), dbg_layer=0):
    nc = bass.Bass("TRN2", target_bir_lowering=False)
    K = Sched(nc, same_engine_sync=(os.environ.get('SAME', '1') == '1'))
    g = G()
    g.nc, g.K, g.dbg, g.stop_after, g.dbg_layer = nc, K, set(dbg), stop_after, dbg_layer
    g.D = {}
    g.outs = {}

    def din(name, shape, dt=F32):
        g.D[name] = nc.dram_tensor(name, list(shape), dt, kind="ExternalInput").ap()
        return g.D[name]

    def dscr(name, shape, dt=F32):
        g.D[name] = nc.dram_tensor(name, list(shape), dt, kind="Internal").ap()
        return g.D[name]

    def dout(name, shape, dt=F32):
        g.outs[name] = nc.dram_tensor(name, list(shape), dt, kind="ExternalOutput").ap()
        return g.outs[name]
    g.din, g.dscr, g.dout = din, dscr, dout

    din('xin', [T, D_MODEL])
    din('cvec', [128, 8, 2])
    din('ada_w', [DEPTH, D_MODEL, 6 * D_MODEL])
    din('ada_bT', [DEPTH, 128, 48])
    din('ada_b', [DEPTH, 6 * D_MODEL])
    din('w_out', [DEPTH, D_MODEL, D_MODEL])
    din('ln_g', [DEPTH, 2, D_MODEL])
    din('ln_b', [DEPTH, 2, D_MODEL])
    din('ffn_w1', [1, D_MODEL, D_FF])
    din('ffn_w3', [1, D_MODEL, D_FF])
    din('ffn_w2', [1, D_FF, D_MODEL])
    din('router_w', [1, D_MODEL, N_EXP])
    din('router_b', [1, N_EXP])
    din('moe_w1', [1, N_EXP, D_MODEL, D_FF])
    din('moe_w3', [1, N_EXP, D_MODEL, D_FF])
    din('moe_w2', [1, N_EXP, D_FF, D_MODEL])
    din('w_in', [DEPTH, D_MODEL, D_PROJ])
    din('ident', [128, 128])
    din('maskL', [128, 128])
    din('maskU', [128, 128])
    din('antiI', [128, 128])
    din('poscols', [128, 4])
    din('rot_cos', [128, 32, 32])
    din('rot_sin', [128, 32, 32])
    din('gla_wa', [DEPTH, 2, 16, 128])
    din('gla_ba', [DEPTH, 2, 128])
    din('gla_ng', [DEPTH, 256])
    din('ret_decay', [DEPTH, 8])
    din('ret_dec_fm', [DEPTH, 2, 128, 2])
    din('ret_gng', [DEPTH, 256])
    din('ret_gnb', [DEPTH, 256])
    din('s5_lam_fm', [DEPTH, 128, 16, 2])
    din('s5_dt_fm', [DEPTH, 128, 16])
    din('s5_BT', [DEPTH, 16, 2, 128, 128])
    din('s5_CT', [DEPTH, 16, 2, 128, 32])
    din('s5_dcol', [DEPTH, 128, 2])
    din('s5_glub', [DEPTH, 128, 2])
    din('s5_glu_w', [DEPTH, 256, 256])
    din('iota_t', [T])
    din('hy_cw', [DEPTH, 128, 6, 3])
    din('hy_cb', [DEPTH, 128, 6])
    din('hy_biasc', [DEPTH, 128, 2])
    din('hy_fw1', [DEPTH, 33, 64])
    din('hy_fw2', [DEPTH, 64, 64])
    din('hy_fw3', [DEPTH, 64, 512])
    din('hy_fcol', [DEPTH, 64, 5])
    din('hy_decay', [DEPTH, 2, 256])
    for sfx, n in (('L', SEQ), ('C', CTXL)):
        nt = n // 128
        din('hy_zemb' + sfx, [33, n])
        din('hy_tn' + sfx, [128, nt])
        din('hy_wk' + sfx, [128, nt + 1])
        din('dftc' + sfx, [nt + 1, 128, nt + 1, 128], BF16)
        din('dfts' + sfx, [nt + 1, 128, nt + 1, 128], BF16)

    pst = ExitStack()
    K.es.enter_context(pst)

    def sbp(name, shape, dt=F32):
        return pst.enter_context(nc.sbuf_tensor(_nm(name), list(shape), dt))
    g.ident = sbp('ident', [128, 128]); g.t_const = Tok()
    g.identb = sbp('identb', [128, 128], BF16)
    g.maskL = sbp('maskL', [128, 128]); g.maskU = sbp('maskU', [128, 128]); g.antiI = sbp('antiI', [128, 128])
    g.antiIb = sbp('antiIb', [128, 128], BF16)
    g.mask2 = sbp('mask2', [128, 2, 128])
    g.ones = sbp('ones', [128, 128]); g.onesb = sbp('onesb', [128, 128], BF16)
    g.epsc = sbp('epsc', [128, 1])
    g.poscols = sbp('poscols', [128, 4])
    g.lnc = sbp('lnc', [128, 2])
    g.modT = sbp('modT', [128, 48, 2]); g.t_mod = Tok()
    g.t_gbc = Tok(); g.t_gbcd = Tok()
    g.mod1 = sbp('mod1', [128, 48, 2])
    NPS = 6
    g.psum = [pst.enter_context(nc.psum_tensor('ps%d' % i, [128, 512], F32)) for i in range(NPS)]
    g.pst = [Tok() for _ in range(NPS)]
    g.pnext = 0
    g.psb = [pst.enter_context(nc.psum_tensor('psb%d' % i, [128, 1024], BF16)) for i in range(2)]
    g.t_psb = [Tok(), Tok()]
    g.psb_next = 0

    def ps():
        i = g.pnext
        g.pnext = (i + 1) % NPS
        return g.psum[i], g.pst[i]
    g.ps = ps

    def psb_half():
        i = g.psb_next
        g.psb_next = 1 - i
        return g.psb[i][:, 0:512], g.t_psb[i]
    g.psb_half = psb_half

    tc = g.t_const
    K.dma(g.ident[:], g.D['ident'][:, :], W=[tc])
    K.dma(g.maskL[:], g.D['maskL'][:, :], W=[tc])
    K.dma(g.maskU[:], g.D['maskU'][:, :], W=[tc])
    K.dma(g.mask2[:, 0, :], g.D['maskL'][:, :], W=[tc])
    K.dma(g.mask2[:, 1, :], g.D['maskU'][:, :], W=[tc])
    K.dma(g.antiI[:], g.D['antiI'][:, :], W=[tc])
    K.dma(g.poscols[:], g.D['poscols'][:, :], W=[tc])
    K.op('pool', lambda e: e.memset(g.ones[:], 1.0), W=[tc])
    K.op('pool', lambda e: e.memset(g.onesb[:], 1.0), W=[tc])
    K.op('pool', lambda e: e.memset(g.epsc[:], LN_EPS), W=[tc])
    K.op('pool', lambda e: e.memset(g.lnc[:, 0:1], math.log(0.125)), W=[tc])
    K.op('pool', lambda e: e.memset(g.lnc[:, 1:2], math.log(32 ** -0.5)), W=[tc])
    K.op('dve', lambda e: e.tensor_copy(out=g.identb[:], in_=g.ident[:]), R=[tc], W=[tc])
    K.op('dve', lambda e: e.tensor_copy(out=g.antiIb[:], in_=g.antiI[:]), R=[tc], W=[tc])
    K.barrier()

    dscr('x_cur', [T, D_MODEL])

    dscr('uT_d', [NCH, 128, 8, 128], BF16)
    dscr('gbc_d', [128, 2, 2, D_MODEL])
    dscr('vT_d', [NCH, 128, 8, 128], BF16)
    dscr('mixT_d', [D_MODEL, T], BF16)
    dscr('x1_d', [T, D_MODEL])
    dscr('ffn_d', [T, D_MODEL])
    dscr('gates_d', [NCH, 128, N_EXP])
    dout('out', [SEQ, D_MODEL])
    g.t_uT = [Tok() for _ in range(NCH)]
    g.t_mix = Tok()
    t_vT = [Tok() for _ in range(NCH)]
    t_x1 = [Tok() for _ in range(NCH)]
    t_ffn = [Tok() for _ in range(NCH)]
    t_gates = [Tok() for _ in range(NCH)]
    t_xcur = [Tok() for _ in range(NCH)]
    t_out = Tok()
    D = g.D
    x_src, t_xsrc = D['xin'], None
    for l in range(DEPTH):
        last = l == DEPTH - 1
        phase_mod(g, l)
        K.barrier()
        if stop_after == ('mod', l):
            break
        phase_lnmod(g, l, x_src, D['uT_d'], g.t_uT, sh_idx=0, sc_idx=8, t_src=t_xsrc)
        K.barrier()
        if 'uT' in g.dbg and l == g.dbg_layer:
            o = dout('dbg_uT', [NCH, 128, 8, 128], BF16)
            K.dma(o[:, :, :, :], D['uT_d'][:, :, :, :], R=g.t_uT, q='pool')
            K.barrier()
        if stop_after == ('lnmod', l):
            break
        _ps = os.environ.get('LA_PASSES', 'g0,g1,r0,r1').split(',')
        for p in range(2):
            if 'g%d' % p in _ps:
                la_pass(g, l, 'gla', p, last)
                K.barrier()
        for p in range(2):
            if 'r%d' % p in _ps:
                la_pass(g, l, 'ret', p, last)
                K.barrier()
        if 's5' in os.environ.get('MIXERS', 's5,hy'):
            phase_s5(g, l)
            K.barrier()
        if 'hy' in os.environ.get('MIXERS', 's5,hy'):
            phase_hyena(g, l, SEQ, 2, 'L')
            K.barrier()
            if not last:
                phase_hyena(g, l, CTXL, 0, 'C')
                K.barrier()
        if 'mix' in g.dbg and l == g.dbg_layer:
            o = dout('dbg_mix', [D_MODEL, T], BF16)
            K.dma(o[:, :], D['mixT_d'][:, :], R=[g.t_mix], q='pool')
            K.barrier()
        if stop_after == ('la', l):
            break
        chunks = list(range(2, NCH)) if last else list(range(NCH))
        phase_wout(g, l, x_src, t_xsrc, chunks, D['x1_d'], t_x1)
        K.barrier()
        if 'gbc' in g.dbg and l == g.dbg_layer:
            o = dout('dbg_gbc', [128, 2, 2, D_MODEL])
            K.dma(o[:, :, :, :], D['gbc_d'][:, :, :, :], R=[g.t_gbcd], q='pool')
            K.barrier()
        if 'x1' in g.dbg and l == g.dbg_layer:
            o = dout('dbg_x1', [T, D_MODEL])
            K.dma(o[:, :], D['x1_d'][:, :], R=t_x1, q='pool')
            K.barrier()
        if stop_after == ('wout', l):
            break
        j = l // 2
        if l % 2 == 0:
            phase_lnmod(g, l, D['x1_d'], D['vT_d'], t_vT, sh_idx=24, sc_idx=32, chunks=chunks, t_src=t_x1)
            K.barrier()
            phase_ffn2(g, l, chunks, [(D['ffn_w1'][j], D['ffn_w3'][j], D['ffn_w2'][j])], D['vT_d'], t_vT, D['ffn_d'], t_ffn)
            K.barrier()
        else:
            phase_lnmod(g, l, D['x1_d'], D['vT_d'], t_vT, sh_idx=24, sc_idx=32, chunks=chunks, t_src=t_x1, router=(j, D['gates_d'], t_gates))
            K.barrier()
            phase_ffn2(g, l, chunks, [(D['moe_w1'][j, e_], D['moe_w3'][j, e_], D['moe_w2'][j, e_]) for e_ in range(N_EXP)],
                       D['vT_d'], t_vT, D['ffn_d'], t_ffn, gates=(D['gates_d'], t_gates))
            K.barrier()
        if 'ffn' in g.dbg and l == g.dbg_layer:
            o = dout('dbg_ffn', [T, D_MODEL])
            K.dma(o[:, :], D['ffn_d'][:, :], R=t_ffn, q='pool')
            K.barrier()
        if stop_after == ('ffn', l):
            break
        if last:
            def dst_fn(c):
                return g.outs['out'][(c - 2) * 128:(c - 1) * 128, :], t_out
        else:
            def dst_fn(c):
                return D['x_cur'][c * 128:(c + 1) * 128, :], t_xcur[c]
        phase_ln2(g, l, chunks, D['x1_d'], t_x1, D['ffn_d'], t_ffn, dst_fn)
        K.barrier()
        if 'x2' in g.dbg and l == g.dbg_layer and not last:
            o = dout('dbg_x2', [T, D_MODEL])
            K.dma(o[:, :], D['x_cur'][:, :], R=t_xcur, q='pool')
            K.barrier()
        if stop_after == ('ln2', l):
            break
        x_src, t_xsrc = D['x_cur'], t_xcur
    K.barrier()
    return nc, g


def phase_mod(g, l):
    nc, K = g.nc, g.K
    with ExitStack() as st:
        def sb(name, shape, dt=F32):
            return st.enter_context(nc.sbuf_tensor(_nm(name), list(shape), dt))
        cv = sb('cv', [128, 8, 2]); t_cv = Tok()
        sv = sb('sv', [128, 8, 2])
        abT = sb('abT', [128, 48]); t_ab = Tok()
        wbuf = [sb('adaw%d' % i, [128, 8, 512]) for i in range(2)]
        t_w = [Tok(), Tok()]
        K.dma(cv[:], g.D['cvec'][:, :, :], W=[t_cv])
        K.dma(abT[:], g.D['ada_bT'][l, :, :], W=[t_ab])
        K.op('act', lambda e: e.activation(out=sv[:], in_=cv[:], func=AF.Silu), R=[t_cv], W=[t_cv])
        wsrc = g.D['ada_w'][l].rearrange("(kt p) n -> p kt n", p=128)
        svrep = sb('svrep', [128, 8, 2, 128])
        g.gbc = sb('gbc', [128, 2, 2, D_MODEL])
        K.op('dve', lambda e: e.tensor_copy(out=svrep[:], in_=sv[:].unsqueeze(3).to_broadcast([128, 8, 2, 128])), R=[t_cv], W=[t_cv])
        K.dma(g.gbc[:, 0, 0, :], g.D['ada_b'][l, 2048:3072].partition_broadcast(128), W=[g.t_gbc])
        K.dma(g.gbc[:, 0, 1, :], g.D['ada_b'][l, 2048:3072].partition_broadcast(128), W=[g.t_gbc])
        K.dma(g.gbc[:, 1, 0, :], g.D['ada_b'][l, 5120:6144].partition_broadcast(128), W=[g.t_gbc])
        K.dma(g.gbc[:, 1, 1, :], g.D['ada_b'][l, 5120:6144].partition_broadcast(128), W=[g.t_gbc])
        for grp in range(12):
            b = grp % 2
            K.dma(wbuf[b][:], wsrc[:, :, grp * 512:(grp + 1) * 512], W=[t_w[b]])
            if grp in (4, 5, 10, 11):
                mf = 0 if grp < 6 else 1
                hf = grp % 2
                for cls in range(2):
                    pq, pqt = g.ps()
                    for kt in range(8):
                        K.op('pe', lambda e, kt=kt, b=b, cls=cls, pq=pq: e.matmul(pq[:, 0:512], lhsT=svrep[:, kt, cls, :], rhs=wbuf[b][:, kt, :],
                                                                                  start=(kt == 0), stop=(kt == 7)), R=[t_w[b], t_cv], W=[pqt])
                    dst = g.gbc[:, mf, cls, hf * 512:(hf + 1) * 512]
                    K.op('dve', lambda e, pq=pq, dst=dst: e.tensor_tensor(out=dst, in0=pq[:, 0:512], in1=dst, op=ALU.add), R=[pqt, g.t_gbc], W=[g.t_gbc])
            pt, ptk = g.ps()
            for jj in range(4):
                for kt in range(8):
                    K.op('pe', lambda e, jj=jj, kt=kt, b=b, pt=pt: e.matmul(
                        pt[:, 2 * jj:2 * jj + 2], lhsT=wbuf[b][:, kt, jj * 128:(jj + 1) * 128], rhs=sv[:, kt, :],
                        start=(kt == 0), stop=(kt == 7)), R=[t_w[b], t_cv], W=[ptk])
            K.op('dve', lambda e, pt=pt, grp=grp: e.tensor_tensor(
                out=g.modT[:, grp * 4:(grp + 1) * 4, :], in0=pt[:, 0:8].rearrange("p (j k) -> p j k", k=2),
                in1=abT[:, grp * 4:(grp + 1) * 4].unsqueeze(2).to_broadcast([128, 4, 2]), op=ALU.add), R=[ptk, t_ab], W=[g.t_mod])
        K.op('dve', lambda e: e.tensor_scalar_add(out=g.mod1[:], in0=g.modT[:], scalar1=1.0), R=[g.t_mod], W=[g.t_mod])
        K.dma(g.D['gbc_d'][:, :, :, :], g.gbc[:], R=[g.t_gbc], W=[g.t_gbcd], q='pool')
        K.barrier()


def ln_stats(g, st_tiles, xc, t_xc, eps=LN_EPS):
    K = g.K
    stt, mv, lnv, rstd, tok = st_tiles
    for h in range(2):
        K.op('dve', lambda e, h=h: e.bn_stats(out=stt[:, h, :], in_=xc[:, h * 512:(h + 1) * 512]), R=[t_xc], W=[tok])
    K.op('dve', lambda e: e.bn_aggr(out=mv[:], in_=stt[:].rearrange("p a b -> p (a b)")), R=[tok], W=[tok])
    K.op('act', lambda e: e.activation(out=lnv[:], in_=mv[:, 1:2], func=AF.Ln, bias=g.epsc[:, 0:1]), R=[tok, g.t_const], W=[tok])
    K.op('act', lambda e: e.activation(out=rstd[:], in_=lnv[:], func=AF.Exp, scale=-0.5), R=[tok], W=[tok])
    return mv, rstd, tok


def phase_lnmod(g, l, x_src, uT_d, t_uT, sh_idx, sc_idx, chunks=None, t_src=None, router=None):
    nc, K = g.nc, g.K
    with ExitStack() as st:
        def sb(name, shape, dt=F32):
            return st.enter_context(nc.sbuf_tensor(_nm(name), list(shape), dt))
        xc = [sb('xc%d' % i, [128, 1024]) for i in range(2)]
        t_xc = [Tok(), Tok()]
        xn = [sb('xn%d' % i, [128, 1024]) for i in range(2)]
        t_xn = [Tok(), Tok()]
        uc = [sb('uc%d' % i, [128, 8, 128], BF16) for i in range(2)]
        t_uc = [Tok(), Tok()]
        sts = [(sb('stt%d' % i, [128, 2, 6]), sb('mv%d' % i, [128, 2]), sb('lnv%d' % i, [128, 1]), sb('rstd%d' % i, [128, 1]), Tok())
               for i in range(2)]
        if router is not None:
            j_moe, gates_d, t_gates = router
            uc32 = [sb('uc32_%d' % i, [128, 8, 128]) for i in range(2)]; t_uc32 = [Tok(), Tok()]
            rw = sb('rw', [128, 8, N_EXP]); rb = sb('rb', [128, N_EXP]); t_rw = Tok()
            K.dma(rw[:], g.D['router_w'][j_moe].rearrange("(kt p) n -> p kt n", p=128), W=[t_rw])
            K.dma(rb[:], g.D['router_b'][j_moe].partition_broadcast(128), W=[t_rw])
            rl = [sb('rl%d' % i, [128, 6, N_EXP]) for i in range(2)]; rs = [sb('rs%d' % i, [128, 4]) for i in range(2)]; t_rl = [Tok(), Tok()]
        cl_ = list(chunks if chunks is not None else range(NCH))

        def stX(n, c):
            b = n % 2
            col = 1 if c < 2 else 0
            K.dma(xc[b][:], x_src[c * 128:(c + 1) * 128, :], R=([t_src[c]] if t_src is not None else []), W=[t_xc[b]])
            mv, rstd, tk = ln_stats(g, sts[b], xc[b], t_xc[b])
            K.op('dve', lambda e, b=b, mv=mv, rstd=rstd: e.tensor_scalar(
                out=xn[b][:], in0=xc[b][:], scalar1=mv[:, 0:1], scalar2=rstd[:, 0:1], op0=ALU.subtract, op1=ALU.mult),
                R=[t_xc[b], tk], W=[t_xn[b]])

        def stY(n, c):
            b = n % 2
            col = 1 if c < 2 else 0
            mv, rstd, tk = None, None, None
            for half in range(2):
                pt, ptk = g.ps()
                for q in range(4):
                    kt = half * 4 + q
                    K.op('pe', lambda e, b=b, kt=kt, q=q, pt=pt: e.transpose(
                        out=pt[:, q * 128:(q + 1) * 128], in_=xn[b][:, kt * 128:(kt + 1) * 128], identity=g.ident[:]),
                        R=[t_xn[b], g.t_const], W=[ptk])
                for q in range(4):
                    kt = half * 4 + q
                    dst, t_dst = (uc[b], t_uc[b]) if router is None else (uc32[b], t_uc32[b])
                    K.op('act', lambda e, kt=kt, q=q, pt=pt, dst=dst, col=col: e.activation(
                        out=dst[:, kt, :], in_=pt[:, q * 128:(q + 1) * 128], func=AF.Identity,
                        scale=g.mod1[:, sc_idx + kt, col:col + 1], bias=g.modT[:, sh_idx + kt, col:col + 1]),
                        R=[ptk, g.t_mod], W=[t_dst])
            if router is not None:
                K.op('pool', lambda e, b=b: e.tensor_copy(out=uc[b][:], in_=uc32[b][:]), R=[t_uc32[b]], W=[t_uc[b]])
                pr, prt = g.ps()
                for kt in range(8):
                    K.op('pe', lambda e, kt=kt, b=b, pr=pr: e.matmul(pr[:, 0:N_EXP], lhsT=uc32[b][:, kt, :], rhs=rw[:, kt, :], start=(kt == 0), stop=(kt == 7)),
                         R=[t_uc32[b], t_rw], W=[prt])
                L_, r4, tk2 = rl[b], rs[b], t_rl[b]
                K.op('dve', lambda e, pr=pr, L_=L_: e.tensor_tensor(out=L_[:, 0, :], in0=pr[:, 0:N_EXP], in1=rb[:], op=ALU.add), R=[prt, t_rw], W=[tk2])
                K.op('dve', lambda e, L_=L_, r4=r4: e.tensor_reduce(out=r4[:, 0:1], in_=L_[:, 0, :], axis=AX.X, op=ALU.max), R=[tk2], W=[tk2])
                K.op('dve', lambda e, L_=L_, r4=r4: e.tensor_scalar(out=L_[:, 1, :], in0=L_[:, 0, :], scalar1=r4[:, 0:1], scalar2=None, op0=ALU.is_equal), R=[tk2], W=[tk2])
                K.op('dve', lambda e, L_=L_: e.scalar_tensor_tensor(out=L_[:, 2, :], in0=L_[:, 1, :], scalar=-1e30, in1=L_[:, 0, :], op0=ALU.mult, op1=ALU.add), R=[tk2], W=[tk2])
                K.op('dve', lambda e, L_=L_, r4=r4: e.tensor_reduce(out=r4[:, 1:2], in_=L_[:, 2, :], axis=AX.X, op=ALU.max), R=[tk2], W=[tk2])
                K.op('dve', lambda e, L_=L_, r4=r4: e.tensor_scalar(out=L_[:, 3, :], in0=L_[:, 0, :], scalar1=r4[:, 1:2], scalar2=None, op0=ALU.is_ge), R=[tk2], W=[tk2])
                K.op('dve', lambda e, r4=r4: e.tensor_scalar_mul(out=r4[:, 2:3], in0=r4[:, 0:1], scalar1=-1.0), R=[tk2], W=[tk2])
                K.op('act', lambda e, L_=L_, r4=r4: e.activation(out=L_[:, 4, :], in_=L_[:, 0, :], func=AF.Exp, bias=r4[:, 2:3]), R=[tk2], W=[tk2])
                K.op('dve', lambda e, L_=L_: e.tensor_tensor(out=L_[:, 4, :], in0=L_[:, 4, :], in1=L_[:, 3, :], op=ALU.mult), R=[tk2], W=[tk2])
                K.op('dve', lambda e, L_=L_, r4=r4: e.tensor_reduce(out=r4[:, 3:4], in_=L_[:, 4, :], axis=AX.X, op=ALU.add), R=[tk2], W=[tk2])
                K.op('dve', lambda e, r4=r4: e.reciprocal(out=r4[:, 3:4], in_=r4[:, 3:4]), R=[tk2], W=[tk2])
                K.op('dve', lambda e, L_=L_, r4=r4: e.tensor_scalar_mul(out=L_[:, 5, :], in0=L_[:, 4, :], scalar1=r4[:, 3:4]), R=[tk2], W=[tk2])
                K.dma(gates_d[c, :, :], L_[:, 5, :], R=[tk2], W=[t_gates[c]], q='pool')
            K.dma(uT_d[c, :, :, :], uc[b][:], R=[t_uc[b]], W=[t_uT[c]], q='pool')

        stX(0, cl_[0])
        for n, c in enumerate(cl_):
            if n + 1 < len(cl_):
                stX(n + 1, cl_[n + 1])
            stY(n, c)


def const_inputs():
    idx = np.arange(128)
    c = {}
    c['ident'] = np.eye(128, dtype=np.float32)
    c['maskL'] = (idx[:, None] <= idx[None, :]).astype(np.float32)
    c['maskU'] = (idx[:, None] >= idx[None, :]).astype(np.float32)
    c['antiI'] = np.ascontiguousarray(np.eye(128, dtype=np.float32)[::-1])
    i = idx.astype(np.float32)
    c['poscols'] = np.stack([i + 1, 128 - i, -(i + 1), -(128 - i)], axis=1).astype(np.float32)
    tpos = np.arange(SEQ)
    row = (tpos // 64).astype(np.float32)
    colp = (tpos % 64).astype(np.float32)
    n_freq = 16
    inv = (10000.0 ** (-np.arange(n_freq, dtype=np.float32) / n_freq)).astype(np.float32)
    ang = np.concatenate([row[:, None] * inv, colp[:, None] * inv], axis=-1).astype(np.float32)
    for sfx, n in (('L', SEQ), ('C', CTXL)):
        nt = n // 128
        tlin = np.linspace(0.0, 1.0, n, dtype=np.float32)
        ii = np.arange(n, dtype=np.float32)[:, None]
        bands = np.linspace(1e-4, 15, 16, dtype=np.float32)[None, :]
        ang2 = (np.float32(2.0 * math.pi / n) * bands * ii).astype(np.float32)
        zemb = np.concatenate([tlin[:, None], np.cos(ang2), -np.sin(ang2)], axis=-1).astype(np.float32)
        c['hy_zemb' + sfx] = np.ascontiguousarray(zemb.T)
        c['hy_tn' + sfx] = np.ascontiguousarray(tlin.reshape(nt, 128).T)
        N2 = 2 * n
        kk = np.arange((nt + 1) * 128)
        wkv = np.where(kk > n, 0.0, np.where((kk == 0) | (kk == n), 1.0 / N2, 2.0 / N2)).astype(np.float32)
        c['hy_wk' + sfx] = np.ascontiguousarray(wkv.reshape(nt + 1, 128).T)
        a = kk.reshape(nt + 1, 128)
        prod = (a.T[None, :, :, None].astype(np.int64) * a[:, None, None, :].astype(np.int64)) % N2
        lut_c = np.cos(2.0 * np.pi * np.arange(N2) / N2)
        lut_s = np.sin(2.0 * np.pi * np.arange(N2) / N2)
        valid = (a.T[None, :, :, None] <= n) & (a[:, None, None, :] <= n)
        c['dftc' + sfx] = np.where(valid, lut_c[prod], 0.0).astype(ml_dtypes.bfloat16)
        c['dfts' + sfx] = np.where(valid, lut_s[prod], 0.0).astype(ml_dtypes.bfloat16)
    c['rot_cos'] = np.ascontiguousarray(np.cos(ang).astype(np.float32).reshape(32, 128, 32).transpose(1, 0, 2))
    c['rot_sin'] = np.ascontiguousarray(np.sin(ang).astype(np.float32).reshape(32, 128, 32).transpose(1, 0, 2))
    return c


def prep_core_inputs(inp, b, shared):
    m = dict(shared)
    m['xin'] = np.ascontiguousarray(np.concatenate([inp['ctx'][b], inp['x'][b]], axis=0))
    cv = np.stack([inp['c'][b], inp['c_ctx']], axis=-1)
    m['cvec'] = np.ascontiguousarray(cv.reshape(8, 128, 2).transpose(1, 0, 2))
    return m


def prep_shared(inp):
    m = const_inputs()
    m['gla_wa'] = inp['gla_wa']
    m['gla_ba'] = inp['gla_ba']
    m['gla_ng'] = inp['gla_norm_g']
    m['ret_decay'] = np.ascontiguousarray(inp['ret_decay'].reshape(DEPTH, 8))
    rd = inp['ret_decay']
    fm = np.zeros((DEPTH, 2, 128, 2), np.float32)
    for p in range(2):
        for h2 in range(2):
            fm[:, p, h2 * 64:(h2 + 1) * 64, :] = rd[:, :, 2 * p + h2][:, None, :]
    m['ret_dec_fm'] = fm
    m['ret_gng'] = inp['ret_gn_g']
    L = DEPTH
    lam = np.stack([inp['s5_lam_re'], inp['s5_lam_im']], axis=-1)
    lam = lam.reshape(L, 2, 8, 2, 64, 2)
    m['s5_lam_fm'] = np.ascontiguousarray(lam.transpose(0, 3, 4, 1, 2, 5).reshape(L, 128, 16, 2))
    ldt = np.broadcast_to(inp['s5_log_dt'].reshape(L, 2, 8, 2, 1), (L, 2, 8, 2, 64))
    m['s5_dt_fm'] = np.ascontiguousarray(ldt.transpose(0, 3, 4, 1, 2).reshape(L, 128, 16))
    BT = np.zeros((L, 2, 8, 2, 128, 128), np.float32)
    CT = np.zeros((L, 2, 8, 2, 128, 32), np.float32)
    for j in range(8):
        for g2 in range(2):
            gi = 2 * j + g2
            r0 = (gi % 8) * 16
            for ri, (bn, cn) in enumerate((('s5_b_re', 's5_c_re'), ('s5_b_im', 's5_c_im'))):
                BT[:, :, j, ri, r0:r0 + 16, g2 * 64:(g2 + 1) * 64] = inp[bn][:, :, gi].transpose(0, 1, 3, 2)
                CT[:, :, j, ri, g2 * 64:(g2 + 1) * 64, g2 * 16:(g2 + 1) * 16] = inp[cn][:, :, gi].transpose(0, 1, 3, 2)
    m['s5_BT'] = BT.reshape(L, 16, 2, 128, 128)
    m['s5_CT'] = CT.reshape(L, 16, 2, 128, 32)
    m['s5_dcol'] = np.ascontiguousarray(inp['s5_d'].reshape(L, 2, 128).transpose(0, 2, 1))
    m['s5_glub'] = np.ascontiguousarray(inp['s5_glu_b'].reshape(L, 2, 128).transpose(0, 2, 1))
    m['s5_glu_w'] = inp['s5_glu_w']
    m['iota_t'] = np.arange(T, dtype=np.float32)
    m['hy_cw'] = np.ascontiguousarray(inp['hy_conv_w'].reshape(L, 3, 6, 128).transpose(0, 3, 2, 1))
    m['hy_cb'] = np.ascontiguousarray(inp['hy_conv_b'].reshape(L, 6, 128).transpose(0, 2, 1))
    m['hy_biasc'] = np.ascontiguousarray(inp['hy_bias'].reshape(L, 2, 128).transpose(0, 2, 1))
    m['hy_fw1'] = inp['hy_fw1']; m['hy_fw2'] = inp['hy_fw2']; m['hy_fw3'] = inp['hy_fw3']
    fc = np.zeros((L, 64, 5), np.float32)
    fc[:, :, 0] = inp['hy_fb1']; fc[:, :, 1] = inp['hy_fb2']; fc[:, :, 2] = inp['hy_freq']
    m['hy_fcol'] = fc
    m['hy_decay'] = inp['hy_decay']
    m['ret_gnb'] = inp['ret_gn_b']
    m['ada_w'] = inp['ada_w']
    m['ada_b'] = inp['ada_b']
    m['w_out'] = inp['w_out']
    m['ln_g'] = np.ascontiguousarray(np.stack([inp['ln_mix_g'], inp['ln_ffn_g']], axis=1))
    m['ln_b'] = np.ascontiguousarray(np.stack([inp['ln_mix_b'], inp['ln_ffn_b']], axis=1))
    for k_ in ('ffn_w1', 'ffn_w3', 'ffn_w2', 'router_w', 'router_b', 'moe_w1', 'moe_w3', 'moe_w2'):
        m[k_] = inp[k_]
    m['ada_bT'] = np.ascontiguousarray(inp['ada_b'].reshape(DEPTH, 48, 128).transpose(0, 2, 1))
    m['w_in'] = inp['w_in']
    return m


def load_w_bf16(g, st, wsrc_cols, ncols_total, name):
    nc, K = g.nc, g.K
    wb = st.enter_context(nc.sbuf_tensor(_nm(name), [128, 8, ncols_total], BF16))
    t_w = Tok()
    o = 0
    for (src, c0, n) in wsrc_cols:
        if src is None:
            K.op('pool', lambda e, o=o, n=n: e.memset(wb[:, :, o:o + n], 0.0), W=[t_w])
            o += n
            continue
        view = src.rearrange("(kt p) n -> p kt n", p=128)
        done = 0
        while done < n:
            m = min(512, n - done)
            K.dma(wb[:, :, o:o + m], view[:, :, c0 + done:c0 + done + m], W=[t_w], q='pool')
            o += m
            done += m
    return wb, t_w


def la_pass(g, l, kind, p, last):
    nc, K, D = g.nc, g.K, g.D
    gla = kind == 'gla'
    H = 2
    dk = 64
    NV = H * 64
    row0 = (256 if gla else 0) + p * 128
    with ExitStack() as st:
        def sb(name, shape, dt=F32):
            return st.enter_context(nc.sbuf_tensor(_nm(name), list(shape), dt))
        W = D['w_in'][l]
        if gla:
            cols = []
            for nm_ in ('gla_q', 'gla_k'):
                for h2 in range(2):
                    cols.append((W, OFF[nm_] + (2 * p + h2) * 32, 32))
                    cols.append((None, 0, 32))
            cols += [(W, OFF['gla_v'] + 128 * p, 128), (W, OFF['gla_r'] + 128 * p, 128), (W, OFF['gla_a'], 32)]
            ncols = 544
        else:
            cols = [(W, OFF['ret_q'] + 128 * p, 128), (W, OFF['ret_k'] + 128 * p, 128),
                    (W, OFF['ret_v'] + 128 * p, 128), (W, OFF['ret_g'] + 128 * p, 128)]
            ncols = 512
        wb, t_w = load_w_bf16(g, st, cols, ncols, 'w_la')
        LT = sb('LT', [128, 4, T], BF16)
        t_LT = [Tok() for _ in range(NCH)]
        vtok = sb('vtok', [128, NCH, NV], BF16); t_v = [Tok() for _ in range(NCH)]
        sgtok = sb('sgtok', [128, NCH, NV], BF16); t_sg = [Tok() for _ in range(NCH)]
        KVGb = sb('KVGb', [128, NCH, 64]); t_kvb = [Tok() for _ in range(NCH)]
        Sbf = sb('Sbf', [128, NCH, 2, 64], BF16); t_S = [[Tok(), Tok()] for _ in range(NCH)]
        Sf = sb('Sf', [128, 64]); Sb = sb('Sb', [128, 64]); t_Sf = Tok(); t_Sb = Tok()
        Gall = sb('Gall', [128, NCH, 2]); t_G = [Tok() for _ in range(NCH)]
        t_par = Tok()
        if gla:
            wa = sb('wa', [16, 2, 128]); ba = sb('ba', [1, 2, 128])
            K.op('pool', lambda e: e.memset(wa[:], 0.0), W=[t_par])
            K.op('pool', lambda e: e.memset(ba[:], 0.0), W=[t_par])
            for h2 in range(2):
                hh = (2 * p + h2) * 32
                K.dma(wa[:, :, h2 * 64:h2 * 64 + 32], D['gla_wa'][l, :, :, hh:hh + 32].rearrange("d r n -> r d n"), W=[t_par])
                K.dma(ba[:, :, h2 * 64:h2 * 64 + 32], D['gla_ba'][l:l + 1, :, hh:hh + 32], W=[t_par])
            ngb = sb('ngb', [128, NV])
            K.dma(ngb[:], D['gla_ng'][l, p * 128:(p + 1) * 128].partition_broadcast(128), W=[t_par])
        else:
            gng = sb('gng', [128, NV]); gnb = sb('gnb', [128, NV])
            K.dma(gng[:], D['ret_gng'][l, p * 128:(p + 1) * 128].partition_broadcast(128), W=[t_par])
            K.dma(gnb[:], D['ret_gnb'][l, p * 128:(p + 1) * 128].partition_broadcast(128), W=[t_par])
            dtm = sb('dtm', [128, 2, 4]); dfm = sb('dfm', [128, 2])
            K.dma(dtm[:].rearrange("p a b -> p (a b)"), D['ret_decay'][l].partition_broadcast(128), W=[t_par])
            K.dma(dfm[:], D['ret_dec_fm'][l, p, :, :], W=[t_par])
            K.op('act', lambda e: e.activation(out=dtm[:], in_=dtm[:], func=AF.Exp, scale=-1.0), R=[t_par], W=[t_par])
            K.op('act', lambda e: e.activation(out=dtm[:], in_=dtm[:], func=AF.Ln, bias=1.0), R=[t_par], W=[t_par])
            K.op('act', lambda e: e.activation(out=dfm[:], in_=dfm[:], func=AF.Exp, scale=-1.0), R=[t_par], W=[t_par])
            K.op('act', lambda e: e.activation(out=dfm[:], in_=dfm[:], func=AF.Ln, bias=1.0), R=[t_par], W=[t_par])
            argt = sb('argt', [128, 2, 2])
            K.op('dve', lambda e: e.tensor_scalar_mul(out=argt[:, 0, :], in0=dtm[:, 0, 2 * p:2 * p + 2], scalar1=g.poscols[:, 2:3]),
                 R=[t_par, g.t_const], W=[t_par])
            K.op('dve', lambda e: e.tensor_scalar_mul(out=argt[:, 1, :], in0=dtm[:, 1, 2 * p:2 * p + 2], scalar1=g.poscols[:, 3:4]),
                 R=[t_par, g.t_const], W=[t_par])
            EpR = sb('EpR', [128, 2, 2]); EmR = sb('EmR', [128, 2, 2])
            K.op('act', lambda e: e.activation(out=EpR[:], in_=argt[:], func=AF.Exp), R=[t_par], W=[t_par])
            K.op('act', lambda e: e.activation(out=EmR[:], in_=argt[:], func=AF.Exp, scale=-1.0, bias=g.lnc[:, 0:1]), R=[t_par, g.t_const], W=[t_par])
            Gret = sb('Gret', [128, 2])
            K.op('act', lambda e: e.activation(out=Gret[:], in_=dfm[:], func=AF.Exp, scale=-128.0), R=[t_par], W=[t_par])
            cosT = sb('cosT', [128, 32, 32]); sinT = sb('sinT', [128, 32, 32])
            K.dma(cosT[:], D['rot_cos'][:, :, :], W=[t_par])
            K.dma(sinT[:], D['rot_sin'][:, :, :], W=[t_par])

        def Gap(c, d):
            return (Gall[:, c, d:d + 1], t_G[c]) if gla else (Gret[:, d:d + 1], t_par)

        def dbl(name, shape, dt=F32):
            return [sb(name + str(i), shape, dt) for i in range(2)], [Tok(), Tok()]
        uc, t_uc = dbl('ucl', [128, 8, 128], BF16)
        qk, t_qk = dbl('qk', [128, 256])
        rt, t_rt = dbl('rt', [128, 4, 4, 32])
        a_sb, t_a = dbl('a_sb', [128, 32])
        aT, t_aT = dbl('aT', [16, 2, 128])
        sp, t_sp = dbl('sp', [128, 256])
        Ep, t_Ep = dbl('Ep', [128, 256]); Em, t_Em = dbl('Em', [128, 256])
        qkt, t_qkt = dbl('qkt', [128, 4, 128], BF16)
        K.op('pool', lambda e: e.memset(Sf[:], 0.0), W=[t_Sf])
        K.op('pool', lambda e: e.memset(Sb[:], 0.0), W=[t_Sb])

        _dbg = os.environ.get('LA_DEBUG', '')
        _n1 = int(_dbg.split(',')[0]) if _dbg else NCH
        _p2 = int(_dbg.split(',')[1]) if _dbg else 1
        _lvl = int(_dbg.split(',')[2]) if _dbg else 99
        def stA(c):
            b = c % 2
            K.dma(uc[b][:], D['uT_d'][c, :, :, :], R=[g.t_uT[c]], W=[t_uc[b]])
            pa, pat = g.ps()
            for kt in range(8):
                K.op('pe', lambda e, kt=kt, b=b, pa=pa: e.matmul(pa[:, 0:512], lhsT=uc[b][:, kt, :], rhs=wb[:, kt, 0:512],
                                                                  start=(kt == 0), stop=(kt == 7)), R=[t_uc[b], t_w], W=[pat])
            if gla:
                pb, pbt = g.ps()
                for kt in range(8):
                    K.op('pe', lambda e, kt=kt, b=b, pb=pb: e.matmul(pb[:, 0:32], lhsT=uc[b][:, kt, :], rhs=wb[:, kt, 512:544],
                                                                      start=(kt == 0), stop=(kt == 7)), R=[t_uc[b], t_w], W=[pbt])
            K.op('act', lambda e, b=b, pa=pa: e.activation(out=qk[b][:], in_=pa[:, 0:256], func=AF.Identity), R=[pat], W=[t_qk[b]])
            K.op('act', lambda e, c=c, pa=pa: e.activation(out=vtok[:, c, :], in_=pa[:, 256:384], func=AF.Identity), R=[pat], W=[t_v[c]])
            K.op('act', lambda e, c=c, pa=pa: e.activation(out=sgtok[:, c, :], in_=pa[:, 384:512], func=AF.Silu), R=[pat], W=[t_sg[c]])
            if gla:
                K.op('dve', lambda e, b=b, pb=pb: e.tensor_copy(out=a_sb[b][:], in_=pb[:, 0:32]), R=[pbt], W=[t_a[b]])

        def stB(c):
            b = c % 2
            if gla:
                pt, ptk = g.ps()
                for d in range(2):
                    K.op('pe', lambda e, d=d, b=b, pt=pt: e.transpose(out=pt[0:16, d * 128:(d + 1) * 128], in_=a_sb[b][:, d * 16:(d + 1) * 16],
                                                                       identity=g.ident[:]), R=[t_a[b], g.t_const], W=[ptk])
                K.op('dve', lambda e, b=b, pt=pt: e.tensor_copy(out=aT[b][:].rearrange("r d n -> r (d n)"), in_=pt[0:16, 0:256]), R=[ptk], W=[t_aT[b]])
                pg, pgt = g.ps()
                for d in range(2):
                    K.op('pe', lambda e, d=d, b=b, pg=pg: e.matmul(pg[:, d * 128:(d + 1) * 128], lhsT=aT[b][:, d, :], rhs=wa[:, d, :],
                                                                    start=True, stop=False), R=[t_aT[b], t_par], W=[pgt])
                    K.op('pe', lambda e, d=d, pg=pg: e.matmul(pg[:, d * 128:(d + 1) * 128], lhsT=g.ones[0:1, :], rhs=ba[:, d, :],
                                                               start=False, stop=True), R=[t_par, g.t_const], W=[pgt])
                K.op('act', lambda e, b=b, pg=pg: e.activation(out=sp[b][:], in_=pg[:, 0:256], func=AF.Exp, scale=-1.0), R=[pgt], W=[t_sp[b]])
                K.op('act', lambda e, b=b: e.activation(out=sp[b][:], in_=sp[b][:], func=AF.Ln, bias=1.0), R=[t_sp[b]], W=[t_sp[b]])
                pc, pct = g.ps()
                K.op('pe', lambda e, b=b, pc=pc: e.matmul(pc[:, 0:128], lhsT=g.maskL[:], rhs=sp[b][:, 0:128], start=True, stop=True),
                     R=[t_sp[b], g.t_const], W=[pct])
                K.op('pe', lambda e, b=b, pc=pc: e.matmul(pc[:, 128:256], lhsT=g.maskU[:], rhs=sp[b][:, 128:256], start=True, stop=True),
                     R=[t_sp[b], g.t_const], W=[pct])
                K.op('pe', lambda e, b=b, pc=pc: e.matmul(pc[:, 256:257], lhsT=sp[b][:, 0:128], rhs=g.ones[:, 0:1], start=True, stop=True),
                     R=[t_sp[b], g.t_const], W=[pct])
                K.op('pe', lambda e, b=b, pc=pc: e.matmul(pc[:, 257:258], lhsT=sp[b][:, 128:256], rhs=g.ones[:, 0:1], start=True, stop=True),
                     R=[t_sp[b], g.t_const], W=[pct])
                K.op('act', lambda e, b=b, pc=pc: e.activation(out=Ep[b][:], in_=pc[:, 0:256], func=AF.Exp, scale=-1.0 / 16, bias=g.lnc[:, 1:2]),
                     R=[pct, g.t_const], W=[t_Ep[b]])
                K.op('act', lambda e, b=b, pc=pc: e.activation(out=Em[b][:], in_=pc[:, 0:256], func=AF.Exp, scale=1.0 / 16), R=[pct], W=[t_Em[b]])
                K.op('act', lambda e, c=c, pc=pc: e.activation(out=Gall[:, c, :], in_=pc[:, 256:258], func=AF.Exp, scale=-1.0 / 16), R=[pct], W=[t_G[c]])
                K.op('dve', lambda e, b=b: e.tensor_tensor(out=qkt[b][:, 0:2, :], in0=qk[b][:, 0:128].unsqueeze(1).to_broadcast([128, 2, 128]),
                                                            in1=Ep[b][:].rearrange("p (d n) -> p d n", d=2), op=ALU.mult),
                     R=[t_qk[b], t_Ep[b]], W=[t_qkt[b]])
                K.op('dve', lambda e, b=b: e.tensor_tensor(out=qkt[b][:, 2:4, :], in0=qk[b][:, 128:256].unsqueeze(1).to_broadcast([128, 2, 128]),
                                                            in1=Em[b][:].rearrange("p (d n) -> p d n", d=2), op=ALU.mult),
                     R=[t_qk[b], t_Em[b]], W=[t_qkt[b]])
            else:
                if c >= 2:
                    v4 = qk[b][:].rearrange("p (a s f) -> p a s f", a=4, s=2)
                    t1, t2 = v4[:, :, 0, :], v4[:, :, 1, :]
                    cs = cosT[:, c - 2, :].unsqueeze(1).to_broadcast([128, 4, 32])
                    sn = sinT[:, c - 2, :].unsqueeze(1).to_broadcast([128, 4, 32])
                    r = rt[b]
                    K.op('dve', lambda e, r=r, t1=t1, cs=cs: e.tensor_tensor(out=r[:, 0], in0=t1, in1=cs, op=ALU.mult), R=[t_qk[b], t_par], W=[t_rt[b]])
                    K.op('pool', lambda e, r=r, t2=t2, sn=sn: e.tensor_tensor(out=r[:, 1], in0=t2, in1=sn, op=ALU.mult), R=[t_qk[b], t_par], W=[t_rt[b]])
                    K.op('dve', lambda e, r=r, t1=t1, sn=sn: e.tensor_tensor(out=r[:, 2], in0=t1, in1=sn, op=ALU.mult), R=[t_qk[b], t_par], W=[t_rt[b]])
                    K.op('pool', lambda e, r=r, t2=t2, cs=cs: e.tensor_tensor(out=r[:, 3], in0=t2, in1=cs, op=ALU.mult), R=[t_qk[b], t_par], W=[t_rt[b]])
                    K.op('dve', lambda e, r=r, t1=t1: e.tensor_tensor(out=t1, in0=r[:, 0], in1=r[:, 1], op=ALU.subtract), R=[t_rt[b]], W=[t_qk[b]])
                    K.op('dve', lambda e, r=r, t2=t2: e.tensor_tensor(out=t2, in0=r[:, 2], in1=r[:, 3], op=ALU.add), R=[t_rt[b]], W=[t_qk[b]])
                K.op('dve', lambda e, b=b: e.tensor_tensor(
                    out=qkt[b][:, 0:2, :].rearrange("p d (h f) -> p d h f", h=2),
                    in0=qk[b][:, 0:128].rearrange("p (h f) -> p h f", h=2).unsqueeze(1).to_broadcast([128, 2, 2, 64]),
                    in1=EpR[:].unsqueeze(3).to_broadcast([128, 2, 2, 64]), op=ALU.mult), R=[t_qk[b], t_par], W=[t_qkt[b]])
                K.op('dve', lambda e, b=b: e.tensor_tensor(
                    out=qkt[b][:, 2:4, :].rearrange("p d (h f) -> p d h f", h=2),
                    in0=qk[b][:, 128:256].rearrange("p (h f) -> p h f", h=2).unsqueeze(1).to_broadcast([128, 2, 2, 64]),
                    in1=EmR[:].unsqueeze(3).to_broadcast([128, 2, 2, 64]), op=ALU.mult), R=[t_qk[b], t_par], W=[t_qkt[b]])

        def stC(c):
            b = c % 2
            pbh, pbht = g.psb_half()
            for s_ in range(4):
                K.op('pe', lambda e, s_=s_, b=b, pbh=pbh: e.transpose(out=pbh[:, s_ * 128:(s_ + 1) * 128], in_=qkt[b][:, s_, :], identity=g.identb[:]),
                     R=[t_qkt[b], g.t_const], W=[pbht])
            K.op('act', lambda e, c=c, pbh=pbh: e.activation(out=LT[:, :, c * 128:(c + 1) * 128], in_=pbh.rearrange("p (s n) -> p s n", s=4),
                                                              func=AF.Identity), R=[pbht], W=[t_LT[c]])
            pk, pkt = g.ps()
            for d in range(2):
                for h in range(H):
                    K.op('pe', lambda e, d=d, h=h, b=b, c=c, pk=pk: e.matmul(
                        pk[h * dk:(h + 1) * dk, d * 64:(d + 1) * 64], lhsT=qkt[b][:, 2 + d, h * dk:(h + 1) * dk],
                        rhs=vtok[:, c, h * 64:(h + 1) * 64], start=True, stop=True), R=[t_qkt[b], t_v[c]], W=[pkt])
            Gf, tGf = Gap(c, 0)
            Gb, tGb = Gap(c, 1)
            K.op('dve', lambda e, c=c: e.tensor_copy(out=Sbf[:, c, 0, :], in_=Sf[:]), R=[t_Sf], W=[t_S[c][0]])
            K.op('dve', lambda e, pk=pk: e.tensor_tensor(out=Sf[:], in0=Sf[:], in1=pk[:, 0:64], op=ALU.add), R=[pkt, t_Sf], W=[t_Sf])
            K.op('dve', lambda e, Gf=Gf: e.tensor_scalar_mul(out=Sf[:], in0=Sf[:], scalar1=Gf), R=[t_Sf, tGf], W=[t_Sf])
            K.op('dve', lambda e, c=c, pk=pk, Gb=Gb: e.tensor_scalar_mul(out=KVGb[:, c, :], in0=pk[:, 64:128], scalar1=Gb),
                 R=[pkt, tGb], W=[t_kvb[c]])

        stA(0)
        for c in range(NCH):
            if c + 1 < NCH:
                stA(c + 1)
            stB(c)
            stC(c)
        for c in [1, 0] + list(range(NCH - 1, 1, -1)):
            Gb, tGb = Gap(c, 1)
            K.op('dve', lambda e, c=c: e.tensor_copy(out=Sbf[:, c, 1, :], in_=Sb[:]), R=[t_Sb], W=[t_S[c][1]])
            K.op('dve', lambda e, c=c, Gb=Gb: e.scalar_tensor_tensor(out=Sb[:], in0=Sb[:], scalar=Gb, in1=KVGb[:, c, :], op0=ALU.mult, op1=ALU.add),
                 R=[t_Sb, tGb, t_kvb[c]], W=[t_Sb])

        att, t_att = dbl('att', [128, 2, 2, H, 128], BF16)
        osb, t_o = dbl('osb', [128, 2, H, 64])
        sq, t_sq = dbl('sq', [128, 2, H, 64])
        stat, t_st = dbl('stat', [128, 4, 2 * H])
        yb, t_yb = dbl('yb', [128, 2, NV], BF16)
        stg, t_stg = dbl('stg', [128, 2, 128], BF16)
        H2 = 2 * H

        def stP(pi, c0_):
            b = pi % 2
            for ci_ in range(2):
                c = c0_ + ci_
                cs = slice(c * 128, (c + 1) * 128)
                pos = []
                for h in range(H):
                    hr = slice(h * dk, (h + 1) * dk)
                    pz, pzt = g.ps()
                    for d in range(2):
                        K.op('pe', lambda e, d=d, hr=hr, pz=pz, cs=cs: e.matmul(
                            pz[:, d * 128:(d + 1) * 128], lhsT=LT[hr, 2 + d, cs], rhs=LT[hr, d, cs], start=True, stop=True),
                            R=[t_LT[c]], W=[pzt])
                    K.op('dve', lambda e, h=h, b=b, pz=pz, ci_=ci_: e.tensor_tensor(
                        out=att[b][:, ci_, :, h, :], in0=pz[:, 0:256].rearrange("p (d n) -> p d n", d=2),
                        in1=g.mask2[:], op=ALU.mult), R=[pzt, g.t_const], W=[t_att[b]])
                for h in range(H):
                    hr = slice(h * dk, (h + 1) * dk)
                    oc = slice(h * 64, (h + 1) * 64)
                    po, pot = g.ps()
                    K.op('pe', lambda e, h=h, b=b, oc=oc, po=po, c=c, ci_=ci_: e.matmul(po[:, 0:64], lhsT=att[b][:, ci_, 0, h, :], rhs=vtok[:, c, oc], start=True, stop=False),
                         R=[t_att[b], t_v[c]], W=[pot])
                    K.op('pe', lambda e, h=h, b=b, oc=oc, po=po, c=c, ci_=ci_: e.matmul(po[:, 0:64], lhsT=att[b][:, ci_, 1, h, :], rhs=vtok[:, c, oc], start=False, stop=False),
                         R=[t_att[b], t_v[c]], W=[pot])
                    K.op('pe', lambda e, hr=hr, po=po, c=c, cs=cs: e.matmul(po[:, 0:64], lhsT=LT[hr, 0, cs], rhs=Sbf[hr, c, 0, :], start=False, stop=False),
                         R=[t_LT[c], t_S[c][0]], W=[pot])
                    K.op('pe', lambda e, hr=hr, po=po, c=c, cs=cs: e.matmul(po[:, 0:64], lhsT=LT[hr, 1, cs], rhs=Sbf[hr, c, 1, :], start=False, stop=True),
                         R=[t_LT[c], t_S[c][1]], W=[pot])
                    K.op('act', lambda e, po=po, b=b, h=h, ci_=ci_: e.activation(out=osb[b][:, ci_, h, :], in_=po[:, 0:64], func=AF.Identity), R=[pot], W=[t_o[b]])

        def stQ(pi, c0_):
            b = pi % 2
            o3 = osb[b][:].rearrange("p c h f -> p (c h) f")
            s4 = stat[b]
            sq3 = sq[b][:].rearrange("p c h f -> p (c h) f")
            if not gla:
                K.op('dve', lambda e: e.tensor_reduce(out=s4[:, 0, :], in_=o3, axis=AX.X, op=ALU.add), R=[t_o[b]], W=[t_st[b]])
                K.op('dve', lambda e: e.tensor_scalar_mul(out=s4[:, 0, :], in0=s4[:, 0, :], scalar1=1.0 / 64), R=[t_st[b]], W=[t_st[b]])
                K.op('dve', lambda e: e.tensor_tensor(out=o3, in0=o3, in1=s4[:, 0, :].unsqueeze(2).to_broadcast([128, H2, 64]), op=ALU.subtract),
                     R=[t_o[b], t_st[b]], W=[t_o[b]])
            K.op('pool', lambda e: e.tensor_tensor(out=sq3, in0=o3, in1=o3, op=ALU.mult), R=[t_o[b]], W=[t_sq[b]])
            K.op('dve', lambda e: e.tensor_reduce(out=s4[:, 1, :], in_=sq3, axis=AX.X, op=ALU.add), R=[t_sq[b]], W=[t_st[b]])
            K.op('act', lambda e: e.activation(out=s4[:, 2, :], in_=s4[:, 1, :], func=AF.Ln, scale=1.0 / 64, bias=g.epsc[:, 0:1]), R=[t_st[b], g.t_const], W=[t_st[b]])
            K.op('act', lambda e: e.activation(out=s4[:, 3, :], in_=s4[:, 2, :], func=AF.Exp, scale=-0.5), R=[t_st[b]], W=[t_st[b]])
            K.op('dve', lambda e: e.tensor_tensor(out=o3, in0=o3, in1=s4[:, 3, :].unsqueeze(2).to_broadcast([128, H2, 64]), op=ALU.mult),
                 R=[t_o[b], t_st[b]], W=[t_o[b]])
            o2 = osb[b][:].rearrange("p c h f -> p c (h f)")
            if gla:
                K.op('pool', lambda e: e.tensor_tensor(out=o2, in0=o2, in1=ngb[:].unsqueeze(1).to_broadcast([128, 2, NV]), op=ALU.mult), R=[t_o[b], t_par], W=[t_o[b]])
            else:
                K.op('pool', lambda e: e.tensor_tensor(out=o2, in0=o2, in1=gng[:].unsqueeze(1).to_broadcast([128, 2, NV]), op=ALU.mult), R=[t_o[b], t_par], W=[t_o[b]])
                K.op('pool', lambda e: e.tensor_tensor(out=o2, in0=o2, in1=gnb[:].unsqueeze(1).to_broadcast([128, 2, NV]), op=ALU.add), R=[t_o[b], t_par], W=[t_o[b]])
            K.op('dve', lambda e: e.tensor_tensor(out=yb[b][:], in0=o2, in1=sgtok[:, c0_:c0_ + 2, :], op=ALU.mult), R=[t_o[b], t_sg[c0_], t_sg[c0_ + 1]], W=[t_yb[b]])
            pbh, pbht = g.psb_half()
            for ci_ in range(2):
                K.op('pe', lambda e, ci_=ci_, pbh=pbh: e.transpose(out=pbh[:, ci_ * 128:(ci_ + 1) * 128], in_=yb[b][:, ci_, :], identity=g.identb[:]),
                     R=[t_yb[b], g.t_const], W=[pbht])
            K.op('act', lambda e, pbh=pbh: e.activation(out=stg[b][:].rearrange("p c n -> p (c n)"), in_=pbh[:, 0:256], func=AF.Identity), R=[pbht], W=[t_stg[b]])
            K.dma(D['mixT_d'][row0:row0 + NV, c0_ * 128:(c0_ + 2) * 128], stg[b][:].rearrange("p c n -> p (c n)"), R=[t_stg[b]], W=[g.t_mix], q='pool')

        pairs = list(range(2, NCH, 2) if last else range(0, NCH, 2))
        stP(0, pairs[0])
        for pi, c0_ in enumerate(pairs):
            if pi + 1 < len(pairs):
                stP(pi + 1, pairs[pi + 1])
            stQ(pi, c0_)


TWO_PI = 2.0 * math.pi


def phase_s5(g, l):
    nc, K, D = g.nc, g.K, g.D
    NP = (T + 511) // 512
    pieces = [(i * 512, min(512, T - i * 512)) for i in range(NP)]
    with ExitStack() as st:
        def sb(name, shape, dt=F32):
            return st.enter_context(nc.sbuf_tensor(_nm(name), list(shape), dt))
        W = D['w_in'][l]
        wb, t_w = load_w_bf16(g, st, [(W, OFF['s5_u'], 256)], 256, 'w_s5')
        t_par = Tok()
        lam = sb('lam', [128, 16, 2]); dtc = sb('dtc', [128, 16])
        K.dma(lam[:], D['s5_lam_fm'][l, :, :, :], W=[t_par])
        K.dma(dtc[:], D['s5_dt_fm'][l, :, :], W=[t_par])
        CT = sb('CT', [128, 16, 2, 32])
        K.dma(CT[:], D['s5_CT'][l].rearrange("t r k m -> k t r m"), W=[t_par])
        dcol = sb('dcol', [128, 2]); glub = sb('glub', [128, 2])
        K.dma(dcol[:], D['s5_dcol'][l, :, :], W=[t_par])
        K.dma(glub[:], D['s5_glub'][l, :, :], W=[t_par])
        gw32 = sb('gw32', [128, 2, 256]); gwb = sb('gwb', [128, 2, 256], BF16)
        K.dma(gw32[:], D['s5_glu_w'][l].rearrange("(kt p) n -> p kt n", p=128), W=[t_par])
        K.op('dve', lambda e: e.tensor_copy(out=gwb[:], in_=gw32[:]), R=[t_par], W=[t_par])
        P_ = {}
        for nm_ in ('dt', 'a', 'th', 'r', 'u', 'ui', 'fr', 'sn', 'u2', 'ui2', 'fr2', 'cs', 'x', 'y', 'den', 'rden', 't1', 't2', 'cr', 'ci', 'ncr', 'thn'):
            P_[nm_] = sb('s5p_' + nm_, [128, 16], I32 if nm_ in ('ui', 'ui2') else F32)

        def dv(fn, rd=True):
            K.op('dve', fn, R=[t_par, g.t_const], W=[t_par])

        def ac(fn):
            K.op('act', fn, R=[t_par, g.t_const], W=[t_par])
        lre, lim = lam[:, :, 0], lam[:, :, 1]
        ac(lambda e: e.activation(out=P_['dt'][:], in_=dtc[:], func=AF.Exp))
        dv(lambda e: e.tensor_tensor(out=P_['a'][:], in0=lre, in1=P_['dt'][:], op=ALU.mult))
        dv(lambda e: e.tensor_tensor(out=P_['th'][:], in0=lim, in1=P_['dt'][:], op=ALU.mult))
        ac(lambda e: e.activation(out=P_['r'][:], in_=P_['a'][:], func=AF.Exp))
        dv(lambda e: e.tensor_scalar_mul(out=P_['u'][:], in0=P_['th'][:], scalar1=1.0 / TWO_PI))
        dv(lambda e: e.tensor_copy(out=P_['ui'][:], in_=P_['u'][:]))
        dv(lambda e: e.tensor_tensor(out=P_['fr'][:], in0=P_['u'][:], in1=P_['ui'][:], op=ALU.subtract))
        ac(lambda e: e.activation(out=P_['sn'][:], in_=P_['fr'][:], func=AF.Sin, scale=TWO_PI))
        dv(lambda e: e.tensor_scalar_add(out=P_['u2'][:], in0=P_['u'][:], scalar1=0.25))
        dv(lambda e: e.tensor_copy(out=P_['ui2'][:], in_=P_['u2'][:]))
        dv(lambda e: e.tensor_tensor(out=P_['fr2'][:], in0=P_['u2'][:], in1=P_['ui2'][:], op=ALU.subtract))
        ac(lambda e: e.activation(out=P_['cs'][:], in_=P_['fr2'][:], func=AF.Sin, scale=TWO_PI))
        dv(lambda e: e.tensor_tensor(out=P_['x'][:], in0=P_['r'][:], in1=P_['cs'][:], op=ALU.mult))
        dv(lambda e: e.tensor_scalar_add(out=P_['x'][:], in0=P_['x'][:], scalar1=-1.0))
        dv(lambda e: e.tensor_tensor(out=P_['y'][:], in0=P_['r'][:], in1=P_['sn'][:], op=ALU.mult))
        dv(lambda e: e.tensor_tensor(out=P_['t1'][:], in0=lre, in1=lre, op=ALU.mult))
        dv(lambda e: e.tensor_tensor(out=P_['t2'][:], in0=lim, in1=lim, op=ALU.mult))
        dv(lambda e: e.tensor_tensor(out=P_['den'][:], in0=P_['t1'][:], in1=P_['t2'][:], op=ALU.add))
        dv(lambda e: e.reciprocal(out=P_['rden'][:], in_=P_['den'][:]))
        dv(lambda e: e.tensor_tensor(out=P_['t1'][:], in0=P_['x'][:], in1=lre, op=ALU.mult))
        dv(lambda e: e.tensor_tensor(out=P_['t2'][:], in0=P_['y'][:], in1=lim, op=ALU.mult))
        dv(lambda e: e.tensor_tensor(out=P_['cr'][:], in0=P_['t1'][:], in1=P_['t2'][:], op=ALU.add))
        dv(lambda e: e.tensor_tensor(out=P_['cr'][:], in0=P_['cr'][:], in1=P_['rden'][:], op=ALU.mult))
        dv(lambda e: e.tensor_tensor(out=P_['t1'][:], in0=P_['y'][:], in1=lre, op=ALU.mult))
        dv(lambda e: e.tensor_tensor(out=P_['t2'][:], in0=P_['x'][:], in1=lim, op=ALU.mult))
        dv(lambda e: e.tensor_tensor(out=P_['ci'][:], in0=P_['t1'][:], in1=P_['t2'][:], op=ALU.subtract))
        dv(lambda e: e.tensor_tensor(out=P_['ci'][:], in0=P_['ci'][:], in1=P_['rden'][:], op=ALU.mult))
        dv(lambda e: e.tensor_scalar_mul(out=P_['thn'][:], in0=P_['th'][:], scalar1=1.0 / TWO_PI))
        Ce = sb('Ce', [128, 16, 2, 32]); Ceb = sb('Ceb', [128, 16, 2, 32], BF16); tmpC = sb('tmpC', [128, 16, 32])
        crb = P_['cr'][:].unsqueeze(2).to_broadcast([128, 16, 32])
        cib = P_['ci'][:].unsqueeze(2).to_broadcast([128, 16, 32])
        dv(lambda e: e.tensor_tensor(out=Ce[:, :, 0, :], in0=CT[:, :, 0, :], in1=crb, op=ALU.mult))
        dv(lambda e: e.tensor_tensor(out=tmpC[:], in0=CT[:, :, 1, :], in1=cib, op=ALU.mult))
        dv(lambda e: e.tensor_tensor(out=Ce[:, :, 0, :], in0=Ce[:, :, 0, :], in1=tmpC[:], op=ALU.subtract))
        dv(lambda e: e.tensor_tensor(out=Ce[:, :, 1, :], in0=CT[:, :, 0, :], in1=cib, op=ALU.mult))
        dv(lambda e: e.tensor_tensor(out=tmpC[:], in0=CT[:, :, 1, :], in1=crb, op=ALU.mult))
        dv(lambda e: e.tensor_tensor(out=Ce[:, :, 1, :], in0=Ce[:, :, 1, :], in1=tmpC[:], op=ALU.add))
        dv(lambda e: e.tensor_scalar_mul(out=Ce[:, :, 1, :], in0=Ce[:, :, 1, :], scalar1=-1.0))
        dv(lambda e: e.tensor_copy(out=Ceb[:], in_=Ce[:]))

        uTf = sb('uTf', [128, 2, T], BF16); t_uf = Tok()
        st2 = ExitStack()

        def sb2(name, shape, dt=F32):
            return st2.enter_context(nc.sbuf_tensor(_nm(name), list(shape), dt))
        yT = sb('yT', [128, 2, T], BF16); t_y = Tok()
        st_p = ExitStack()
        ucs = [st_p.enter_context(nc.sbuf_tensor(_nm('ucs%d' % i), [128, 4, 8, 128], BF16)) for i in range(2)]; t_ucs = [Tok(), Tok()]
        for gi_, c4 in enumerate(range(0, NCH, 4)):
            cc = list(range(c4, min(c4 + 4, NCH)))
            ncc = len(cc)
            pts = [g.ps(), g.ps()]
            b = gi_ % 2
            K.dma(ucs[b][:, 0:ncc], D['uT_d'][c4:c4 + ncc].rearrange("c p k n -> p c k n"), R=[g.t_uT[c] for c in cc], W=[t_ucs[b]])
            for ft in range(2):
                pt, ptk = pts[ft]
                for kt in range(8):
                    K.op('pe', lambda e, kt=kt, b=b, ft=ft, pt=pt, ncc=ncc: e.matmul(
                        pt[:, 0:ncc * 128].rearrange("p (c n) -> p c n", c=ncc), lhsT=wb[:, kt, ft * 128:(ft + 1) * 128], rhs=ucs[b][:, 0:ncc, kt, :],
                        start=(kt == 0), stop=(kt == 7)), R=[t_w, t_ucs[b]], W=[ptk])
            n = len(cc) * 128
            for ft in range(2):
                pt, ptk = pts[ft]
                K.op('act', lambda e, ft=ft, pt=pt, c4=c4, n=n: e.activation(out=uTf[:, ft, c4 * 128:c4 * 128 + n], in_=pt[:, 0:n], func=AF.Identity),
                     R=[ptk], W=[t_uf])

        K.barrier()
        st_p.close()
        uTb = sb2('uTb', [128, 1, T], BF16); t_ub = Tok()
        iot = sb2('iot', [128, 544])
        K.dma(iot[:], D['iota_t'][0:544].partition_broadcast(128), W=[t_par])
        offc = sb2('offc', [128, 16, 8, 2])
        for q4 in range(8):
            for which, off in ((0, 0.0), (1, 0.25)):
                dv(lambda e, q4=q4, which=which, off=off: e.tensor_scalar(out=offc[:, :, q4, which], in0=P_['thn'][:], scalar1=float(q4 * 544), scalar2=off,
                                                                         op0=ALU.mult, op1=ALU.add))
        cosT = sb2('s5cos', [128, T]); sinT = sb2('s5sin', [128, T]); t_tab = Tok()
        uus = [sb2('s5uu%d' % i, [128, 544]) for i in range(2)]; uis = [sb2('s5ui%d' % i, [128, 544], I32) for i in range(2)]; t_uus = [Tok(), Tok()]
        _tc = [0]
        cre = sb2('s5cre', [128, T]); cim = sb2('s5cim', [128, T]); t_c = Tok()
        hh = sb2('s5h', [128, 2, 2, T], BF16); t_h = [Tok(), Tok()]
        BT32 = sb2('BT32', [128, 2, 128]); BTb = sb2('BTb', [128, 2, 128], BF16); t_B = Tok()
        m1a = [sb2('s5m%d' % i, [128, 512]) for i in range(8)]; t_ma = [Tok() for _ in range(8)]
        _pc = [0]
        for j in range(8):
            ft = j // 4
            if j % 4 == 0:
                K.op('pool', lambda e, ft=ft: e.tensor_copy(out=uTb[:, 0, 0:CTXL], in_=uTf[:, ft, CTXL - 1::-1]), R=[t_uf], W=[t_ub])
                K.op('pool', lambda e, ft=ft: e.tensor_copy(out=uTb[:, 0, CTXL:T], in_=uTf[:, ft, T - 1:CTXL - 1:-1]), R=[t_uf], W=[t_ub])
            for d in range(2):
                col = d * 8 + j
                usrc, t_us, uft = (uTf, t_uf, ft) if d == 0 else (uTb, t_ub, 0)
                K.dma(BT32[:], D['s5_BT'][l, col].rearrange("r k m -> k r m"), W=[t_B])
                K.op('pool', lambda e: e.tensor_copy(out=BTb[:], in_=BT32[:]), R=[t_B], W=[t_B])
                for which, tab, off in ((0, sinT, 0.0), (1, cosT, 0.25)):
                    for q4 in range(8):
                        qs = slice(q4 * 544, (q4 + 1) * 544)
                        _b = _tc[0] % 2
                        _tc[0] += 1
                        uu, ui, t_uu = uus[_b], uis[_b], t_uus[_b]
                        K.op('dve', lambda e, col=col, which=which, q4=q4, uu=uu: e.tensor_scalar(out=uu[:], in0=iot[:], scalar1=P_['thn'][:, col:col + 1],
                                                                                  scalar2=offc[:, col, q4, which:which + 1], op0=ALU.mult, op1=ALU.add), R=[t_par], W=[t_uu])
                        K.op('dve', lambda e, uu=uu, ui=ui: e.tensor_copy(out=ui[:], in_=uu[:]), R=[t_uu], W=[t_uu])
                        K.op('pool', lambda e, uu=uu, ui=ui: e.tensor_tensor(out=uu[:], in0=uu[:], in1=ui[:], op=ALU.subtract), R=[t_uu], W=[t_uu])
                        K.op('act', lambda e, tab=tab, qs=qs, uu=uu: e.activation(out=tab[:, qs], in_=uu[:], func=AF.Sin, scale=TWO_PI), R=[t_uu], W=[t_tab])
                for (p0, n) in pieces:
                    pr, prt = g.ps()
                    pi_, pit = g.ps()
                    K.op('pe', lambda e, pr=pr, p0=p0, n=n, usrc=usrc, uft=uft: e.matmul(pr[:, 0:n], lhsT=BTb[:, 0, :], rhs=usrc[:, uft, p0:p0 + n], start=True, stop=True),
                         R=[t_B, t_us], W=[prt])
                    K.op('pe', lambda e, pi_=pi_, p0=p0, n=n, usrc=usrc, uft=uft: e.matmul(pi_[:, 0:n], lhsT=BTb[:, 1, :], rhs=usrc[:, uft, p0:p0 + n], start=True, stop=True),
                         R=[t_B, t_us], W=[pit])
                    sl = slice(p0, p0 + n)
                    _o = 4 * (_pc[0] % 2)
                    _pc[0] += 1
                    m1 = m1a[_o:_o + 4]
                    t_m = t_ma[_o:_o + 4]
                    K.op('dve', lambda e, pr=pr, sl=sl, n=n, m1=m1: e.tensor_tensor(out=m1[0][:, 0:n], in0=pr[:, 0:n], in1=cosT[:, sl], op=ALU.mult), R=[prt, t_tab], W=[t_m[0]])
                    K.op('dve', lambda e, pi_=pi_, sl=sl, n=n, m1=m1: e.tensor_tensor(out=m1[1][:, 0:n], in0=pi_[:, 0:n], in1=sinT[:, sl], op=ALU.mult), R=[pit, t_tab], W=[t_m[1]])
                    K.op('dve', lambda e, pi_=pi_, sl=sl, n=n, m1=m1: e.tensor_tensor(out=m1[2][:, 0:n], in0=pi_[:, 0:n], in1=cosT[:, sl], op=ALU.mult), R=[pit, t_tab], W=[t_m[2]])
                    K.op('dve', lambda e, pr=pr, sl=sl, n=n, m1=m1: e.tensor_tensor(out=m1[3][:, 0:n], in0=pr[:, 0:n], in1=sinT[:, sl], op=ALU.mult), R=[prt, t_tab], W=[t_m[3]])
                    K.op('pool', lambda e, sl=sl, n=n, m1=m1: e.tensor_tensor(out=cre[:, sl], in0=m1[0][:, 0:n], in1=m1[1][:, 0:n], op=ALU.add), R=[t_m[0], t_m[1]], W=[t_c])
                    K.op('pool', lambda e, sl=sl, n=n, m1=m1: e.tensor_tensor(out=cim[:, sl], in0=m1[2][:, 0:n], in1=m1[3][:, 0:n], op=ALU.subtract), R=[t_m[2], t_m[3]], W=[t_c])
                rb = P_['r'][:, col:col + 1].to_broadcast([128, T])
                K.op('dve', lambda e, rb=rb: e.tensor_tensor_scan(out=cre[:], data0=rb, data1=cre[:], initial=0.0, op0=ALU.mult, op1=ALU.add), R=[t_c, t_par], W=[t_c])
                K.op('dve', lambda e, rb=rb: e.tensor_tensor_scan(out=cim[:], data0=rb, data1=cim[:], initial=0.0, op0=ALU.mult, op1=ALU.add), R=[t_c, t_par], W=[t_c])
                if d == 0:
                    segs = [(slice(0, T), slice(0, T))]
                else:
                    segs = [(slice(0, CTXL), slice(CTXL - 1, None, -1)), (slice(CTXL, T), slice(T - 1, CTXL - 1, -1))]
                for (so, si) in segs:
                    n = so.stop - so.start
                    for q0 in range(0, n, 1024):
                        qn = min(1024, n - q0)
                        o_sl = slice(so.start + q0, so.start + q0 + qn)
                        if d == 0:
                            i_sl = o_sl
                        else:
                            hi = si.start - q0
                            lo = hi - qn
                            i_sl = slice(hi, lo if lo >= 0 else None, -1)
                        for h0 in range(0, qn, 512):
                            hn = min(512, qn - h0)
                            oo = slice(o_sl.start + h0, o_sl.start + h0 + hn)
                            _o = 4 * (_pc[0] % 2)
                            _pc[0] += 1
                            mA, mB, mC, mD = m1a[_o:_o + 4]
                            t_m = t_ma[_o:_o + 4]
                            if d == 0:
                                ii = oo
                            else:
                                a_ = i_sl.start - h0
                                b_ = a_ - hn
                                ii = slice(a_, b_ if b_ >= 0 else None, -1)
                            K.op('dve', lambda e, ii=ii, hn=hn: e.tensor_tensor(out=mA[:, 0:hn], in0=cre[:, ii], in1=cosT[:, ii], op=ALU.mult), R=[t_c, t_tab], W=[t_m[0]])
                            K.op('pool', lambda e, ii=ii, hn=hn: e.tensor_tensor(out=mB[:, 0:hn], in0=cim[:, ii], in1=sinT[:, ii], op=ALU.mult), R=[t_c, t_tab], W=[t_m[1]])
                            K.op('dve', lambda e, ii=ii, hn=hn: e.tensor_tensor(out=mC[:, 0:hn], in0=cre[:, ii], in1=sinT[:, ii], op=ALU.mult), R=[t_c, t_tab], W=[t_m[2]])
                            K.op('pool', lambda e, ii=ii, hn=hn: e.tensor_tensor(out=mD[:, 0:hn], in0=cim[:, ii], in1=cosT[:, ii], op=ALU.mult), R=[t_c, t_tab], W=[t_m[3]])
                            K.op('dve', lambda e, oo=oo, hn=hn, d=d: e.tensor_tensor(out=hh[:, d, 0, oo], in0=mA[:, 0:hn], in1=mB[:, 0:hn], op=ALU.subtract), R=[t_m[0], t_m[1]], W=[t_h[d]])
                            K.op('dve', lambda e, oo=oo, hn=hn, d=d: e.tensor_tensor(out=hh[:, d, 1, oo], in0=mC[:, 0:hn], in1=mD[:, 0:hn], op=ALU.add), R=[t_m[2], t_m[3]], W=[t_h[d]])
            for (p0, n) in pieces:
                py, pyt = g.ps()
                k = 0
                for d in range(2):
                    for r_ in range(2):
                        K.op('pe', lambda e, d=d, r_=r_, py=py, p0=p0, n=n, k=k, j=j: e.matmul(
                            py[0:32, 0:n], lhsT=Ceb[:, d * 8 + j, r_, :], rhs=hh[:, d, r_, p0:p0 + n], start=(k == 0), stop=(k == 3)),
                            R=[t_par, t_h[d]], W=[pyt])
                        k += 1
                rows = slice(32 * (j % 4), 32 * (j % 4) + 32)
                K.op('act', lambda e, py=py, p0=p0, n=n, rows=rows, ft=ft: e.activation(out=yT[rows, ft, p0:p0 + n], in_=py[0:32, 0:n], func=AF.Identity),
                     R=[pyt], W=[t_y])
        K.barrier()
        st2.close()
        gT = sb('gT', [128, 2, T], BF16); t_g = Tok()
        ytmp = [sb('ytmp%d' % i, [128, 512]) for i in range(2)]; t_yt = [Tok(), Tok()]
        k = 0
        for ft in range(2):
            for (p0, n) in pieces:
                b = k % 2
                k += 1
                K.op('dve', lambda e, b=b, ft=ft, p0=p0, n=n: e.scalar_tensor_tensor(
                    out=ytmp[b][:, 0:n], in0=uTf[:, ft, p0:p0 + n], scalar=dcol[:, ft:ft + 1], in1=yT[:, ft, p0:p0 + n], op0=ALU.mult, op1=ALU.add),
                    R=[t_uf, t_y, t_par], W=[t_yt[b]])
                K.op('act', lambda e, b=b, ft=ft, p0=p0, n=n: e.activation(out=gT[:, ft, p0:p0 + n], in_=ytmp[b][:, 0:n], func=AF.Gelu), R=[t_yt[b]], W=[t_g])
        ob = [sb('s5ob%d' % i, [128, 512], BF16) for i in range(2)]; t_ob = [Tok(), Tok()]
        sg = [sb('s5sg%d' % i, [128, 512]) for i in range(2)]; t_sgm = [Tok(), Tok()]
        k = 0
        for ft in range(2):
            for (p0, n) in pieces:
                b = k % 2
                k += 1
                pz, pzt = g.ps()
                for kt in range(2):
                    K.op('pe', lambda e, kt=kt, ft=ft, pz=pz, p0=p0, n=n: e.matmul(pz[:, 0:n], lhsT=gwb[:, kt, ft * 128:(ft + 1) * 128], rhs=gT[:, kt, p0:p0 + n],
                                                                                   start=(kt == 0), stop=(kt == 1)), R=[t_par, t_g], W=[pzt])
                K.op('act', lambda e, b=b, pz=pz, n=n, ft=ft: e.activation(out=sg[b][:, 0:n], in_=pz[:, 0:n], func=AF.Sigmoid, bias=glub[:, ft:ft + 1]), R=[pzt, t_par], W=[t_sgm[b]])
                K.op('dve', lambda e, b=b, ft=ft, p0=p0, n=n: e.tensor_tensor(out=ob[b][:, 0:n], in0=gT[:, ft, p0:p0 + n], in1=sg[b][:, 0:n], op=ALU.mult), R=[t_g, t_sgm[b]], W=[t_ob[b]])
                K.dma(D['mixT_d'][512 + ft * 128:512 + (ft + 1) * 128, p0:p0 + n], ob[b][:, 0:n], R=[t_ob[b]], W=[g.t_mix], q='pool')


def phase_hyena(g, l, n, c0, sfx):
    nc, K, D = g.nc, g.K, g.D
    NT = n // 128
    NKT = NT + 1
    with ExitStack() as st:
        def sb(name, shape, dt=F32):
            return st.enter_context(nc.sbuf_tensor(_nm(name), list(shape), dt))
        W = D['w_in'][l]
        t_par = Tok()
        cw = sb('hy_cw', [128, 6, 3]); cb = sb('hy_cb', [128, 6])
        K.dma(cw[:], D['hy_cw'][l, :, :, :], W=[t_par])
        K.dma(cb[:], D['hy_cb'][l, :, :], W=[t_par])
        hbias = sb('hy_bias', [128, 2])
        K.dma(hbias[:], D['hy_biasc'][l, :, :], W=[t_par])
        zT = sb('hy_zT', [128, 2, n], BF16); t_z = Tok()
        x0T = sb('hy_x0T', [128, 2, n], BF16); t_x0 = Tok()
        data = sb('hy_data', [128, NT, 768], BF16); t_data = [Tok() for _ in range(NT)]
        rnorm = sb('hy_rnorm', [128, 2]); t_rn = Tok()
        with ExitStack() as st2:
            def sb2(name, shape, dt=F32):
                return st2.enter_context(nc.sbuf_tensor(_nm(name), list(shape), dt))
            wb, t_w = load_w_bf16(g, st2, [(W, OFF['hy_p'], 768)], 768, 'w_hy')
            ucs = [sb2('hucs%d' % i, [128, 4, 8, 128], BF16) for i in range(2)]; t_ucs = [Tok(), Tok()]
            _ug = [0]
            pp = [sb2('hy_pp%d' % i, [128, n + 2]) for i in range(2)]; t_pp = [Tok(), Tok()]
            sv = [sb2('hy_sv%d' % i, [128, n]) for i in range(2)]; t_sv = [Tok(), Tok()]
            for i in range(2):
                K.op('pool', lambda e, i=i: e.memset(pp[i][:, 0:1], 0.0), W=[t_pp[i]])
                K.op('pool', lambda e, i=i: e.memset(pp[i][:, n + 1:n + 2], 0.0), W=[t_pp[i]])

            _sub = int(os.environ.get('HY_SUB', '9'))

            def proj_conv(ft, slot):
                for c4 in range(0, NT, 4):
                    cc = list(range(c4, min(c4 + 4, NT)))
                    ncc = len(cc)
                    pt, ptk = g.ps()
                    b = _ug[0] % 2
                    _ug[0] += 1
                    K.dma(ucs[b][:, 0:ncc], D['uT_d'][c0 + c4:c0 + c4 + ncc].rearrange("c p k n -> p c k n"),
                          R=[g.t_uT[c0 + c] for c in cc], W=[t_ucs[b]])
                    for kt in range(8):
                        K.op('pe', lambda e, kt=kt, b=b, pt=pt, ncc=ncc: e.matmul(
                            pt[:, 0:ncc * 128].rearrange("p (c n) -> p c n", c=ncc), lhsT=wb[:, kt, ft * 128:(ft + 1) * 128], rhs=ucs[b][:, 0:ncc, kt, :],
                            start=(kt == 0), stop=(kt == 7)), R=[t_w, t_ucs[b]], W=[ptk])
                    nn = len(cc) * 128
                    K.op('act', lambda e, pt=pt, c4=c4, nn=nn: e.activation(out=pp[slot][:, 1 + c4 * 128:1 + c4 * 128 + nn], in_=pt[:, 0:nn], func=AF.Identity),
                         R=[ptk], W=[t_pp[slot]])
                if _sub < 2:
                    return
                for q0 in range(0, n, 2048):
                    qn = min(2048, n - q0)
                    K.op('dve', lambda e, q0=q0, qn=qn: e.tensor_scalar(out=sv[slot][:, q0:q0 + qn], in0=pp[slot][:, q0:q0 + qn], scalar1=cw[:, ft, 0:1], scalar2=cb[:, ft:ft + 1],
                                                                         op0=ALU.mult, op1=ALU.add), R=[t_pp[slot], t_par], W=[t_sv[slot]])
                    K.op('dve', lambda e, q0=q0, qn=qn: e.scalar_tensor_tensor(out=sv[slot][:, q0:q0 + qn], in0=pp[slot][:, q0 + 1:q0 + 1 + qn], scalar=cw[:, ft, 1:2],
                                                                                 in1=sv[slot][:, q0:q0 + qn], op0=ALU.mult, op1=ALU.add), R=[t_pp[slot], t_par, t_sv[slot]], W=[t_sv[slot]])
                    K.op('dve', lambda e, q0=q0, qn=qn: e.scalar_tensor_tensor(out=sv[slot][:, q0:q0 + qn], in0=pp[slot][:, q0 + 2:q0 + 2 + qn], scalar=cw[:, ft, 2:3],
                                                                                 in1=sv[slot][:, q0:q0 + qn], op0=ALU.mult, op1=ALU.add), R=[t_pp[slot], t_par, t_sv[slot]], W=[t_sv[slot]])
            for ci in range(2):
                proj_conv(2 + ci, 0)
                if _sub < 3:
                    break
                proj_conv(4 + ci, 1)
                K.op('pool', lambda e, ci=ci: e.tensor_tensor(out=zT[:, ci, :], in0=sv[0][:], in1=sv[1][:], op=ALU.mult), R=[t_sv[0], t_sv[1]], W=[t_z])
                if _sub < 4:
                    break
                proj_conv(ci, 0)
                K.op('pool', lambda e, ci=ci: e.tensor_copy(out=x0T[:, ci, :], in_=sv[0][:]), R=[t_sv[0]], W=[t_x0])
            for i in (range(NT) if _sub >= 6 else [int(x) for x in os.environ.get("HY_IT", "0,1").split(",") if int(x) < NT]) if _sub >= 5 else []:
                pbh, pbht = g.psb_half()
                for ci in range(2):
                    K.op('pe', lambda e, ci=ci, i=i, pbh=pbh: e.transpose(out=pbh[:, ci * 128:(ci + 1) * 128], in_=zT[:, ci, i * 128:(i + 1) * 128], identity=g.identb[:]),
                         R=[t_z, g.t_const], W=[pbht])
                K.op('act', lambda e, i=i, pbh=pbh: e.activation(out=data[:, i, 0:256], in_=pbh[:, 0:256], func=AF.Identity), R=[pbht], W=[t_data[i]])
            K.barrier()
        _hs = int(os.environ.get('HY_STAGE', '9'))
        if _hs < 2:
            return
        with ExitStack() as st2:
            def sb2(name, shape, dt=F32):
                return st2.enter_context(nc.sbuf_tensor(_nm(name), list(shape), dt))
            zemb = sb2('hy_zemb', [33, n]); fw1 = sb2('hy_fw1', [33, 64]); fw2 = sb2('hy_fw2', [64, 64]); fw3 = sb2('hy_fw3', [64, 512])
            fcol = sb2('hy_fcol', [64, 5]); fs = sb2('hy_fs', [64, 2])
            K.dma(zemb[:], D['hy_zemb' + sfx][:, :], W=[t_par])
            K.dma(fw1[:], D['hy_fw1'][l, :, :], W=[t_par])
            K.dma(fw2[:], D['hy_fw2'][l, :, :], W=[t_par])
            K.dma(fw3[:], D['hy_fw3'][l, :, :], W=[t_par])
            K.dma(fcol[:], D['hy_fcol'][l, :, :], W=[t_par])
            K.op('dve', lambda e: e.tensor_scalar_mul(out=fs[:, 0:1], in0=fcol[:, 2:3], scalar1=1.0 / TWO_PI), R=[t_par], W=[t_par])
            absd = sb2('hy_absd', [128, 512])
            K.dma(absd[:], D['hy_decay'][l].rearrange("d c -> (d c)").partition_broadcast(128), W=[t_par])
            K.op('dve', lambda e: e.scalar_tensor_tensor(out=absd[:], in0=absd[:], scalar=-1.0, in1=absd[:], op0=ALU.mult, op1=ALU.max), R=[t_par], W=[t_par])
            tn = sb2('hy_tn', [128, NT])
            K.dma(tn[:], D['hy_tn' + sfx][:, :], W=[t_par])
            K.op('dve', lambda e: e.tensor_scalar_mul(out=tn[:], in0=tn[:], scalar1=-1.0), R=[t_par], W=[t_par])
            h1 = sb2('hy_h1', [64, n]); h2 = sb2('hy_h2', [64, n]); t_h1 = Tok(); t_h2 = Tok()
            uus = [sb2('hy_uu%d' % i, [64, 512]) for i in range(2)]; uis = [sb2('hy_ui%d' % i, [64, 512], I32) for i in range(2)]; t_uus = [Tok(), Tok()]
            _fc = [0]
            for (src, t_src, wgt, bcol, dst, t_dst) in ((zemb, t_par, fw1, 0, h1, t_h1), (h1, t_h1, fw2, 1, h2, t_h2)):
                for q0 in range(0, n, 512):
                    qn = min(512, n - q0)
                    pt, ptk = g.ps()
                    uu, ui, t_uu = uus[_fc[0] % 2], uis[_fc[0] % 2], t_uus[_fc[0] % 2]
                    _fc[0] += 1
                    K.op('pe', lambda e, pt=pt, q0=q0, qn=qn, src=src, wgt=wgt: e.matmul(pt[0:64, 0:qn], lhsT=wgt[:], rhs=src[:, q0:q0 + qn], start=True, stop=True),
                         R=[t_src, t_par], W=[ptk])
                    K.op('dve', lambda e, pt=pt, qn=qn, bcol=bcol, uu=uu: e.tensor_scalar(out=uu[:, 0:qn], in0=pt[0:64, 0:qn], scalar1=fcol[:, bcol:bcol + 1], scalar2=fs[:, 0:1],
                                                                                   op0=ALU.add, op1=ALU.mult), R=[ptk, t_par], W=[t_uu])
                    K.op('dve', lambda e, qn=qn, uu=uu, ui=ui: e.tensor_copy(out=ui[:, 0:qn], in_=uu[:, 0:qn]), R=[t_uu], W=[t_uu])
                    K.op('dve', lambda e, qn=qn, uu=uu, ui=ui: e.tensor_tensor(out=uu[:, 0:qn], in0=uu[:, 0:qn], in1=ui[:, 0:qn], op=ALU.subtract), R=[t_uu], W=[t_uu])
                    K.op('act', lambda e, q0=q0, qn=qn, dst=dst, uu=uu: e.activation(out=dst[:, q0:q0 + qn], in_=uu[:, 0:qn], func=AF.Sin, scale=TWO_PI), R=[t_uu], W=[t_dst])
            acc = sb2('hy_acc', [128, 512]); t_acc = Tok()
            K.op('pool', lambda e: e.memset(acc[:], 0.0), W=[t_acc])
            win = [sb2('hy_win%d' % i, [128, 512]) for i in range(2)]; t_win = [Tok(), Tok()]
            fl = [sb2('hy_fl%d' % i, [128, 512]) for i in range(2)]; t_fl = [Tok(), Tok()]
            fa = [sb2('hy_fa%d' % i, [128, 512]) for i in range(2)]; t_fa = [Tok(), Tok()]
            for i in range(NT):
                b = i % 2
                pt, ptk = g.ps()
                K.op('pe', lambda e, pt=pt, i=i: e.matmul(pt[:, 0:512], lhsT=h2[:, i * 128:(i + 1) * 128], rhs=fw3[:], start=True, stop=True), R=[t_h2, t_par], W=[ptk])
                K.op('act', lambda e, b=b, i=i: e.activation(out=win[b][:], in_=absd[:], func=AF.Exp, scale=tn[:, i:i + 1]), R=[t_par], W=[t_win[b]])
                K.op('dve', lambda e, b=b, pt=pt: e.tensor_tensor(out=fl[b][:], in0=pt[:, 0:512], in1=win[b][:], op=ALU.mult), R=[ptk, t_win[b]], W=[t_fl[b]])
                if i == 0:
                    K.op('dve', lambda e, b=b: e.memset(fl[b][0:1, 256:512], 0.0), W=[t_fl[b]])
                K.op('dve', lambda e, b=b: e.scalar_tensor_tensor(out=fa[b][:], in0=fl[b][:], scalar=-1.0, in1=fl[b][:], op0=ALU.mult, op1=ALU.max), R=[t_fl[b]], W=[t_fa[b]])
                K.op('pool', lambda e, b=b: e.tensor_tensor(out=acc[:], in0=acc[:], in1=fa[b][:], op=ALU.add), R=[t_fa[b], t_acc], W=[t_acc])
                K.op('pool', lambda e, b=b, i=i: e.tensor_copy(out=data[:, i, 256:768], in_=fl[b][:]), R=[t_fl[b]], W=[t_data[i]])
            pt, ptk = g.ps()
            for ci in range(2):
                K.op('pe', lambda e, ci=ci, pt=pt: e.matmul(pt[:, ci:ci + 1], lhsT=acc[:, ci * 128:(ci + 1) * 128], rhs=g.ones[:, 0:1], start=True, stop=False),
                     R=[t_acc, g.t_const], W=[ptk])
                K.op('pe', lambda e, ci=ci, pt=pt: e.matmul(pt[:, ci:ci + 1], lhsT=acc[:, 256 + ci * 128:256 + (ci + 1) * 128], rhs=g.ones[:, 0:1], start=False, stop=True),
                     R=[t_acc, g.t_const], W=[ptk])
            K.op('dve', lambda e, pt=pt: e.reciprocal(out=rnorm[:], in_=pt[:, 0:2]), R=[ptk], W=[t_rn])
            K.barrier()
        if _hs < 3:
            return
        Yw = sb('hy_Yw', [128, NKT, 2, 256], BF16); t_Y = [Tok() for _ in range(NKT)]
        wk = sb('hy_wk', [128, NKT])
        K.dma(wk[:], D['hy_wk' + sfx][:, :], W=[t_par])
        tabc = [sb('hy_tc%d' % i, [128, NKT, 128], BF16) for i in range(2)]
        tabs = [sb('hy_ts%d' % i, [128, NKT, 128], BF16) for i in range(2)]
        t_tab = [Tok(), Tok()]
        f1c = sb('hy_f1c', [128, 256]); f1s = sb('hy_f1s', [128, 256]); t_f1 = Tok()
        rre = sb('hy_rre', [128, 256]); rim = sb('hy_rim', [128, 256]); t_r = Tok()
        tt = [sb('hy_tt%d' % i, [128, 256]) for i in range(4)]; t_tt = [Tok() for _ in range(4)]
        yy = [sb('hy_yy%d' % i, [128, 256]) for i in range(2)]; t_yy = [Tok(), Tok()]
        for j in range(NKT):
            b = j % 2
            K.dma(tabc[b][:], D['dftc' + sfx][j, :, :, :], W=[t_tab[b]])
            K.dma(tabs[b][:], D['dfts' + sfx][j, :, :, :], W=[t_tab[b]])
            pA, pAt = g.ps(); pB, pBt = g.ps(); pC, pCt = g.ps(); pD, pDt = g.ps()
            for (tab_, pX, pXt, pY, pYt) in ((tabc, pA, pAt, pB, pBt), (tabs, pC, pCt, pD, pDt)):
                for i in range(NT):
                    fl_ = dict(start=(i == 0), stop=(i == NT - 1))
                    K.op('pe', lambda e, i=i, b=b, pX=pX, fl_=fl_, tab_=tab_: e.matmul(pX[:, 0:512], lhsT=tab_[b][:, i, :], rhs=data[:, i, 0:512], **fl_), R=[t_tab[b], t_data[i]], W=[pXt])
                    K.op('pe', lambda e, i=i, b=b, pY=pY, fl_=fl_, tab_=tab_: e.matmul(pY[:, 0:256], lhsT=tab_[b][:, i, :], rhs=data[:, i, 512:768], **fl_), R=[t_tab[b], t_data[i]], W=[pYt])
            K.op('act', lambda e, pB=pB: e.activation(out=f1c[:], in_=pB[:, 0:256], func=AF.Identity), R=[pBt], W=[t_f1])
            K.op('act', lambda e, pD=pD: e.activation(out=f1s[:], in_=pD[:, 0:256], func=AF.Identity), R=[pDt], W=[t_f1])
            K.op('dve', lambda e, pA=pA: e.tensor_tensor(out=rre[:], in0=pA[:, 256:512], in1=f1c[:], op=ALU.add), R=[pAt, t_f1], W=[t_r])
            K.op('dve', lambda e, pC=pC: e.tensor_tensor(out=rim[:], in0=f1s[:], in1=pC[:, 256:512], op=ALU.subtract), R=[pCt, t_f1], W=[t_r])
            K.op('dve', lambda e, pA=pA: e.tensor_tensor(out=tt[0][:], in0=pA[:, 0:256], in1=rre[:], op=ALU.mult), R=[pAt, t_r], W=[t_tt[0]])
            K.op('dve', lambda e, pC=pC: e.tensor_tensor(out=tt[1][:], in0=pC[:, 0:256], in1=rim[:], op=ALU.mult), R=[pCt, t_r], W=[t_tt[1]])
            K.op('dve', lambda e, pA=pA: e.tensor_tensor(out=tt[2][:], in0=pA[:, 0:256], in1=rim[:], op=ALU.mult), R=[pAt, t_r], W=[t_tt[2]])
            K.op('dve', lambda e, pC=pC: e.tensor_tensor(out=tt[3][:], in0=pC[:, 0:256], in1=rre[:], op=ALU.mult), R=[pCt, t_r], W=[t_tt[3]])
            K.op('pool', lambda e: e.tensor_tensor(out=yy[0][:], in0=tt[0][:], in1=tt[1][:], op=ALU.add), R=[t_tt[0], t_tt[1]], W=[t_yy[0]])
            K.op('pool', lambda e: e.tensor_tensor(out=yy[1][:], in0=tt[3][:], in1=tt[2][:], op=ALU.subtract), R=[t_tt[2], t_tt[3]], W=[t_yy[1]])
            K.op('act', lambda e, j=j: e.activation(out=Yw[:, j, 0, :], in_=yy[0][:], func=AF.Identity, scale=wk[:, j:j + 1]), R=[t_yy[0], t_par], W=[t_Y[j]])
            K.op('act', lambda e, j=j: e.activation(out=Yw[:, j, 1, :], in_=yy[1][:], func=AF.Identity, scale=wk[:, j:j + 1]), R=[t_yy[1], t_par], W=[t_Y[j]])
        if _hs < 4:
            return
        ysb = [sb('hy_ysb%d' % i, [128, 256]) for i in range(2)]; t_ys = [Tok(), Tok()]
        tmp = [sb('hy_tmp%d' % i, [128, 256]) for i in range(2)]; t_tmp = [Tok(), Tok()]
        ob = [sb('hy_ob%d' % i, [128, 2, 128], BF16) for i in range(2)]; t_ob = [Tok(), Tok()]
        for i in range(NT):
            b = i % 2
            K.dma(tabc[b][:], D['dftc' + sfx][i, :, :, :], W=[t_tab[b]])
            K.dma(tabs[b][:], D['dfts' + sfx][i, :, :, :], W=[t_tab[b]])
            py, pyt = g.ps()
            for kt in range(NKT):
                K.op('pe', lambda e, kt=kt, b=b, py=py: e.matmul(py[:, 0:256], lhsT=tabc[b][:, kt, :], rhs=Yw[:, kt, 0, :], start=(kt == 0), stop=False), R=[t_tab[b], t_Y[kt]], W=[pyt])
                K.op('pe', lambda e, kt=kt, b=b, py=py: e.matmul(py[:, 0:256], lhsT=tabs[b][:, kt, :], rhs=Yw[:, kt, 1, :], start=False, stop=(kt == NKT - 1)), R=[t_tab[b], t_Y[kt]], W=[pyt])
            K.op('act', lambda e, b=b, py=py: e.activation(out=ysb[b][:], in_=py[:, 0:256], func=AF.Identity), R=[pyt], W=[t_ys[b]])
            pt, ptk = g.ps()
            for ci in range(2):
                K.op('pe', lambda e, ci=ci, b=b, pt=pt: e.transpose(out=pt[:, ci * 128:(ci + 1) * 128], in_=ysb[b][:, ci * 128:(ci + 1) * 128], identity=g.ident[:]),
                     R=[t_ys[b], g.t_const], W=[ptk])
            cs = slice(i * 128, (i + 1) * 128)
            for ci in range(2):
                K.op('act', lambda e, ci=ci, b=b, pt=pt: e.activation(out=tmp[b][:, ci * 128:(ci + 1) * 128], in_=pt[:, ci * 128:(ci + 1) * 128], func=AF.Identity, scale=rnorm[:, ci:ci + 1]),
                     R=[ptk, t_rn], W=[t_tmp[b]])
                K.op('dve', lambda e, ci=ci, b=b, cs=cs: e.scalar_tensor_tensor(out=tmp[b][:, ci * 128:(ci + 1) * 128], in0=zT[:, ci, cs], scalar=hbias[:, ci:ci + 1],
                                                                             in1=tmp[b][:, ci * 128:(ci + 1) * 128], op0=ALU.mult, op1=ALU.add), R=[t_z, t_par, t_tmp[b]], W=[t_tmp[b]])
                K.op('dve', lambda e, ci=ci, b=b, cs=cs: e.tensor_tensor(out=ob[b][:, ci, :], in0=tmp[b][:, ci * 128:(ci + 1) * 128], in1=x0T[:, ci, cs], op=ALU.mult),
                     R=[t_tmp[b], t_x0], W=[t_ob[b]])
            col0 = (c0 + i) * 128
            K.dma(D['mixT_d'][768:1024, col0:col0 + 128].rearrange("(m p) n -> p m n", p=128), ob[b][:], R=[t_ob[b]], W=[g.t_mix], q='pool')


def load_ln_tables(g, st, l, which):
    nc, K = g.nc, g.K
    t = st.enter_context(nc.sbuf_tensor(_nm('lntab'), [128, 2, D_MODEL], F32))
    tk = Tok()
    K.dma(t[:, 0, :], g.D['ln_g'][l, which, :].partition_broadcast(128), W=[tk])
    K.dma(t[:, 1, :], g.D['ln_b'][l, which, :].partition_broadcast(128), W=[tk])
    return t, tk


def deepnorm_chunk(g, l, c, b, which, y_ap, t_y, xres, t_xres, T_, lntab, t_ln, dst_ap, t_dst_tok):
    K = g.K
    cls = 1 if c < 2 else 0
    tmp, t_tmp = T_['tmp'][b], T_['t_tmp'][b]
    for h in range(2):
        K.op('dve', lambda e, h=h: e.tensor_tensor(out=tmp[:, h * 512:(h + 1) * 512], in0=y_ap[h], in1=g.gbc[:, 0, cls, h * 512:(h + 1) * 512], op=ALU.mult),
             R=[t_y[h], g.t_gbc], W=[t_tmp])
    K.op('dve', lambda e: e.scalar_tensor_tensor(out=tmp[:], in0=xres[:], scalar=float(ALPHA), in1=tmp[:], op0=ALU.mult, op1=ALU.add), R=[t_xres, t_tmp], W=[t_tmp])
    mv, rstd, tk = ln_stats(g, T_['sts'][b], tmp, t_tmp)
    K.op('dve', lambda e: e.tensor_scalar(out=tmp[:], in0=tmp[:], scalar1=mv[:, 0:1], scalar2=rstd[:, 0:1], op0=ALU.subtract, op1=ALU.mult), R=[t_tmp, tk], W=[t_tmp])
    K.op('pool', lambda e: e.tensor_tensor(out=tmp[:], in0=tmp[:], in1=lntab[:, 0, :], op=ALU.mult), R=[t_tmp, t_ln], W=[t_tmp])
    K.op('pool', lambda e: e.tensor_tensor(out=dst_ap, in0=tmp[:], in1=lntab[:, 1, :], op=ALU.add), R=[t_tmp, t_ln], W=[t_dst_tok])


def phase_wout(g, l, x_src, t_xsrc, chunks, x1_d, t_x1):
    nc, K, D = g.nc, g.K, g.D
    with ExitStack() as st:
        def sb(name, shape, dt=F32):
            return st.enter_context(nc.sbuf_tensor(_nm(name), list(shape), dt))
        wb, t_w = load_w_bf16(g, st, [(D['w_out'][l], 0, D_MODEL)], D_MODEL, 'w_out')
        lntab, t_ln = load_ln_tables(g, st, l, 0)
        g.gbc = sb('gbcw', [128, 1, 2, D_MODEL])
        K.dma(g.gbc[:], D['gbc_d'][:, 0:1, :, :], R=[g.t_gbcd], W=[g.t_gbc])
        mx = [sb('mx%d' % i, [128, 8, 128], BF16) for i in range(2)]; t_mx = [Tok(), Tok()]
        xr = [sb('xr%d' % i, [128, D_MODEL]) for i in range(2)]; t_xr = [Tok(), Tok()]
        xo = [sb('xo%d' % i, [128, D_MODEL]) for i in range(2)]; t_xo = [Tok(), Tok()]
        T_ = {'tmp': [sb('dn_tmp%d' % i, [128, D_MODEL]) for i in range(2)], 't_tmp': [Tok(), Tok()],
              'sts': [(sb('dstt%d' % i, [128, 2, 6]), sb('dmv%d' % i, [128, 2]), sb('dlnv%d' % i, [128, 1]), sb('drstd%d' % i, [128, 1]), Tok()) for i in range(2)]}
        for n_, c in enumerate(chunks):
            b = n_ % 2
            cs = slice(c * 128, (c + 1) * 128)
            K.dma(mx[b][:], D['mixT_d'][:, cs].rearrange("(kt p) n -> p kt n", p=128), R=[g.t_mix], W=[t_mx[b]])
            K.dma(xr[b][:], x_src[cs, :], R=([t_xsrc[c]] if t_xsrc is not None else []), W=[t_xr[b]])
            pys = []
            for h in range(2):
                py, pyt = g.ps()
                pys.append((py, pyt))
                for kt in range(8):
                    K.op('pe', lambda e, kt=kt, b=b, h=h, py=py: e.matmul(py[:, 0:512], lhsT=mx[b][:, kt, :], rhs=wb[:, kt, h * 512:(h + 1) * 512],
                                                                          start=(kt == 0), stop=(kt == 7)), R=[t_mx[b], t_w], W=[pyt])
            deepnorm_chunk(g, l, c, b, 0, [pys[0][0][:, 0:512], pys[1][0][:, 0:512]], [pys[0][1], pys[1][1]], xr[b], t_xr[b], T_, lntab, t_ln, xo[b][:], t_xo[b])
            K.dma(x1_d[cs, :], xo[b][:], R=[t_xo[b]], W=[t_x1[c]], q='pool')


def phase_ffn(g, l, chunks, w1_src, w3_src, w2_src, vT_d, t_vT, ffn_d, t_ffn, gate=None, first=True):
    nc, K, D = g.nc, g.K, g.D
    with ExitStack() as st:
        def sb(name, shape, dt=F32):
            return st.enter_context(nc.sbuf_tensor(_nm(name), list(shape), dt))
        w1b = sb('w1b', [128, 8, D_FF], BF16); w3b = sb('w3b', [128, 8, D_FF], BF16); w2b = sb('w2b', [128, NF, D_MODEL], BF16)
        t_w = Tok()
        stg = [sb('fstg%d' % i, [128, 8, 256]) for i in range(3)]; t_stg = [Tok() for _ in range(3)]
        k = 0
        for (src, dstw, nk, ncol) in ((w1_src, w1b, 8, D_FF), (w3_src, w3b, 8, D_FF), (w2_src, w2b, NF, D_MODEL)):
            view = src.rearrange("(kt p) n -> p kt n", p=128)
            for k0 in range(0, nk, 8):
                kn = min(8, nk - k0)
                for c0 in range(0, ncol, 256):
                    cn = min(256, ncol - c0)
                    i = k % 3
                    K.dma(stg[i][:, 0:kn, 0:cn], view[:, k0:k0 + kn, c0:c0 + cn], W=[t_stg[i]])
                    eng = ('dve', 'pool', 'act')[k % 3]
                    if eng == 'act':
                        K.op('act', lambda e, i=i, kn=kn, cn=cn, k0=k0, c0=c0, dstw=dstw: e.activation(out=dstw[:, k0:k0 + kn, c0:c0 + cn], in_=stg[i][:, 0:kn, 0:cn], func=AF.Identity),
                             R=[t_stg[i]], W=[t_w])
                    else:
                        K.op(eng, lambda e, i=i, kn=kn, cn=cn, k0=k0, c0=c0, dstw=dstw: e.tensor_copy(out=dstw[:, k0:k0 + kn, c0:c0 + cn], in_=stg[i][:, 0:kn, 0:cn]),
                             R=[t_stg[i]], W=[t_w])
                    k += 1
        vt = [sb('vt%d' % i, [128, 8, 256], BF16) for i in range(2)]; t_vt = [Tok(), Tok()]
        hT = sb('hT', [128, NF, 256], BF16); t_hT = Tok()
        sil = [sb('sil%d' % i, [128, 256]) for i in range(2)]; t_sil = [Tok(), Tok()]
        fo = [sb('fo%d' % i, [128, D_MODEL]) for i in range(2)]; t_fo = [Tok(), Tok()]
        if gate is not None:
            e_idx, gates_d, t_gates = gate
            gt = [sb('gt%d' % i, [128, N_EXP]) for i in range(2)]; t_gt = [Tok(), Tok()]
        nfo = 0
        for ti in range(0, len(chunks), 2):
            cc = chunks[ti:ti + 2]
            b = (ti // 2) % 2
            nt_ = len(cc) * 128
            for j, c in enumerate(cc):
                K.dma(vt[b][:, :, j * 128:(j + 1) * 128], vT_d[c, :, :, :], R=[t_vT[c]], W=[t_vt[b]])
            for f in range(NF):
                ph, pht = g.ps()
                fs = slice(f * 128, (f + 1) * 128)
                for kt in range(8):
                    K.op('pe', lambda e, kt=kt, b=b, ph=ph, fs=fs, nt_=nt_: e.matmul(ph[:, 0:nt_], lhsT=w1b[:, kt, fs], rhs=vt[b][:, kt, 0:nt_], start=(kt == 0), stop=(kt == 7)),
                         R=[t_w, t_vt[b]], W=[pht])
                for kt in range(8):
                    K.op('pe', lambda e, kt=kt, b=b, ph=ph, fs=fs, nt_=nt_: e.matmul(ph[:, 256:256 + nt_], lhsT=w3b[:, kt, fs], rhs=vt[b][:, kt, 0:nt_], start=(kt == 0), stop=(kt == 7)),
                         R=[t_w, t_vt[b]], W=[pht])
                sb_ = f % 2
                K.op('act', lambda e, ph=ph, sb_=sb_, nt_=nt_: e.activation(out=sil[sb_][:, 0:nt_], in_=ph[:, 0:nt_], func=AF.Silu), R=[pht], W=[t_sil[sb_]])
                K.op('dve', lambda e, ph=ph, sb_=sb_, nt_=nt_, f=f: e.tensor_tensor(out=hT[:, f, 0:nt_], in0=ph[:, 256:256 + nt_], in1=sil[sb_][:, 0:nt_], op=ALU.mult),
                     R=[pht, t_sil[sb_]], W=[t_hT])
            for j, c in enumerate(cc):
                ob_ = nfo % 2
                nfo += 1
                if gate is not None:
                    K.dma(gt[ob_][:], gates_d[c, :, :], R=[t_gates[c]], W=[t_gt[ob_]])
                for h in range(2):
                    po, pot = g.ps()
                    for f in range(NF):
                        K.op('pe', lambda e, f=f, j=j, h=h, po=po: e.matmul(po[:, 0:512], lhsT=hT[:, f, j * 128:(j + 1) * 128], rhs=w2b[:, f, h * 512:(h + 1) * 512],
                                                                            start=(f == 0), stop=(f == NF - 1)), R=[t_hT, t_w], W=[pot])
                    if gate is None:
                        K.op('act', lambda e, po=po, ob_=ob_, h=h: e.activation(out=fo[ob_][:, h * 512:(h + 1) * 512], in_=po[:, 0:512], func=AF.Identity), R=[pot], W=[t_fo[ob_]])
                    else:
                        K.op('act', lambda e, po=po, ob_=ob_, h=h: e.activation(out=fo[ob_][:, h * 512:(h + 1) * 512], in_=po[:, 0:512], func=AF.Identity,
                                                                              scale=gt[ob_][:, e_idx:e_idx + 1]), R=[pot, t_gt[ob_]], W=[t_fo[ob_]])
                cs = slice(c * 128, (c + 1) * 128)
                if first:
                    K.dma(ffn_d[cs, :], fo[ob_][:], R=[t_fo[ob_]], W=[t_ffn[c]], q='pool')
                else:
                    K.dma(ffn_d[cs, :], fo[ob_][:], R=[t_fo[ob_]], W=[t_ffn[c]], q='pool', accum_op=ALU.add)


def phase_ln2(g, l, chunks, x1_d, t_x1, ffn_d, t_ffn, dst_fn):
    nc, K, D = g.nc, g.K, g.D
    with ExitStack() as st:
        def sb(name, shape, dt=F32):
            return st.enter_context(nc.sbuf_tensor(_nm(name), list(shape), dt))
        lntab, t_ln = load_ln_tables(g, st, l, 1)
        g.gbc = sb('gbcf', [128, 1, 2, D_MODEL])
        K.dma(g.gbc[:], D['gbc_d'][:, 1:2, :, :], R=[g.t_gbcd], W=[g.t_gbc])
        xr = [sb('l2x%d' % i, [128, D_MODEL]) for i in range(2)]; t_xr = [Tok(), Tok()]
        fr = [sb('l2f%d' % i, [128, D_MODEL]) for i in range(2)]; t_fr = [Tok(), Tok()]
        xo = [sb('l2o%d' % i, [128, D_MODEL]) for i in range(2)]; t_xo = [Tok(), Tok()]
        T_ = {'tmp': [sb('l2tmp%d' % i, [128, D_MODEL]) for i in range(2)], 't_tmp': [Tok(), Tok()],
              'sts': [(sb('l2stt%d' % i, [128, 2, 6]), sb('l2mv%d' % i, [128, 2]), sb('l2lnv%d' % i, [128, 1]), sb('l2rstd%d' % i, [128, 1]), Tok()) for i in range(2)]}
        for n_, c in enumerate(chunks):
            b = n_ % 2
            cs = slice(c * 128, (c + 1) * 128)
            K.dma(xr[b][:], x1_d[cs, :], R=[t_x1[c]], W=[t_xr[b]])
            K.dma(fr[b][:], ffn_d[cs, :], R=[t_ffn[c]], W=[t_fr[b]])
            deepnorm_chunk(g, l, c, b, 1, [fr[b][:, 0:512], fr[b][:, 512:1024]], [t_fr[b], t_fr[b]], xr[b], t_xr[b], T_, lntab, t_ln, xo[b][:], t_xo[b])
            dst, t_dst = dst_fn(c)
            K.dma(dst, xo[b][:], R=[t_xo[b]], W=[t_dst], q='pool')


_CACHE = {}


def kernel(**inputs):
    inp = {k: np.asarray(v) for k, v in inputs.items()}
    if 'prog' not in _CACHE:
        _CACHE['prog'] = build_program()
    nc, g = _CACHE['prog']
    shared = prep_shared(inp)
    maps = []
    for b in range(8):
        m = prep_core_inputs(inp, b, shared)
        maps.append({k: v for k, v in m.items() if k in g.D})
    res = run_bass_kernel_spmd(nc, maps, core_ids=list(range(8)))
    out = np.stack([np.asarray(r['out']) for r in res.results], axis=0)
    return out.astype(np.float32)


def phase_ffn2(g, l, chunks, experts, vT_d, t_vT, ffn_d, t_ffn, gates=None):
    nc, K, D = g.nc, g.K, g.D
    HF = NF // 2
    with ExitStack() as st:
        def sb(name, shape, dt=F32):
            return st.enter_context(nc.sbuf_tensor(_nm(name), list(shape), dt))
        w1s = [sb('w1s%d' % i, [128, 8, HF * 128], BF16) for i in range(2)]
        w3s = [sb('w3s%d' % i, [128, 8, HF * 128], BF16) for i in range(2)]
        w2s = [sb('w2s%d' % i, [128, HF, D_MODEL], BF16) for i in range(2)]
        t_ws = [Tok(), Tok()]
        vt = [sb('vt%d' % i, [128, 8, 512], BF16) for i in range(2)]; t_vt = [Tok(), Tok()]
        hT = [sb('hT%d' % i, [128, HF, 512], BF16) for i in range(2)]; t_hT = [Tok(), Tok()]
        sil = [sb('sil%d' % i, [128, 512]) for i in range(2)]; t_sil = [Tok(), Tok()]
        fo = [sb('fo%d' % i, [128, D_MODEL]) for i in range(2)]; t_fo = [Tok(), Tok()]
        if gates is not None:
            gates_d, t_gates = gates
            gt = [sb('gt%d' % i, [128, N_EXP]) for i in range(2)]; t_gt = [Tok(), Tok()]
        units = [(e_, hf) for e_ in range(len(experts)) for hf in range(2)]

        def load_unit(u):
            e_, hf = units[u]
            w1, w3, w2 = experts[e_]
            s_ = u % 2
            f0 = hf * HF * 128
            v1 = w1.rearrange("(kt p) n -> p kt n", p=128)
            v3 = w3.rearrange("(kt p) n -> p kt n", p=128)
            v2 = w2.rearrange("(ft p) n -> p ft n", p=128)
            for c0 in range(0, HF * 128, 704):
                K.dma(w1s[s_][:, :, c0:c0 + 704], v1[:, :, f0 + c0:f0 + c0 + 704], W=[t_ws[s_]], q='pool')
                K.dma(w3s[s_][:, :, c0:c0 + 704], v3[:, :, f0 + c0:f0 + c0 + 704], W=[t_ws[s_]], q='pool')
            for f_ in range(0, HF, 4):
                fn_ = min(4, HF - f_)
                K.dma(w2s[s_][:, f_:f_ + fn_, :], v2[:, hf * HF + f_:hf * HF + f_ + fn_, :], W=[t_ws[s_]], q='pool')
        load_unit(0)
        nfo = 0
        ntile = 0
        for u in range(len(units)):
            e_, hf = units[u]
            s_ = u % 2
            if u + 1 < len(units):
                load_unit(u + 1)
            _ft = int(os.environ.get('FFN_TILE', '2'))
            for ti in range(0, len(chunks), _ft):
                cc = chunks[ti:ti + _ft]
                b = ntile % 2
                ntile += 1
                nt_ = len(cc) * 128
                for j, c in enumerate(cc):
                    K.dma(vt[b][:, :, j * 128:(j + 1) * 128], vT_d[c, :, :, :], R=[t_vT[c]], W=[t_vt[b]])
                for f in range(HF):
                    ph, pht = g.ps()
                    ph3, pht3 = g.ps()
                    fs = slice(f * 128, (f + 1) * 128)
                    for kt in range(8):
                        K.op('pe', lambda e, kt=kt, b=b, ph=ph, fs=fs, nt_=nt_, s_=s_: e.matmul(ph[:, 0:nt_], lhsT=w1s[s_][:, kt, fs], rhs=vt[b][:, kt, 0:nt_], start=(kt == 0), stop=(kt == 7)),
                             R=[t_ws[s_], t_vt[b]], W=[pht])
                    for kt in range(8):
                        K.op('pe', lambda e, kt=kt, b=b, ph3=ph3, fs=fs, nt_=nt_, s_=s_: e.matmul(ph3[:, 0:nt_], lhsT=w3s[s_][:, kt, fs], rhs=vt[b][:, kt, 0:nt_], start=(kt == 0), stop=(kt == 7)),
                             R=[t_ws[s_], t_vt[b]], W=[pht3])
                    sb_ = f % 2
                    K.op('act', lambda e, ph=ph, sb_=sb_, nt_=nt_: e.activation(out=sil[sb_][:, 0:nt_], in_=ph[:, 0:nt_], func=AF.Silu), R=[pht], W=[t_sil[sb_]])
                    K.op('dve', lambda e, ph3=ph3, sb_=sb_, nt_=nt_, f=f, b=b: e.tensor_tensor(out=hT[b][:, f, 0:nt_], in0=ph3[:, 0:nt_], in1=sil[sb_][:, 0:nt_], op=ALU.mult),
                         R=[pht3, t_sil[sb_]], W=[t_hT[b]])
                for j, c in enumerate(cc):
                    ob_ = nfo % 2
                    nfo += 1
                    if gates is not None:
                        K.dma(gt[ob_][:], gates_d[c, :, :], R=[t_gates[c]], W=[t_gt[ob_]])
                    for h in range(2):
                        po, pot = g.ps()
                        for f in range(HF):
                            K.op('pe', lambda e, f=f, j=j, h=h, po=po, b=b, s_=s_: e.matmul(po[:, 0:512], lhsT=hT[b][:, f, j * 128:(j + 1) * 128], rhs=w2s[s_][:, f, h * 512:(h + 1) * 512],
                                                                                    start=(f == 0), stop=(f == HF - 1)), R=[t_hT[b], t_ws[s_]], W=[pot])
                        if gates is None:
                            K.op('act', lambda e, po=po, ob_=ob_, h=h: e.activation(out=fo[ob_][:, h * 512:(h + 1) * 512], in_=po[:, 0:512], func=AF.Identity), R=[pot], W=[t_fo[ob_]])
                        else:
                            K.op('act', lambda e, po=po, ob_=ob_, h=h, e_=e_: e.activation(out=fo[ob_][:, h * 512:(h + 1) * 512], in_=po[:, 0:512], func=AF.Identity,
                                                                                  scale=gt[ob_][:, e_:e_ + 1]), R=[pot, t_gt[ob_]], W=[t_fo[ob_]])
                    cs = slice(c * 128, (c + 1) * 128)
                    if u == 0:
                        K.dma(ffn_d[cs, :], fo[ob_][:], R=[t_fo[ob_]], W=[t_ffn[c]], q='pool')
                    else:
                        K.dma(ffn_d[cs, :], fo[ob_][:], R=[t_fo[ob_]], W=[t_ffn[c]], q='pool', accum_op=ALU.add)
```
